# Optimizing a Trainium2 kernel written in Bass

```python
import math
import jax, jax.numpy as jnp
from jax import lax
import numpy as np

D_MODEL = 1024
BATCH = 16
SEQ = 4096
DEPTH = 2

CHUNK = 64
RMS_EPS = 1e-6
DN_WIDTH = D_MODEL // 2
DN_DHEAD = 128
DN_HEADS = DN_WIDTH // DN_DHEAD
DN_CONV = 4
SA_WIDTH = D_MODEL // 4
SA_DHEAD = 64
SA_HEADS = SA_WIDTH // SA_DHEAD
SA_DKV = SA_DHEAD
IDX_HEADS = 4
IDX_DHEAD = 64
IDX_TOPK_MAX = 256
Q_BLOCK = 128
HG_WIDTH = D_MODEL // 4
HG_DHEAD = 64
HG_HEADS = HG_WIDTH // HG_DHEAD
D_MIX = DN_WIDTH + SA_WIDTH + HG_WIDTH
D_FF = 2816
FFN_CONV = 3

SPLIT_SIZES = (DN_WIDTH, DN_WIDTH, DN_WIDTH, DN_WIDTH, DN_HEADS, DN_HEADS,
               SA_WIDTH, SA_DKV, SA_DKV, IDX_HEADS * IDX_DHEAD, IDX_DHEAD, IDX_HEADS,
               HG_WIDTH, HG_WIDTH, HG_WIDTH, HG_WIDTH)
D_IN = sum(SPLIT_SIZES)
SPLIT_POINTS = tuple(int(p) for p in np.cumsum(SPLIT_SIZES)[:-1])

kernel_name = 'hybrid_parallel_heads_streaming_encoder'


def rmsnorm(x, g):
    xf = x.astype(jnp.float32)
    y = xf * lax.rsqrt(jnp.mean(xf * xf, axis=-1, keepdims=True) + RMS_EPS)
    return (y * g.astype(jnp.float32)).astype(x.dtype)


def l2norm(x):
    return x * lax.rsqrt(jnp.sum(x * x, axis=-1, keepdims=True) + 1e-6)


def causal_depthwise_conv(x, w):
    K, C = w.shape
    return lax.conv_general_dilated(x, w[:, None, :].astype(x.dtype), window_strides=(1,),
                                    padding=[(K - 1, 0)],
                                    dimension_numbers=('NWC', 'WIO', 'NWC'),
                                    feature_group_count=C)


def to_chunks(t, n_heads, d_head):
    B, S = t.shape[:2]
    return t.reshape(B, S // CHUNK, CHUNK, n_heads, d_head).transpose(0, 3, 1, 2, 4)


def gate_chunks(t):
    B, S, H = t.shape
    return t.reshape(B, S // CHUNK, CHUNK, H).transpose(0, 3, 1, 2)


def from_chunks(o):
    N, B, H, C, d = o.shape
    return o.transpose(1, 0, 3, 2, 4).reshape(B, N * C, H, d)


def gated_deltanet(q, k, v, z, b, a, conv_w, a_log, dt_bias, norm_g):
    dtype = q.dtype
    Bsz, S, _ = q.shape
    qkv = jax.nn.silu(causal_depthwise_conv(jnp.concatenate([q, k, v], axis=-1), conv_w))
    q, k, v = jnp.split(qkv.astype(jnp.float32), 3, axis=-1)
    q = l2norm(to_chunks(q, DN_HEADS, DN_DHEAD)) * (DN_DHEAD ** -0.5)
    k = l2norm(to_chunks(k, DN_HEADS, DN_DHEAD))
    v = to_chunks(v, DN_HEADS, DN_DHEAD)
    beta = gate_chunks(jax.nn.sigmoid(b.astype(jnp.float32)))
    g = -jnp.exp(a_log.astype(jnp.float32)) * jax.nn.softplus(
        a.astype(jnp.float32) + dt_bias.astype(jnp.float32))
    G = jnp.cumsum(gate_chunks(g), axis=-1)
    causal = jnp.tril(jnp.ones((CHUNK, CHUNK), bool))
    strict = jnp.tril(jnp.ones((CHUNK, CHUNK), bool), k=-1)
    decay = jnp.exp(jnp.where(causal, G[..., :, None] - G[..., None, :], -jnp.inf))
    kk = jnp.einsum('bhnck,bhnsk->bhncs', k, k)
    M = jnp.where(strict, beta[..., :, None] * kk * decay, 0.0)
    eye = jnp.eye(CHUNK, dtype=jnp.float32)
    T = lax.linalg.triangular_solve(eye + M, jnp.broadcast_to(eye, M.shape),
                                    left_side=True, lower=True, unit_diagonal=True)
    u0 = jnp.einsum('bhncs,bhnsv->bhncv', T, beta[..., None] * v)
    wk = jnp.einsum('bhncs,bhnsk->bhnck', T, (beta * jnp.exp(G))[..., None] * k)
    qk = jnp.einsum('bhnck,bhnsk->bhncs', q, k) * decay
    q_dec = q * jnp.exp(G)[..., None]
    k_dec = k * jnp.exp(G[..., -1:] - G)[..., None]
    g_last = jnp.exp(G[..., -1])

    def step(state, inp):
        u0_c, w_c, qk_c, qd_c, kd_c, gl_c = inp
        u = u0_c - jnp.einsum('bhck,bhkv->bhcv', w_c, state)
        o = jnp.einsum('bhck,bhkv->bhcv', qd_c, state) + jnp.einsum('bhcs,bhsv->bhcv', qk_c, u)
        state = state * gl_c[..., None, None] + jnp.einsum('bhck,bhcv->bhkv', kd_c, u)
        return state, o

    xs = tuple(jnp.moveaxis(t, 2, 0) for t in (u0, wk, qk, q_dec, k_dec, g_last))
    s0 = jnp.zeros((Bsz, DN_HEADS, DN_DHEAD, DN_DHEAD), jnp.float32)
    _, o = lax.scan(step, s0, xs)
    o = rmsnorm(from_chunks(o), norm_g)
    o = o * jax.nn.silu(z.astype(jnp.float32).reshape(Bsz, S, DN_HEADS, DN_DHEAD))
    return o.reshape(Bsz, S, DN_WIDTH).astype(dtype)


def dsa_attention(q, k, v, qi, ki, wi):
    dtype = q.dtype
    Bsz, S, _ = q.shape
    nb = S // Q_BLOCK
    topk = min(IDX_TOPK_MAX, S // 4)
    f32 = jnp.float32
    qb_all = q.astype(f32).reshape(Bsz, nb, Q_BLOCK, SA_HEADS, SA_DHEAD).transpose(1, 0, 2, 3, 4)
    qib_all = qi.astype(f32).reshape(Bsz, nb, Q_BLOCK, IDX_HEADS, IDX_DHEAD).transpose(1, 0, 2, 3, 4)
    wib_all = (wi.astype(f32) * (IDX_HEADS ** -0.5 * IDX_DHEAD ** -0.5)).reshape(
        Bsz, nb, Q_BLOCK, IDX_HEADS).transpose(1, 0, 2, 3)
    k = k.astype(f32)
    v = v.astype(f32)
    ki = ki.astype(f32)
    key_chunk = jnp.arange(S) // CHUNK
    gather = jax.vmap(lambda t, idx: t[idx])

    def block(args):
        qb, qib, wib, blk = args
        q_chunk = (blk * Q_BLOCK + jnp.arange(Q_BLOCK)) // CHUNK
        score = jnp.einsum('bqhs,bqh->bqs',
                           jax.nn.relu(jnp.einsum('bqhd,bsd->bqhs', qib, ki)), wib)
        admissible = key_chunk[None, :] <= q_chunk[:, None]
        score = jnp.where(admissible[None], score, -jnp.inf)
        _, idx = lax.top_k(score, topk)
        k_sel = gather(k, idx)
        v_sel = gather(v, idx)
        valid = key_chunk[idx] <= q_chunk[None, :, None]
        logits = jnp.einsum('bqhd,bqkd->bqhk', qb, k_sel) * (SA_DHEAD ** -0.5)
        logits = jnp.where(valid[:, :, None, :], logits, -jnp.inf)
        p = jax.nn.softmax(logits, axis=-1)
        return jnp.einsum('bqhk,bqkd->bqhd', p, v_sel)

    o = lax.map(block, (qb_all, qib_all, wib_all, jnp.arange(nb)))
    return o.transpose(1, 0, 2, 3, 4).reshape(Bsz, S, SA_WIDTH).astype(dtype)


def hgrn2(q, f, i, g, lb, norm_g):
    dtype = q.dtype
    Bsz, S, _ = q.shape
    f32 = jnp.float32
    fg = lb + (1.0 - lb) * jax.nn.sigmoid(f.astype(f32))
    qc = to_chunks(jax.nn.silu(q.astype(f32)), HG_HEADS, HG_DHEAD)
    kc = to_chunks(1.0 - fg, HG_HEADS, HG_DHEAD)
    vc = to_chunks(i.astype(f32), HG_HEADS, HG_DHEAD)
    Bc = jnp.cumsum(to_chunks(jnp.log(fg), HG_HEADS, HG_DHEAD), axis=-2)
    q_dec = qc * jnp.exp(Bc)
    k_dec = kc * jnp.exp(Bc[..., -1:, :] - Bc)
    g_last = jnp.exp(Bc[..., -1, :])
    causal = jnp.tril(jnp.ones((CHUNK, CHUNK), bool))[..., None]

    def step(state, inp):
        q_c, k_c, v_c, b_c, qd_c, kd_c, gl_c = inp
        dec = jnp.exp(jnp.where(causal, b_c[..., :, None, :] - b_c[..., None, :, :], -jnp.inf))
        A = jnp.einsum('bhck,bhsk,bhcsk->bhcs', q_c, k_c, dec)
        o = jnp.einsum('bhck,bhkv->bhcv', qd_c, state) + jnp.einsum('bhcs,bhsv->bhcv', A, v_c)
        state = gl_c[..., :, None] * state + jnp.einsum('bhck,bhcv->bhkv', kd_c, v_c)
        return state, o

    xs = tuple(jnp.moveaxis(t, 2, 0) for t in (qc, kc, vc, Bc, q_dec, k_dec, g_last))
    s0 = jnp.zeros((Bsz, HG_HEADS, HG_DHEAD, HG_DHEAD), f32)
    _, o = lax.scan(step, s0, xs)
    o = rmsnorm(from_chunks(o), norm_g)
    o = o * jax.nn.silu(g.astype(f32).reshape(Bsz, S, HG_HEADS, HG_DHEAD))
    return o.reshape(Bsz, S, HG_WIDTH).astype(dtype)


def conv_geglu_ffn(h, w_up, conv_w, w_down):
    up = causal_depthwise_conv(h @ w_up, conv_w)
    gate, val = jnp.split(up, 2, axis=-1)
    return (jax.nn.gelu(gate, approximate=True) * val) @ w_down


def setup_inputs(seed: int = 0) -> dict:
    key = jax.random.key(seed)
    ks = jax.random.split(key, 16)
    f32 = jnp.float32
    nrm = lambda k, s: jax.random.normal(k, s, f32)
    x = nrm(ks[0], (BATCH, SEQ, D_MODEL))
    w_in = nrm(ks[1], (DEPTH, D_MODEL, D_IN)) * D_MODEL ** -0.5
    dn_conv = nrm(ks[2], (DEPTH, DN_CONV, 3 * DN_WIDTH)) * DN_CONV ** -0.5
    dn_a_log = jnp.log(jax.random.uniform(ks[3], (DEPTH, DN_HEADS), f32, 1.0, 16.0))
    dt = jnp.exp(jax.random.uniform(ks[4], (DEPTH, DN_HEADS), f32,
                                    math.log(1e-3), math.log(1e-1)))
    dn_dt_bias = dt + jnp.log(-jnp.expm1(-dt))
    dn_norm = 1.0 + 0.02 * nrm(ks[5], (DEPTH, DN_DHEAD))
    hg_lb = 0.1 * nrm(ks[6], (DEPTH, HG_WIDTH))
    hg_norm = 1.0 + 0.02 * nrm(ks[7], (DEPTH, HG_DHEAD))
    w_out = nrm(ks[8], (DEPTH, D_MIX, D_MODEL)) * D_MIX ** -0.5
    g_mix_pre = 1.0 + 0.02 * nrm(ks[9], (DEPTH, D_MODEL))
    g_mix_post = 1.0 + 0.02 * nrm(ks[10], (DEPTH, D_MODEL))
    g_ffn_pre = 1.0 + 0.02 * nrm(ks[11], (DEPTH, D_MODEL))
    g_ffn_post = 1.0 + 0.02 * nrm(ks[12], (DEPTH, D_MODEL))
    ffn_w_up = nrm(ks[13], (DEPTH, D_MODEL, 2 * D_FF)) * D_MODEL ** -0.5
    ffn_conv = nrm(ks[14], (DEPTH, FFN_CONV, 2 * D_FF)) * FFN_CONV ** -0.5
    ffn_w_down = nrm(ks[15], (DEPTH, D_FF, D_MODEL)) * D_FF ** -0.5
    return {'x': x, 'w_in': w_in, 'dn_conv': dn_conv, 'dn_a_log': dn_a_log,
            'dn_dt_bias': dn_dt_bias, 'dn_norm': dn_norm, 'hg_lb': hg_lb, 'hg_norm': hg_norm,
            'w_out': w_out, 'g_mix_pre': g_mix_pre, 'g_mix_post': g_mix_post,
            'g_ffn_pre': g_ffn_pre, 'g_ffn_post': g_ffn_post, 'ffn_w_up': ffn_w_up,
            'ffn_conv': ffn_conv, 'ffn_w_down': ffn_w_down}


def reference(x, w_in, dn_conv, dn_a_log, dn_dt_bias, dn_norm, hg_lb, hg_norm, w_out,
              g_mix_pre, g_mix_post, g_ffn_pre, g_ffn_post, ffn_w_up, ffn_conv, ffn_w_down):
    cs = jnp.cumsum(jax.nn.softmax(hg_lb.astype(jnp.float32), axis=0), axis=0)
    lower_bounds = cs - cs[0:1]
    for l in range(DEPTH):
        h = rmsnorm(x, g_mix_pre[l])
        (a_q, a_k, a_v, a_z, a_b, a_a,
         b_q, b_k, b_v, b_qi, b_ki, b_wi,
         c_q, c_f, c_i, c_g) = jnp.split(h @ w_in[l], SPLIT_POINTS, axis=-1)
        o_a = gated_deltanet(a_q, a_k, a_v, a_z, a_b, a_a, dn_conv[l], dn_a_log[l],
                             dn_dt_bias[l], dn_norm[l])
        o_b = dsa_attention(b_q, b_k, b_v, b_qi, b_ki, b_wi)
        o_c = hgrn2(c_q, c_f, c_i, c_g, lower_bounds[l], hg_norm[l])
        mix = jnp.concatenate([o_a, o_b, o_c], axis=-1) @ w_out[l]
        x = x + rmsnorm(mix, g_mix_post[l])
        h = rmsnorm(x, g_ffn_pre[l])
        x = x + rmsnorm(conv_geglu_ffn(h, ffn_w_up[l], ffn_conv[l], ffn_w_down[l]), g_ffn_post[l])
    return x
```

```python
import numpy as np
import concourse.bass as bass
import concourse.mybir as mybir
from concourse.bass_utils import run_bass_kernel_spmd

F32 = mybir.dt.float32
BF16 = mybir.dt.bfloat16
AF = mybir.ActivationFunctionType
ALU = mybir.AluOpType
AX = mybir.AxisListType

D = 1024
SEQ = 4096
NSEQ = 2
NTOK = NSEQ * SEQ
DEPTH = 2
D_IN = 3788
D_FF = 2816
EPS = 1e-6
NEG = -30000.0

TM_GROUPS = [(1536, 2056), (2376, 2440), (2760, 2764), (3020, 3788)]
TM_W = sum(b - a for a, b in TM_GROUPS)
TM_AZ, TM_AB, TM_AA = 0, 512, 516
TM_BV = 520
TM_WI = 584
TM_CF, TM_CI, TM_CG = 588, 844, 1100
FM_GROUPS = [(i * 128, 128) for i in range(12)] + [(2056, 128), (2184, 128), (2312, 64),
             (2440, 128), (2568, 128), (2696, 64), (2764, 128), (2892, 128), (3020, 128), (3148, 128)]
FM_ROW = {}
_r = 0
for _c, _n in FM_GROUPS:
    FM_ROW[_c] = _r
    _r += _n
FM_H = _r


class Sched:
    def __init__(self, nc):
        self.nc = nc
        self.eng = {"pe": nc.tensor, "act": nc.scalar, "dve": nc.vector, "pool": nc.gpsimd,
                    "sp": nc.sync}
        self.sems = {}
        self.cnt = {}
        for k in ("pe", "act", "dve", "pool"):
            self.sems[k] = nc.alloc_semaphore("s_" + k)
            self.cnt[k] = 0
        self.seen = {k: {} for k in self.eng}
        self.bufs = {}
        self.ninstr = 0

    def _buf(self, key):
        b = self.bufs.get(key)
        if b is None:
            b = {"w": None, "r": {}}
            self.bufs[key] = b
        return b

    def _deps(self, engine, reads, writes):
        deps = {}

        def add(ev, same_ok):
            if ev is None:
                return
            sk, val = ev
            if sk == engine and not same_ok:
                return
            if deps.get(sk, 0) < val:
                deps[sk] = val

        for k in reads:
            b = self._buf(k)
            add(b["w"], engine != "pe")
            if isinstance(k, tuple) and k[0] in ("ps", "psb"):
                for sk, val in b["r"].items():
                    add((sk, val), False)
        for k in writes:
            b = self._buf(k)
            add(b["w"], engine != "pe")
            for sk, val in b["r"].items():
                add((sk, val), False)
        return deps

    def _emit_waits(self, engine, deps):
        e = self.eng[engine]
        seen = self.seen[engine]
        for sk, val in deps.items():
            if seen.get(sk, 0) >= val:
                continue
            e.wait_ge(self.sems[sk], val)
            self.ninstr += 1
            seen[sk] = val

    def _record(self, ev, reads, writes):
        for k in writes:
            b = self._buf(k)
            b["w"] = ev
            b["r"] = {}
        for k in reads:
            b = self._buf(k)
            if b["r"].get(ev[0], 0) < ev[1]:
                b["r"][ev[0]] = ev[1]

    def op(self, engine, fn, reads=(), writes=()):
        deps = self._deps(engine, reads, writes)
        self._emit_waits(engine, deps)
        ins = fn(self.eng[engine])
        self.cnt[engine] += 1
        ins.then_inc(self.sems[engine], 1)
        self.ninstr += 1
        self._record((engine, self.cnt[engine]), reads, writes)

    def dma(self, out, in_, sem, reads=(), writes=(), q="sp"):
        if sem not in self.sems:
            self.sems[sem] = self.nc.alloc_semaphore("d_" + sem)
            self.cnt[sem] = 0
        deps = self._deps(q, reads, writes)
        self._emit_waits(q, deps)
        ins = self.eng[q].dma_start(out=out, in_=in_)
        self.cnt[sem] += 16
        ins.then_inc(self.sems[sem], 16)
        self.ninstr += 1
        self._record((sem, self.cnt[sem]), reads, writes)

    def barrier(self):
        allv = {k: v for k, v in self.cnt.items() if v > 0}
        for engine in self.eng:
            self._emit_waits(engine, dict(allv))
        self.bufs = {}


def bcast_rows(ap2d_row, nparts):
    return ap2d_row.partition_broadcast(nparts)


class Prog:
    def __init__(self, layers=(0, 1), phases="AHDSEF", dbg=()):
        self.nc = nc = bass.Bass("TRN2", target_bir_lowering=False)
        self.S = Sched(nc)
        self.dbg = dbg
        dt = nc.dram_tensor
        self.x_in = dt("x", [NTOK, D], F32, kind="ExternalInput").ap()
        self.w_in = dt("w_in", [DEPTH, D, D_IN], F32, kind="ExternalInput").ap()
        self.dn_conv = dt("dn_conv", [DEPTH, 4, 1536], F32, kind="ExternalInput").ap()
        self.dn_a_log = dt("dn_a_log", [DEPTH, 4], F32, kind="ExternalInput").ap()
        self.dn_dt_bias = dt("dn_dt_bias", [DEPTH, 4], F32, kind="ExternalInput").ap()
        self.dn_norm = dt("dn_norm", [DEPTH, 128], F32, kind="ExternalInput").ap()
        self.hg_lb = dt("hg_lb", [DEPTH, 256], F32, kind="ExternalInput").ap()
        self.hg_norm = dt("hg_norm", [DEPTH, 64], F32, kind="ExternalInput").ap()
        self.w_out = dt("w_out", [DEPTH, D, D], F32, kind="ExternalInput").ap()
        self.g_mix_pre = dt("g_mix_pre", [DEPTH, D], F32, kind="ExternalInput").ap()
        self.g_mix_post = dt("g_mix_post", [DEPTH, D], F32, kind="ExternalInput").ap()
        self.g_ffn_pre = dt("g_ffn_pre", [DEPTH, D], F32, kind="ExternalInput").ap()
        self.g_ffn_post = dt("g_ffn_post", [DEPTH, D], F32, kind="ExternalInput").ap()
        self.w_up = dt("ffn_w_up", [DEPTH, D, 2 * D_FF], F32, kind="ExternalInput").ap()
        self.ffn_conv = dt("ffn_conv", [DEPTH, 3, 2 * D_FF], F32, kind="ExternalInput").ap()
        self.w_down = dt("ffn_w_down", [DEPTH, D_FF, D], F32, kind="ExternalInput").ap()
        self.y = dt("y", [NTOK, D], F32, kind="ExternalOutput").ap()

        def scratch(name, shape, dtype):
            kind = "ExternalOutput" if name in dbg else "Internal"
            return dt(name, shape, dtype, kind=kind).ap()

        self.ptok = scratch("ptok", [NTOK, TM_W], F32)
        self.pfeat = scratch("pfeat", [FM_H, NTOK], F32)
        self.mixin = scratch("mixin", [NTOK, D], F32)
        self.x1 = scratch("x1", [NTOK, D], F32)
        self.h2T = scratch("h2T", [D, NTOK], BF16)
        self.xmid = scratch("xmid", [NTOK, D], F32)

        self.ps = [nc.alloc_psum_tensor("ps%d" % i, [128, 512], F32) for i in range(6)]
        self.psb = [nc.alloc_psum_tensor("psb%d" % i, [128, 1024], BF16) for i in range(2)]
        self.ident_b = nc.alloc_sbuf_tensor("ident_b", [128, 128], BF16)
        self.ident_f = nc.alloc_sbuf_tensor("ident_f", [128, 128], F32)
        self.ones_f = nc.alloc_sbuf_tensor("ones_f", [128, 128], F32)
        self._consts()
        self.sb_base = nc.sbuf_base
        self.layers = layers
        self.phases = phases

    def _consts(self):
        nc, S = self.nc, self.S
        S.op("pool", lambda e: e.memset(self.ones_f[:], 1.0), writes=["ones_f"])
        S.op("pool", lambda e: e.memset(self.ident_f[:], 0.0), writes=["ident_f"])
        S.op("pool", lambda e: e.affine_select(out=self.ident_f[:], in_=self.ident_f[:],
                                                pattern=[[-1, 128]], compare_op=ALU.not_equal,
                                                fill=1.0, base=0, channel_multiplier=1),
             reads=["ident_f"], writes=["ident_f"])
        S.op("dve", lambda e: e.tensor_copy(out=self.ident_b[:], in_=self.ident_f[:]),
             reads=["ident_f"], writes=["ident_b"])

    def sbuf_reset(self):
        self.nc.sbuf_base = self.sb_base

    def sb(self, name, shape, dtype):
        self._uid = getattr(self, "_uid", 0) + 1
        return self.nc.alloc_sbuf_tensor("%s_u%d" % (name, self._uid), shape, dtype)

    def load_bcast(self, name, row_ap, n):
        t = self.sb(name, [128, n], F32)
        self.S.dma(out=t[:], in_=bcast_rows(row_ap, 128), sem="ld_" + name, writes=[name])
        return t

    def load_weight_bf16(self, name, w_ap, K, N, stage, stage_keys):
        S = self.S
        wt = self.sb(name, [128, K, N], BF16)
        CH = stage[0].shape[1]
        i = 0
        for k in range(K):
            for c0 in range(0, N, CH):
                cw = min(CH, N - c0)
                st, sk = stage[i % 2], stage_keys[i % 2]
                S.dma(out=st[:, :cw], in_=w_ap[k * 128:(k + 1) * 128, c0:c0 + cw], sem=sk,
                      writes=[sk])
                eng = ("dve", "pool", "act")[i % 3]
                if eng == "act":
                    S.op("act", lambda e, st=st, k=k, c0=c0, cw=cw: e.copy(
                        out=wt[:, k, c0:c0 + cw], in_=st[:, :cw]), reads=[sk], writes=[name])
                else:
                    S.op(eng, lambda e, st=st, k=k, c0=c0, cw=cw: e.tensor_copy(
                        out=wt[:, k, c0:c0 + cw], in_=st[:, :cw]), reads=[sk], writes=[name])
                i += 1
        return wt

    def evac(self, i, out, in_, reads, writes):
        if i % 2 == 0:
            self.S.op("act", lambda e: e.copy(out=out, in_=in_), reads=reads, writes=writes)
        else:
            self.S.op("dve", lambda e: e.tensor_copy(out=out, in_=in_), reads=reads, writes=writes)

    def norm_transpose(self, src_dram, tok0, gbc, gkey, T, tag, xt, hb, hT, ss, rs, slot,
                       do_norm=True):
        S = self.S
        kx = lambda j: (tag + "xt", slot, j)
        for j in range(4):
            S.dma(out=xt[slot][:, j, :], in_=src_dram[tok0 + j * 128: tok0 + (j + 1) * 128, :],
                  sem="%sxt%d_%d" % (tag, slot, j), writes=[kx(j)])
        kss, krs = (tag + "ss", slot), (tag + "rs", slot)
        if do_norm:
            for j in range(4):
                S.op("act", lambda e, j=j: e.activation(out=T["junk"][:], in_=xt[slot][:, j, :],
                                                         func=AF.Square,
                                                         accum_out=ss[slot][:, j:j + 1]),
                     reads=[kx(j)], writes=[kss])
            S.op("dve", lambda e: e.tensor_scalar(out=rs[slot][:], in0=ss[slot][:],
                                                   scalar1=1.0 / D, scalar2=EPS, op0=ALU.mult,
                                                   op1=ALU.add), reads=[kss], writes=[krs])
            S.op("act", lambda e: e.sqrt(out=rs[slot][:], in_=rs[slot][:]), reads=[krs],
                 writes=[krs])
            S.op("dve", lambda e: e.reciprocal(out=rs[slot][:], in_=rs[slot][:]), reads=[krs],
                 writes=[krs])
        for j in range(4):
            khb = (tag + "hb", j % 2)
            hbj = hb[j % 2]
            if do_norm:
                S.op("dve", lambda e, j=j, hbj=hbj: e.scalar_tensor_tensor(
                    out=hbj[:], in0=xt[slot][:, j, :], scalar=rs[slot][:, j:j + 1], in1=gbc[:],
                    op0=ALU.mult, op1=ALU.mult), reads=[kx(j), krs, gkey], writes=[khb])
            else:
                S.op("pool", lambda e, j=j, hbj=hbj: e.tensor_copy(out=hbj[:],
                                                                  in_=xt[slot][:, j, :]),
                     reads=[kx(j)], writes=[khb])
            pb = self.psb[j % 2]
            kpb = ("psb", j % 2)

            def tr(e, hbj=hbj, pb=pb):
                ins = None
                for k in range(8):
                    ins = e.transpose(out=pb[:, k * 128:(k + 1) * 128],
                                      in_=hbj[:, k * 128:(k + 1) * 128], identity=self.ident_b[:])
                return ins
            S.op("pe", tr, reads=[khb, "ident_b"], writes=[kpb])
            self.evac(j, hT[slot][:, :, j * 128:(j + 1) * 128],
                      pb[:].rearrange("p (k t) -> p k t", k=8), [kpb], [(tag + "hT", slot, j)])

    def phase_A(self, l, xsrc):
        nc, S = self.nc, self.S
        self.sbuf_reset()
        stage = [self.sb("A_stage%d" % i, [128, 3788], F32) for i in range(2)]
        Wi = self.load_weight_bf16("A_Wi", self.w_in[l], 8, D_IN, stage, ["A_stg0", "A_stg1"])
        gbc = self.load_bcast("A_gbc", self.g_mix_pre[l:l + 1, :], D)
        xt = [self.sb("A_xt%d" % i, [128, 4, D], F32) for i in range(2)]
        hb = [self.sb("A_hb%d" % i, [128, D], BF16) for i in range(2)]
        hT = [self.sb("A_hT%d" % i, [128, 8, 512], BF16) for i in range(2)]
        ss = [self.sb("A_ss%d" % i, [128, 4], F32) for i in range(2)]
        rs = [self.sb("A_rs%d" % i, [128, 4], F32) for i in range(2)]
        T = {"junk": self.sb("A_junk", [128, D], F32)}
        ofm = [self.sb("A_ofm%d" % i, [128, 512], F32) for i in range(4)]
        otm = [self.sb("A_otm%d" % i, [128, TM_W], F32) for i in range(2)]
        tmch = []
        off = 0
        for a, b in TM_GROUPS:
            c = a
            while c < b:
                w = min(512, b - c)
                tmch.append((c, w, off))
                off += w
                c += w
        nev = 0
        for blk in range(NTOK // 512):
            slot = blk % 2
            tok0 = blk * 512
            self.norm_transpose(xsrc, tok0, gbc, "A_gbc", T, "A_", xt, hb, hT, ss, rs, slot)
            hkeys = [("A_hT", slot, j) for j in range(4)]
            for gi, (c0, n) in enumerate(FM_GROUPS):
                ps = self.ps[gi % 4]
                kps = ("ps", gi % 4)

                def mm(e, ps=ps, c0=c0, n=n):
                    ins = None
                    for k in range(8):
                        ins = e.matmul(ps[:n, :], lhsT=Wi[:, k, c0:c0 + n], rhs=hT[slot][:, k, :],
                                       start=(k == 0), stop=(k == 7))
                    return ins
                S.op("pe", mm, reads=["A_Wi"] + hkeys, writes=[kps])
                o = ofm[gi % 4]
                ko = ("A_ofm", gi % 4)
                self.evac(nev, o[:n, :], ps[:n, :], [kps], [ko])
                nev += 1
                r0 = FM_ROW[c0]
                S.dma(out=self.pfeat[r0:r0 + n, tok0:tok0 + 512], in_=o[:n, :],
                      sem="A_ofm%d" % (gi % 4), reads=[ko])
            for j in range(4):
                o = otm[j % 2]
                ko = ("A_otm", j % 2)
                for ci, (c, w, dst) in enumerate(tmch):
                    ps = self.ps[4 + ci % 2]
                    kps = ("ps", 4 + ci % 2)

                    def mm(e, ps=ps, c=c, w=w, j=j):
                        ins = None
                        for k in range(8):
                            ins = e.matmul(ps[:, :w], lhsT=hT[slot][:, k, j * 128:(j + 1) * 128],
                                           rhs=Wi[:, k, c:c + w], start=(k == 0), stop=(k == 7))
                        return ins
                    S.op("pe", mm, reads=["A_Wi", hkeys[j]], writes=[kps])
                    self.evac(nev, o[:, dst:dst + w], ps[:, :w], [kps], [(ko, ci)])
                    nev += 1
                S.dma(out=self.ptok[tok0 + j * 128: tok0 + (j + 1) * 128, :], in_=o[:, :],
                      sem="A_otm%d" % (j % 2), reads=[(ko, ci) for ci in range(len(tmch))])
        S.barrier()

    def transp8(self, hbj, khb, dst, kdst, j):
        pb = self.psb[j % 2]
        kpb = ("psb", j % 2)

        def tr(e):
            ins = None
            for k in range(8):
                ins = e.transpose(out=pb[:, k * 128:(k + 1) * 128],
                                  in_=hbj[:, k * 128:(k + 1) * 128], identity=self.ident_b[:])
            return ins
        self.S.op("pe", tr, reads=[khb, "ident_b"], writes=[kpb])
        self.evac(j, dst, pb[:].rearrange("p (k t) -> p k t", k=8), [kpb], [kdst])

    def rstd_from_ss(self, ss, kss, rs, krs, n):
        S = self.S
        S.op("dve", lambda e: e.tensor_scalar(out=rs, in0=ss, scalar1=1.0 / n, scalar2=EPS,
                                               op0=ALU.mult, op1=ALU.add), reads=[kss],
             writes=[krs])
        S.op("act", lambda e: e.sqrt(out=rs, in_=rs), reads=[krs], writes=[krs])
        S.op("dve", lambda e: e.reciprocal(out=rs, in_=rs), reads=[krs], writes=[krs])

    def phase_E(self, l, xsrc):
        S = self.S
        self.sbuf_reset()
        stage = [self.sb("E_stage%d" % i, [128, 1024], F32) for i in range(2)]
        Wo = self.load_weight_bf16("E_Wo", self.w_out[l], 8, D, stage, ["E_stg0", "E_stg1"])
        gpost = self.load_bcast("E_gpost", self.g_mix_post[l:l + 1, :], D)
        gpre = self.load_bcast("E_gpre", self.g_ffn_pre[l:l + 1, :], D)
        xt = [self.sb("E_xt%d" % i, [128, 4, D], F32) for i in range(2)]
        xr = [self.sb("E_xr%d" % i, [128, 4, D], F32) for i in range(2)]
        hb = [self.sb("E_hb%d" % i, [128, D], BF16) for i in range(2)]
        mT = [self.sb("E_mT%d" % i, [128, 8, 512], BF16) for i in range(2)]
        h2s = [self.sb("E_h2s%d" % i, [128, 8, 512], BF16) for i in range(2)]
        yt = [self.sb("E_yt%d" % i, [128, D], F32) for i in range(2)]
        x1t = [self.sb("E_x1t%d" % i, [128, D], F32) for i in range(2)]
        h2b = [self.sb("E_h2b%d" % i, [128, D], BF16) for i in range(2)]
        junk = self.sb("E_junk", [128, D], F32)
        st = [self.sb("E_st%d" % i, [128, 8], F32) for i in range(2)]
        h2T_v = self.h2T.rearrange("(k p) t -> p k t", p=128)
        for blk in range(NTOK // 512):
            slot = blk % 2
            tok0 = blk * 512
            self.norm_transpose(self.mixin, tok0, None, None, None, "E_", xt, hb, mT, None, None,
                                slot, do_norm=False)
            for j in range(4):
                S.dma(out=xr[slot][:, j, :], in_=xsrc[tok0 + j * 128: tok0 + (j + 1) * 128, :],
                      sem="E_xr%d_%d" % (slot, j), writes=[("E_xr", slot, j)])
            for j in range(4):
                p2 = j % 2
                kss, krs = ("E_ss", p2), ("E_rs", p2)
                for hf in range(2):
                    ps = self.ps[2 * p2 + hf]
                    kps = ("ps", 2 * p2 + hf)

                    def mm(e, ps=ps, hf=hf, j=j):
                        ins = None
                        for k in range(8):
                            ins = e.matmul(ps[:, :], lhsT=mT[slot][:, k, j * 128:(j + 1) * 128],
                                           rhs=Wo[:, k, hf * 512:(hf + 1) * 512], start=(k == 0),
                                           stop=(k == 7))
                        return ins
                    S.op("pe", mm, reads=["E_Wo", ("E_hT", slot, j)], writes=[kps])
                    S.op("act", lambda e, ps=ps, hf=hf, p2=p2: e.activation(
                        out=junk[:, :512], in_=ps[:, :], func=AF.Square,
                        accum_out=st[p2][:, hf:hf + 1]), reads=[kps], writes=[(kss, hf)])
                S.op("dve", lambda e, p2=p2: e.tensor_tensor(out=st[p2][:, 2:3], in0=st[p2][:, 0:1],
                                                              in1=st[p2][:, 1:2], op=ALU.add),
                     reads=[(kss, 0), (kss, 1)], writes=[kss])
                self.rstd_from_ss(st[p2][:, 2:3], kss, st[p2][:, 3:4], krs, D)
                kyt = ("E_yt", p2)
                for hf in range(2):
                    ps = self.ps[2 * p2 + hf]
                    kps = ("ps", 2 * p2 + hf)
                    S.op("act", lambda e, ps=ps, hf=hf, p2=p2: e.activation(
                        out=yt[p2][:, hf * 512:(hf + 1) * 512], in_=ps[:, :], func=AF.Copy,
                        scale=st[p2][:, 3:4]), reads=[kps, krs], writes=[(kyt, hf)])
                S.op("dve", lambda e, p2=p2: e.tensor_tensor(out=yt[p2][:], in0=yt[p2][:],
                                                              in1=gpost[:], op=ALU.mult),
                     reads=[(kyt, 0), (kyt, 1), "E_gpost"], writes=[kyt])
                kx1 = ("E_x1t", p2)
                S.op("pool", lambda e, p2=p2, j=j: e.tensor_tensor(out=x1t[p2][:], in0=yt[p2][:],
                                                                    in1=xr[slot][:, j, :],
                                                                    op=ALU.add),
                     reads=[kyt, ("E_xr", slot, j)], writes=[kx1])
                S.dma(out=self.x1[tok0 + j * 128: tok0 + (j + 1) * 128, :], in_=x1t[p2][:],
                      sem="E_x1t%d" % p2, reads=[kx1])
                kss2, krs2 = ("E_ss2", p2), ("E_rs2", p2)
                S.op("act", lambda e, p2=p2: e.activation(out=junk[:], in_=x1t[p2][:],
                                                           func=AF.Square,
                                                           accum_out=st[p2][:, 4:5]),
                     reads=[kx1], writes=[kss2])
                self.rstd_from_ss(st[p2][:, 4:5], kss2, st[p2][:, 5:6], krs2, D)
                kh2b = ("E_h2b", p2)
                S.op("dve", lambda e, p2=p2: e.scalar_tensor_tensor(
                    out=h2b[p2][:], in0=x1t[p2][:], scalar=st[p2][:, 5:6], in1=gpre[:],
                    op0=ALU.mult, op1=ALU.mult), reads=[kx1, krs2, "E_gpre"], writes=[kh2b])
                self.transp8(h2b[p2], kh2b, h2s[slot][:, :, j * 128:(j + 1) * 128],
                             ("E_h2s", slot, j), j)
            S.dma(out=h2T_v[:, :, tok0:tok0 + 512], in_=h2s[slot][:],
                  sem="E_h2s%d" % slot, reads=[("E_h2s", slot, j) for j in range(4)])
        S.barrier()

    def load_convw(self, name, conv_ap, ntaps, ntiles):
        S = self.S
        raw = self.sb(name + "_raw", [ntiles, ntaps, 128], F32)
        cw = self.sb(name, [128, ntaps, ntiles], F32)
        S.dma(out=raw[:], in_=conv_ap.rearrange("j (t p) -> t j p", p=128), sem="ld_" + name,
              writes=[name + "_raw"])
        for j in range(ntaps):
            ps = self.ps[j % 2]
            kps = ("ps", j % 2)
            S.op("pe", lambda e, j=j, ps=ps: e.transpose(out=ps[:, :ntiles], in_=raw[:, j, :],
                                                         identity=self.ident_f[:ntiles, :ntiles]),
                 reads=[name + "_raw", "ident_f"], writes=[kps])
            S.op("dve", lambda e, j=j, ps=ps: e.tensor_copy(out=cw[:, j, :], in_=ps[:, :ntiles]),
                 reads=[kps], writes=[name])
        return cw

    def phase_F(self, l, dst):
        S = self.S
        self.sbuf_reset()
        NT = 22
        stage = [self.sb("F_stage%d" % i, [128, 512], F32) for i in range(2)]
        Wu = self.load_weight_bf16("F_Wu", self.w_up[l], 8, 2 * D_FF, stage, ["F_stg0", "F_stg1"])
        Wd = self.load_weight_bf16("F_Wd", self.w_down[l], NT, D, stage, ["F_stg0", "F_stg1"])
        gpost = self.load_bcast("F_gpost", self.g_ffn_post[l:l + 1, :], D)
        cw = self.load_convw("F_cw", self.ffn_conv[l], 3, 2 * NT)
        hT = self.sb("F_hT", [128, 8, 512], BF16)
        gT = self.sb("F_gT", [128, NT, 512], BF16)
        U = [self.sb("F_U%d" % i, [128, 514], F32) for i in range(2)]
        C = [self.sb("F_C%d" % i, [128, 512], F32) for i in range(2)]
        GL = self.sb("F_GL", [128, 512], F32)
        halo = self.sb("F_halo", [128, 2 * NT, 2], F32)
        x1t = [self.sb("F_x1t%d" % i, [128, D], F32) for i in range(2)]
        yt = [self.sb("F_yt%d" % i, [128, D], F32) for i in range(2)]
        junk = self.sb("F_junk", [128, 512], F32)
        st = [self.sb("F_st%d" % i, [128, 8], F32) for i in range(2)]
        h2T_v = self.h2T.rearrange("(k p) t -> p k t", p=128)
        for blk in range(NTOK // 512):
            tok0 = blk * 512
            if blk % (SEQ // 512) == 0:
                S.op("pool", lambda e: e.memset(halo[:], 0.0), writes=["F_halo"])
            S.dma(out=hT[:], in_=h2T_v[:, :, tok0:tok0 + 512], sem="F_hT", writes=["F_hT"])
            for i in range(NT):
                for gv in range(2):
                    ti = gv * NT + i
                    c0 = ti * 128
                    ps = self.ps[gv * 2 + i % 2]
                    kps = ("ps", gv * 2 + i % 2)

                    def mm(e, ps=ps, c0=c0):
                        ins = None
                        for k in range(8):
                            ins = e.matmul(ps[:, :], lhsT=Wu[:, k, c0:c0 + 128], rhs=hT[:, k, :],
                                           start=(k == 0), stop=(k == 7))
                        return ins
                    S.op("pe", mm, reads=["F_Wu", "F_hT"], writes=[kps])
                    u, ku = U[gv], ("F_U", gv)
                    c, kc = C[gv], ("F_C", gv)
                    S.op("act", lambda e, u=u, ps=ps: e.copy(out=u[:, 2:514], in_=ps[:, :]),
                         reads=[kps], writes=[(ku, "b")])
                    S.op("pool", lambda e, u=u, ti=ti: e.tensor_copy(out=u[:, 0:2],
                                                                     in_=halo[:, ti, :]),
                         reads=["F_halo"], writes=[(ku, "h")])
                    S.op("act", lambda e, u=u, c=c, ti=ti: e.activation(
                        out=c[:], in_=u[:, 0:512], func=AF.Copy, scale=cw[:, 0, ti:ti + 1]),
                        reads=[(ku, "b"), (ku, "h"), "F_cw"], writes=[kc])
                    for tap in (1, 2):
                        S.op("dve", lambda e, u=u, c=c, ti=ti, tap=tap: e.scalar_tensor_tensor(
                            out=c[:], in0=u[:, tap:tap + 512], scalar=cw[:, tap, ti:ti + 1],
                            in1=c[:], op0=ALU.mult, op1=ALU.add),
                            reads=[(ku, "b"), (ku, "h"), "F_cw", kc], writes=[kc])
                    S.op("pool", lambda e, u=u, ti=ti: e.tensor_copy(out=halo[:, ti, :],
                                                                     in_=u[:, 512:514]),
                         reads=[(ku, "b")], writes=["F_halo"])
                S.op("act", lambda e: e.activation(out=GL[:], in_=C[0][:],
                                                    func=AF.Gelu_apprx_tanh),
                     reads=[("F_C", 0)], writes=["F_GL"])
                S.op("pool", lambda e, i=i: e.tensor_tensor(out=gT[:, i, :], in0=GL[:],
                                                             in1=C[1][:], op=ALU.mult),
                     reads=["F_GL", ("F_C", 1)], writes=[("F_gT", i)])
            gkeys = [("F_gT", i) for i in range(NT)]
            for j in range(4):
                p2 = j % 2
                S.dma(out=x1t[p2][:], in_=self.x1[tok0 + j * 128: tok0 + (j + 1) * 128, :],
                      sem="F_x1t%d" % p2, writes=[("F_x1t", p2)])
                kss, krs = ("F_ss", p2), ("F_rs", p2)
                for hf in range(2):
                    ps = self.ps[4 + hf]
                    kps = ("ps", 4 + hf)

                    def mm(e, ps=ps, hf=hf, j=j):
                        ins = None
                        for k in range(NT):
                            ins = e.matmul(ps[:, :], lhsT=gT[:, k, j * 128:(j + 1) * 128],
                                           rhs=Wd[:, k, hf * 512:(hf + 1) * 512], start=(k == 0),
                                           stop=(k == NT - 1))
                        return ins
                    S.op("pe", mm, reads=["F_Wd"] + gkeys, writes=[kps])
                    S.op("act", lambda e, ps=ps, hf=hf, p2=p2: e.activation(
                        out=junk[:, :], in_=ps[:, :], func=AF.Square,
                        accum_out=st[p2][:, hf:hf + 1]), reads=[kps], writes=[(kss, hf)])
                S.op("dve", lambda e, p2=p2: e.tensor_tensor(out=st[p2][:, 2:3], in0=st[p2][:, 0:1],
                                                              in1=st[p2][:, 1:2], op=ALU.add),
                     reads=[(kss, 0), (kss, 1)], writes=[kss])
                self.rstd_from_ss(st[p2][:, 2:3], kss, st[p2][:, 3:4], krs, D)
                kyt = ("F_yt", p2)
                for hf in range(2):
                    ps = self.ps[4 + hf]
                    kps = ("ps", 4 + hf)
                    S.op("act", lambda e, ps=ps, hf=hf, p2=p2: e.activation(
                        out=yt[p2][:, hf * 512:(hf + 1) * 512], in_=ps[:, :], func=AF.Copy,
                        scale=st[p2][:, 3:4]), reads=[kps, krs], writes=[(kyt, hf)])
                S.op("dve", lambda e, p2=p2: e.tensor_tensor(out=yt[p2][:], in0=yt[p2][:],
                                                              in1=gpost[:], op=ALU.mult),
                     reads=[(kyt, 0), (kyt, 1), "F_gpost"], writes=[kyt])
                S.op("pool", lambda e, p2=p2: e.tensor_tensor(out=yt[p2][:], in0=yt[p2][:],
                                                               in1=x1t[p2][:], op=ALU.add),
                     reads=[kyt, ("F_x1t", p2)], writes=[kyt])
                S.dma(out=dst[tok0 + j * 128: tok0 + (j + 1) * 128, :], in_=yt[p2][:],
                      sem="F_yt%d" % p2, reads=[kyt])
        S.barrier()

    def phase_H(self, l):
        S = self.S
        self.sbuf_reset()
        sb = self.sb
        UU = sb("H_UU", [64, 128], F32)
        UL = sb("H_UL", [64, 64], F32)
        tmpm = sb("H_tmpm", [64, 64], F32)
        S.op("pool", lambda e: e.memset(UU[:], 1.0), writes=["H_UU"])
        S.op("pool", lambda e: e.affine_select(out=UU[:, 0:64], in_=UU[:, 0:64], pattern=[[1, 64]],
                                                compare_op=ALU.is_ge, fill=0.0, base=0,
                                                channel_multiplier=-1),
             reads=["H_UU"], writes=["H_UU"])
        S.op("pool", lambda e: e.memset(tmpm[:], 1.0), writes=["H_tmpm"])
        S.op("pool", lambda e: e.affine_select(out=tmpm[:], in_=tmpm[:], pattern=[[0, 64]],
                                                compare_op=ALU.is_ge, fill=0.0, base=31,
                                                channel_multiplier=-1),
             reads=["H_tmpm"], writes=["H_tmpm"])
        S.op("pool", lambda e: e.tensor_tensor(out=UU[:, 64:128], in0=UU[:, 0:64], in1=tmpm[:],
                                                op=ALU.subtract),
             reads=["H_UU", "H_tmpm"], writes=["H_UU"])
        S.op("pool", lambda e: e.memset(UL[:], 1.0), writes=["H_UL"])
        S.op("pool", lambda e: e.affine_select(out=UL[:], in_=UL[:], pattern=[[-1, 64]],
                                                compare_op=ALU.is_gt, fill=0.0, base=0,
                                                channel_multiplier=1),
             reads=["H_UL"], writes=["H_UL"])
        lb = sb("H_lb", [64, 256], F32)
        oml = sb("H_oml", [64, 256], F32)
        if l == 0:
            S.op("pool", lambda e: e.memset(lb[:], 0.0), writes=["H_lb"])
        else:
            r0 = sb("H_r0", [64, 256], F32)
            S.dma(out=r0[:], in_=bcast_rows(self.hg_lb[0:1, :], 64), sem="H_r0", writes=["H_r0"])
            S.dma(out=lb[:], in_=bcast_rows(self.hg_lb[1:2, :], 64), sem="H_lbl", writes=["H_lb"])
            S.op("dve", lambda e: e.tensor_tensor(out=lb[:], in0=lb[:], in1=r0[:], op=ALU.subtract),
                 reads=["H_lb", "H_r0"], writes=["H_lb"])
            S.op("act", lambda e: e.activation(out=lb[:], in_=lb[:], func=AF.Sigmoid),
                 reads=["H_lb"], writes=["H_lb"])
        S.op("dve", lambda e: e.tensor_scalar(out=oml[:], in0=lb[:], scalar1=-1.0, scalar2=1.0,
                                               op0=ALU.mult, op1=ALU.add),
             reads=["H_lb"], writes=["H_oml"])
        lbT = sb("H_lbT", [64, 4, 2], F32)
        for h in range(4):
            for which, src, ksrc in ((0, lb, "H_lb"), (1, oml, "H_oml")):
                ps = self.ps[(2 * h + which) % 4]
                kps = ("ps", (2 * h + which) % 4)
                S.op("pe", lambda e, ps=ps, src=src, h=h: e.transpose(
                    out=ps[:64, :64], in_=src[:, h * 64:(h + 1) * 64],
                    identity=self.ident_f[:64, :64]), reads=[ksrc, "ident_f"], writes=[kps])
                S.op("dve", lambda e, ps=ps, h=h, which=which: e.tensor_copy(
                    out=lbT[:, h, which:which + 1], in_=ps[:64, 0:1]), reads=[kps],
                    writes=["H_lbT"])
        ng = sb("H_ng", [64, 64], F32)
        S.dma(out=ng[:], in_=bcast_rows(self.hg_norm[l:l + 1, :], 64), sem="H_ng", writes=["H_ng"])

        qf = [sb("H_qf%d" % i, [64, 2, 4, 512], F32) for i in range(2)]
        sq = [sb("H_sq%d" % i, [64, 4, 512], F32) for i in range(2)]
        kc = [sb("H_kc%d" % i, [64, 4, 512], F32) for i in range(2)]
        tk = [sb("H_tk%d" % i, [64, 768], F32) for i in range(3)]
        St = [sb("H_S%d" % i, [64, 4, 64], F32) for i in range(2)]
        Sb = [sb("H_Sb%d" % i, [64, 4, 64], BF16) for i in range(2)]
        pf_q = self.pfeat[FM_ROW[2764]:FM_ROW[2764] + 256, :].rearrange("(h k) t -> k h t", k=64)
        pf_f = self.pfeat[FM_ROW[3020]:FM_ROW[3020] + 256, :].rearrange("(h k) t -> k h t", k=64)
        NB = 3

        def t3(name, shape, dtype):
            return [sb("%s%d" % (name, i), shape, dtype) for i in range(NB)]
        sig, fg, logf, kct, kd = (t3("H_sig", [64, 256], F32), t3("H_fg", [64, 256], F32),
                                  t3("H_logf", [64, 256], F32), t3("H_kct", [64, 256], F32),
                                  t3("H_kd", [64, 256], BF16))
        vb = t3("H_vb", [64, 256], BF16)
        gs = t3("H_gs", [64, 4, 64], F32)
        EB, EN = t3("H_EB", [64, 4, 128], F32), t3("H_EN", [64, 4, 64], F32)
        qd, qp, kp = (t3("H_qd", [64, 4, 64], BF16), t3("H_qp", [64, 4, 64], BF16),
                      t3("H_kp", [64, 4, 64], BF16))
        Am = t3("H_Am", [64, 4, 64], BF16)
        osb, osq = t3("H_osb", [64, 4, 64], F32), t3("H_osq", [64, 4, 64], F32)
        stt = t3("H_stt", [64, 8], F32)
        stmp = t3("H_stmp", [64, 4, 64], F32)
        it = 0
        for blk8 in range(SEQ // 512):
            for s in range(NSEQ):
                slot = (blk8 * NSEQ + s) % 2
                tb = s * SEQ + blk8 * 512
                kqf = ("H_qf", slot)
                S.dma(out=qf[slot][:, 0, :, :], in_=pf_q[:, :, tb:tb + 512], sem="H_qfq%d" % slot,
                      writes=[(kqf, 0)])
                S.dma(out=qf[slot][:, 1, :, :], in_=pf_f[:, :, tb:tb + 512], sem="H_qff%d" % slot,
                      writes=[(kqf, 1)])
                S.op("act", lambda e, slot=slot: e.activation(out=sq[slot][:], in_=qf[slot][:, 0],
                                                               func=AF.Silu),
                     reads=[(kqf, 0)], writes=[("H_sq", slot)])
                S.op("act", lambda e, slot=slot: e.activation(out=kc[slot][:], in_=qf[slot][:, 1],
                                                               func=AF.Sigmoid),
                     reads=[(kqf, 1)], writes=[("H_kc", slot)])
                for h in range(4):
                    S.op("dve", lambda e, slot=slot, h=h: e.tensor_scalar(
                        out=kc[slot][:, h, :], in0=kc[slot][:, h, :], scalar1=lbT[:, h, 1:2],
                        scalar2=-1.0, op0=ALU.mult, op1=ALU.mult),
                        reads=[("H_kc", slot), "H_lbT"], writes=[("H_kc", slot)])
                    S.op("dve", lambda e, slot=slot, h=h: e.tensor_scalar(
                        out=kc[slot][:, h, :], in0=kc[slot][:, h, :], scalar1=lbT[:, h, 1:2],
                        scalar2=None, op0=ALU.add),
                        reads=[("H_kc", slot), "H_lbT"], writes=[("H_kc", slot)])
                for cn in range(8):
                    b = it % NB
                    it += 1
                    c0 = cn * 64
                    tok = tb + c0
                    ktk = ("H_tk", b)
                    S.dma(out=tk[b][:], in_=self.ptok[tok:tok + 64, TM_CF:TM_CF + 768],
                          sem="H_tk%d" % b, writes=[ktk])
                    first = (blk8 == 0 and cn == 0)
                    S.op("act", lambda e, b=b: e.activation(out=sig[b][:], in_=tk[b][:, 0:256],
                                                             func=AF.Sigmoid),
                         reads=[ktk], writes=[("H_sig", b)])
                    S.op("dve", lambda e, b=b: e.tensor_tensor(out=fg[b][:], in0=sig[b][:],
                                                                in1=oml[:], op=ALU.mult),
                         reads=[("H_sig", b), "H_oml"], writes=[("H_fg", b)])
                    S.op("dve", lambda e, b=b: e.tensor_tensor(out=fg[b][:], in0=fg[b][:],
                                                                in1=lb[:], op=ALU.add),
                         reads=[("H_fg", b), "H_lb"], writes=[("H_fg", b)])
                    S.op("act", lambda e, b=b: e.activation(out=logf[b][:], in_=fg[b][:],
                                                             func=AF.Ln),
                         reads=[("H_fg", b)], writes=[("H_logf", b)])
                    S.op("pool", lambda e, b=b: e.tensor_scalar(out=kct[b][:], in0=fg[b][:],
                                                                 scalar1=-1.0, scalar2=1.0,
                                                                 op0=ALU.mult, op1=ALU.add),
                         reads=[("H_fg", b)], writes=[("H_kct", b)])
                    S.op("pool", lambda e, b=b: e.tensor_copy(out=vb[b][:], in_=tk[b][:, 256:512]),
                         reads=[ktk], writes=[("H_vb", b)])
                    S.op("act", lambda e, b=b: e.activation(
                        out=gs[b][:], in_=tk[b][:, 512:768].rearrange("p (h v) -> p h v", h=4),
                        func=AF.Silu), reads=[ktk], writes=[("H_gs", b)])
                    S.op("pool", lambda e, b=b: e.tensor_tensor(
                        out=gs[b][:], in0=gs[b][:],
                        in1=ng[:].unsqueeze(1).to_broadcast([64, 4, 64]), op=ALU.mult),
                        reads=[("H_gs", b), "H_ng"], writes=[("H_gs", b)])
                    p0, kp0 = self.ps[0], ("ps", 0)

                    def mmb(e, b=b, p0=p0):
                        ins = None
                        for h in range(4):
                            ins = e.matmul(p0[:64, h * 128:(h + 1) * 128],
                                           lhsT=logf[b][:, h * 64:(h + 1) * 64], rhs=UU[:, :],
                                           start=True, stop=True)
                        return ins
                    S.op("pe", mmb, reads=[("H_logf", b), "H_UU"], writes=[kp0])
                    p0v = p0[:64, :].rearrange("p (h t) -> p h t", h=4)
                    S.op("act", lambda e, b=b, p0v=p0v: e.activation(out=EB[b][:], in_=p0v,
                                                                      func=AF.Exp),
                         reads=[kp0], writes=[("H_EB", b)])
                    S.op("act", lambda e, b=b, p0v=p0v: e.activation(out=EN[b][:],
                                                                      in_=p0v[:, :, 64:128],
                                                                      func=AF.Exp, scale=-1.0),
                         reads=[kp0], writes=[("H_EN", b)])
                    p1, kp1 = self.ps[1], ("ps", 1)
                    S.op("pe", lambda e, b=b, p1=p1: e.matmul(p1[:64, :256], lhsT=UL[:, :],
                                                              rhs=logf[b][:, :], start=True,
                                                              stop=True),
                         reads=[("H_logf", b), "H_UL"], writes=[kp1])
                    S.op("act", lambda e, b=b, p1=p1: e.activation(out=sig[b][:], in_=p1[:64, :256],
                                                                    func=AF.Exp),
                         reads=[kp1], writes=[("H_sig", b)])
                    S.op("dve", lambda e, b=b: e.tensor_tensor(out=kd[b][:], in0=sig[b][:],
                                                                in1=kct[b][:], op=ALU.mult),
                         reads=[("H_sig", b), ("H_kct", b)], writes=[("H_kd", b)])
                    sqv = sq[slot][:, :, c0:c0 + 64]
                    kcv = kc[slot][:, :, c0:c0 + 64]
                    S.op("dve", lambda e, b=b, sqv=sqv: e.tensor_tensor(
                        out=qd[b][:], in0=sqv, in1=EB[b][:, :, 0:64], op=ALU.mult),
                        reads=[("H_sq", slot), ("H_EB", b)], writes=[("H_qd", b)])
                    S.op("pool", lambda e, b=b, sqv=sqv: e.tensor_tensor(
                        out=qp[b][:], in0=sqv, in1=EB[b][:, :, 64:128], op=ALU.mult),
                        reads=[("H_sq", slot), ("H_EB", b)], writes=[("H_qp", b)])
                    S.op("dve", lambda e, b=b, kcv=kcv: e.tensor_tensor(
                        out=kp[b][:], in0=kcv, in1=EN[b][:], op=ALU.mult),
                        reads=[("H_kc", slot), ("H_EN", b)], writes=[("H_kp", b)])
                    p2, kp2 = self.ps[2], ("ps", 2)

                    def mma(e, b=b, p2=p2):
                        ins = None
                        for h in range(4):
                            ins = e.matmul(p2[:64, h * 64:(h + 1) * 64], lhsT=kp[b][:, h, :],
                                           rhs=qp[b][:, h, :], start=True, stop=True)
                        return ins
                    S.op("pe", mma, reads=[("H_kp", b), ("H_qp", b)], writes=[kp2])
                    S.op("dve", lambda e, b=b, p2=p2: e.tensor_tensor(
                        out=Am[b][:], in0=p2[:64, :256].rearrange("p (h c) -> p h c", h=4),
                        in1=UU[:, 0:64].unsqueeze(1).to_broadcast([64, 4, 64]), op=ALU.mult),
                        reads=[kp2, "H_UU"], writes=[("H_Am", b)])
                    if first:
                        S.op("pool", lambda e, s=s: e.memset(St[s][:], 0.0), writes=[("H_S", s)])
                        S.op("pool", lambda e, s=s: e.memset(Sb[s][:], 0.0), writes=[("H_Sb", s)])
                    p3, kp3 = self.ps[3], ("ps", 3)

                    def mmo(e, b=b, p3=p3, s=s):
                        ins = None
                        for h in range(4):
                            e.matmul(p3[:64, h * 64:(h + 1) * 64], lhsT=qd[b][:, h, :],
                                     rhs=Sb[s][:, h, :], start=True, stop=False)
                            ins = e.matmul(p3[:64, h * 64:(h + 1) * 64], lhsT=Am[b][:, h, :],
                                           rhs=vb[b][:, h * 64:(h + 1) * 64], start=False, stop=True)
                        return ins
                    S.op("pe", mmo, reads=[("H_qd", b), ("H_Sb", s), ("H_Am", b), ("H_vb", b)],
                         writes=[kp3])
                    p4, kp4 = self.ps[4], ("ps", 4)

                    def mms(e, b=b, p4=p4):
                        ins = None
                        for h in range(4):
                            ins = e.matmul(p4[:64, h * 64:(h + 1) * 64],
                                           lhsT=kd[b][:, h * 64:(h + 1) * 64],
                                           rhs=vb[b][:, h * 64:(h + 1) * 64], start=True, stop=True)
                        return ins
                    S.op("pe", mms, reads=[("H_kd", b), ("H_vb", b)], writes=[kp4])
                    S.op("dve", lambda e, b=b, s=s: e.tensor_tensor(
                        out=stmp[b][:], in0=St[s][:],
                        in1=EB[b][:, :, 63:64].to_broadcast([64, 4, 64]), op=ALU.mult),
                        reads=[("H_S", s), ("H_EB", b)], writes=[("H_stmp", b)])
                    S.op("dve", lambda e, b=b, s=s, p4=p4: e.tensor_tensor(
                        out=St[s][:], in0=stmp[b][:],
                        in1=p4[:64, :256].rearrange("p (h v) -> p h v", h=4), op=ALU.add),
                        reads=[("H_stmp", b), kp4], writes=[("H_S", s)])
                    S.op("act", lambda e, s=s: e.copy(out=Sb[s][:], in_=St[s][:]),
                         reads=[("H_S", s)], writes=[("H_Sb", s)])
                    S.op("act", lambda e, b=b, p3=p3: e.copy(
                        out=osb[b][:], in_=p3[:64, :256].rearrange("p (h v) -> p h v", h=4)),
                        reads=[kp3], writes=[("H_osb", b)])
                    S.op("pool", lambda e, b=b: e.tensor_tensor(out=osq[b][:], in0=osb[b][:],
                                                                 in1=osb[b][:], op=ALU.mult),
                         reads=[("H_osb", b)], writes=[("H_osq", b)])
                    S.op("dve", lambda e, b=b: e.tensor_reduce(out=stt[b][:, 0:4], in_=osq[b][:],
                                                                axis=AX.X, op=ALU.add),
                         reads=[("H_osq", b)], writes=[("H_stt", b)])
                    self.rstd_from_ss(stt[b][:, 0:4], ("H_stt", b), stt[b][:, 4:8], ("H_rs", b), 64)
                    S.op("dve", lambda e, b=b: e.tensor_tensor(
                        out=osb[b][:], in0=osb[b][:],
                        in1=stt[b][:, 4:8].unsqueeze(2).to_broadcast([64, 4, 64]), op=ALU.mult),
                        reads=[("H_osb", b), ("H_rs", b)], writes=[("H_osb", b)])
                    S.op("pool", lambda e, b=b: e.tensor_tensor(out=osq[b][:], in0=osb[b][:],
                                                                 in1=gs[b][:], op=ALU.mult),
                         reads=[("H_osb", b), ("H_gs", b)], writes=[("H_osq", b)])
                    S.dma(out=self.mixin[tok:tok + 64, 768:1024],
                          in_=osq[b][:].rearrange("p h v -> p (h v)"), sem="H_out%d" % b,
                          reads=[("H_osq", b)])
        S.barrier()

    def phase_S(self, l):
        S = self.S
        self.sbuf_reset()
        sb = self.sb
        BIG = -1.0e30
        stg = sb("S_stg", [64, SEQ], F32)
        kiT = sb("S_kiT", [64, SEQ], F32)
        kT = sb("S_kT", [64, SEQ], BF16)
        vst = sb("S_vst", [128, 32, 64], F32)
        v1 = sb("S_v1", [128, 32, 65], BF16)
        qq = [sb("S_qq%d" % i, [64, 2, 4, 512], F32) for i in range(2)]
        qqb = [sb("S_qqb%d" % i, [64, 2, 4, 512], BF16) for i in range(2)]
        acc = [sb("S_acc%d" % i, [128, SEQ], F32) for i in range(2)]
        wk = sb("S_wk", [128, SEQ], F32)
        selb = [sb("S_selb%d" % i, [128, SEQ], BF16) for i in range(2)]
        rr = [sb("S_rr%d" % i, [128, 512], F32) for i in range(3)]
        pT = [sb("S_pT%d" % i, [128, 512], BF16) for i in range(3)]
        wi = [sb("S_wi%d" % i, [128, 12], F32) for i in range(2)]
        m8 = [sb("S_m8%d" % i, [128, 8], F32) for i in range(2)]
        oT = [sb("S_oT%d" % i, [65, 512], F32) for i in range(2)]
        osb = [sb("S_osb%d" % i, [128, 4, 64], F32) for i in range(2)]
        rc = [sb("S_rc%d" % i, [128, 4, 1], F32) for i in range(2)]
        pf_q = self.pfeat[FM_ROW[2056]:FM_ROW[2056] + 256, :].rearrange("(h k) t -> k h t", k=64)
        pf_qi = self.pfeat[FM_ROW[2440]:FM_ROW[2440] + 256, :].rearrange("(h k) t -> k h t", k=64)
        pf_k = self.pfeat[FM_ROW[2312]:FM_ROW[2312] + 64, :]
        pf_ki = self.pfeat[FM_ROW[2696]:FM_ROW[2696] + 64, :]
        nr = 0
        tbi = wk[:].bitcast(mybir.dt.int32)
        tb = sb("S_tb", [128, SEQ], F32)
        S.op("pool", lambda e: e.iota(tbi, pattern=[[1, SEQ]], base=0, channel_multiplier=0),
             writes=["S_wk"])
        S.op("dve", lambda e: e.tensor_copy(out=tb[:], in_=tbi), reads=["S_wk"],
             writes=["S_tb"])
        S.op("dve", lambda e: e.tensor_scalar(out=tb[:], in0=tb[:], scalar1=-1.0e-35, scalar2=None,
                                               op0=ALU.mult), reads=["S_tb"], writes=["S_tb"])
        for s in range(NSEQ):
            t0 = s * SEQ
            S.dma(out=kiT[:], in_=pf_ki[:, t0:t0 + SEQ], sem="S_kiT", writes=["S_kiT"])
            S.dma(out=stg[:], in_=pf_k[:, t0:t0 + SEQ], sem="S_stg", writes=["S_stg"])
            S.op("pool", lambda e: e.tensor_copy(out=kT[:], in_=stg[:]), reads=["S_stg"],
                 writes=["S_kT"])
            for q4 in range(4):
                S.dma(out=vst[:, q4 * 8:(q4 + 1) * 8, :],
                      in_=self.ptok[t0 + q4 * 1024: t0 + (q4 + 1) * 1024,
                                    TM_BV:TM_BV + 64].rearrange("(kt p) d -> p kt d", p=128),
                      sem="S_vst", writes=[("S_vst", q4)])
            S.op("pool", lambda e: e.memset(v1[:], 1.0), writes=["S_v1"])
            S.op("pool", lambda e: e.tensor_copy(out=v1[:, :, 0:64], in_=vst[:]),
                 reads=[("S_vst", q4) for q4 in range(4)], writes=["S_v1"])
            for j in range(SEQ // 128):
                a2 = j % 2
                tq = t0 + j * 128
                NK = (j + 1) * 128
                if j % 4 == 0:
                    slot = (j // 4) % 2
                    kqq = ("S_qq", slot)
                    S.dma(out=qq[slot][:, 0], in_=pf_q[:, :, tq:tq + 512], sem="S_qq%da" % slot,
                          writes=[(kqq, 0)])
                    S.dma(out=qq[slot][:, 1], in_=pf_qi[:, :, tq:tq + 512], sem="S_qq%db" % slot,
                          writes=[(kqq, 1)])
                    S.op("act", lambda e, slot=slot: e.copy(out=qqb[slot][:], in_=qq[slot][:]),
                         reads=[(kqq, 0), (kqq, 1)], writes=[("S_qqb", slot)])
                c0 = (j % 4) * 128
                kwi = ("S_wi", a2)
                S.dma(out=wi[a2][:, 0:4], in_=self.ptok[tq:tq + 128, TM_WI:TM_WI + 4],
                      sem="S_wi%d" % a2, writes=[(kwi, 0)])
                S.op("act", lambda e, a2=a2: e.activation(out=wi[a2][:, 4:8], in_=wi[a2][:, 0:4],
                                                           func=AF.Abs),
                     reads=[(kwi, 0)], writes=[(kwi, 1)])
                S.op("act", lambda e, a2=a2: e.activation(out=wi[a2][:, 8:12], in_=wi[a2][:, 0:4],
                                                           func=AF.Sign),
                     reads=[(kwi, 0)], writes=[(kwi, 2)])
                kacc = ("S_acc", a2)
                for kb in range((NK + 511) // 512):
                    w = min(512, NK - kb * 512)
                    for h in range(4):
                        ps, kps = self.ps[h % 2], ("ps", h % 2)
                        S.op("pe", lambda e, ps=ps, h=h, kb=kb, w=w, slot=slot, c0=c0: e.matmul(
                            ps[:, :w], lhsT=qq[slot][:, 1, h, c0:c0 + 128],
                            rhs=kiT[:, kb * 512: kb * 512 + w], start=True, stop=True),
                            reads=[(("S_qq", slot), 1), "S_kiT"], writes=[kps])
                        r, kr = rr[nr % 3], ("S_rr", nr % 3)
                        nr += 1
                        S.op("act", lambda e, ps=ps, r=r, w=w, a2=a2, h=h: e.activation(
                            out=r[:, :w], in_=ps[:, :w], func=AF.Relu, scale=wi[a2][:, 4 + h:5 + h]),
                            reads=[kps, (kwi, 1)], writes=[kr])
                        av = acc[a2][:, kb * 512: kb * 512 + w]
                        if h == 0:
                            tbv = tb[:, kb * 512: kb * 512 + w]
                            S.op("dve", lambda e, av=av, r=r, w=w, a2=a2, h=h, tbv=tbv:
                                 e.scalar_tensor_tensor(out=av, in0=r[:, :w],
                                                        scalar=wi[a2][:, 8 + h:9 + h], in1=tbv,
                                                        op0=ALU.mult, op1=ALU.add),
                                 reads=[kr, (kwi, 2), "S_tb"], writes=[(kacc, kb)])
                        else:
                            S.op("dve", lambda e, av=av, r=r, w=w, a2=a2, h=h:
                                 e.scalar_tensor_tensor(out=av, in0=r[:, :w],
                                                        scalar=wi[a2][:, 8 + h:9 + h], in1=av,
                                                        op0=ALU.mult, op1=ALU.add),
                                 reads=[kr, (kwi, 2), (kacc, kb)], writes=[(kacc, kb)])
                kall = [(kacc, kb) for kb in range((NK + 511) // 512)]
                S.op("pool", lambda e, a2=a2, NK=NK: e.memset(acc[a2][0:64, NK - 64:NK], BIG),
                     reads=kall, writes=[kacc])
                km8 = ("S_m8", a2)
                if j >= 2:
                    for rnd in range(32):
                        src = acc[a2] if rnd == 0 else wk
                        S.op("dve", lambda e, src=src, a2=a2, NK=NK: e.max(out=m8[a2][:],
                                                                             in_=src[:, :NK]),
                             reads=[kacc, "S_wk"], writes=[km8])
                        if rnd < 31:
                            S.op("dve", lambda e, src=src, a2=a2, NK=NK: e.match_replace(
                                out=wk[:, :NK], in_to_replace=m8[a2][:], in_values=src[:, :NK],
                                imm_value=BIG), reads=[kacc, km8, "S_wk"], writes=["S_wk"])
                else:
                    S.op("dve", lambda e, a2=a2: e.memset(m8[a2][:], BIG / 2), writes=[km8])
                ksel = ("S_selb", a2)
                S.op("pool", lambda e, a2=a2, NK=NK: e.tensor_scalar(
                    out=selb[a2][:, :NK], in0=acc[a2][:, :NK], scalar1=m8[a2][:, 7:8], scalar2=NEG,
                    op0=ALU.is_lt, op1=ALU.mult), reads=[kacc, km8], writes=[ksel])
                po, kpo = self.ps[4], ("ps", 4)
                for kt in range(j + 1):
                    pl, kpl = self.ps[2 + kt % 2], ("ps", 2 + kt % 2)

                    def mml(e, pl=pl, kt=kt, a2=a2, slot=slot, c0=c0):
                        e.matmul(pl[:, :].rearrange("p (h t) -> p h t", h=4),
                                 lhsT=kT[:, kt * 128:(kt + 1) * 128],
                                 rhs=qqb[slot][:, 0, :, c0:c0 + 128], start=True, stop=False)
                        ins = None
                        for h in range(4):
                            ins = e.matmul(pl[:, h * 128:(h + 1) * 128],
                                           lhsT=selb[a2][:, kt * 128:(kt + 1) * 128],
                                           rhs=self.ident_b[:, :], start=False, stop=(h == 3))
                        return ins
                    S.op("pe", mml, reads=["S_kT", ("S_qqb", slot), ksel, "ident_b"], writes=[kpl])
                    p, kp = pT[kt % 3], ("S_pT", kt % 3)
                    S.op("act", lambda e, p=p, pl=pl: e.activation(out=p[:], in_=pl[:, :],
                                                                    func=AF.Exp, scale=0.125),
                         reads=[kpl], writes=[kp])
                    S.op("pe", lambda e, p=p, kt=kt, j=j, po=po: e.matmul(
                        po[:65, :], lhsT=v1[:, kt, :], rhs=p[:], start=(kt == 0), stop=(kt == j)),
                        reads=["S_v1", kp], writes=[kpo])
                koT = ("S_oT", a2)
                S.op("act", lambda e, a2=a2, po=po: e.copy(out=oT[a2][:], in_=po[:65, :]),
                     reads=[kpo], writes=[koT])
                pt, kpt = self.ps[5], ("ps", 5)

                def trs(e, a2=a2, pt=pt):
                    ins = None
                    for h in range(4):
                        ins = e.transpose(out=pt[:, h * 65:(h + 1) * 65],
                                          in_=oT[a2][:, h * 128:(h + 1) * 128],
                                          identity=self.ident_f[:65, :65])
                    return ins
                S.op("pe", trs, reads=[koT, "ident_f"], writes=[kpt])
                ptv = pt[:, :260].rearrange("p (h d) -> p h d", h=4)
                S.op("dve", lambda e, a2=a2, ptv=ptv: e.reciprocal(out=rc[a2][:],
                                                                    in_=ptv[:, :, 64:65]),
                     reads=[kpt], writes=[("S_rc", a2)])
                S.op("dve", lambda e, a2=a2, ptv=ptv: e.tensor_tensor(
                    out=osb[a2][:], in0=ptv[:, :, 0:64], in1=rc[a2][:].to_broadcast([128, 4, 64]),
                    op=ALU.mult), reads=[kpt, ("S_rc", a2)], writes=[("S_osb", a2)])
                S.dma(out=self.mixin[tq:tq + 128, 512:768],
                      in_=osb[a2][:].rearrange("p h d -> p (h d)"), sem="S_out%d" % a2,
                      reads=[("S_osb", a2)])
        S.barrier()

    def phase_D(self, l):
        S = self.S
        self.sbuf_reset()
        sb = self.sb
        NB = 2
        U64 = sb("D_U", [64, 64], F32)
        UL = sb("D_UL", [64, 64], F32)
        Lst = sb("D_Lst", [64, 64], F32)
        mb = sb("D_mb", [64, 64], F32)
        S.op("pool", lambda e: e.memset(U64[:], 1.0), writes=["D_U"])
        S.op("pool", lambda e: e.affine_select(out=U64[:], in_=U64[:], pattern=[[1, 64]],
                                                compare_op=ALU.is_ge, fill=0.0, base=0,
                                                channel_multiplier=-1), reads=["D_U"],
             writes=["D_U"])
        for t_, kk_ in ((UL, "D_UL"), (Lst, "D_Lst")):
            S.op("pool", lambda e, t_=t_: e.memset(t_[:], 1.0), writes=[kk_])
            S.op("pool", lambda e, t_=t_: e.affine_select(out=t_[:], in_=t_[:], pattern=[[-1, 64]],
                                                          compare_op=ALU.is_gt, fill=0.0, base=0,
                                                          channel_multiplier=1), reads=[kk_],
                 writes=[kk_])
        S.op("pool", lambda e: e.memset(mb[:], 0.0), writes=["D_mb"])
        S.op("pool", lambda e: e.affine_select(out=mb[:], in_=mb[:], pattern=[[-1, 64]],
                                                compare_op=ALU.is_ge, fill=-1.0e4, base=0,
                                                channel_multiplier=1), reads=["D_mb"],
             writes=["D_mb"])
        cw = self.load_convw("D_cw", self.dn_conv[l], 4, 12)
        alog = sb("D_alog", [64, 4], F32)
        dtb = sb("D_dtb", [64, 4], F32)
        S.dma(out=alog[:], in_=bcast_rows(self.dn_a_log[l:l + 1, :], 64), sem="D_alog",
              writes=["D_alog"])
        S.dma(out=dtb[:], in_=bcast_rows(self.dn_dt_bias[l:l + 1, :], 64), sem="D_dtb",
              writes=["D_dtb"])
        S.op("act", lambda e: e.activation(out=alog[:], in_=alog[:], func=AF.Exp), reads=["D_alog"],
             writes=["D_alog"])
        S.op("dve", lambda e: e.tensor_scalar(out=alog[:], in0=alog[:], scalar1=-1.0, scalar2=None,
                                               op0=ALU.mult), reads=["D_alog"], writes=["D_alog"])
        ng = sb("D_ng", [64, 128], F32)
        S.dma(out=ng[:], in_=bcast_rows(self.dn_norm[l:l + 1, :], 64), sem="D_ng", writes=["D_ng"])

        X = [sb("D_X%d" % i, [128, 515], F32) for i in range(2)]
        Cc = [sb("D_C%d" % i, [128, 512], F32) for i in range(2)]
        Y = [sb("D_Y%d" % i, [128, 12, 512], F32) for i in range(2)]
        St = [sb("D_S%d" % i, [128, 4, 128], F32) for i in range(NSEQ)]
        Sb = [sb("D_Sb%d" % i, [128, 4, 128], BF16) for i in range(NSEQ)]

        def t2(name, shape, dtype):
            return [sb("%s%d" % (name, i), shape, dtype) for i in range(NB)]
        tg = t2("D_tg", [64, 8], F32)
        gt = t2("D_gt", [64, 24], F32)
        zt = t2("D_zt", [64, 4, 128], F32)
        gs = t2("D_gs", [64, 4, 128], F32)
        QK = t2("D_QK", [64, 8, 128], F32)
        Vt = t2("D_Vt", [64, 4, 128], F32)
        sqq = t2("D_sqq", [64, 8, 128], F32)
        nrm = t2("D_nrm", [64, 16], F32)
        qdk = t2("D_qdk", [64, 4, 128], F32)
        TT = t2("D_TT", [128, 12, 64], BF16)
        kd = t2("D_kd", [64, 4, 128], BF16)
        bk = t2("D_bk", [64, 4, 128], BF16)
        bv = t2("D_bv", [64, 4, 128], BF16)
        gU = t2("D_gU", [64, 8, 64], F32)
        dec = t2("D_dec", [64, 4, 64], F32)
        Mm = t2("D_M", [64, 4, 64], F32)
        QKm = t2("D_QKm", [64, 4, 64], F32)
        QKT = t2("D_QKT", [64, 4, 64], BF16)
        Qa = t2("D_Qa", [64, 4, 64], F32)
        Qta = t2("D_Qta", [64, 4, 64], F32)
        Qb = t2("D_Qb", [64, 4, 64], F32)
        Qtb = t2("D_Qtb", [64, 4, 64], F32)
        Bt = t2("D_Bt", [64, 4, 64], F32)
        Tb = t2("D_Tb", [64, 4, 64], BF16)
        u0 = t2("D_u0", [64, 4, 128], F32)
        ub = t2("D_ub", [64, 4, 128], BF16)
        wkT = t2("D_wkT", [128, 4, 64], BF16)
        glb = t2("D_glb", [128, 4], F32)
        osb = t2("D_osb", [64, 4, 128], F32)
        osq = t2("D_osq", [64, 4, 128], F32)
        identI = sb("D_I", [64, 4, 64], F32)
        for h in range(4):
            S.op("pool", lambda e, h=h: e.tensor_copy(out=identI[:, h, :], in_=self.ident_f[:64, :64]),
                 reads=["ident_f"], writes=["D_I"])

        def v4(ps, w):
            return ps[:64, :4 * w].rearrange("p (h x) -> p h x", h=4)

        def bc(ap_h1, w):
            a = ap_h1 if len(ap_h1.shape) == 3 else ap_h1.unsqueeze(2)
            return a.to_broadcast([64, 4, w])

        it = 0
        for blk8 in range(SEQ // 512):
            for s in range(NSEQ):
                ys = (blk8 * NSEQ + s) % 2
                tb = s * SEQ + blk8 * 512
                for ct in range(12):
                    x = X[ct % 2]
                    kx = ("D_X", ct % 2)
                    if blk8 == 0:
                        S.op("pool", lambda e, x=x: e.memset(x[:, 0:3], 0.0), writes=[(kx, "h")])
                        S.dma(out=x[:, 3:515], in_=self.pfeat[ct * 128:(ct + 1) * 128, tb:tb + 512],
                              sem="D_X%d" % (ct % 2), writes=[(kx, "b")])
                    else:
                        S.dma(out=x[:, :], in_=self.pfeat[ct * 128:(ct + 1) * 128, tb - 3:tb + 512],
                              sem="D_X%d" % (ct % 2), writes=[(kx, "h"), (kx, "b")])
                    c, kc = Cc[ct % 2], ("D_C", ct % 2)
                    S.op("act", lambda e, x=x, c=c, ct=ct: e.activation(
                        out=c[:], in_=x[:, 0:512], func=AF.Copy, scale=cw[:, 0, ct:ct + 1]),
                        reads=[(kx, "h"), (kx, "b"), "D_cw"], writes=[kc])
                    for tap in (1, 2, 3):
                        S.op("dve", lambda e, x=x, c=c, ct=ct, tap=tap: e.scalar_tensor_tensor(
                            out=c[:], in0=x[:, tap:tap + 512], scalar=cw[:, tap, ct:ct + 1],
                            in1=c[:], op0=ALU.mult, op1=ALU.add),
                            reads=[(kx, "h"), (kx, "b"), "D_cw", kc], writes=[kc])
                    S.op("act", lambda e, c=c, ct=ct, ys=ys: e.activation(out=Y[ys][:, ct, :],
                                                                           in_=c[:], func=AF.Silu),
                         reads=[kc], writes=[("D_Y", ys, ct)])
                ykeys = [("D_Y", ys, ct) for ct in range(12)]
                for cn in range(8):
                    b = it % NB
                    it += 1
                    c0 = cn * 64
                    tok = tb + c0
                    K = lambda nm: (nm, b)
                    first = (blk8 == 0 and cn == 0)
                    if first:
                        S.op("pool", lambda e, s=s: e.memset(St[s][:], 0.0), writes=[("D_S", s)])
                        S.op("pool", lambda e, s=s: e.memset(Sb[s][:], 0.0), writes=[("D_Sb", s)])
                    S.dma(out=tg[b][:], in_=self.ptok[tok:tok + 64, TM_AB:TM_AB + 8],
                          sem="D_tg%d" % b, writes=[K("tg")])
                    S.dma(out=zt[b][:].rearrange("p h v -> p (h v)"),
                          in_=self.ptok[tok:tok + 64, TM_AZ:TM_AZ + 512], sem="D_zt%d" % b,
                          writes=[K("zt")])
                    G = gt[b]
                    S.op("act", lambda e, b=b, G=G: e.activation(out=G[:, 0:4], in_=tg[b][:, 0:4],
                                                                  func=AF.Sigmoid),
                         reads=[K("tg")], writes=[K("beta")])
                    S.op("dve", lambda e, b=b, G=G: e.tensor_tensor(out=G[:, 20:24], in0=tg[b][:, 4:8],
                                                                     in1=dtb[:], op=ALU.add),
                         reads=[K("tg"), "D_dtb"], writes=[K("gtmp")])
                    S.op("act", lambda e, G=G: e.activation(out=G[:, 20:24], in_=G[:, 20:24],
                                                             func=AF.Exp),
                         reads=[K("gtmp")], writes=[K("gtmp")])
                    S.op("act", lambda e, G=G: e.activation(out=G[:, 20:24], in_=G[:, 20:24],
                                                             func=AF.Ln, bias=1.0),
                         reads=[K("gtmp")], writes=[K("gtmp")])
                    S.op("dve", lambda e, G=G: e.tensor_tensor(out=G[:, 4:8], in0=G[:, 20:24],
                                                                in1=alog[:], op=ALU.mult),
                         reads=[K("gtmp"), "D_alog"], writes=[K("g")])
                    pg, kpg = self.ps[5], ("ps", 5)

                    def mmg(e, G=G, pg=pg):
                        e.matmul(pg[:64, 0:4], lhsT=U64[:, :], rhs=G[:, 4:8], start=True, stop=True)
                        e.matmul(pg[:64, 4:8], lhsT=UL[:, :], rhs=G[:, 4:8], start=True, stop=True)
                        return e.matmul(pg[:, 8:12], lhsT=self.ones_f[:64, :], rhs=G[:, 4:8],
                                        start=True, stop=True)
                    S.op("pe", mmg, reads=[K("g"), "D_U", "D_UL", "ones_f"], writes=[kpg])
                    S.op("act", lambda e, G=G, pg=pg: e.activation(out=G[:, 8:16], in_=pg[:64, 0:8],
                                                                    func=AF.Exp),
                         reads=[kpg], writes=[K("eG")])
                    S.op("act", lambda e, b=b, pg=pg: e.activation(out=glb[b][:], in_=pg[:, 8:12],
                                                                    func=AF.Exp),
                         reads=[kpg], writes=[K("glb")])
                    S.op("dve", lambda e, G=G: e.tensor_tensor(out=G[:, 16:20], in0=G[:, 0:4],
                                                                in1=G[:, 8:12], op=ALU.mult),
                         reads=[K("beta"), K("eG")], writes=[K("beG")])
                    S.op("dve", lambda e, b=b, G=G: e.tensor_tensor(
                        out=gU[b][:, 0:4, :], in0=U64[:].unsqueeze(1).to_broadcast([64, 4, 64]),
                        in1=bc(G[:, 4:8], 64), op=ALU.mult), reads=["D_U", K("g")],
                        writes=[K("gU")])
                    S.op("pool", lambda e, b=b: e.tensor_scalar(out=gU[b][:, 4:8, :],
                                                                 in0=gU[b][:, 0:4, :], scalar1=-1.0,
                                                                 scalar2=None, op0=ALU.mult),
                         reads=[K("gU")], writes=[K("ngU")])
                    pd, kpd = self.ps[4], ("ps", 4)

                    def mmd(e, b=b, pd=pd):
                        ins = None
                        for h in range(4):
                            e.matmul(pd[:64, h * 64:(h + 1) * 64], lhsT=gU[b][:, h, :],
                                     rhs=self.ones_f[:64, :64], start=True, stop=False)
                            ins = e.matmul(pd[:64, h * 64:(h + 1) * 64], lhsT=self.ones_f[:64, :64],
                                           rhs=gU[b][:, 4 + h, :], start=False, stop=True)
                        return ins
                    S.op("pe", mmd, reads=[K("gU"), K("ngU"), "ones_f"], writes=[kpd])
                    S.op("dve", lambda e, b=b, pd=pd: e.tensor_tensor(
                        out=dec[b][:], in0=v4(pd, 64),
                        in1=mb[:].unsqueeze(1).to_broadcast([64, 4, 64]), op=ALU.add),
                        reads=[kpd, "D_mb"], writes=[K("dec")])
                    S.op("act", lambda e, b=b: e.activation(out=dec[b][:], in_=dec[b][:],
                                                             func=AF.Exp),
                         reads=[K("dec")], writes=[K("dec")])
                    for grp in range(3):
                        pt, kpt = self.ps[grp], ("ps", grp)

                        def trq(e, grp=grp, pt=pt):
                            ins = None
                            for h in range(4):
                                ins = e.transpose(out=pt[:64, h * 128:(h + 1) * 128],
                                                  in_=Y[ys][:, grp * 4 + h, c0:c0 + 64],
                                                  identity=self.ident_f[:, :])
                            return ins
                        S.op("pe", trq, reads=ykeys + ["ident_f"], writes=[kpt])
                        dstv = Vt[b][:] if grp == 2 else QK[b][:, grp * 4:(grp + 1) * 4, :]
                        S.op("act", lambda e, dstv=dstv, pt=pt: e.copy(out=dstv, in_=v4(pt, 128)),
                             reads=[kpt], writes=[K("QK%d" % grp)])
                    S.op("pool", lambda e, b=b: e.tensor_tensor(out=sqq[b][:], in0=QK[b][:],
                                                                 in1=QK[b][:], op=ALU.mult),
                         reads=[K("QK0"), K("QK1")], writes=[K("sqq")])
                    S.op("dve", lambda e, b=b: e.tensor_reduce(out=nrm[b][:, 0:8], in_=sqq[b][:],
                                                                axis=AX.X, op=ALU.add),
                         reads=[K("sqq")], writes=[K("nrm")])
                    S.op("dve", lambda e, b=b: e.tensor_scalar(out=nrm[b][:, 0:8], in0=nrm[b][:, 0:8],
                                                                scalar1=1.0e-6, scalar2=None,
                                                                op0=ALU.add),
                         reads=[K("nrm")], writes=[K("nrm")])
                    S.op("act", lambda e, b=b: e.sqrt(out=nrm[b][:, 0:8], in_=nrm[b][:, 0:8]),
                         reads=[K("nrm")], writes=[K("nrm")])
                    S.op("dve", lambda e, b=b: e.reciprocal(out=nrm[b][:, 8:16], in_=nrm[b][:, 0:8]),
                         reads=[K("nrm")], writes=[K("rn")])
                    S.op("dve", lambda e, b=b: e.tensor_scalar(out=nrm[b][:, 8:12],
                                                                in0=nrm[b][:, 8:12],
                                                                scalar1=128.0 ** -0.5, scalar2=None,
                                                                op0=ALU.mult),
                         reads=[K("rn")], writes=[K("rn")])
                    S.op("dve", lambda e, b=b: e.tensor_tensor(
                        out=QK[b][:], in0=QK[b][:],
                        in1=nrm[b][:, 8:16].unsqueeze(2).to_broadcast([64, 8, 128]), op=ALU.mult),
                        reads=[K("QK0"), K("QK1"), K("rn")], writes=[K("QKn")])
                    for h in range(4):
                        for dst_, src_, col, kd_, kr_ in ((qdk[b], QK[b][:, h, :], 8, "qdk", "eG"),
                                                       (kd[b], QK[b][:, 4 + h, :], 12, "kd", "eG"),
                                                       (bk[b], QK[b][:, 4 + h, :], 16, "bk", "beG"),
                                                       (bv[b], Vt[b][:, h, :], 0, "bv", "beta")):
                            S.op("dve", lambda e, dst_=dst_, src_=src_, col=col, h=h, G=G:
                                 e.tensor_scalar(out=dst_[:, h, :], in0=src_,
                                                 scalar1=G[:, col + h:col + h + 1], scalar2=None,
                                                 op0=ALU.mult),
                                 reads=[K("QKn"), K("QK2"), K(kr_)], writes=[(K(kd_), h)])
                    for grp, (src, ksrc) in enumerate(((qdk[b][:], [*[(K("qdk"), h_) for h_ in range(4)]]),
                                                       (QK[b][:, 4:8, :], [K("QKn")]),
                                                       (QK[b][:, 0:4, :], [K("QKn")]))):
                        pt, kpt = self.ps[grp], ("ps", grp)

                        def trt(e, src=src, pt=pt):
                            ins = None
                            for h in range(4):
                                ins = e.transpose(out=pt[:, h * 64:(h + 1) * 64], in_=src[:, h, :],
                                                  identity=self.ident_f[:64, :64])
                            return ins
                        S.op("pe", trt, reads=ksrc + ["ident_f"], writes=[kpt])
                        self.evac(grp, TT[b][:, grp * 4:(grp + 1) * 4, :],
                                  pt[:, :256].rearrange("p (h t) -> p h t", h=4), [kpt],
                                  [K("TT%d" % grp)])
                    pk, kpk = self.ps[3], ("ps", 3)

                    def mmk(e, b=b, pk=pk):
                        ins = None
                        for h in range(4):
                            e.matmul(pk[:64, h * 64:(h + 1) * 64], lhsT=TT[b][:, 4 + h, :],
                                     rhs=TT[b][:, 4 + h, :], start=True, stop=True)
                            ins = e.matmul(pk[:64, 256 + h * 64:256 + (h + 1) * 64],
                                           lhsT=TT[b][:, 8 + h, :], rhs=TT[b][:, 4 + h, :],
                                           start=True, stop=True)
                        return ins
                    S.op("pe", mmk, reads=[K("TT1"), K("TT2")], writes=[kpk])
                    S.op("dve", lambda e, b=b, pk=pk: e.tensor_tensor(
                        out=Mm[b][:], in0=v4(pk, 64), in1=dec[b][:], op=ALU.mult),
                        reads=[kpk, K("dec")], writes=[K("M")])
                    S.op("dve", lambda e, b=b, pk=pk: e.tensor_tensor(
                        out=QKm[b][:], in0=pk[:64, 256:512].rearrange("p (h x) -> p h x", h=4),
                        in1=dec[b][:], op=ALU.mult), reads=[kpk, K("dec")], writes=[K("QKm")])
                    S.op("pool", lambda e, b=b: e.tensor_tensor(
                        out=Mm[b][:], in0=Mm[b][:],
                        in1=Lst[:].unsqueeze(1).to_broadcast([64, 4, 64]), op=ALU.mult),
                        reads=[K("M"), "D_Lst"], writes=[K("M")])
                    S.op("dve", lambda e, b=b, G=G: e.tensor_tensor(
                        out=Mm[b][:], in0=Mm[b][:], in1=bc(G[:, 0:4], 64), op=ALU.mult),
                        reads=[K("M"), K("beta")], writes=[K("M")])
                    pn, kpn = self.ps[0], ("ps", 0)

                    def trn(e, b=b, pn=pn):
                        ins = None
                        for h in range(4):
                            e.transpose(out=pn[:64, h * 64:(h + 1) * 64], in_=Mm[b][:, h, :],
                                        identity=self.ident_f[:64, :64])
                            ins = e.transpose(out=pn[:64, 256 + h * 64:256 + (h + 1) * 64],
                                              in_=QKm[b][:, h, :], identity=self.ident_f[:64, :64])
                        return ins
                    S.op("pe", trn, reads=[K("M"), K("QKm"), "ident_f"], writes=[kpn])
                    S.op("act", lambda e, b=b, pn=pn: e.copy(out=Qa[b][:], in_=v4(pn, 64)),
                         reads=[kpn], writes=[K("Qa")])
                    S.op("act", lambda e, b=b, pn=pn: e.copy(
                        out=QKT[b][:], in_=pn[:64, 256:512].rearrange("p (h x) -> p h x", h=4)),
                        reads=[kpn], writes=[K("QKT")])
                    S.op("dve", lambda e, b=b: e.tensor_tensor(out=Bt[b][:], in0=identI[:],
                                                                in1=Qa[b][:], op=ALU.subtract),
                         reads=[K("Qa"), "D_I"], writes=[K("Bt")])
                    Q, Qt, kQ, kQt = Qa[b], Mm[b], K("Qa"), K("M")
                    alt = [(Qb[b], Qtb[b], K("Qb"), K("Qtb")), (Qa[b], Qta[b], K("Qa"), K("Qta"))]
                    for step in range(5):
                        Q2, Qt2, kQ2, kQt2 = alt[step % 2]
                        p1, kp1 = self.ps[1], ("ps", 1)

                        def mq(e, Q=Q, Qt=Qt, p1=p1, step=step):
                            ins = None
                            for h in range(4):
                                ins = e.matmul(p1[:64, h * 64:(h + 1) * 64], lhsT=Q[:, h, :],
                                               rhs=Qt[:, h, :], start=True, stop=True)
                                if step < 4:
                                    ins = e.matmul(p1[:64, 256 + h * 64:256 + (h + 1) * 64],
                                                   lhsT=Qt[:, h, :], rhs=Q[:, h, :], start=True,
                                                   stop=True)
                            return ins
                        S.op("pe", mq, reads=[kQ, kQt], writes=[kp1])
                        S.op("act", lambda e, Qt2=Qt2, p1=p1: e.copy(out=Qt2[:], in_=v4(p1, 64)),
                             reads=[kp1], writes=[kQt2])
                        if step < 4:
                            S.op("act", lambda e, Q2=Q2, p1=p1: e.copy(
                                out=Q2[:], in_=p1[:64, 256:512].rearrange("p (h x) -> p h x", h=4)),
                                reads=[kp1], writes=[kQ2])
                        p2, kp2 = self.ps[2], ("ps", 2)

                        def mbm(e, Qt2=Qt2, b=b, p2=p2):
                            ins = None
                            for h in range(4):
                                ins = e.matmul(p2[:64, h * 64:(h + 1) * 64], lhsT=Qt2[:, h, :],
                                               rhs=Bt[b][:, h, :], start=True, stop=True)
                            return ins
                        S.op("pe", mbm, reads=[kQt2, K("Bt")], writes=[kp2])
                        S.op("dve", lambda e, b=b, p2=p2: e.tensor_tensor(out=Bt[b][:], in0=Bt[b][:],
                                                                           in1=v4(p2, 64),
                                                                           op=ALU.add),
                             reads=[kp2, K("Bt")], writes=[K("Bt")])
                        Q, Qt, kQ, kQt = Q2, Qt2, kQ2, kQt2
                    S.op("act", lambda e, b=b: e.copy(out=Tb[b][:], in_=Bt[b][:]), reads=[K("Bt")],
                         writes=[K("Tb")])
                    pu0, kpu0 = self.ps[3], ("ps", 3)

                    def mu0(e, b=b, pu0=pu0):
                        ins = None
                        for h in range(4):
                            ins = e.matmul(pu0[:64, h * 128:(h + 1) * 128], lhsT=Tb[b][:, h, :],
                                           rhs=bv[b][:, h, :], start=True, stop=True)
                        return ins
                    S.op("pe", mu0, reads=[K("Tb"), *[(K("bv"), h_) for h_ in range(4)]], writes=[kpu0])
                    S.op("act", lambda e, b=b, pu0=pu0: e.copy(out=u0[b][:], in_=v4(pu0, 128)),
                         reads=[kpu0], writes=[K("u0")])
                    pw, kpw = self.ps[4], ("ps", 4)

                    def mwk(e, b=b, pw=pw):
                        ins = None
                        for h in range(4):
                            ins = e.matmul(pw[:, h * 64:(h + 1) * 64], lhsT=bk[b][:, h, :],
                                           rhs=Tb[b][:, h, :], start=True, stop=True)
                        return ins
                    S.op("pe", mwk, reads=[K("Tb"), *[(K("bk"), h_) for h_ in range(4)]], writes=[kpw])
                    S.op("act", lambda e, b=b, pw=pw: e.copy(
                        out=wkT[b][:], in_=pw[:, :256].rearrange("p (h t) -> p h t", h=4)),
                        reads=[kpw], writes=[K("wkT")])
                    pu, kpu = self.ps[5], ("ps", 5)

                    def mpu(e, b=b, pu=pu, s=s):
                        ins = None
                        for h in range(4):
                            ins = e.matmul(pu[:64, h * 128:(h + 1) * 128], lhsT=wkT[b][:, h, :],
                                           rhs=Sb[s][:, h, :], start=True, stop=True)
                        return ins
                    S.op("pe", mpu, reads=[K("wkT"), ("D_Sb", s)], writes=[kpu])
                    S.op("dve", lambda e, b=b, pu=pu: e.tensor_tensor(out=ub[b][:], in0=u0[b][:],
                                                                       in1=v4(pu, 128),
                                                                       op=ALU.subtract),
                         reads=[K("u0"), kpu], writes=[K("ub")])
                    po, kpo = self.ps[0], ("ps", 0)

                    def mpo(e, b=b, po=po, s=s):
                        ins = None
                        for h in range(4):
                            e.matmul(po[:64, h * 128:(h + 1) * 128], lhsT=TT[b][:, h, :],
                                     rhs=Sb[s][:, h, :], start=True, stop=False)
                            ins = e.matmul(po[:64, h * 128:(h + 1) * 128], lhsT=QKT[b][:, h, :],
                                           rhs=ub[b][:, h, :], start=False, stop=True)
                        return ins
                    S.op("pe", mpo, reads=[K("TT0"), ("D_Sb", s), K("QKT"), K("ub")], writes=[kpo])
                    psn, kpsn = self.ps[1], ("ps", 1)

                    def mps(e, b=b, psn=psn):
                        ins = None
                        for h in range(4):
                            ins = e.matmul(psn[:, h * 128:(h + 1) * 128], lhsT=kd[b][:, h, :],
                                           rhs=ub[b][:, h, :], start=True, stop=True)
                        return ins
                    S.op("pe", mps, reads=[*[(K("kd"), h_) for h_ in range(4)], K("ub")], writes=[kpsn])
                    for h in range(4):
                        S.op("dve", lambda e, b=b, s=s, h=h, psn=psn: e.scalar_tensor_tensor(
                            out=St[s][:, h, :], in0=St[s][:, h, :], scalar=glb[b][:, h:h + 1],
                            in1=psn[:, h * 128:(h + 1) * 128], op0=ALU.mult, op1=ALU.add),
                            reads=[("D_S", s), K("glb"), kpsn], writes=[("D_S", s)])
                    S.op("act", lambda e, s=s: e.copy(out=Sb[s][:], in_=St[s][:]),
                         reads=[("D_S", s)], writes=[("D_Sb", s)])
                    S.op("act", lambda e, b=b: e.activation(out=gs[b][:], in_=zt[b][:], func=AF.Silu),
                         reads=[K("zt")], writes=[K("gs")])
                    S.op("pool", lambda e, b=b: e.tensor_tensor(
                        out=gs[b][:], in0=gs[b][:],
                        in1=ng[:].unsqueeze(1).to_broadcast([64, 4, 128]), op=ALU.mult),
                        reads=[K("gs"), "D_ng"], writes=[K("gs")])
                    S.op("act", lambda e, b=b, po=po: e.copy(out=osb[b][:], in_=v4(po, 128)),
                         reads=[kpo], writes=[K("osb")])
                    S.op("pool", lambda e, b=b: e.tensor_tensor(out=osq[b][:], in0=osb[b][:],
                                                                 in1=osb[b][:], op=ALU.mult),
                         reads=[K("osb")], writes=[K("osq")])
                    S.op("dve", lambda e, b=b: e.tensor_reduce(out=nrm[b][:, 0:4], in_=osq[b][:],
                                                                axis=AX.X, op=ALU.add),
                         reads=[K("osq")], writes=[K("oss")])
                    self.rstd_from_ss(nrm[b][:, 0:4], K("oss"), nrm[b][:, 4:8], K("ors"), 128)
                    S.op("dve", lambda e, b=b: e.tensor_tensor(out=osb[b][:], in0=osb[b][:],
                                                                in1=bc(nrm[b][:, 4:8], 128),
                                                                op=ALU.mult),
                         reads=[K("osb"), K("ors")], writes=[K("osb")])
                    S.op("pool", lambda e, b=b: e.tensor_tensor(out=osq[b][:], in0=osb[b][:],
                                                                 in1=gs[b][:], op=ALU.mult),
                         reads=[K("osb"), K("gs")], writes=[K("osq")])
                    S.dma(out=self.mixin[tok:tok + 64, 0:512],
                          in_=osq[b][:].rearrange("p h v -> p (h v)"), sem="D_out%d" % b,
                          reads=[K("osq")])
        S.barrier()


def build_program():
    P = Prog()
    src = P.x_in
    for l in range(DEPTH):
        dst = P.xmid if l < DEPTH - 1 else P.y
        P.phase_A(l, src)
        P.phase_D(l)
        P.phase_S(l)
        P.phase_H(l)
        P.phase_E(l, src)
        P.phase_F(l, dst)
        src = dst
    return P


def kernel(**inputs):
    n = 8
    P = build_program()
    x = np.ascontiguousarray(inputs["x"], dtype=np.float32)
    shared = {k: np.ascontiguousarray(v, dtype=np.float32) for k, v in inputs.items() if k != "x"}
    in_maps = []
    for c in range(n):
        m = dict(shared)
        m["x"] = np.ascontiguousarray(x[c * NSEQ:(c + 1) * NSEQ].reshape(NTOK, D))
        in_maps.append(m)
    res = run_bass_kernel_spmd(P.nc, in_maps, core_ids=list(range(n)))
    out = np.stack([np.asarray(r["y"]).reshape(NSEQ, SEQ, D) for r in res.results], axis=0)
    return out.reshape(n * NSEQ, SEQ, D).astype(np.float32)
```

```python
import numpy as np
import concourse.bass as bass
import concourse.mybir as mybir
from concourse.bass_utils import run_bass_kernel_spmd

F32 = mybir.dt.float32
BF16 = mybir.dt.bfloat16
AF = mybir.ActivationFunctionType
ALU = mybir.AluOpType
AX = mybir.AxisListType

D = 1024
SEQ = 4096
NSEQ = 2
NTOK = NSEQ * SEQ
DEPTH = 2
D_IN = 3788
D_FF = 2816
EPS = 1e-6
NEG = -30000.0

TM_GROUPS = [(1536, 2056), (2376, 2440), (2760, 2764), (3020, 3788)]
TM_W = sum(b - a for a, b in TM_GROUPS)
TM_AZ, TM_AB, TM_AA = 0, 512, 516
TM_BV = 520
TM_WI = 584
TM_CF, TM_CI, TM_CG = 588, 844, 1100
FM_GROUPS = [(i * 128, 128) for i in range(12)] + [(2056, 128), (2184, 128), (2312, 64),
             (2440, 128), (2568, 128), (2696, 64), (2764, 128), (2892, 128), (3020, 128), (3148, 128)]
FM_ROW = {}
_r = 0
for _c, _n in FM_GROUPS:
    FM_ROW[_c] = _r
    _r += _n
FM_H = _r


class Sched:
    def __init__(self, nc):
        self.nc = nc
        self.eng = {"pe": nc.tensor, "act": nc.scalar, "dve": nc.vector, "pool": nc.gpsimd,
                    "sp": nc.sync}
        self.sems = {}
        self.cnt = {}
        for k in ("pe", "act", "dve", "pool"):
            self.sems[k] = nc.alloc_semaphore("s_" + k)
            self.cnt[k] = 0
        self.seen = {k: {} for k in self.eng}
        self.bufs = {}
        self.ninstr = 0

    def _buf(self, key):
        b = self.bufs.get(key)
        if b is None:
            b = {"w": None, "r": {}}
            self.bufs[key] = b
        return b

    def _deps(self, engine, reads, writes):
        deps = {}

        def add(ev, same_ok):
            if ev is None:
                return
            sk, val = ev
            if sk == engine and not same_ok:
                return
            if deps.get(sk, 0) < val:
                deps[sk] = val

        for k in reads:
            b = self._buf(k)
            add(b["w"], engine != "pe")
            if isinstance(k, tuple) and k[0] in ("ps", "psb"):
                for sk, val in b["r"].items():
                    add((sk, val), False)
        for k in writes:
            b = self._buf(k)
            add(b["w"], engine != "pe")
            for sk, val in b["r"].items():
                add((sk, val), False)
        return deps

    def _emit_waits(self, engine, deps):
        e = self.eng[engine]
        seen = self.seen[engine]
        for sk, val in deps.items():
            if seen.get(sk, 0) >= val:
                continue
            e.wait_ge(self.sems[sk], val)
            self.ninstr += 1
            seen[sk] = val

    def _record(self, ev, reads, writes):
        for k in writes:
            b = self._buf(k)
            b["w"] = ev
            b["r"] = {}
        for k in reads:
            b = self._buf(k)
            if b["r"].get(ev[0], 0) < ev[1]:
                b["r"][ev[0]] = ev[1]

    def op(self, engine, fn, reads=(), writes=()):
        deps = self._deps(engine, reads, writes)
        self._emit_waits(engine, deps)
        ins = fn(self.eng[engine])
        self.cnt[engine] += 1
        ins.then_inc(self.sems[engine], 1)
        self.ninstr += 1
        self._record((engine, self.cnt[engine]), reads, writes)
        self._yield()

    def dma(self, out, in_, sem, reads=(), writes=(), q="sp"):
        if sem not in self.sems:
            self.sems[sem] = self.nc.alloc_semaphore("d_" + sem)
            self.cnt[sem] = 0
        deps = self._deps(q, reads, writes)
        self._emit_waits(q, deps)
        ins = self.eng[q].dma_start(out=out, in_=in_)
        self.cnt[sem] += 16
        ins.then_inc(self.sems[sem], 16)
        self.ninstr += 1
        self._record((sem, self.cnt[sem]), reads, writes)
        self._yield()

    def interleave(self, fns):
        import threading
        n = len(fns)
        st = {"cur": 0, "alive": [True] * n, "err": None}
        cond = threading.Condition()

        def nxt(i):
            for d in range(1, n + 1):
                k = (i + d) % n
                if st["alive"][k]:
                    return k
            return -1

        def pass_turn(me):
            with cond:
                st["cur"] = nxt(me)
                cond.notify_all()
                while st["alive"][me] and st["cur"] != me and st["err"] is None:
                    cond.wait()
                if st["err"] is not None and st["alive"][me]:
                    raise RuntimeError("interleave aborted")

        def worker(i):
            with cond:
                while st["cur"] != i and st["err"] is None:
                    cond.wait()
            try:
                if st["err"] is None:
                    self._tl.me = i
                    fns[i]()
            except BaseException as e:
                if st["err"] is None:
                    st["err"] = e
            finally:
                with cond:
                    st["alive"][i] = False
                    if st["cur"] == i:
                        st["cur"] = nxt(i)
                    cond.notify_all()

        self._tl = threading.local()
        self._pass = pass_turn
        ths = [threading.Thread(target=worker, args=(i,)) for i in range(n)]
        for t in ths:
            t.start()
        for t in ths:
            t.join()
        self._pass = None
        if st["err"] is not None:
            raise st["err"]

    def _yield(self):
        p = getattr(self, "_pass", None)
        if p is not None:
            p(self._tl.me)

    def barrier(self):
        allv = {k: v for k, v in self.cnt.items() if v > 0}
        for engine in self.eng:
            self._emit_waits(engine, dict(allv))
        self.bufs = {}


def bcast_rows(ap2d_row, nparts):
    return ap2d_row.partition_broadcast(nparts)


class Prog:
    def __init__(self, layers=(0, 1), phases="AHDSEF", dbg=()):
        self.nc = nc = bass.Bass("TRN2", target_bir_lowering=False)
        self.S = Sched(nc)
        self.dbg = dbg
        dt = nc.dram_tensor
        self.x_in = dt("x", [NTOK, D], F32, kind="ExternalInput").ap()
        self.w_in = dt("w_in", [DEPTH, D, D_IN], F32, kind="ExternalInput").ap()
        self.dn_conv = dt("dn_conv", [DEPTH, 4, 1536], F32, kind="ExternalInput").ap()
        self.dn_a_log = dt("dn_a_log", [DEPTH, 4], F32, kind="ExternalInput").ap()
        self.dn_dt_bias = dt("dn_dt_bias", [DEPTH, 4], F32, kind="ExternalInput").ap()
        self.dn_norm = dt("dn_norm", [DEPTH, 128], F32, kind="ExternalInput").ap()
        self.hg_lb = dt("hg_lb", [DEPTH, 256], F32, kind="ExternalInput").ap()
        self.hg_norm = dt("hg_norm", [DEPTH, 64], F32, kind="ExternalInput").ap()
        self.w_out = dt("w_out", [DEPTH, D, D], F32, kind="ExternalInput").ap()
        self.g_mix_pre = dt("g_mix_pre", [DEPTH, D], F32, kind="ExternalInput").ap()
        self.g_mix_post = dt("g_mix_post", [DEPTH, D], F32, kind="ExternalInput").ap()
        self.g_ffn_pre = dt("g_ffn_pre", [DEPTH, D], F32, kind="ExternalInput").ap()
        self.g_ffn_post = dt("g_ffn_post", [DEPTH, D], F32, kind="ExternalInput").ap()
        self.w_up = dt("ffn_w_up", [DEPTH, D, 2 * D_FF], F32, kind="ExternalInput").ap()
        self.ffn_conv = dt("ffn_conv", [DEPTH, 3, 2 * D_FF], F32, kind="ExternalInput").ap()
        self.w_down = dt("ffn_w_down", [DEPTH, D_FF, D], F32, kind="ExternalInput").ap()
        self.y = dt("y", [NTOK, D], F32, kind="ExternalOutput").ap()

        def scratch(name, shape, dtype):
            kind = "ExternalOutput" if name in dbg else "Internal"
            return dt(name, shape, dtype, kind=kind).ap()

        self.ptok = scratch("ptok", [NTOK, TM_W], F32)
        self.pfeat = scratch("pfeat", [FM_H, NTOK], F32)
        self.mixin = scratch("mixin", [NTOK, D], F32)
        self.x1 = scratch("x1", [NTOK, D], F32)
        self.h2T = scratch("h2T", [D, NTOK], BF16)
        self.xmid = scratch("xmid", [NTOK, D], F32)

        self.ps = [nc.alloc_psum_tensor("ps%d" % i, [128, 512], F32) for i in range(6)]
        self.psb = [nc.alloc_psum_tensor("psb%d" % i, [128, 1024], BF16) for i in range(2)]
        self.ps8 = [p[:, :] for p in self.ps] + [p[:, :].bitcast(F32) for p in self.psb]
        self.ident_b = nc.alloc_sbuf_tensor("ident_b", [128, 128], BF16)
        self.ident_f = nc.alloc_sbuf_tensor("ident_f", [128, 128], F32)
        self.ones_f = nc.alloc_sbuf_tensor("ones_f", [128, 128], F32)
        self._consts()
        self.sb_base = nc.sbuf_base
        self.layers = layers
        self.phases = phases

    def _consts(self):
        nc, S = self.nc, self.S
        S.op("pool", lambda e: e.memset(self.ones_f[:], 1.0), writes=["ones_f"])
        S.op("pool", lambda e: e.memset(self.ident_f[:], 0.0), writes=["ident_f"])
        S.op("pool", lambda e: e.affine_select(out=self.ident_f[:], in_=self.ident_f[:],
                                                pattern=[[-1, 128]], compare_op=ALU.not_equal,
                                                fill=1.0, base=0, channel_multiplier=1),
             reads=["ident_f"], writes=["ident_f"])
        S.op("dve", lambda e: e.tensor_copy(out=self.ident_b[:], in_=self.ident_f[:]),
             reads=["ident_f"], writes=["ident_b"])

    def sbuf_reset(self):
        self.nc.sbuf_base = self.sb_base

    def sb(self, name, shape, dtype):
        self._uid = getattr(self, "_uid", 0) + 1
        return self.nc.alloc_sbuf_tensor("%s_u%d" % (name, self._uid), shape, dtype)

    def load_bcast(self, name, row_ap, n):
        t = self.sb(name, [128, n], F32)
        self.S.dma(out=t[:], in_=bcast_rows(row_ap, 128), sem="ld_" + name, writes=[name])
        return t

    def load_weight_bf16(self, name, w_ap, K, N, stage, stage_keys):
        S = self.S
        wt = self.sb(name, [128, K, N], BF16)
        CH = stage[0].shape[1]
        i = 0
        for k in range(K):
            for c0 in range(0, N, CH):
                cw = min(CH, N - c0)
                st, sk = stage[i % 2], stage_keys[i % 2]
                S.dma(out=st[:, :cw], in_=w_ap[k * 128:(k + 1) * 128, c0:c0 + cw], sem=sk,
                      writes=[sk])
                eng = ("dve", "pool", "act")[i % 3]
                if eng == "act":
                    S.op("act", lambda e, st=st, k=k, c0=c0, cw=cw: e.copy(
                        out=wt[:, k, c0:c0 + cw], in_=st[:, :cw]), reads=[sk], writes=[name])
                else:
                    S.op(eng, lambda e, st=st, k=k, c0=c0, cw=cw: e.tensor_copy(
                        out=wt[:, k, c0:c0 + cw], in_=st[:, :cw]), reads=[sk], writes=[name])
                i += 1
        return wt

    def evac(self, i, out, in_, reads, writes):
        if i % 2 == 0:
            self.S.op("act", lambda e: e.copy(out=out, in_=in_), reads=reads, writes=writes)
        else:
            self.S.op("dve", lambda e: e.tensor_copy(out=out, in_=in_), reads=reads, writes=writes)

    def norm_transpose(self, src_dram, tok0, gbc, gkey, T, tag, xt, hb, hT, ss, rs, slot,
                       do_norm=True):
        S = self.S
        kx = lambda j: (tag + "xt", slot, j)
        for j in range(4):
            S.dma(out=xt[slot][:, j, :], in_=src_dram[tok0 + j * 128: tok0 + (j + 1) * 128, :],
                  sem="%sxt%d_%d" % (tag, slot, j), writes=[kx(j)])
        kss, krs = (tag + "ss", slot), (tag + "rs", slot)
        if do_norm:
            for j in range(4):
                S.op("act", lambda e, j=j: e.activation(out=T["junk"][:], in_=xt[slot][:, j, :],
                                                         func=AF.Square,
                                                         accum_out=ss[slot][:, j:j + 1]),
                     reads=[kx(j)], writes=[kss])
            S.op("dve", lambda e: e.tensor_scalar(out=rs[slot][:], in0=ss[slot][:],
                                                   scalar1=1.0 / D, scalar2=EPS, op0=ALU.mult,
                                                   op1=ALU.add), reads=[kss], writes=[krs])
            S.op("act", lambda e: e.sqrt(out=rs[slot][:], in_=rs[slot][:]), reads=[krs],
                 writes=[krs])
            S.op("dve", lambda e: e.reciprocal(out=rs[slot][:], in_=rs[slot][:]), reads=[krs],
                 writes=[krs])
        for j in range(4):
            khb = (tag + "hb", j % 2)
            hbj = hb[j % 2]
            if do_norm:
                S.op("dve", lambda e, j=j, hbj=hbj: e.scalar_tensor_tensor(
                    out=hbj[:], in0=xt[slot][:, j, :], scalar=rs[slot][:, j:j + 1], in1=gbc[:],
                    op0=ALU.mult, op1=ALU.mult), reads=[kx(j), krs, gkey], writes=[khb])
            else:
                S.op("pool", lambda e, j=j, hbj=hbj: e.tensor_copy(out=hbj[:],
                                                                  in_=xt[slot][:, j, :]),
                     reads=[kx(j)], writes=[khb])
            pb = self.psb[j % 2]
            kpb = ("psb", j % 2)

            def tr(e, hbj=hbj, pb=pb):
                ins = None
                for k in range(8):
                    ins = e.transpose(out=pb[:, k * 128:(k + 1) * 128],
                                      in_=hbj[:, k * 128:(k + 1) * 128], identity=self.ident_b[:])
                return ins
            S.op("pe", tr, reads=[khb, "ident_b"], writes=[kpb])
            self.evac(j, hT[slot][:, :, j * 128:(j + 1) * 128],
                      pb[:].rearrange("p (k t) -> p k t", k=8), [kpb], [(tag + "hT", slot, j)])

    def phase_A(self, l, xsrc):
        nc, S = self.nc, self.S
        self.sbuf_reset()
        stage = [self.sb("A_stage%d" % i, [128, 3788], F32) for i in range(2)]
        Wi = self.load_weight_bf16("A_Wi", self.w_in[l], 8, D_IN, stage, ["A_stg0", "A_stg1"])
        gbc = self.load_bcast("A_gbc", self.g_mix_pre[l:l + 1, :], D)
        xt = [self.sb("A_xt%d" % i, [128, 4, D], F32) for i in range(2)]
        hb = [self.sb("A_hb%d" % i, [128, D], BF16) for i in range(2)]
        hT = [self.sb("A_hT%d" % i, [128, 8, 512], BF16) for i in range(2)]
        ss = [self.sb("A_ss%d" % i, [128, 4], F32) for i in range(2)]
        rs = [self.sb("A_rs%d" % i, [128, 4], F32) for i in range(2)]
        T = {"junk": self.sb("A_junk", [128, D], F32)}
        ofm = [self.sb("A_ofm%d" % i, [128, 512], F32) for i in range(4)]
        otm = [self.sb("A_otm%d" % i, [128, TM_W], F32) for i in range(2)]
        tmch = []
        off = 0
        for a, b in TM_GROUPS:
            c = a
            while c < b:
                w = min(512, b - c)
                tmch.append((c, w, off))
                off += w
                c += w
        nev = 0
        for blk in range(NTOK // 512):
            slot = blk % 2
            tok0 = blk * 512
            self.norm_transpose(xsrc, tok0, gbc, "A_gbc", T, "A_", xt, hb, hT, ss, rs, slot)
            hkeys = [("A_hT", slot, j) for j in range(4)]
            for gi, (c0, n) in enumerate(FM_GROUPS):
                ps = self.ps[gi % 4]
                kps = ("ps", gi % 4)

                def mm(e, ps=ps, c0=c0, n=n):
                    ins = None
                    for k in range(8):
                        ins = e.matmul(ps[:n, :], lhsT=Wi[:, k, c0:c0 + n], rhs=hT[slot][:, k, :],
                                       start=(k == 0), stop=(k == 7))
                    return ins
                S.op("pe", mm, reads=["A_Wi"] + hkeys, writes=[kps])
                o = ofm[gi % 4]
                ko = ("A_ofm", gi % 4)
                self.evac(nev, o[:n, :], ps[:n, :], [kps], [ko])
                nev += 1
                r0 = FM_ROW[c0]
                S.dma(out=self.pfeat[r0:r0 + n, tok0:tok0 + 512], in_=o[:n, :],
                      sem="A_ofm%d" % (gi % 4), reads=[ko])
            for j in range(4):
                o = otm[j % 2]
                ko = ("A_otm", j % 2)
                for ci, (c, w, dst) in enumerate(tmch):
                    ps = self.ps[4 + ci % 2]
                    kps = ("ps", 4 + ci % 2)

                    def mm(e, ps=ps, c=c, w=w, j=j):
                        ins = None
                        for k in range(8):
                            ins = e.matmul(ps[:, :w], lhsT=hT[slot][:, k, j * 128:(j + 1) * 128],
                                           rhs=Wi[:, k, c:c + w], start=(k == 0), stop=(k == 7))
                        return ins
                    S.op("pe", mm, reads=["A_Wi", hkeys[j]], writes=[kps])
                    self.evac(nev, o[:, dst:dst + w], ps[:, :w], [kps], [(ko, ci)])
                    nev += 1
                S.dma(out=self.ptok[tok0 + j * 128: tok0 + (j + 1) * 128, :], in_=o[:, :],
                      sem="A_otm%d" % (j % 2), reads=[(ko, ci) for ci in range(len(tmch))])
        S.barrier()

    def transp8(self, hbj, khb, dst, kdst, j):
        pb = self.psb[j % 2]
        kpb = ("psb", j % 2)

        def tr(e):
            ins = None
            for k in range(8):
                ins = e.transpose(out=pb[:, k * 128:(k + 1) * 128],
                                  in_=hbj[:, k * 128:(k + 1) * 128], identity=self.ident_b[:])
            return ins
        self.S.op("pe", tr, reads=[khb, "ident_b"], writes=[kpb])
        self.evac(j, dst, pb[:].rearrange("p (k t) -> p k t", k=8), [kpb], [kdst])

    def rstd_from_ss(self, ss, kss, rs, krs, n):
        S = self.S
        S.op("dve", lambda e: e.tensor_scalar(out=rs, in0=ss, scalar1=1.0 / n, scalar2=EPS,
                                               op0=ALU.mult, op1=ALU.add), reads=[kss],
             writes=[krs])
        S.op("act", lambda e: e.sqrt(out=rs, in_=rs), reads=[krs], writes=[krs])
        S.op("dve", lambda e: e.reciprocal(out=rs, in_=rs), reads=[krs], writes=[krs])

    def phase_E(self, l, xsrc):
        S = self.S
        self.sbuf_reset()
        stage = [self.sb("E_stage%d" % i, [128, 1024], F32) for i in range(2)]
        Wo = self.load_weight_bf16("E_Wo", self.w_out[l], 8, D, stage, ["E_stg0", "E_stg1"])
        gpost = self.load_bcast("E_gpost", self.g_mix_post[l:l + 1, :], D)
        gpre = self.load_bcast("E_gpre", self.g_ffn_pre[l:l + 1, :], D)
        xt = [self.sb("E_xt%d" % i, [128, 4, D], F32) for i in range(2)]
        xr = [self.sb("E_xr%d" % i, [128, 4, D], F32) for i in range(2)]
        hb = [self.sb("E_hb%d" % i, [128, D], BF16) for i in range(2)]
        mT = [self.sb("E_mT%d" % i, [128, 8, 512], BF16) for i in range(2)]
        h2s = [self.sb("E_h2s%d" % i, [128, 8, 512], BF16) for i in range(2)]
        yt = [self.sb("E_yt%d" % i, [128, D], F32) for i in range(2)]
        x1t = [self.sb("E_x1t%d" % i, [128, D], F32) for i in range(2)]
        h2b = [self.sb("E_h2b%d" % i, [128, D], BF16) for i in range(2)]
        junk = self.sb("E_junk", [128, D], F32)
        st = [self.sb("E_st%d" % i, [128, 8], F32) for i in range(2)]
        h2T_v = self.h2T.rearrange("(k p) t -> p k t", p=128)
        for blk in range(NTOK // 512):
            slot = blk % 2
            tok0 = blk * 512
            self.norm_transpose(self.mixin, tok0, None, None, None, "E_", xt, hb, mT, None, None,
                                slot, do_norm=False)
            for j in range(4):
                S.dma(out=xr[slot][:, j, :], in_=xsrc[tok0 + j * 128: tok0 + (j + 1) * 128, :],
                      sem="E_xr%d_%d" % (slot, j), writes=[("E_xr", slot, j)])
            for j in range(4):
                p2 = j % 2
                kss, krs = ("E_ss", p2), ("E_rs", p2)
                for hf in range(2):
                    ps = self.ps[2 * p2 + hf]
                    kps = ("ps", 2 * p2 + hf)

                    def mm(e, ps=ps, hf=hf, j=j):
                        ins = None
                        for k in range(8):
                            ins = e.matmul(ps[:, :], lhsT=mT[slot][:, k, j * 128:(j + 1) * 128],
                                           rhs=Wo[:, k, hf * 512:(hf + 1) * 512], start=(k == 0),
                                           stop=(k == 7))
                        return ins
                    S.op("pe", mm, reads=["E_Wo", ("E_hT", slot, j)], writes=[kps])
                    S.op("act", lambda e, ps=ps, hf=hf, p2=p2: e.activation(
                        out=junk[:, :512], in_=ps[:, :], func=AF.Square,
                        accum_out=st[p2][:, hf:hf + 1]), reads=[kps], writes=[(kss, hf)])
                S.op("dve", lambda e, p2=p2: e.tensor_tensor(out=st[p2][:, 2:3], in0=st[p2][:, 0:1],
                                                              in1=st[p2][:, 1:2], op=ALU.add),
                     reads=[(kss, 0), (kss, 1)], writes=[kss])
                self.rstd_from_ss(st[p2][:, 2:3], kss, st[p2][:, 3:4], krs, D)
                kyt = ("E_yt", p2)
                for hf in range(2):
                    ps = self.ps[2 * p2 + hf]
                    kps = ("ps", 2 * p2 + hf)
                    S.op("act", lambda e, ps=ps, hf=hf, p2=p2: e.activation(
                        out=yt[p2][:, hf * 512:(hf + 1) * 512], in_=ps[:, :], func=AF.Copy,
                        scale=st[p2][:, 3:4]), reads=[kps, krs], writes=[(kyt, hf)])
                S.op("dve", lambda e, p2=p2: e.tensor_tensor(out=yt[p2][:], in0=yt[p2][:],
                                                              in1=gpost[:], op=ALU.mult),
                     reads=[(kyt, 0), (kyt, 1), "E_gpost"], writes=[kyt])
                kx1 = ("E_x1t", p2)
                S.op("pool", lambda e, p2=p2, j=j: e.tensor_tensor(out=x1t[p2][:], in0=yt[p2][:],
                                                                    in1=xr[slot][:, j, :],
                                                                    op=ALU.add),
                     reads=[kyt, ("E_xr", slot, j)], writes=[kx1])
                S.dma(out=self.x1[tok0 + j * 128: tok0 + (j + 1) * 128, :], in_=x1t[p2][:],
                      sem="E_x1t%d" % p2, reads=[kx1])
                kss2, krs2 = ("E_ss2", p2), ("E_rs2", p2)
                S.op("act", lambda e, p2=p2: e.activation(out=junk[:], in_=x1t[p2][:],
                                                           func=AF.Square,
                                                           accum_out=st[p2][:, 4:5]),
                     reads=[kx1], writes=[kss2])
                self.rstd_from_ss(st[p2][:, 4:5], kss2, st[p2][:, 5:6], krs2, D)
                kh2b = ("E_h2b", p2)
                S.op("dve", lambda e, p2=p2: e.scalar_tensor_tensor(
                    out=h2b[p2][:], in0=x1t[p2][:], scalar=st[p2][:, 5:6], in1=gpre[:],
                    op0=ALU.mult, op1=ALU.mult), reads=[kx1, krs2, "E_gpre"], writes=[kh2b])
                self.transp8(h2b[p2], kh2b, h2s[slot][:, :, j * 128:(j + 1) * 128],
                             ("E_h2s", slot, j), j)
            S.dma(out=h2T_v[:, :, tok0:tok0 + 512], in_=h2s[slot][:],
                  sem="E_h2s%d" % slot, reads=[("E_h2s", slot, j) for j in range(4)])
        S.barrier()

    def load_convw(self, name, conv_ap, ntaps, ntiles):
        S = self.S
        raw = self.sb(name + "_raw", [ntiles, ntaps, 128], F32)
        cw = self.sb(name, [128, ntaps, ntiles], F32)
        S.dma(out=raw[:], in_=conv_ap.rearrange("j (t p) -> t j p", p=128), sem="ld_" + name,
              writes=[name + "_raw"])
        for j in range(ntaps):
            ps = self.ps[j % 2]
            kps = ("ps", j % 2)
            S.op("pe", lambda e, j=j, ps=ps: e.transpose(out=ps[:, :ntiles], in_=raw[:, j, :],
                                                         identity=self.ident_f[:ntiles, :ntiles]),
                 reads=[name + "_raw", "ident_f"], writes=[kps])
            S.op("dve", lambda e, j=j, ps=ps: e.tensor_copy(out=cw[:, j, :], in_=ps[:, :ntiles]),
                 reads=[kps], writes=[name])
        return cw

    def phase_F(self, l, dst):
        S = self.S
        self.sbuf_reset()
        NT = 22
        stage = [self.sb("F_stage%d" % i, [128, 512], F32) for i in range(2)]
        Wu = self.load_weight_bf16("F_Wu", self.w_up[l], 8, 2 * D_FF, stage, ["F_stg0", "F_stg1"])
        Wd = self.load_weight_bf16("F_Wd", self.w_down[l], NT, D, stage, ["F_stg0", "F_stg1"])
        gpost = self.load_bcast("F_gpost", self.g_ffn_post[l:l + 1, :], D)
        cw = self.load_convw("F_cw", self.ffn_conv[l], 3, 2 * NT)
        hT = self.sb("F_hT", [128, 8, 512], BF16)
        gT = self.sb("F_gT", [128, NT, 512], BF16)
        U = [self.sb("F_U%d" % i, [128, 514], F32) for i in range(2)]
        C = [self.sb("F_C%d" % i, [128, 512], F32) for i in range(2)]
        GL = self.sb("F_GL", [128, 512], F32)
        halo = self.sb("F_halo", [128, 2 * NT, 2], F32)
        x1t = [self.sb("F_x1t%d" % i, [128, D], F32) for i in range(2)]
        yt = [self.sb("F_yt%d" % i, [128, D], F32) for i in range(2)]
        junk = self.sb("F_junk", [128, 512], F32)
        st = [self.sb("F_st%d" % i, [128, 8], F32) for i in range(2)]
        h2T_v = self.h2T.rearrange("(k p) t -> p k t", p=128)
        for blk in range(NTOK // 512):
            tok0 = blk * 512
            if blk % (SEQ // 512) == 0:
                S.op("pool", lambda e: e.memset(halo[:], 0.0), writes=["F_halo"])
            S.dma(out=hT[:], in_=h2T_v[:, :, tok0:tok0 + 512], sem="F_hT", writes=["F_hT"])
            for i in range(NT):
                for gv in range(2):
                    ti = gv * NT + i
                    c0 = ti * 128
                    ps = self.ps[gv * 2 + i % 2]
                    kps = ("ps", gv * 2 + i % 2)

                    def mm(e, ps=ps, c0=c0):
                        ins = None
                        for k in range(8):
                            ins = e.matmul(ps[:, :], lhsT=Wu[:, k, c0:c0 + 128], rhs=hT[:, k, :],
                                           start=(k == 0), stop=(k == 7))
                        return ins
                    S.op("pe", mm, reads=["F_Wu", "F_hT"], writes=[kps])
                    u, ku = U[gv], ("F_U", gv)
                    c, kc = C[gv], ("F_C", gv)
                    S.op("act", lambda e, u=u, ps=ps: e.copy(out=u[:, 2:514], in_=ps[:, :]),
                         reads=[kps], writes=[(ku, "b")])
                    S.op("pool", lambda e, u=u, ti=ti: e.tensor_copy(out=u[:, 0:2],
                                                                     in_=halo[:, ti, :]),
                         reads=["F_halo"], writes=[(ku, "h")])
                    S.op("act", lambda e, u=u, c=c, ti=ti: e.activation(
                        out=c[:], in_=u[:, 0:512], func=AF.Copy, scale=cw[:, 0, ti:ti + 1]),
                        reads=[(ku, "b"), (ku, "h"), "F_cw"], writes=[kc])
                    for tap in (1, 2):
                        S.op("dve", lambda e, u=u, c=c, ti=ti, tap=tap: e.scalar_tensor_tensor(
                            out=c[:], in0=u[:, tap:tap + 512], scalar=cw[:, tap, ti:ti + 1],
                            in1=c[:], op0=ALU.mult, op1=ALU.add),
                            reads=[(ku, "b"), (ku, "h"), "F_cw", kc], writes=[kc])
                    S.op("pool", lambda e, u=u, ti=ti: e.tensor_copy(out=halo[:, ti, :],
                                                                     in_=u[:, 512:514]),
                         reads=[(ku, "b")], writes=["F_halo"])
                S.op("act", lambda e: e.activation(out=GL[:], in_=C[0][:],
                                                    func=AF.Gelu_apprx_tanh),
                     reads=[("F_C", 0)], writes=["F_GL"])
                S.op("pool", lambda e, i=i: e.tensor_tensor(out=gT[:, i, :], in0=GL[:],
                                                             in1=C[1][:], op=ALU.mult),
                     reads=["F_GL", ("F_C", 1)], writes=[("F_gT", i)])
            gkeys = [("F_gT", i) for i in range(NT)]
            for j in range(4):
                p2 = j % 2
                S.dma(out=x1t[p2][:], in_=self.x1[tok0 + j * 128: tok0 + (j + 1) * 128, :],
                      sem="F_x1t%d" % p2, writes=[("F_x1t", p2)])
                kss, krs = ("F_ss", p2), ("F_rs", p2)
                for hf in range(2):
                    ps = self.ps[4 + hf]
                    kps = ("ps", 4 + hf)

                    def mm(e, ps=ps, hf=hf, j=j):
                        ins = None
                        for k in range(NT):
                            ins = e.matmul(ps[:, :], lhsT=gT[:, k, j * 128:(j + 1) * 128],
                                           rhs=Wd[:, k, hf * 512:(hf + 1) * 512], start=(k == 0),
                                           stop=(k == NT - 1))
                        return ins
                    S.op("pe", mm, reads=["F_Wd"] + gkeys, writes=[kps])
                    S.op("act", lambda e, ps=ps, hf=hf, p2=p2: e.activation(
                        out=junk[:, :], in_=ps[:, :], func=AF.Square,
                        accum_out=st[p2][:, hf:hf + 1]), reads=[kps], writes=[(kss, hf)])
                S.op("dve", lambda e, p2=p2: e.tensor_tensor(out=st[p2][:, 2:3], in0=st[p2][:, 0:1],
                                                              in1=st[p2][:, 1:2], op=ALU.add),
                     reads=[(kss, 0), (kss, 1)], writes=[kss])
                self.rstd_from_ss(st[p2][:, 2:3], kss, st[p2][:, 3:4], krs, D)
                kyt = ("F_yt", p2)
                for hf in range(2):
                    ps = self.ps[4 + hf]
                    kps = ("ps", 4 + hf)
                    S.op("act", lambda e, ps=ps, hf=hf, p2=p2: e.activation(
                        out=yt[p2][:, hf * 512:(hf + 1) * 512], in_=ps[:, :], func=AF.Copy,
                        scale=st[p2][:, 3:4]), reads=[kps, krs], writes=[(kyt, hf)])
                S.op("dve", lambda e, p2=p2: e.tensor_tensor(out=yt[p2][:], in0=yt[p2][:],
                                                              in1=gpost[:], op=ALU.mult),
                     reads=[(kyt, 0), (kyt, 1), "F_gpost"], writes=[kyt])
                S.op("pool", lambda e, p2=p2: e.tensor_tensor(out=yt[p2][:], in0=yt[p2][:],
                                                               in1=x1t[p2][:], op=ALU.add),
                     reads=[kyt, ("F_x1t", p2)], writes=[kyt])
                S.dma(out=dst[tok0 + j * 128: tok0 + (j + 1) * 128, :], in_=yt[p2][:],
                      sem="F_yt%d" % p2, reads=[kyt])
        S.barrier()

    def phase_H(self, l):
        S = self.S
        self.sbuf_reset()
        sb = self.sb
        UU = sb("H_UU", [64, 128], F32)
        UL = sb("H_UL", [64, 64], F32)
        tmpm = sb("H_tmpm", [64, 64], F32)
        S.op("pool", lambda e: e.memset(UU[:], 1.0), writes=["H_UU"])
        S.op("pool", lambda e: e.affine_select(out=UU[:, 0:64], in_=UU[:, 0:64], pattern=[[1, 64]],
                                                compare_op=ALU.is_ge, fill=0.0, base=0,
                                                channel_multiplier=-1),
             reads=["H_UU"], writes=["H_UU"])
        S.op("pool", lambda e: e.memset(tmpm[:], 1.0), writes=["H_tmpm"])
        S.op("pool", lambda e: e.affine_select(out=tmpm[:], in_=tmpm[:], pattern=[[0, 64]],
                                                compare_op=ALU.is_ge, fill=0.0, base=31,
                                                channel_multiplier=-1),
             reads=["H_tmpm"], writes=["H_tmpm"])
        S.op("pool", lambda e: e.tensor_tensor(out=UU[:, 64:128], in0=UU[:, 0:64], in1=tmpm[:],
                                                op=ALU.subtract),
             reads=["H_UU", "H_tmpm"], writes=["H_UU"])
        S.op("pool", lambda e: e.memset(UL[:], 1.0), writes=["H_UL"])
        S.op("pool", lambda e: e.affine_select(out=UL[:], in_=UL[:], pattern=[[-1, 64]],
                                                compare_op=ALU.is_gt, fill=0.0, base=0,
                                                channel_multiplier=1),
             reads=["H_UL"], writes=["H_UL"])
        lb = sb("H_lb", [64, 256], F32)
        oml = sb("H_oml", [64, 256], F32)
        if l == 0:
            S.op("pool", lambda e: e.memset(lb[:], 0.0), writes=["H_lb"])
        else:
            r0 = sb("H_r0", [64, 256], F32)
            S.dma(out=r0[:], in_=bcast_rows(self.hg_lb[0:1, :], 64), sem="H_r0", writes=["H_r0"])
            S.dma(out=lb[:], in_=bcast_rows(self.hg_lb[1:2, :], 64), sem="H_lbl", writes=["H_lb"])
            S.op("dve", lambda e: e.tensor_tensor(out=lb[:], in0=lb[:], in1=r0[:], op=ALU.subtract),
                 reads=["H_lb", "H_r0"], writes=["H_lb"])
            S.op("act", lambda e: e.activation(out=lb[:], in_=lb[:], func=AF.Sigmoid),
                 reads=["H_lb"], writes=["H_lb"])
        S.op("dve", lambda e: e.tensor_scalar(out=oml[:], in0=lb[:], scalar1=-1.0, scalar2=1.0,
                                               op0=ALU.mult, op1=ALU.add),
             reads=["H_lb"], writes=["H_oml"])
        lbT = sb("H_lbT", [64, 4, 2], F32)
        for h in range(4):
            for which, src, ksrc in ((0, lb, "H_lb"), (1, oml, "H_oml")):
                ps = self.ps[(2 * h + which) % 4]
                kps = ("ps", (2 * h + which) % 4)
                S.op("pe", lambda e, ps=ps, src=src, h=h: e.transpose(
                    out=ps[:64, :64], in_=src[:, h * 64:(h + 1) * 64],
                    identity=self.ident_f[:64, :64]), reads=[ksrc, "ident_f"], writes=[kps])
                S.op("dve", lambda e, ps=ps, h=h, which=which: e.tensor_copy(
                    out=lbT[:, h, which:which + 1], in_=ps[:64, 0:1]), reads=[kps],
                    writes=["H_lbT"])
        ng = sb("H_ng", [64, 64], F32)
        S.dma(out=ng[:], in_=bcast_rows(self.hg_norm[l:l + 1, :], 64), sem="H_ng", writes=["H_ng"])

        qf = [sb("H_qf%d" % i, [64, 2, 4, 512], F32) for i in range(2)]
        sq = [sb("H_sq%d" % i, [64, 4, 512], F32) for i in range(2)]
        kc = [sb("H_kc%d" % i, [64, 4, 512], F32) for i in range(2)]
        tk = [sb("H_tk%d" % i, [64, 768], F32) for i in range(3)]
        St = [sb("H_S%d" % i, [64, 4, 64], F32) for i in range(2)]
        Sb = [sb("H_Sb%d" % i, [64, 4, 64], BF16) for i in range(2)]
        pf_q = self.pfeat[FM_ROW[2764]:FM_ROW[2764] + 256, :].rearrange("(h k) t -> k h t", k=64)
        pf_f = self.pfeat[FM_ROW[3020]:FM_ROW[3020] + 256, :].rearrange("(h k) t -> k h t", k=64)
        NB = 3

        def t3(name, shape, dtype):
            return [sb("%s%d" % (name, i), shape, dtype) for i in range(NB)]
        sig, fg, logf, kct, kd = (t3("H_sig", [64, 256], F32), t3("H_fg", [64, 256], F32),
                                  t3("H_logf", [64, 256], F32), t3("H_kct", [64, 256], F32),
                                  t3("H_kd", [64, 256], BF16))
        vb = t3("H_vb", [64, 256], BF16)
        gs = t3("H_gs", [64, 4, 64], F32)
        EB, EN = t3("H_EB", [64, 4, 128], F32), t3("H_EN", [64, 4, 64], F32)
        qd, qp, kp = (t3("H_qd", [64, 4, 64], BF16), t3("H_qp", [64, 4, 64], BF16),
                      t3("H_kp", [64, 4, 64], BF16))
        Am = t3("H_Am", [64, 4, 64], BF16)
        osb, osq = t3("H_osb", [64, 4, 64], F32), t3("H_osq", [64, 4, 64], F32)
        stt = t3("H_stt", [64, 8], F32)
        stmp = t3("H_stmp", [64, 4, 64], F32)
        def seq_thread(s):
            pb = lambda i: 4 * s + i % 4
            for blk8 in range(SEQ // 512):
                slot = (blk8 * NSEQ + s) % 2
                tb = s * SEQ + blk8 * 512
                kqf = ("H_qf", slot)
                S.dma(out=qf[slot][:, 0, :, :], in_=pf_q[:, :, tb:tb + 512], sem="H_qfq%d" % slot,
                      writes=[(kqf, 0)])
                S.dma(out=qf[slot][:, 1, :, :], in_=pf_f[:, :, tb:tb + 512], sem="H_qff%d" % slot,
                      writes=[(kqf, 1)])
                S.op("act", lambda e, slot=slot: e.activation(out=sq[slot][:], in_=qf[slot][:, 0],
                                                               func=AF.Silu),
                     reads=[(kqf, 0)], writes=[("H_sq", slot)])
                S.op("act", lambda e, slot=slot: e.activation(out=kc[slot][:], in_=qf[slot][:, 1],
                                                               func=AF.Sigmoid),
                     reads=[(kqf, 1)], writes=[("H_kc", slot)])
                for h in range(4):
                    S.op("dve", lambda e, slot=slot, h=h: e.tensor_scalar(
                        out=kc[slot][:, h, :], in0=kc[slot][:, h, :], scalar1=lbT[:, h, 1:2],
                        scalar2=-1.0, op0=ALU.mult, op1=ALU.mult),
                        reads=[("H_kc", slot), "H_lbT"], writes=[("H_kc", slot)])
                    S.op("dve", lambda e, slot=slot, h=h: e.tensor_scalar(
                        out=kc[slot][:, h, :], in0=kc[slot][:, h, :], scalar1=lbT[:, h, 1:2],
                        scalar2=None, op0=ALU.add),
                        reads=[("H_kc", slot), "H_lbT"], writes=[("H_kc", slot)])
                for cn in range(8):
                    b = s
                    c0 = cn * 64
                    tok = tb + c0
                    ktk = ("H_tk", b)
                    S.dma(out=tk[b][:], in_=self.ptok[tok:tok + 64, TM_CF:TM_CF + 768],
                          sem="H_tk%d" % b, writes=[ktk])
                    first = (blk8 == 0 and cn == 0)
                    S.op("act", lambda e, b=b: e.activation(out=sig[b][:], in_=tk[b][:, 0:256],
                                                             func=AF.Sigmoid),
                         reads=[ktk], writes=[("H_sig", b)])
                    S.op("dve", lambda e, b=b: e.tensor_tensor(out=fg[b][:], in0=sig[b][:],
                                                                in1=oml[:], op=ALU.mult),
                         reads=[("H_sig", b), "H_oml"], writes=[("H_fg", b)])
                    S.op("dve", lambda e, b=b: e.tensor_tensor(out=fg[b][:], in0=fg[b][:],
                                                                in1=lb[:], op=ALU.add),
                         reads=[("H_fg", b), "H_lb"], writes=[("H_fg", b)])
                    S.op("act", lambda e, b=b: e.activation(out=logf[b][:], in_=fg[b][:],
                                                             func=AF.Ln),
                         reads=[("H_fg", b)], writes=[("H_logf", b)])
                    S.op("pool", lambda e, b=b: e.tensor_scalar(out=kct[b][:], in0=fg[b][:],
                                                                 scalar1=-1.0, scalar2=1.0,
                                                                 op0=ALU.mult, op1=ALU.add),
                         reads=[("H_fg", b)], writes=[("H_kct", b)])
                    S.op("pool", lambda e, b=b: e.tensor_copy(out=vb[b][:], in_=tk[b][:, 256:512]),
                         reads=[ktk], writes=[("H_vb", b)])
                    S.op("act", lambda e, b=b: e.activation(
                        out=gs[b][:], in_=tk[b][:, 512:768].rearrange("p (h v) -> p h v", h=4),
                        func=AF.Silu), reads=[ktk], writes=[("H_gs", b)])
                    S.op("pool", lambda e, b=b: e.tensor_tensor(
                        out=gs[b][:], in0=gs[b][:],
                        in1=ng[:].unsqueeze(1).to_broadcast([64, 4, 64]), op=ALU.mult),
                        reads=[("H_gs", b), "H_ng"], writes=[("H_gs", b)])
                    p0, kp0 = self.ps8[pb(0)], ("ps", pb(0))

                    def mmb(e, b=b, p0=p0):
                        ins = None
                        for h in range(4):
                            ins = e.matmul(p0[:64, h * 128:(h + 1) * 128],
                                           lhsT=logf[b][:, h * 64:(h + 1) * 64], rhs=UU[:, :],
                                           start=True, stop=True)
                        return ins
                    S.op("pe", mmb, reads=[("H_logf", b), "H_UU"], writes=[kp0])
                    p0v = p0[:64, :].rearrange("p (h t) -> p h t", h=4)
                    S.op("act", lambda e, b=b, p0v=p0v: e.activation(out=EB[b][:], in_=p0v,
                                                                      func=AF.Exp),
                         reads=[kp0], writes=[("H_EB", b)])
                    S.op("act", lambda e, b=b, p0v=p0v: e.activation(out=EN[b][:],
                                                                      in_=p0v[:, :, 64:128],
                                                                      func=AF.Exp, scale=-1.0),
                         reads=[kp0], writes=[("H_EN", b)])
                    p1, kp1 = self.ps8[pb(1)], ("ps", pb(1))
                    S.op("pe", lambda e, b=b, p1=p1: e.matmul(p1[:64, :256], lhsT=UL[:, :],
                                                              rhs=logf[b][:, :], start=True,
                                                              stop=True),
                         reads=[("H_logf", b), "H_UL"], writes=[kp1])
                    S.op("act", lambda e, b=b, p1=p1: e.activation(out=sig[b][:], in_=p1[:64, :256],
                                                                    func=AF.Exp),
                         reads=[kp1], writes=[("H_sig", b)])
                    S.op("dve", lambda e, b=b: e.tensor_tensor(out=kd[b][:], in0=sig[b][:],
                                                                in1=kct[b][:], op=ALU.mult),
                         reads=[("H_sig", b), ("H_kct", b)], writes=[("H_kd", b)])
                    sqv = sq[slot][:, :, c0:c0 + 64]
                    kcv = kc[slot][:, :, c0:c0 + 64]
                    S.op("dve", lambda e, b=b, sqv=sqv: e.tensor_tensor(
                        out=qd[b][:], in0=sqv, in1=EB[b][:, :, 0:64], op=ALU.mult),
                        reads=[("H_sq", slot), ("H_EB", b)], writes=[("H_qd", b)])
                    S.op("pool", lambda e, b=b, sqv=sqv: e.tensor_tensor(
                        out=qp[b][:], in0=sqv, in1=EB[b][:, :, 64:128], op=ALU.mult),
                        reads=[("H_sq", slot), ("H_EB", b)], writes=[("H_qp", b)])
                    S.op("dve", lambda e, b=b, kcv=kcv: e.tensor_tensor(
                        out=kp[b][:], in0=kcv, in1=EN[b][:], op=ALU.mult),
                        reads=[("H_kc", slot), ("H_EN", b)], writes=[("H_kp", b)])
                    p2, kp2 = self.ps8[pb(2)], ("ps", pb(2))

                    def mma(e, b=b, p2=p2):
                        ins = None
                        for h in range(4):
                            ins = e.matmul(p2[:64, h * 64:(h + 1) * 64], lhsT=kp[b][:, h, :],
                                           rhs=qp[b][:, h, :], start=True, stop=True)
                        return ins
                    S.op("pe", mma, reads=[("H_kp", b), ("H_qp", b)], writes=[kp2])
                    S.op("dve", lambda e, b=b, p2=p2: e.tensor_tensor(
                        out=Am[b][:], in0=p2[:64, :256].rearrange("p (h c) -> p h c", h=4),
                        in1=UU[:, 0:64].unsqueeze(1).to_broadcast([64, 4, 64]), op=ALU.mult),
                        reads=[kp2, "H_UU"], writes=[("H_Am", b)])
                    if first:
                        S.op("pool", lambda e, s=s: e.memset(St[s][:], 0.0), writes=[("H_S", s)])
                        S.op("pool", lambda e, s=s: e.memset(Sb[s][:], 0.0), writes=[("H_Sb", s)])
                    p3, kp3 = self.ps8[pb(3)], ("ps", pb(3))

                    def mmo(e, b=b, p3=p3, s=s):
                        ins = None
                        for h in range(4):
                            e.matmul(p3[:64, h * 64:(h + 1) * 64], lhsT=qd[b][:, h, :],
                                     rhs=Sb[s][:, h, :], start=True, stop=False)
                            ins = e.matmul(p3[:64, h * 64:(h + 1) * 64], lhsT=Am[b][:, h, :],
                                           rhs=vb[b][:, h * 64:(h + 1) * 64], start=False, stop=True)
                        return ins
                    S.op("pe", mmo, reads=[("H_qd", b), ("H_Sb", s), ("H_Am", b), ("H_vb", b)],
                         writes=[kp3])
                    p4, kp4 = self.ps8[pb(4)], ("ps", pb(4))

                    def mms(e, b=b, p4=p4):
                        ins = None
                        for h in range(4):
                            ins = e.matmul(p4[:64, h * 64:(h + 1) * 64],
                                           lhsT=kd[b][:, h * 64:(h + 1) * 64],
                                           rhs=vb[b][:, h * 64:(h + 1) * 64], start=True, stop=True)
                        return ins
                    S.op("pe", mms, reads=[("H_kd", b), ("H_vb", b)], writes=[kp4])
                    S.op("dve", lambda e, b=b, s=s: e.tensor_tensor(
                        out=stmp[b][:], in0=St[s][:],
                        in1=EB[b][:, :, 63:64].to_broadcast([64, 4, 64]), op=ALU.mult),
                        reads=[("H_S", s), ("H_EB", b)], writes=[("H_stmp", b)])
                    S.op("dve", lambda e, b=b, s=s, p4=p4: e.tensor_tensor(
                        out=St[s][:], in0=stmp[b][:],
                        in1=p4[:64, :256].rearrange("p (h v) -> p h v", h=4), op=ALU.add),
                        reads=[("H_stmp", b), kp4], writes=[("H_S", s)])
                    S.op("act", lambda e, s=s: e.copy(out=Sb[s][:], in_=St[s][:]),
                         reads=[("H_S", s)], writes=[("H_Sb", s)])
                    S.op("act", lambda e, b=b, p3=p3: e.copy(
                        out=osb[b][:], in_=p3[:64, :256].rearrange("p (h v) -> p h v", h=4)),
                        reads=[kp3], writes=[("H_osb", b)])
                    S.op("pool", lambda e, b=b: e.tensor_tensor(out=osq[b][:], in0=osb[b][:],
                                                                 in1=osb[b][:], op=ALU.mult),
                         reads=[("H_osb", b)], writes=[("H_osq", b)])
                    S.op("dve", lambda e, b=b: e.tensor_reduce(out=stt[b][:, 0:4], in_=osq[b][:],
                                                                axis=AX.X, op=ALU.add),
                         reads=[("H_osq", b)], writes=[("H_stt", b)])
                    self.rstd_from_ss(stt[b][:, 0:4], ("H_stt", b), stt[b][:, 4:8], ("H_rs", b), 64)
                    S.op("dve", lambda e, b=b: e.tensor_tensor(
                        out=osb[b][:], in0=osb[b][:],
                        in1=stt[b][:, 4:8].unsqueeze(2).to_broadcast([64, 4, 64]), op=ALU.mult),
                        reads=[("H_osb", b), ("H_rs", b)], writes=[("H_osb", b)])
                    S.op("pool", lambda e, b=b: e.tensor_tensor(out=osq[b][:], in0=osb[b][:],
                                                                 in1=gs[b][:], op=ALU.mult),
                         reads=[("H_osb", b), ("H_gs", b)], writes=[("H_osq", b)])
                    S.dma(out=self.mixin[tok:tok + 64, 768:1024],
                          in_=osq[b][:].rearrange("p h v -> p (h v)"), sem="H_out%d" % b,
                          reads=[("H_osq", b)])
        S.interleave([lambda s=s: seq_thread(s) for s in range(NSEQ)])
        S.barrier()

    def phase_S(self, l):
        S = self.S
        self.sbuf_reset()
        sb = self.sb
        BIG = -1.0e30
        KIT = 32
        kiT = sb("S_kiT", [64, SEQ], F32)
        kT = sb("S_kT", [64, SEQ], BF16)
        kst = sb("S_kst", [64, 1024], F32)
        vst = sb("S_vst", [128, 8, 64], F32)
        v1 = sb("S_v1", [128, 32, 65], BF16)
        junk = sb("S_junk", [128, SEQ], BF16)
        ckn = sb("S_ckn", [128, KIT + 1], F32)
        for k in range(KIT + 1):
            S.op("pool", lambda e, k=k: e.memset(ckn[:, k:k + 1], -2.1 / 2.0 ** (k + 1)),
                 writes=[("S_ckn", k)])
        kck = [("S_ckn", k) for k in range(KIT + 1)]

        NT = 2

        def t2(name, shape, dtype):
            return [sb("%s%d" % (name, i), shape, dtype) for i in range(NT)]
        qq = t2("S_qq", [64, 2, 4, 128], F32)
        qqb = t2("S_qqb", [64, 4, 128], BF16)
        acc = t2("S_acc", [128, SEQ], F32)
        wkA = t2("S_wkA", [128, SEQ], F32)
        gtt = t2("S_gt", [128, SEQ], BF16)
        eqt = t2("S_eq", [128, SEQ], BF16)
        selb = t2("S_selb", [128, SEQ], BF16)
        rr = [sb("S_rr%d" % i, [128, 512], F32) for i in range(2 * NT)]
        pT = [sb("S_pT%d" % i, [128, 512], BF16) for i in range(2 * NT)]
        wi = t2("S_wi", [128, 12], F32)
        sc = t2("S_sc", [128, 16], F32)
        nst = t2("S_nst", [128, KIT + 1], F32)
        oT = t2("S_oT", [65, 512], F32)
        osb = t2("S_osb", [128, 4, 64], F32)
        rc = t2("S_rc", [128, 4, 1], F32)
        pf_q = self.pfeat[FM_ROW[2056]:FM_ROW[2056] + 256, :].rearrange("(h k) t -> k h t", k=64)
        pf_qi = self.pfeat[FM_ROW[2440]:FM_ROW[2440] + 256, :].rearrange("(h k) t -> k h t", k=64)
        pf_k = self.pfeat[FM_ROW[2312]:FM_ROW[2312] + 64, :]
        pf_ki = self.pfeat[FM_ROW[2696]:FM_ROW[2696] + 64, :]

        def tile_thread(s, a2):
            t0 = s * SEQ
            nr = 0
            for j in range(a2, SEQ // 128, NT):
                tq = t0 + j * 128
                NK = (j + 1) * 128
                kqq = ("S_qq", a2)
                S.dma(out=qq[a2][:, 0], in_=pf_q[:, :, tq:tq + 128], sem="S_qq%da" % a2,
                      writes=[(kqq, 0)])
                S.dma(out=qq[a2][:, 1], in_=pf_qi[:, :, tq:tq + 128], sem="S_qq%db" % a2,
                      writes=[(kqq, 1)])
                S.op("pool", lambda e: e.tensor_copy(out=qqb[a2][:], in_=qq[a2][:, 0]),
                     reads=[(kqq, 0)], writes=[("S_qqb", a2)])
                kwi = ("S_wi", a2)
                S.dma(out=wi[a2][:, 0:4], in_=self.ptok[tq:tq + 128, TM_WI:TM_WI + 4],
                      sem="S_wi%d" % a2, writes=[(kwi, 0)])
                S.op("act", lambda e: e.activation(out=wi[a2][:, 4:8], in_=wi[a2][:, 0:4],
                                                    func=AF.Abs),
                     reads=[(kwi, 0)], writes=[(kwi, 1)])
                S.op("act", lambda e: e.activation(out=wi[a2][:, 8:12], in_=wi[a2][:, 0:4],
                                                    func=AF.Sign),
                     reads=[(kwi, 0)], writes=[(kwi, 2)])
                kacc = ("S_acc", a2)
                nkb = (NK + 511) // 512
                for kb in range(nkb):
                    w = min(512, NK - kb * 512)
                    for h in range(4):
                        pi = 2 * a2 + h % 2
                        ps, kps = self.ps8[pi], ("ps", pi)
                        S.op("pe", lambda e, ps=ps, h=h, kb=kb, w=w: e.matmul(
                            ps[:, :w], lhsT=qq[a2][:, 1, h, :],
                            rhs=kiT[:, kb * 512: kb * 512 + w], start=True, stop=True),
                            reads=[(kqq, 1), "S_kiT"], writes=[kps])
                        ri = 2 * a2 + nr % 2
                        r, kr = rr[ri], ("S_rr", ri)
                        nr += 1
                        S.op("act", lambda e, ps=ps, r=r, w=w, h=h: e.activation(
                            out=r[:, :w], in_=ps[:, :w], func=AF.Relu, scale=wi[a2][:, 4 + h:5 + h]),
                            reads=[kps, (kwi, 1)], writes=[kr])
                        av = acc[a2][:, kb * 512: kb * 512 + w]
                        if h == 0:
                            S.op("dve", lambda e, av=av, r=r, w=w, h=h: e.tensor_scalar(
                                out=av, in0=r[:, :w], scalar1=wi[a2][:, 8 + h:9 + h], scalar2=None,
                                op0=ALU.mult), reads=[kr, (kwi, 2)], writes=[(kacc, kb)])
                        else:
                            S.op("dve", lambda e, av=av, r=r, w=w, h=h: e.scalar_tensor_tensor(
                                out=av, in0=r[:, :w], scalar=wi[a2][:, 8 + h:9 + h], in1=av,
                                op0=ALU.mult, op1=ALU.add),
                                reads=[kr, (kwi, 2), (kacc, kb)], writes=[(kacc, kb)])
                kall = [(kacc, kb) for kb in range(nkb)]
                X = sc[a2]
                ksc = lambda nm: ("S_sc", a2, nm)
                accv = acc[a2][:, :NK]
                if j >= 2:
                    S.op("dve", lambda e: e.tensor_reduce(out=X[:, 0:1], in_=accv, axis=AX.X,
                                                           op=ALU.max, apply_absolute_value=True),
                         reads=kall, writes=[ksc("rm")])
                    S.op("dve", lambda e: e.tensor_scalar(out=X[:, 0:1], in0=X[:, 0:1],
                                                           scalar1=1.0e-20, scalar2=None,
                                                           op0=ALU.max),
                         reads=[ksc("rm")], writes=[ksc("rm")])
                    S.op("dve", lambda e: e.tensor_scalar(out=nst[a2][:], in0=ckn[:],
                                                           scalar1=X[:, 0:1], scalar2=None,
                                                           op0=ALU.mult),
                         reads=[ksc("rm")] + kck, writes=[("S_nst", a2)])
                    S.op("dve", lambda e: e.tensor_scalar(out=X[:, 1:2], in0=X[:, 0:1],
                                                           scalar1=-0.02, scalar2=None,
                                                           op0=ALU.mult),
                         reads=[ksc("rm")], writes=[ksc("nc")])
                    S.op("pool", lambda e: e.memset(X[:, 4:5], float(NK) - 511.5),
                         writes=[ksc("cb")])
                S.op("pool", lambda e: e.memset(acc[a2][0:64, NK - 64:NK], BIG),
                     reads=kall + [ksc("rm")], writes=[kacc])
                if j >= 2:
                    for k in range(KIT):
                        S.op("act", lambda e: e.activation(out=junk[:, :NK], in_=accv, func=AF.Sign,
                                                            bias=X[:, 1:2], scale=1.0,
                                                            accum_out=X[:, 2:3]),
                             reads=[kacc, ksc("nc")], writes=[ksc("sg")])
                        S.op("act", lambda e: e.activation(out=X[:, 3:4], in_=X[:, 2:3],
                                                            func=AF.Sign, bias=X[:, 4:5], scale=1.0),
                             reads=[ksc("sg"), ksc("cb")], writes=[ksc("dd")])
                        S.op("act", lambda e, k=k: e.activation(out=X[:, 1:2], in_=X[:, 3:4],
                                                                 func=AF.Identity,
                                                                 scale=nst[a2][:, k + 1:k + 2],
                                                                 bias=X[:, 1:2]),
                             reads=[ksc("dd"), ("S_nst", a2), ksc("nc")], writes=[ksc("nc")])
                    S.op("dve", lambda e: e.scalar_tensor_tensor(
                        out=X[:, 5:6], in0=X[:, 1:2], scalar=-1.0, in1=nst[a2][:, KIT:KIT + 1],
                        op0=ALU.mult, op1=ALU.add), reads=[ksc("nc"), ("S_nst", a2)],
                        writes=[ksc("thr")])
                else:
                    S.op("pool", lambda e: e.memset(X[:, 5:6], BIG / 2), writes=[ksc("thr")])
                kwk = ("S_wkA", a2)
                wv = wkA[a2][:, :NK]
                S.op("dve", lambda e: e.tensor_scalar(out=wv, in0=accv, scalar1=X[:, 5:6],
                                                       scalar2=3.0e30, op0=ALU.is_lt, op1=ALU.mult),
                     reads=[kacc, ksc("thr")], writes=[kwk])
                S.op("dve", lambda e: e.tensor_tensor(out=wv, in0=wv, in1=accv, op=ALU.add),
                     reads=[kacc, kwk], writes=[kwk])
                S.op("dve", lambda e: e.tensor_reduce(out=X[:, 6:7], in_=wv, axis=AX.X, op=ALU.min),
                     reads=[kwk], writes=[ksc("v")])
                gv, ev = gtt[a2][:, :NK], eqt[a2][:, :NK]
                S.op("dve", lambda e: e.tensor_scalar(out=gv, in0=accv, scalar1=X[:, 6:7],
                                                       scalar2=None, op0=ALU.is_gt, op1=ALU.add,
                                                       accum_out=X[:, 7:8]),
                     reads=[kacc, ksc("v")], writes=[("S_gt", a2), ksc("cg")])
                S.op("dve", lambda e: e.tensor_scalar(out=ev, in0=accv, scalar1=X[:, 6:7],
                                                       scalar2=None, op0=ALU.is_equal),
                     reads=[kacc, ksc("v")], writes=[("S_eq", a2)])
                S.op("dve", lambda e: e.tensor_tensor_scan(out=wv, data0=ev, data1=ev, initial=0.0,
                                                            op0=ALU.add, op1=ALU.max),
                     reads=[("S_eq", a2), kwk], writes=[kwk])
                S.op("dve", lambda e: e.tensor_scalar(out=X[:, 8:9], in0=X[:, 7:8], scalar1=-1.0,
                                                       scalar2=256.0, op0=ALU.mult, op1=ALU.add),
                     reads=[ksc("cg")], writes=[ksc("need")])
                S.op("dve", lambda e: e.scalar_tensor_tensor(out=ev, in0=wv, scalar=X[:, 8:9],
                                                              in1=ev, op0=ALU.is_le, op1=ALU.mult),
                     reads=[kwk, ksc("need"), ("S_eq", a2)], writes=[("S_eq", a2)])
                S.op("pool", lambda e: e.tensor_tensor(out=gv, in0=gv, in1=ev, op=ALU.add),
                     reads=[("S_gt", a2), ("S_eq", a2)], writes=[("S_gt", a2)])
                ksel = ("S_selb", a2)
                S.op("pool", lambda e: e.tensor_scalar(out=selb[a2][:, :NK], in0=gv, scalar1=-1.0,
                                                        scalar2=-NEG, op0=ALU.add, op1=ALU.mult),
                     reads=[("S_gt", a2)], writes=[ksel])
                po, kpo = self.ps8[4 + a2], ("ps", 4 + a2)
                for kt in range(j + 1):
                    li = (0, 1, 6, 7)[2 * a2 + kt % 2]
                    pl, kpl = self.ps8[li], ("ps", li)

                    def mml(e, pl=pl, kt=kt):
                        e.matmul(pl[:, :].rearrange("p (h t) -> p h t", h=4),
                                 lhsT=kT[:, kt * 128:(kt + 1) * 128], rhs=qqb[a2][:, :, :],
                                 start=True, stop=False)
                        ins = None
                        for h in range(4):
                            ins = e.matmul(pl[:, h * 128:(h + 1) * 128],
                                           lhsT=selb[a2][:, kt * 128:(kt + 1) * 128],
                                           rhs=self.ident_b[:, :], start=False, stop=(h == 3))
                        return ins
                    S.op("pe", mml, reads=["S_kT", ("S_qqb", a2), ksel, "ident_b"], writes=[kpl])
                    pi_ = 2 * a2 + kt % 2
                    p, kp = pT[pi_], ("S_pT", pi_)
                    S.op("act", lambda e, p=p, pl=pl: e.activation(out=p[:], in_=pl[:, :],
                                                                    func=AF.Exp, scale=0.125),
                         reads=[kpl], writes=[kp])
                    S.op("pe", lambda e, p=p, kt=kt: e.matmul(
                        po[:65, :], lhsT=v1[:, kt, :], rhs=p[:], start=(kt == 0), stop=(kt == j)),
                        reads=["S_v1", kp], writes=[kpo])
                koT = ("S_oT", a2)
                S.op("act", lambda e: e.copy(out=oT[a2][:], in_=po[:65, :]),
                     reads=[kpo], writes=[koT])
                pt, kpt = self.ps8[2 * a2], ("ps", 2 * a2)

                def trs(e):
                    ins = None
                    for h in range(4):
                        ins = e.transpose(out=pt[:, h * 65:(h + 1) * 65],
                                          in_=oT[a2][:, h * 128:(h + 1) * 128],
                                          identity=self.ident_f[:65, :65])
                    return ins
                S.op("pe", trs, reads=[koT, "ident_f"], writes=[kpt])
                ptv = pt[:, :260].rearrange("p (h d) -> p h d", h=4)
                S.op("dve", lambda e: e.reciprocal(out=rc[a2][:], in_=ptv[:, :, 64:65]),
                     reads=[kpt], writes=[("S_rc", a2)])
                S.op("dve", lambda e: e.tensor_tensor(
                    out=osb[a2][:], in0=ptv[:, :, 0:64], in1=rc[a2][:].to_broadcast([128, 4, 64]),
                    op=ALU.mult), reads=[kpt, ("S_rc", a2)], writes=[("S_osb", a2)])
                S.dma(out=self.mixin[tq:tq + 128, 512:768],
                      in_=osb[a2][:].rearrange("p h d -> p (h d)"), sem="S_out%d" % a2,
                      reads=[("S_osb", a2)])

        for s in range(NSEQ):
            t0 = s * SEQ
            S.dma(out=kiT[:], in_=pf_ki[:, t0:t0 + SEQ], sem="S_kiT", writes=["S_kiT"])
            for q4 in range(4):
                S.dma(out=kst[:], in_=pf_k[:, t0 + q4 * 1024:t0 + (q4 + 1) * 1024], sem="S_kst",
                      writes=["S_kst"])
                S.op("pool", lambda e, q4=q4: e.tensor_copy(out=kT[:, q4 * 1024:(q4 + 1) * 1024],
                                                            in_=kst[:]),
                     reads=["S_kst"], writes=["S_kT"])
            S.op("pool", lambda e: e.memset(v1[:], 1.0), writes=["S_v1"])
            for q4 in range(4):
                S.dma(out=vst[:],
                      in_=self.ptok[t0 + q4 * 1024: t0 + (q4 + 1) * 1024,
                                    TM_BV:TM_BV + 64].rearrange("(kt p) d -> p kt d", p=128),
                      sem="S_vst", writes=["S_vst"])
                S.op("pool", lambda e, q4=q4: e.tensor_copy(out=v1[:, q4 * 8:(q4 + 1) * 8, 0:64],
                                                            in_=vst[:]),
                     reads=["S_vst"], writes=["S_v1"])
            S.interleave([lambda s=s, a=a: tile_thread(s, a) for a in range(NT)])
        S.barrier()

    def phase_D(self, l):
        S = self.S
        self.sbuf_reset()
        sb = self.sb
        NB = 2
        U64 = sb("D_U", [64, 64], F32)
        UL = sb("D_UL", [64, 64], F32)
        Lst = sb("D_Lst", [64, 64], F32)
        mb = sb("D_mb", [64, 64], F32)
        S.op("pool", lambda e: e.memset(U64[:], 1.0), writes=["D_U"])
        S.op("pool", lambda e: e.affine_select(out=U64[:], in_=U64[:], pattern=[[1, 64]],
                                                compare_op=ALU.is_ge, fill=0.0, base=0,
                                                channel_multiplier=-1), reads=["D_U"],
             writes=["D_U"])
        for t_, kk_ in ((UL, "D_UL"), (Lst, "D_Lst")):
            S.op("pool", lambda e, t_=t_: e.memset(t_[:], 1.0), writes=[kk_])
            S.op("pool", lambda e, t_=t_: e.affine_select(out=t_[:], in_=t_[:], pattern=[[-1, 64]],
                                                          compare_op=ALU.is_gt, fill=0.0, base=0,
                                                          channel_multiplier=1), reads=[kk_],
                 writes=[kk_])
        S.op("pool", lambda e: e.memset(mb[:], 0.0), writes=["D_mb"])
        S.op("pool", lambda e: e.affine_select(out=mb[:], in_=mb[:], pattern=[[-1, 64]],
                                                compare_op=ALU.is_ge, fill=-1.0e4, base=0,
                                                channel_multiplier=1), reads=["D_mb"],
             writes=["D_mb"])
        cw = self.load_convw("D_cw", self.dn_conv[l], 4, 12)
        alog = sb("D_alog", [64, 4], F32)
        dtb = sb("D_dtb", [64, 4], F32)
        S.dma(out=alog[:], in_=bcast_rows(self.dn_a_log[l:l + 1, :], 64), sem="D_alog",
              writes=["D_alog"])
        S.dma(out=dtb[:], in_=bcast_rows(self.dn_dt_bias[l:l + 1, :], 64), sem="D_dtb",
              writes=["D_dtb"])
        S.op("act", lambda e: e.activation(out=alog[:], in_=alog[:], func=AF.Exp), reads=["D_alog"],
             writes=["D_alog"])
        S.op("dve", lambda e: e.tensor_scalar(out=alog[:], in0=alog[:], scalar1=-1.0, scalar2=None,
                                               op0=ALU.mult), reads=["D_alog"], writes=["D_alog"])
        ng = sb("D_ng", [64, 128], F32)
        S.dma(out=ng[:], in_=bcast_rows(self.dn_norm[l:l + 1, :], 64), sem="D_ng", writes=["D_ng"])

        X = [sb("D_X%d" % i, [128, 515], F32) for i in range(4)]
        Cc = [sb("D_C%d" % i, [128, 512], F32) for i in range(4)]
        Y = [sb("D_Y%d" % i, [128, 12, 512], F32) for i in range(2)]
        St = [sb("D_S%d" % i, [128, 4, 128], F32) for i in range(NSEQ)]
        Sb = [sb("D_Sb%d" % i, [128, 4, 128], BF16) for i in range(NSEQ)]

        def t2(name, shape, dtype):
            return [sb("%s%d" % (name, i), shape, dtype) for i in range(NB)]
        tg = t2("D_tg", [64, 8], F32)
        gt = t2("D_gt", [64, 24], F32)
        zt = t2("D_zt", [64, 4, 128], F32)
        gs = t2("D_gs", [64, 4, 128], F32)
        QK = t2("D_QK", [64, 8, 128], F32)
        Vt = t2("D_Vt", [64, 4, 128], F32)
        sqq = t2("D_sqq", [64, 8, 128], F32)
        nrm = t2("D_nrm", [64, 16], F32)
        qdk = t2("D_qdk", [64, 4, 128], F32)
        TT = t2("D_TT", [128, 12, 64], BF16)
        kd = t2("D_kd", [64, 4, 128], BF16)
        bk = t2("D_bk", [64, 4, 128], BF16)
        bv = t2("D_bv", [64, 4, 128], BF16)
        gU = t2("D_gU", [64, 8, 64], F32)
        dec = t2("D_dec", [64, 4, 64], F32)
        Mm = t2("D_M", [64, 4, 64], F32)
        QKm = t2("D_QKm", [64, 4, 64], F32)
        QKT = t2("D_QKT", [64, 4, 64], BF16)
        Qa = t2("D_Qa", [64, 4, 64], F32)
        Qta = t2("D_Qta", [64, 4, 64], F32)
        Qb = t2("D_Qb", [64, 4, 64], F32)
        Qtb = t2("D_Qtb", [64, 4, 64], F32)
        Bt = t2("D_Bt", [64, 4, 64], F32)
        Tb = t2("D_Tb", [64, 4, 64], BF16)
        u0 = t2("D_u0", [64, 4, 128], F32)
        ub = t2("D_ub", [64, 4, 128], BF16)
        wkT = t2("D_wkT", [128, 4, 64], BF16)
        glb = t2("D_glb", [128, 4], F32)
        osb = t2("D_osb", [64, 4, 128], F32)
        osq = t2("D_osq", [64, 4, 128], F32)
        identI = sb("D_I", [64, 4, 64], F32)
        for h in range(4):
            S.op("pool", lambda e, h=h: e.tensor_copy(out=identI[:, h, :], in_=self.ident_f[:64, :64]),
                 reads=["ident_f"], writes=["D_I"])

        def v4(ps, w):
            return ps[:64, :4 * w].rearrange("p (h x) -> p h x", h=4)

        def bc(ap_h1, w):
            a = ap_h1 if len(ap_h1.shape) == 3 else ap_h1.unsqueeze(2)
            return a.to_broadcast([64, 4, w])

        def seq_thread(s):
            pb = lambda i: 4 * s + i % 4
            for blk8 in range(SEQ // 512):
                ys = (blk8 * NSEQ + s) % 2
                tb = s * SEQ + blk8 * 512
                for ct in range(12):
                    xi = 2 * s + ct % 2
                    x = X[xi]
                    kx = ("D_X", xi)
                    if blk8 == 0:
                        S.op("pool", lambda e, x=x: e.memset(x[:, 0:3], 0.0), writes=[(kx, "h")])
                        S.dma(out=x[:, 3:515], in_=self.pfeat[ct * 128:(ct + 1) * 128, tb:tb + 512],
                              sem="D_X%d" % xi, writes=[(kx, "b")])
                    else:
                        S.dma(out=x[:, :], in_=self.pfeat[ct * 128:(ct + 1) * 128, tb - 3:tb + 512],
                              sem="D_X%d" % xi, writes=[(kx, "h"), (kx, "b")])
                    c, kc = Cc[xi], ("D_C", xi)
                    S.op("act", lambda e, x=x, c=c, ct=ct: e.activation(
                        out=c[:], in_=x[:, 0:512], func=AF.Copy, scale=cw[:, 0, ct:ct + 1]),
                        reads=[(kx, "h"), (kx, "b"), "D_cw"], writes=[kc])
                    for tap in (1, 2, 3):
                        S.op("dve", lambda e, x=x, c=c, ct=ct, tap=tap: e.scalar_tensor_tensor(
                            out=c[:], in0=x[:, tap:tap + 512], scalar=cw[:, tap, ct:ct + 1],
                            in1=c[:], op0=ALU.mult, op1=ALU.add),
                            reads=[(kx, "h"), (kx, "b"), "D_cw", kc], writes=[kc])
                    S.op("act", lambda e, c=c, ct=ct, ys=ys: e.activation(out=Y[ys][:, ct, :],
                                                                           in_=c[:], func=AF.Silu),
                         reads=[kc], writes=[("D_Y", ys, ct)])
                ykeys = [("D_Y", ys, ct) for ct in range(12)]
                for cn in range(8):
                    b = s
                    c0 = cn * 64
                    tok = tb + c0
                    K = lambda nm: (nm, b)
                    first = (blk8 == 0 and cn == 0)
                    if first:
                        S.op("pool", lambda e, s=s: e.memset(St[s][:], 0.0), writes=[("D_S", s)])
                        S.op("pool", lambda e, s=s: e.memset(Sb[s][:], 0.0), writes=[("D_Sb", s)])
                    S.dma(out=tg[b][:], in_=self.ptok[tok:tok + 64, TM_AB:TM_AB + 8],
                          sem="D_tg%d" % b, writes=[K("tg")])
                    S.dma(out=zt[b][:].rearrange("p h v -> p (h v)"),
                          in_=self.ptok[tok:tok + 64, TM_AZ:TM_AZ + 512], sem="D_zt%d" % b,
                          writes=[K("zt")])
                    G = gt[b]
                    S.op("act", lambda e, b=b, G=G: e.activation(out=G[:, 0:4], in_=tg[b][:, 0:4],
                                                                  func=AF.Sigmoid),
                         reads=[K("tg")], writes=[K("beta")])
                    S.op("dve", lambda e, b=b, G=G: e.tensor_tensor(out=G[:, 20:24], in0=tg[b][:, 4:8],
                                                                     in1=dtb[:], op=ALU.add),
                         reads=[K("tg"), "D_dtb"], writes=[K("gtmp")])
                    S.op("act", lambda e, G=G: e.activation(out=G[:, 20:24], in_=G[:, 20:24],
                                                             func=AF.Exp),
                         reads=[K("gtmp")], writes=[K("gtmp")])
                    S.op("act", lambda e, G=G: e.activation(out=G[:, 20:24], in_=G[:, 20:24],
                                                             func=AF.Ln, bias=1.0),
                         reads=[K("gtmp")], writes=[K("gtmp")])
                    S.op("dve", lambda e, G=G: e.tensor_tensor(out=G[:, 4:8], in0=G[:, 20:24],
                                                                in1=alog[:], op=ALU.mult),
                         reads=[K("gtmp"), "D_alog"], writes=[K("g")])
                    pg, kpg = self.ps8[pb(5)], ("ps", pb(5))

                    def mmg(e, G=G, pg=pg):
                        e.matmul(pg[:64, 0:4], lhsT=U64[:, :], rhs=G[:, 4:8], start=True, stop=True)
                        e.matmul(pg[:64, 4:8], lhsT=UL[:, :], rhs=G[:, 4:8], start=True, stop=True)
                        return e.matmul(pg[:, 8:12], lhsT=self.ones_f[:64, :], rhs=G[:, 4:8],
                                        start=True, stop=True)
                    S.op("pe", mmg, reads=[K("g"), "D_U", "D_UL", "ones_f"], writes=[kpg])
                    S.op("act", lambda e, G=G, pg=pg: e.activation(out=G[:, 8:16], in_=pg[:64, 0:8],
                                                                    func=AF.Exp),
                         reads=[kpg], writes=[K("eG")])
                    S.op("act", lambda e, b=b, pg=pg: e.activation(out=glb[b][:], in_=pg[:, 8:12],
                                                                    func=AF.Exp),
                         reads=[kpg], writes=[K("glb")])
                    S.op("dve", lambda e, G=G: e.tensor_tensor(out=G[:, 16:20], in0=G[:, 0:4],
                                                                in1=G[:, 8:12], op=ALU.mult),
                         reads=[K("beta"), K("eG")], writes=[K("beG")])
                    S.op("dve", lambda e, b=b, G=G: e.tensor_tensor(
                        out=gU[b][:, 0:4, :], in0=U64[:].unsqueeze(1).to_broadcast([64, 4, 64]),
                        in1=bc(G[:, 4:8], 64), op=ALU.mult), reads=["D_U", K("g")],
                        writes=[K("gU")])
                    S.op("pool", lambda e, b=b: e.tensor_scalar(out=gU[b][:, 4:8, :],
                                                                 in0=gU[b][:, 0:4, :], scalar1=-1.0,
                                                                 scalar2=None, op0=ALU.mult),
                         reads=[K("gU")], writes=[K("ngU")])
                    pd, kpd = self.ps8[pb(4)], ("ps", pb(4))

                    def mmd(e, b=b, pd=pd):
                        ins = None
                        for h in range(4):
                            e.matmul(pd[:64, h * 64:(h + 1) * 64], lhsT=gU[b][:, h, :],
                                     rhs=self.ones_f[:64, :64], start=True, stop=False)
                            ins = e.matmul(pd[:64, h * 64:(h + 1) * 64], lhsT=self.ones_f[:64, :64],
                                           rhs=gU[b][:, 4 + h, :], start=False, stop=True)
                        return ins
                    S.op("pe", mmd, reads=[K("gU"), K("ngU"), "ones_f"], writes=[kpd])
                    S.op("dve", lambda e, b=b, pd=pd: e.tensor_tensor(
                        out=dec[b][:], in0=v4(pd, 64),
                        in1=mb[:].unsqueeze(1).to_broadcast([64, 4, 64]), op=ALU.add),
                        reads=[kpd, "D_mb"], writes=[K("dec")])
                    S.op("act", lambda e, b=b: e.activation(out=dec[b][:], in_=dec[b][:],
                                                             func=AF.Exp),
                         reads=[K("dec")], writes=[K("dec")])
                    for grp in range(3):
                        pt, kpt = self.ps8[pb(grp)], ("ps", pb(grp))

                        def trq(e, grp=grp, pt=pt):
                            ins = None
                            for h in range(4):
                                ins = e.transpose(out=pt[:64, h * 128:(h + 1) * 128],
                                                  in_=Y[ys][:, grp * 4 + h, c0:c0 + 64],
                                                  identity=self.ident_f[:, :])
                            return ins
                        S.op("pe", trq, reads=ykeys + ["ident_f"], writes=[kpt])
                        dstv = Vt[b][:] if grp == 2 else QK[b][:, grp * 4:(grp + 1) * 4, :]
                        S.op("act", lambda e, dstv=dstv, pt=pt: e.copy(out=dstv, in_=v4(pt, 128)),
                             reads=[kpt], writes=[K("QK%d" % grp)])
                    S.op("pool", lambda e, b=b: e.tensor_tensor(out=sqq[b][:], in0=QK[b][:],
                                                                 in1=QK[b][:], op=ALU.mult),
                         reads=[K("QK0"), K("QK1")], writes=[K("sqq")])
                    S.op("dve", lambda e, b=b: e.tensor_reduce(out=nrm[b][:, 0:8], in_=sqq[b][:],
                                                                axis=AX.X, op=ALU.add),
                         reads=[K("sqq")], writes=[K("nrm")])
                    S.op("dve", lambda e, b=b: e.tensor_scalar(out=nrm[b][:, 0:8], in0=nrm[b][:, 0:8],
                                                                scalar1=1.0e-6, scalar2=None,
                                                                op0=ALU.add),
                         reads=[K("nrm")], writes=[K("nrm")])
                    S.op("act", lambda e, b=b: e.sqrt(out=nrm[b][:, 0:8], in_=nrm[b][:, 0:8]),
                         reads=[K("nrm")], writes=[K("nrm")])
                    S.op("dve", lambda e, b=b: e.reciprocal(out=nrm[b][:, 8:16], in_=nrm[b][:, 0:8]),
                         reads=[K("nrm")], writes=[K("rn")])
                    S.op("dve", lambda e, b=b: e.tensor_scalar(out=nrm[b][:, 8:12],
                                                                in0=nrm[b][:, 8:12],
                                                                scalar1=128.0 ** -0.5, scalar2=None,
                                                                op0=ALU.mult),
                         reads=[K("rn")], writes=[K("rn")])
                    S.op("dve", lambda e, b=b: e.tensor_tensor(
                        out=QK[b][:], in0=QK[b][:],
                        in1=nrm[b][:, 8:16].unsqueeze(2).to_broadcast([64, 8, 128]), op=ALU.mult),
                        reads=[K("QK0"), K("QK1"), K("rn")], writes=[K("QKn")])
                    for h in range(4):
                        for dst_, src_, col, kd_, kr_ in ((qdk[b], QK[b][:, h, :], 8, "qdk", "eG"),
                                                       (kd[b], QK[b][:, 4 + h, :], 12, "kd", "eG"),
                                                       (bk[b], QK[b][:, 4 + h, :], 16, "bk", "beG"),
                                                       (bv[b], Vt[b][:, h, :], 0, "bv", "beta")):
                            S.op("dve", lambda e, dst_=dst_, src_=src_, col=col, h=h, G=G:
                                 e.tensor_scalar(out=dst_[:, h, :], in0=src_,
                                                 scalar1=G[:, col + h:col + h + 1], scalar2=None,
                                                 op0=ALU.mult),
                                 reads=[K("QKn"), K("QK2"), K(kr_)], writes=[(K(kd_), h)])
                    for grp, (src, ksrc) in enumerate(((qdk[b][:], [*[(K("qdk"), h_) for h_ in range(4)]]),
                                                       (QK[b][:, 4:8, :], [K("QKn")]),
                                                       (QK[b][:, 0:4, :], [K("QKn")]))):
                        pt, kpt = self.ps8[pb(grp)], ("ps", pb(grp))

                        def trt(e, src=src, pt=pt):
                            ins = None
                            for h in range(4):
                                ins = e.transpose(out=pt[:, h * 64:(h + 1) * 64], in_=src[:, h, :],
                                                  identity=self.ident_f[:64, :64])
                            return ins
                        S.op("pe", trt, reads=ksrc + ["ident_f"], writes=[kpt])
                        self.evac(grp, TT[b][:, grp * 4:(grp + 1) * 4, :],
                                  pt[:, :256].rearrange("p (h t) -> p h t", h=4), [kpt],
                                  [K("TT%d" % grp)])
                    pk, kpk = self.ps8[pb(3)], ("ps", pb(3))

                    def mmk(e, b=b, pk=pk):
                        ins = None
                        for h in range(4):
                            e.matmul(pk[:64, h * 64:(h + 1) * 64], lhsT=TT[b][:, 4 + h, :],
                                     rhs=TT[b][:, 4 + h, :], start=True, stop=True)
                            ins = e.matmul(pk[:64, 256 + h * 64:256 + (h + 1) * 64],
                                           lhsT=TT[b][:, 8 + h, :], rhs=TT[b][:, 4 + h, :],
                                           start=True, stop=True)
                        return ins
                    S.op("pe", mmk, reads=[K("TT1"), K("TT2")], writes=[kpk])
                    S.op("dve", lambda e, b=b, pk=pk: e.tensor_tensor(
                        out=Mm[b][:], in0=v4(pk, 64), in1=dec[b][:], op=ALU.mult),
                        reads=[kpk, K("dec")], writes=[K("M")])
                    S.op("dve", lambda e, b=b, pk=pk: e.tensor_tensor(
                        out=QKm[b][:], in0=pk[:64, 256:512].rearrange("p (h x) -> p h x", h=4),
                        in1=dec[b][:], op=ALU.mult), reads=[kpk, K("dec")], writes=[K("QKm")])
                    S.op("pool", lambda e, b=b: e.tensor_tensor(
                        out=Mm[b][:], in0=Mm[b][:],
                        in1=Lst[:].unsqueeze(1).to_broadcast([64, 4, 64]), op=ALU.mult),
                        reads=[K("M"), "D_Lst"], writes=[K("M")])
                    S.op("dve", lambda e, b=b, G=G: e.tensor_tensor(
                        out=Mm[b][:], in0=Mm[b][:], in1=bc(G[:, 0:4], 64), op=ALU.mult),
                        reads=[K("M"), K("beta")], writes=[K("M")])
                    pn, kpn = self.ps8[pb(0)], ("ps", pb(0))

                    def trn(e, b=b, pn=pn):
                        ins = None
                        for h in range(4):
                            e.transpose(out=pn[:64, h * 64:(h + 1) * 64], in_=Mm[b][:, h, :],
                                        identity=self.ident_f[:64, :64])
                            ins = e.transpose(out=pn[:64, 256 + h * 64:256 + (h + 1) * 64],
                                              in_=QKm[b][:, h, :], identity=self.ident_f[:64, :64])
                        return ins
                    S.op("pe", trn, reads=[K("M"), K("QKm"), "ident_f"], writes=[kpn])
                    S.op("act", lambda e, b=b, pn=pn: e.copy(out=Qa[b][:], in_=v4(pn, 64)),
                         reads=[kpn], writes=[K("Qa")])
                    S.op("act", lambda e, b=b, pn=pn: e.copy(
                        out=QKT[b][:], in_=pn[:64, 256:512].rearrange("p (h x) -> p h x", h=4)),
                        reads=[kpn], writes=[K("QKT")])
                    S.op("dve", lambda e, b=b: e.tensor_tensor(out=Bt[b][:], in0=identI[:],
                                                                in1=Qa[b][:], op=ALU.subtract),
                         reads=[K("Qa"), "D_I"], writes=[K("Bt")])
                    Q, Qt, kQ, kQt = Qa[b], Mm[b], K("Qa"), K("M")
                    alt = [(Qb[b], Qtb[b], K("Qb"), K("Qtb")), (Qa[b], Qta[b], K("Qa"), K("Qta"))]
                    for step in range(5):
                        Q2, Qt2, kQ2, kQt2 = alt[step % 2]
                        p1, kp1 = self.ps8[pb(1)], ("ps", pb(1))

                        def mq(e, Q=Q, Qt=Qt, p1=p1, step=step):
                            ins = None
                            for h in range(4):
                                ins = e.matmul(p1[:64, h * 64:(h + 1) * 64], lhsT=Q[:, h, :],
                                               rhs=Qt[:, h, :], start=True, stop=True)
                                if step < 4:
                                    ins = e.matmul(p1[:64, 256 + h * 64:256 + (h + 1) * 64],
                                                   lhsT=Qt[:, h, :], rhs=Q[:, h, :], start=True,
                                                   stop=True)
                            return ins
                        S.op("pe", mq, reads=[kQ, kQt], writes=[kp1])
                        S.op("act", lambda e, Qt2=Qt2, p1=p1: e.copy(out=Qt2[:], in_=v4(p1, 64)),
                             reads=[kp1], writes=[kQt2])
                        if step < 4:
                            S.op("act", lambda e, Q2=Q2, p1=p1: e.copy(
                                out=Q2[:], in_=p1[:64, 256:512].rearrange("p (h x) -> p h x", h=4)),
                                reads=[kp1], writes=[kQ2])
                        p2, kp2 = self.ps8[pb(2)], ("ps", pb(2))

                        def mbm(e, Qt2=Qt2, b=b, p2=p2):
                            ins = None
                            for h in range(4):
                                ins = e.matmul(p2[:64, h * 64:(h + 1) * 64], lhsT=Qt2[:, h, :],
                                               rhs=Bt[b][:, h, :], start=True, stop=True)
                            return ins
                        S.op("pe", mbm, reads=[kQt2, K("Bt")], writes=[kp2])
                        S.op("dve", lambda e, b=b, p2=p2: e.tensor_tensor(out=Bt[b][:], in0=Bt[b][:],
                                                                           in1=v4(p2, 64),
                                                                           op=ALU.add),
                             reads=[kp2, K("Bt")], writes=[K("Bt")])
                        Q, Qt, kQ, kQt = Q2, Qt2, kQ2, kQt2
                    S.op("act", lambda e, b=b: e.copy(out=Tb[b][:], in_=Bt[b][:]), reads=[K("Bt")],
                         writes=[K("Tb")])
                    pu0, kpu0 = self.ps8[pb(3)], ("ps", pb(3))

                    def mu0(e, b=b, pu0=pu0):
                        ins = None
                        for h in range(4):
                            ins = e.matmul(pu0[:64, h * 128:(h + 1) * 128], lhsT=Tb[b][:, h, :],
                                           rhs=bv[b][:, h, :], start=True, stop=True)
                        return ins
                    S.op("pe", mu0, reads=[K("Tb"), *[(K("bv"), h_) for h_ in range(4)]], writes=[kpu0])
                    S.op("act", lambda e, b=b, pu0=pu0: e.copy(out=u0[b][:], in_=v4(pu0, 128)),
                         reads=[kpu0], writes=[K("u0")])
                    pw, kpw = self.ps8[pb(4)], ("ps", pb(4))

                    def mwk(e, b=b, pw=pw):
                        ins = None
                        for h in range(4):
                            ins = e.matmul(pw[:, h * 64:(h + 1) * 64], lhsT=bk[b][:, h, :],
                                           rhs=Tb[b][:, h, :], start=True, stop=True)
                        return ins
                    S.op("pe", mwk, reads=[K("Tb"), *[(K("bk"), h_) for h_ in range(4)]], writes=[kpw])
                    S.op("act", lambda e, b=b, pw=pw: e.copy(
                        out=wkT[b][:], in_=pw[:, :256].rearrange("p (h t) -> p h t", h=4)),
                        reads=[kpw], writes=[K("wkT")])
                    pu, kpu = self.ps8[pb(5)], ("ps", pb(5))

                    def mpu(e, b=b, pu=pu, s=s):
                        ins = None
                        for h in range(4):
                            ins = e.matmul(pu[:64, h * 128:(h + 1) * 128], lhsT=wkT[b][:, h, :],
                                           rhs=Sb[s][:, h, :], start=True, stop=True)
                        return ins
                    S.op("pe", mpu, reads=[K("wkT"), ("D_Sb", s)], writes=[kpu])
                    S.op("dve", lambda e, b=b, pu=pu: e.tensor_tensor(out=ub[b][:], in0=u0[b][:],
                                                                       in1=v4(pu, 128),
                                                                       op=ALU.subtract),
                         reads=[K("u0"), kpu], writes=[K("ub")])
                    po, kpo = self.ps8[pb(0)], ("ps", pb(0))

                    def mpo(e, b=b, po=po, s=s):
                        ins = None
                        for h in range(4):
                            e.matmul(po[:64, h * 128:(h + 1) * 128], lhsT=TT[b][:, h, :],
                                     rhs=Sb[s][:, h, :], start=True, stop=False)
                            ins = e.matmul(po[:64, h * 128:(h + 1) * 128], lhsT=QKT[b][:, h, :],
                                           rhs=ub[b][:, h, :], start=False, stop=True)
                        return ins
                    S.op("pe", mpo, reads=[K("TT0"), ("D_Sb", s), K("QKT"), K("ub")], writes=[kpo])
                    psn, kpsn = self.ps8[pb(1)], ("ps", pb(1))

                    def mps(e, b=b, psn=psn):
                        ins = None
                        for h in range(4):
                            ins = e.matmul(psn[:, h * 128:(h + 1) * 128], lhsT=kd[b][:, h, :],
                                           rhs=ub[b][:, h, :], start=True, stop=True)
                        return ins
                    S.op("pe", mps, reads=[*[(K("kd"), h_) for h_ in range(4)], K("ub")], writes=[kpsn])
                    for h in range(4):
                        S.op("dve", lambda e, b=b, s=s, h=h, psn=psn: e.scalar_tensor_tensor(
                            out=St[s][:, h, :], in0=St[s][:, h, :], scalar=glb[b][:, h:h + 1],
                            in1=psn[:, h * 128:(h + 1) * 128], op0=ALU.mult, op1=ALU.add),
                            reads=[("D_S", s), K("glb"), kpsn], writes=[("D_S", s)])
                    S.op("act", lambda e, s=s: e.copy(out=Sb[s][:], in_=St[s][:]),
                         reads=[("D_S", s)], writes=[("D_Sb", s)])
                    S.op("act", lambda e, b=b: e.activation(out=gs[b][:], in_=zt[b][:], func=AF.Silu),
                         reads=[K("zt")], writes=[K("gs")])
                    S.op("pool", lambda e, b=b: e.tensor_tensor(
                        out=gs[b][:], in0=gs[b][:],
                        in1=ng[:].unsqueeze(1).to_broadcast([64, 4, 128]), op=ALU.mult),
                        reads=[K("gs"), "D_ng"], writes=[K("gs")])
                    S.op("act", lambda e, b=b, po=po: e.copy(out=osb[b][:], in_=v4(po, 128)),
                         reads=[kpo], writes=[K("osb")])
                    S.op("pool", lambda e, b=b: e.tensor_tensor(out=osq[b][:], in0=osb[b][:],
                                                                 in1=osb[b][:], op=ALU.mult),
                         reads=[K("osb")], writes=[K("osq")])
                    S.op("dve", lambda e, b=b: e.tensor_reduce(out=nrm[b][:, 0:4], in_=osq[b][:],
                                                                axis=AX.X, op=ALU.add),
                         reads=[K("osq")], writes=[K("oss")])
                    self.rstd_from_ss(nrm[b][:, 0:4], K("oss"), nrm[b][:, 4:8], K("ors"), 128)
                    S.op("dve", lambda e, b=b: e.tensor_tensor(out=osb[b][:], in0=osb[b][:],
                                                                in1=bc(nrm[b][:, 4:8], 128),
                                                                op=ALU.mult),
                         reads=[K("osb"), K("ors")], writes=[K("osb")])
                    S.op("pool", lambda e, b=b: e.tensor_tensor(out=osq[b][:], in0=osb[b][:],
                                                                 in1=gs[b][:], op=ALU.mult),
                         reads=[K("osb"), K("gs")], writes=[K("osq")])
                    S.dma(out=self.mixin[tok:tok + 64, 0:512],
                          in_=osq[b][:].rearrange("p h v -> p (h v)"), sem="D_out%d" % b,
                          reads=[K("osq")])
        S.interleave([lambda s=s: seq_thread(s) for s in range(NSEQ)])
        S.barrier()


def build_program():
    P = Prog()
    src = P.x_in
    for l in range(DEPTH):
        dst = P.xmid if l < DEPTH - 1 else P.y
        P.phase_A(l, src)
        P.phase_D(l)
        P.phase_S(l)
        P.phase_H(l)
        P.phase_E(l, src)
        P.phase_F(l, dst)
        src = dst
    return P


def kernel(**inputs):
    n = 8
    P = build_program()
    x = np.ascontiguousarray(inputs["x"], dtype=np.float32)
    shared = {k: np.ascontiguousarray(v, dtype=np.float32) for k, v in inputs.items() if k != "x"}
    in_maps = []
    for c in range(n):
        m = dict(shared)
        m["x"] = np.ascontiguousarray(x[c * NSEQ:(c + 1) * NSEQ].reshape(NTOK, D))
        in_maps.append(m)
    res = run_bass_kernel_spmd(P.nc, in_maps, core_ids=list(range(n)))
    out = np.stack([np.asarray(r["y"]).reshape(NSEQ, SEQ, D) for r in res.results], axis=0)
    return out.reshape(n * NSEQ, SEQ, D).astype(np.float32)
```

```python
import numpy as np
import concourse.bass as bass
import concourse.mybir as mybir
from concourse.bass_utils import run_bass_kernel_spmd

F32 = mybir.dt.float32
BF16 = mybir.dt.bfloat16
AF = mybir.ActivationFunctionType
ALU = mybir.AluOpType
AX = mybir.AxisListType

D = 1024
SEQ = 4096
NSEQ = 2
NTOK = NSEQ * SEQ
DEPTH = 2
D_IN = 3788
D_FF = 2816
EPS = 1e-6
NEG = -30000.0

TM_GROUPS = [(1536, 2056), (2376, 2440), (2760, 2764), (3020, 3788)]
TM_W = sum(b - a for a, b in TM_GROUPS)
TM_AZ, TM_AB, TM_AA = 0, 512, 516
TM_BV = 520
TM_WI = 584
TM_CF, TM_CI, TM_CG = 588, 844, 1100
FM_GROUPS = [(i * 128, 128) for i in range(12)] + [(2056, 128), (2184, 128), (2312, 64),
             (2440, 128), (2568, 128), (2696, 64), (2764, 128), (2892, 128), (3020, 128), (3148, 128)]
FM_ROW = {}
_r = 0
for _c, _n in FM_GROUPS:
    FM_ROW[_c] = _r
    _r += _n
FM_H = _r


class Sched:
    def __init__(self, nc):
        self.nc = nc
        self.eng = {"pe": nc.tensor, "act": nc.scalar, "dve": nc.vector, "pool": nc.gpsimd,
                    "sp": nc.sync}
        self.sems = {}
        self.cnt = {}
        for k in ("pe", "act", "dve", "pool"):
            self.sems[k] = nc.alloc_semaphore("s_" + k)
            self.cnt[k] = 0
        self.seen = {k: {} for k in self.eng}
        self.bufs = {}
        self.ninstr = 0

    def _buf(self, key):
        b = self.bufs.get(key)
        if b is None:
            b = {"w": None, "r": {}}
            self.bufs[key] = b
        return b

    def _deps(self, engine, reads, writes):
        deps = {}

        def add(ev, same_ok):
            if ev is None:
                return
            sk, val = ev
            if sk == engine and not same_ok:
                return
            if deps.get(sk, 0) < val:
                deps[sk] = val

        for k in reads:
            b = self._buf(k)
            add(b["w"], engine != "pe")
            if isinstance(k, tuple) and k[0] in ("ps", "psb"):
                for sk, val in b["r"].items():
                    add((sk, val), False)
        for k in writes:
            b = self._buf(k)
            add(b["w"], engine != "pe")
            for sk, val in b["r"].items():
                add((sk, val), False)
        return deps

    def _emit_waits(self, engine, deps):
        e = self.eng[engine]
        seen = self.seen[engine]
        for sk, val in deps.items():
            if seen.get(sk, 0) >= val:
                continue
            e.wait_ge(self.sems[sk], val)
            self.ninstr += 1
            seen[sk] = val

    def _record(self, ev, reads, writes):
        for k in writes:
            b = self._buf(k)
            b["w"] = ev
            b["r"] = {}
        for k in reads:
            b = self._buf(k)
            if b["r"].get(ev[0], 0) < ev[1]:
                b["r"][ev[0]] = ev[1]

    def op(self, engine, fn, reads=(), writes=()):
        deps = self._deps(engine, reads, writes)
        self._emit_waits(engine, deps)
        ins = fn(self.eng[engine])
        self.cnt[engine] += 1
        ins.then_inc(self.sems[engine], 1)
        self.ninstr += 1
        self._record((engine, self.cnt[engine]), reads, writes)
        self._yield()

    def dma(self, out, in_, sem, reads=(), writes=(), q="sp"):
        if sem not in self.sems:
            self.sems[sem] = self.nc.alloc_semaphore("d_" + sem)
            self.cnt[sem] = 0
        deps = self._deps(q, reads, writes)
        self._emit_waits(q, deps)
        ins = self.eng[q].dma_start(out=out, in_=in_)
        self.cnt[sem] += 16
        ins.then_inc(self.sems[sem], 16)
        self.ninstr += 1
        self._record((sem, self.cnt[sem]), reads, writes)
        self._yield()

    def interleave(self, fns):
        import threading
        n = len(fns)
        st = {"cur": 0, "alive": [True] * n, "err": None}
        cond = threading.Condition()

        def nxt(i):
            for d in range(1, n + 1):
                k = (i + d) % n
                if st["alive"][k]:
                    return k
            return -1

        def pass_turn(me):
            with cond:
                st["cur"] = nxt(me)
                cond.notify_all()
                while st["alive"][me] and st["cur"] != me and st["err"] is None:
                    cond.wait()
                if st["err"] is not None and st["alive"][me]:
                    raise RuntimeError("interleave aborted")

        def worker(i):
            with cond:
                while st["cur"] != i and st["err"] is None:
                    cond.wait()
            try:
                if st["err"] is None:
                    self._tl.me = i
                    fns[i]()
            except BaseException as e:
                if st["err"] is None:
                    st["err"] = e
            finally:
                with cond:
                    st["alive"][i] = False
                    if st["cur"] == i:
                        st["cur"] = nxt(i)
                    cond.notify_all()

        self._tl = threading.local()
        self._pass = pass_turn
        ths = [threading.Thread(target=worker, args=(i,)) for i in range(n)]
        for t in ths:
            t.start()
        for t in ths:
            t.join()
        self._pass = None
        if st["err"] is not None:
            raise st["err"]

    def _yield(self):
        p = getattr(self, "_pass", None)
        if p is not None:
            p(self._tl.me)

    def barrier(self):
        allv = {k: v for k, v in self.cnt.items() if v > 0}
        for engine in self.eng:
            self._emit_waits(engine, dict(allv))
        self.bufs = {}


def bcast_rows(ap2d_row, nparts):
    return ap2d_row.partition_broadcast(nparts)


class Prog:
    def __init__(self, layers=(0, 1), phases="AHDSEF", dbg=()):
        self.nc = nc = bass.Bass("TRN2", target_bir_lowering=False)
        self.S = Sched(nc)
        self.dbg = dbg
        dt = nc.dram_tensor
        self.x_in = dt("x", [NTOK, D], F32, kind="ExternalInput").ap()
        self.w_in = dt("w_in", [DEPTH, D, D_IN], F32, kind="ExternalInput").ap()
        self.dn_conv = dt("dn_conv", [DEPTH, 4, 1536], F32, kind="ExternalInput").ap()
        self.dn_a_log = dt("dn_a_log", [DEPTH, 4], F32, kind="ExternalInput").ap()
        self.dn_dt_bias = dt("dn_dt_bias", [DEPTH, 4], F32, kind="ExternalInput").ap()
        self.dn_norm = dt("dn_norm", [DEPTH, 128], F32, kind="ExternalInput").ap()
        self.hg_lb = dt("hg_lb", [DEPTH, 256], F32, kind="ExternalInput").ap()
        self.hg_norm = dt("hg_norm", [DEPTH, 64], F32, kind="ExternalInput").ap()
        self.w_out = dt("w_out", [DEPTH, D, D], F32, kind="ExternalInput").ap()
        self.g_mix_pre = dt("g_mix_pre", [DEPTH, D], F32, kind="ExternalInput").ap()
        self.g_mix_post = dt("g_mix_post", [DEPTH, D], F32, kind="ExternalInput").ap()
        self.g_ffn_pre = dt("g_ffn_pre", [DEPTH, D], F32, kind="ExternalInput").ap()
        self.g_ffn_post = dt("g_ffn_post", [DEPTH, D], F32, kind="ExternalInput").ap()
        self.w_up = dt("ffn_w_up", [DEPTH, D, 2 * D_FF], F32, kind="ExternalInput").ap()
        self.ffn_conv = dt("ffn_conv", [DEPTH, 3, 2 * D_FF], F32, kind="ExternalInput").ap()
        self.w_down = dt("ffn_w_down", [DEPTH, D_FF, D], F32, kind="ExternalInput").ap()
        self.y = dt("y", [NTOK, D], F32, kind="ExternalOutput").ap()

        def scratch(name, shape, dtype):
            kind = "ExternalOutput" if name in dbg else "Internal"
            return dt(name, shape, dtype, kind=kind).ap()

        self.ptok = scratch("ptok", [NTOK, TM_W], F32)
        self.pfeat = scratch("pfeat", [FM_H, NTOK], F32)
        self.mixin = scratch("mixin", [NTOK, D], F32)
        self.x1 = scratch("x1", [NTOK, D], F32)
        self.h2T = scratch("h2T", [D, NTOK], BF16)
        self.xmid = scratch("xmid", [NTOK, D], F32)

        self.ps = [nc.alloc_psum_tensor("ps%d" % i, [128, 512], F32) for i in range(6)]
        self.psb = [nc.alloc_psum_tensor("psb%d" % i, [128, 1024], BF16) for i in range(2)]
        self.ps8 = [p[:, :] for p in self.ps] + [p[:, :].bitcast(F32) for p in self.psb]
        self.ident_b = nc.alloc_sbuf_tensor("ident_b", [128, 128], BF16)
        self.ident_f = nc.alloc_sbuf_tensor("ident_f", [128, 128], F32)
        self.ones_f = nc.alloc_sbuf_tensor("ones_f", [128, 128], F32)
        self._consts()
        self.sb_base = nc.sbuf_base
        self.layers = layers
        self.phases = phases

    def _consts(self):
        nc, S = self.nc, self.S
        S.op("pool", lambda e: e.memset(self.ones_f[:], 1.0), writes=["ones_f"])
        S.op("pool", lambda e: e.memset(self.ident_f[:], 0.0), writes=["ident_f"])
        S.op("pool", lambda e: e.affine_select(out=self.ident_f[:], in_=self.ident_f[:],
                                                pattern=[[-1, 128]], compare_op=ALU.not_equal,
                                                fill=1.0, base=0, channel_multiplier=1),
             reads=["ident_f"], writes=["ident_f"])
        S.op("dve", lambda e: e.tensor_copy(out=self.ident_b[:], in_=self.ident_f[:]),
             reads=["ident_f"], writes=["ident_b"])

    def sbuf_reset(self):
        self.nc.sbuf_base = self.sb_base

    def sb(self, name, shape, dtype):
        self._uid = getattr(self, "_uid", 0) + 1
        return self.nc.alloc_sbuf_tensor("%s_u%d" % (name, self._uid), shape, dtype)

    def load_bcast(self, name, row_ap, n):
        t = self.sb(name, [128, n], F32)
        self.S.dma(out=t[:], in_=bcast_rows(row_ap, 128), sem="ld_" + name, writes=[name])
        return t

    def load_weight_bf16(self, name, w_ap, K, N, stage, stage_keys):
        S = self.S
        wt = self.sb(name, [128, K, N], BF16)
        CH = stage[0].shape[1]
        i = 0
        for k in range(K):
            for c0 in range(0, N, CH):
                cw = min(CH, N - c0)
                st, sk = stage[i % 2], stage_keys[i % 2]
                S.dma(out=st[:, :cw], in_=w_ap[k * 128:(k + 1) * 128, c0:c0 + cw], sem=sk,
                      writes=[sk])
                eng = ("dve", "pool", "act")[i % 3]
                if eng == "act":
                    S.op("act", lambda e, st=st, k=k, c0=c0, cw=cw: e.copy(
                        out=wt[:, k, c0:c0 + cw], in_=st[:, :cw]), reads=[sk], writes=[name])
                else:
                    S.op(eng, lambda e, st=st, k=k, c0=c0, cw=cw: e.tensor_copy(
                        out=wt[:, k, c0:c0 + cw], in_=st[:, :cw]), reads=[sk], writes=[name])
                i += 1
        return wt

    def evac(self, i, out, in_, reads, writes):
        if i % 2 == 0:
            self.S.op("act", lambda e: e.copy(out=out, in_=in_), reads=reads, writes=writes)
        else:
            self.S.op("dve", lambda e: e.tensor_copy(out=out, in_=in_), reads=reads, writes=writes)

    def norm_transpose(self, src_dram, tok0, gbc, gkey, T, tag, xt, hb, hT, ss, rs, slot,
                       do_norm=True):
        S = self.S
        kx = lambda j: (tag + "xt", slot, j)
        for j in range(4):
            S.dma(out=xt[slot][:, j, :], in_=src_dram[tok0 + j * 128: tok0 + (j + 1) * 128, :],
                  sem="%sxt%d_%d" % (tag, slot, j), writes=[kx(j)])
        kss, krs = (tag + "ss", slot), (tag + "rs", slot)
        if do_norm:
            for j in range(4):
                S.op("act", lambda e, j=j: e.activation(out=T["junk"][:], in_=xt[slot][:, j, :],
                                                         func=AF.Square,
                                                         accum_out=ss[slot][:, j:j + 1]),
                     reads=[kx(j)], writes=[kss])
            S.op("dve", lambda e: e.tensor_scalar(out=rs[slot][:], in0=ss[slot][:],
                                                   scalar1=1.0 / D, scalar2=EPS, op0=ALU.mult,
                                                   op1=ALU.add), reads=[kss], writes=[krs])
            S.op("act", lambda e: e.sqrt(out=rs[slot][:], in_=rs[slot][:]), reads=[krs],
                 writes=[krs])
            S.op("dve", lambda e: e.reciprocal(out=rs[slot][:], in_=rs[slot][:]), reads=[krs],
                 writes=[krs])
        for j in range(4):
            khb = (tag + "hb", j % 2)
            hbj = hb[j % 2]
            if do_norm:
                S.op("dve", lambda e, j=j, hbj=hbj: e.scalar_tensor_tensor(
                    out=hbj[:], in0=xt[slot][:, j, :], scalar=rs[slot][:, j:j + 1], in1=gbc[:],
                    op0=ALU.mult, op1=ALU.mult), reads=[kx(j), krs, gkey], writes=[khb])
            else:
                S.op("pool", lambda e, j=j, hbj=hbj: e.tensor_copy(out=hbj[:],
                                                                  in_=xt[slot][:, j, :]),
                     reads=[kx(j)], writes=[khb])
            pb = self.psb[j % 2]
            kpb = ("psb", j % 2)

            def tr(e, hbj=hbj, pb=pb):
                ins = None
                for k in range(8):
                    ins = e.transpose(out=pb[:, k * 128:(k + 1) * 128],
                                      in_=hbj[:, k * 128:(k + 1) * 128], identity=self.ident_b[:])
                return ins
            S.op("pe", tr, reads=[khb, "ident_b"], writes=[kpb])
            self.evac(j, hT[slot][:, :, j * 128:(j + 1) * 128],
                      pb[:].rearrange("p (k t) -> p k t", k=8), [kpb], [(tag + "hT", slot, j)])

    def phase_A(self, l, xsrc):
        nc, S = self.nc, self.S
        self.sbuf_reset()
        stage = [self.sb("A_stage%d" % i, [128, 3788], F32) for i in range(2)]
        Wi = self.load_weight_bf16("A_Wi", self.w_in[l], 8, D_IN, stage, ["A_stg0", "A_stg1"])
        gbc = self.load_bcast("A_gbc", self.g_mix_pre[l:l + 1, :], D)
        S.barrier()
        xt = [self.sb("A_xt%d" % i, [128, 4, D], F32) for i in range(2)]
        hb = [self.sb("A_hb%d" % i, [128, D], BF16) for i in range(2)]
        hT = [self.sb("A_hT%d" % i, [128, 8, 512], BF16) for i in range(2)]
        ss = [self.sb("A_ss%d" % i, [128, 4], F32) for i in range(2)]
        rs = [self.sb("A_rs%d" % i, [128, 4], F32) for i in range(2)]
        T = {"junk": self.sb("A_junk", [128, D], F32)}
        ofm = [self.sb("A_ofm%d" % i, [128, 512], F32) for i in range(4)]
        otm = [self.sb("A_otm%d" % i, [128, TM_W], F32) for i in range(2)]
        tmch = []
        off = 0
        for a, b in TM_GROUPS:
            c = a
            while c < b:
                w = min(512, b - c)
                tmch.append((c, w, off))
                off += w
                c += w
        nev = 0
        for blk in range(NTOK // 512):
            slot = blk % 2
            tok0 = blk * 512
            self.norm_transpose(xsrc, tok0, gbc, "A_gbc", T, "A_", xt, hb, hT, ss, rs, slot)
            hkeys = [("A_hT", slot, j) for j in range(4)]
            for gi, (c0, n) in enumerate(FM_GROUPS):
                ps = self.ps[gi % 4]
                kps = ("ps", gi % 4)

                def mm(e, ps=ps, c0=c0, n=n):
                    ins = None
                    for k in range(8):
                        ins = e.matmul(ps[:n, :], lhsT=Wi[:, k, c0:c0 + n], rhs=hT[slot][:, k, :],
                                       start=(k == 0), stop=(k == 7))
                    return ins
                S.op("pe", mm, reads=["A_Wi"] + hkeys, writes=[kps])
                o = ofm[gi % 4]
                ko = ("A_ofm", gi % 4)
                self.evac(nev, o[:n, :], ps[:n, :], [kps], [ko])
                nev += 1
                r0 = FM_ROW[c0]
                S.dma(out=self.pfeat[r0:r0 + n, tok0:tok0 + 512], in_=o[:n, :],
                      sem="A_ofm%d" % (gi % 4), reads=[ko])
            for j in range(4):
                o = otm[j % 2]
                ko = ("A_otm", j % 2)
                for ci, (c, w, dst) in enumerate(tmch):
                    ps = self.ps[4 + ci % 2]
                    kps = ("ps", 4 + ci % 2)

                    def mm(e, ps=ps, c=c, w=w, j=j):
                        ins = None
                        for k in range(8):
                            ins = e.matmul(ps[:, :w], lhsT=hT[slot][:, k, j * 128:(j + 1) * 128],
                                           rhs=Wi[:, k, c:c + w], start=(k == 0), stop=(k == 7))
                        return ins
                    S.op("pe", mm, reads=["A_Wi", hkeys[j]], writes=[kps])
                    self.evac(nev, o[:, dst:dst + w], ps[:, :w], [kps], [(ko, ci)])
                    nev += 1
                S.dma(out=self.ptok[tok0 + j * 128: tok0 + (j + 1) * 128, :], in_=o[:, :],
                      sem="A_otm%d" % (j % 2), reads=[(ko, ci) for ci in range(len(tmch))])
        S.barrier()

    def transp8(self, hbj, khb, dst, kdst, j):
        pb = self.psb[j % 2]
        kpb = ("psb", j % 2)

        def tr(e):
            ins = None
            for k in range(8):
                ins = e.transpose(out=pb[:, k * 128:(k + 1) * 128],
                                  in_=hbj[:, k * 128:(k + 1) * 128], identity=self.ident_b[:])
            return ins
        self.S.op("pe", tr, reads=[khb, "ident_b"], writes=[kpb])
        self.evac(j, dst, pb[:].rearrange("p (k t) -> p k t", k=8), [kpb], [kdst])

    def rstd_from_ss(self, ss, kss, rs, krs, n):
        S = self.S
        S.op("dve", lambda e: e.tensor_scalar(out=rs, in0=ss, scalar1=1.0 / n, scalar2=EPS,
                                               op0=ALU.mult, op1=ALU.add), reads=[kss],
             writes=[krs])
        S.op("act", lambda e: e.sqrt(out=rs, in_=rs), reads=[krs], writes=[krs])
        S.op("dve", lambda e: e.reciprocal(out=rs, in_=rs), reads=[krs], writes=[krs])

    def phase_E(self, l, xsrc):
        S = self.S
        self.sbuf_reset()
        stage = [self.sb("E_stage%d" % i, [128, 1024], F32) for i in range(2)]
        Wo = self.load_weight_bf16("E_Wo", self.w_out[l], 8, D, stage, ["E_stg0", "E_stg1"])
        gpost = self.load_bcast("E_gpost", self.g_mix_post[l:l + 1, :], D)
        gpre = self.load_bcast("E_gpre", self.g_ffn_pre[l:l + 1, :], D)
        S.barrier()
        xt = [self.sb("E_xt%d" % i, [128, 4, D], F32) for i in range(2)]
        xr = [self.sb("E_xr%d" % i, [128, 4, D], F32) for i in range(2)]
        hb = [self.sb("E_hb%d" % i, [128, D], BF16) for i in range(2)]
        mT = [self.sb("E_mT%d" % i, [128, 8, 512], BF16) for i in range(2)]
        h2s = [self.sb("E_h2s%d" % i, [128, 8, 512], BF16) for i in range(2)]
        yt = [self.sb("E_yt%d" % i, [128, D], F32) for i in range(2)]
        x1t = [self.sb("E_x1t%d" % i, [128, D], F32) for i in range(2)]
        h2b = [self.sb("E_h2b%d" % i, [128, D], BF16) for i in range(2)]
        junk = self.sb("E_junk", [128, D], F32)
        st = [self.sb("E_st%d" % i, [128, 8], F32) for i in range(2)]
        h2T_v = self.h2T.rearrange("(k p) t -> p k t", p=128)
        for blk in range(NTOK // 512):
            slot = blk % 2
            tok0 = blk * 512
            self.norm_transpose(self.mixin, tok0, None, None, None, "E_", xt, hb, mT, None, None,
                                slot, do_norm=False)
            for j in range(4):
                S.dma(out=xr[slot][:, j, :], in_=xsrc[tok0 + j * 128: tok0 + (j + 1) * 128, :],
                      sem="E_xr%d_%d" % (slot, j), writes=[("E_xr", slot, j)])
            for j in range(4):
                p2 = j % 2
                kss, krs = ("E_ss", p2), ("E_rs", p2)
                for hf in range(2):
                    ps = self.ps[2 * p2 + hf]
                    kps = ("ps", 2 * p2 + hf)

                    def mm(e, ps=ps, hf=hf, j=j):
                        ins = None
                        for k in range(8):
                            ins = e.matmul(ps[:, :], lhsT=mT[slot][:, k, j * 128:(j + 1) * 128],
                                           rhs=Wo[:, k, hf * 512:(hf + 1) * 512], start=(k == 0),
                                           stop=(k == 7))
                        return ins
                    S.op("pe", mm, reads=["E_Wo", ("E_hT", slot, j)], writes=[kps])
                    S.op("act", lambda e, ps=ps, hf=hf, p2=p2: e.activation(
                        out=junk[:, :512], in_=ps[:, :], func=AF.Square,
                        accum_out=st[p2][:, hf:hf + 1]), reads=[kps], writes=[(kss, hf)])
                S.op("dve", lambda e, p2=p2: e.tensor_tensor(out=st[p2][:, 2:3], in0=st[p2][:, 0:1],
                                                              in1=st[p2][:, 1:2], op=ALU.add),
                     reads=[(kss, 0), (kss, 1)], writes=[kss])
                self.rstd_from_ss(st[p2][:, 2:3], kss, st[p2][:, 3:4], krs, D)
                kyt = ("E_yt", p2)
                for hf in range(2):
                    ps = self.ps[2 * p2 + hf]
                    kps = ("ps", 2 * p2 + hf)
                    S.op("act", lambda e, ps=ps, hf=hf, p2=p2: e.activation(
                        out=yt[p2][:, hf * 512:(hf + 1) * 512], in_=ps[:, :], func=AF.Copy,
                        scale=st[p2][:, 3:4]), reads=[kps, krs], writes=[(kyt, hf)])
                S.op("dve", lambda e, p2=p2: e.tensor_tensor(out=yt[p2][:], in0=yt[p2][:],
                                                              in1=gpost[:], op=ALU.mult),
                     reads=[(kyt, 0), (kyt, 1), "E_gpost"], writes=[kyt])
                kx1 = ("E_x1t", p2)
                S.op("pool", lambda e, p2=p2, j=j: e.tensor_tensor(out=x1t[p2][:], in0=yt[p2][:],
                                                                    in1=xr[slot][:, j, :],
                                                                    op=ALU.add),
                     reads=[kyt, ("E_xr", slot, j)], writes=[kx1])
                S.dma(out=self.x1[tok0 + j * 128: tok0 + (j + 1) * 128, :], in_=x1t[p2][:],
                      sem="E_x1t%d" % p2, reads=[kx1])
                kss2, krs2 = ("E_ss2", p2), ("E_rs2", p2)
                S.op("act", lambda e, p2=p2: e.activation(out=junk[:], in_=x1t[p2][:],
                                                           func=AF.Square,
                                                           accum_out=st[p2][:, 4:5]),
                     reads=[kx1], writes=[kss2])
                self.rstd_from_ss(st[p2][:, 4:5], kss2, st[p2][:, 5:6], krs2, D)
                kh2b = ("E_h2b", p2)
                S.op("dve", lambda e, p2=p2: e.scalar_tensor_tensor(
                    out=h2b[p2][:], in0=x1t[p2][:], scalar=st[p2][:, 5:6], in1=gpre[:],
                    op0=ALU.mult, op1=ALU.mult), reads=[kx1, krs2, "E_gpre"], writes=[kh2b])
                self.transp8(h2b[p2], kh2b, h2s[slot][:, :, j * 128:(j + 1) * 128],
                             ("E_h2s", slot, j), j)
            S.dma(out=h2T_v[:, :, tok0:tok0 + 512], in_=h2s[slot][:],
                  sem="E_h2s%d" % slot, reads=[("E_h2s", slot, j) for j in range(4)])
        S.barrier()

    def load_convw(self, name, conv_ap, ntaps, ntiles):
        S = self.S
        raw = self.sb(name + "_raw", [ntiles, ntaps, 128], F32)
        cw = self.sb(name, [128, ntaps, ntiles], F32)
        S.dma(out=raw[:], in_=conv_ap.rearrange("j (t p) -> t j p", p=128), sem="ld_" + name,
              writes=[name + "_raw"])
        for j in range(ntaps):
            ps = self.ps[j % 2]
            kps = ("ps", j % 2)
            S.op("pe", lambda e, j=j, ps=ps: e.transpose(out=ps[:, :ntiles], in_=raw[:, j, :],
                                                         identity=self.ident_f[:ntiles, :ntiles]),
                 reads=[name + "_raw", "ident_f"], writes=[kps])
            S.op("dve", lambda e, j=j, ps=ps: e.tensor_copy(out=cw[:, j, :], in_=ps[:, :ntiles]),
                 reads=[kps], writes=[name])
        return cw

    def phase_F(self, l, dst):
        S = self.S
        self.sbuf_reset()
        NT = 22
        stage = [self.sb("F_stage%d" % i, [128, 512], F32) for i in range(2)]
        Wu = self.load_weight_bf16("F_Wu", self.w_up[l], 8, 2 * D_FF, stage, ["F_stg0", "F_stg1"])
        Wd = self.load_weight_bf16("F_Wd", self.w_down[l], NT, D, stage, ["F_stg0", "F_stg1"])
        gpost = self.load_bcast("F_gpost", self.g_ffn_post[l:l + 1, :], D)
        cw = self.load_convw("F_cw", self.ffn_conv[l], 3, 2 * NT)
        S.barrier()
        hT = self.sb("F_hT", [128, 8, 512], BF16)
        gT = self.sb("F_gT", [128, NT, 512], BF16)
        U = [self.sb("F_U%d" % i, [128, 514], F32) for i in range(2)]
        C = [self.sb("F_C%d" % i, [128, 512], F32) for i in range(2)]
        GL = self.sb("F_GL", [128, 512], F32)
        halo = self.sb("F_halo", [128, 2 * NT, 2], F32)
        x1t = [self.sb("F_x1t%d" % i, [128, D], F32) for i in range(2)]
        yt = [self.sb("F_yt%d" % i, [128, D], F32) for i in range(2)]
        junk = self.sb("F_junk", [128, 512], F32)
        st = [self.sb("F_st%d" % i, [128, 8], F32) for i in range(2)]
        h2T_v = self.h2T.rearrange("(k p) t -> p k t", p=128)
        for blk in range(NTOK // 512):
            tok0 = blk * 512
            if blk % (SEQ // 512) == 0:
                S.op("pool", lambda e: e.memset(halo[:], 0.0), writes=["F_halo"])
            S.dma(out=hT[:], in_=h2T_v[:, :, tok0:tok0 + 512], sem="F_hT", writes=["F_hT"])
            for i in range(NT):
                for gv in range(2):
                    ti = gv * NT + i
                    c0 = ti * 128
                    ps = self.ps[gv * 2 + i % 2]
                    kps = ("ps", gv * 2 + i % 2)

                    def mm(e, ps=ps, c0=c0):
                        ins = None
                        for k in range(8):
                            ins = e.matmul(ps[:, :], lhsT=Wu[:, k, c0:c0 + 128], rhs=hT[:, k, :],
                                           start=(k == 0), stop=(k == 7))
                        return ins
                    S.op("pe", mm, reads=["F_Wu", "F_hT"], writes=[kps])
                    u, ku = U[gv], ("F_U", gv)
                    c, kc = C[gv], ("F_C", gv)
                    S.op("act", lambda e, u=u, ps=ps: e.copy(out=u[:, 2:514], in_=ps[:, :]),
                         reads=[kps], writes=[(ku, "b")])
                    S.op("pool", lambda e, u=u, ti=ti: e.tensor_copy(out=u[:, 0:2],
                                                                     in_=halo[:, ti, :]),
                         reads=["F_halo"], writes=[(ku, "h")])
                    S.op("act", lambda e, u=u, c=c, ti=ti: e.activation(
                        out=c[:], in_=u[:, 0:512], func=AF.Copy, scale=cw[:, 0, ti:ti + 1]),
                        reads=[(ku, "b"), (ku, "h"), "F_cw"], writes=[kc])
                    for tap in (1, 2):
                        S.op("dve", lambda e, u=u, c=c, ti=ti, tap=tap: e.scalar_tensor_tensor(
                            out=c[:], in0=u[:, tap:tap + 512], scalar=cw[:, tap, ti:ti + 1],
                            in1=c[:], op0=ALU.mult, op1=ALU.add),
                            reads=[(ku, "b"), (ku, "h"), "F_cw", kc], writes=[kc])
                    S.op("pool", lambda e, u=u, ti=ti: e.tensor_copy(out=halo[:, ti, :],
                                                                     in_=u[:, 512:514]),
                         reads=[(ku, "b")], writes=["F_halo"])
                S.op("act", lambda e: e.activation(out=GL[:], in_=C[0][:],
                                                    func=AF.Gelu_apprx_tanh),
                     reads=[("F_C", 0)], writes=["F_GL"])
                S.op("pool", lambda e, i=i: e.tensor_tensor(out=gT[:, i, :], in0=GL[:],
                                                             in1=C[1][:], op=ALU.mult),
                     reads=["F_GL", ("F_C", 1)], writes=[("F_gT", i)])
            gkeys = [("F_gT", i) for i in range(NT)]
            for j in range(4):
                p2 = j % 2
                S.dma(out=x1t[p2][:], in_=self.x1[tok0 + j * 128: tok0 + (j + 1) * 128, :],
                      sem="F_x1t%d" % p2, writes=[("F_x1t", p2)])
                kss, krs = ("F_ss", p2), ("F_rs", p2)
                for hf in range(2):
                    ps = self.ps[4 + hf]
                    kps = ("ps", 4 + hf)

                    def mm(e, ps=ps, hf=hf, j=j):
                        ins = None
                        for k in range(NT):
                            ins = e.matmul(ps[:, :], lhsT=gT[:, k, j * 128:(j + 1) * 128],
                                           rhs=Wd[:, k, hf * 512:(hf + 1) * 512], start=(k == 0),
                                           stop=(k == NT - 1))
                        return ins
                    S.op("pe", mm, reads=["F_Wd"] + gkeys, writes=[kps])
                    S.op("act", lambda e, ps=ps, hf=hf, p2=p2: e.activation(
                        out=junk[:, :], in_=ps[:, :], func=AF.Square,
                        accum_out=st[p2][:, hf:hf + 1]), reads=[kps], writes=[(kss, hf)])
                S.op("dve", lambda e, p2=p2: e.tensor_tensor(out=st[p2][:, 2:3], in0=st[p2][:, 0:1],
                                                              in1=st[p2][:, 1:2], op=ALU.add),
                     reads=[(kss, 0), (kss, 1)], writes=[kss])
                self.rstd_from_ss(st[p2][:, 2:3], kss, st[p2][:, 3:4], krs, D)
                kyt = ("F_yt", p2)
                for hf in range(2):
                    ps = self.ps[4 + hf]
                    kps = ("ps", 4 + hf)
                    S.op("act", lambda e, ps=ps, hf=hf, p2=p2: e.activation(
                        out=yt[p2][:, hf * 512:(hf + 1) * 512], in_=ps[:, :], func=AF.Copy,
                        scale=st[p2][:, 3:4]), reads=[kps, krs], writes=[(kyt, hf)])
                S.op("dve", lambda e, p2=p2: e.tensor_tensor(out=yt[p2][:], in0=yt[p2][:],
                                                              in1=gpost[:], op=ALU.mult),
                     reads=[(kyt, 0), (kyt, 1), "F_gpost"], writes=[kyt])
                S.op("pool", lambda e, p2=p2: e.tensor_tensor(out=yt[p2][:], in0=yt[p2][:],
                                                               in1=x1t[p2][:], op=ALU.add),
                     reads=[kyt, ("F_x1t", p2)], writes=[kyt])
                S.dma(out=dst[tok0 + j * 128: tok0 + (j + 1) * 128, :], in_=yt[p2][:],
                      sem="F_yt%d" % p2, reads=[kyt])
        S.barrier()

    def phase_H(self, l):
        S = self.S
        self.sbuf_reset()
        sb = self.sb
        UU = sb("H_UU", [64, 128], F32)
        UL = sb("H_UL", [64, 64], F32)
        tmpm = sb("H_tmpm", [64, 64], F32)
        S.op("pool", lambda e: e.memset(UU[:], 1.0), writes=["H_UU"])
        S.op("pool", lambda e: e.affine_select(out=UU[:, 0:64], in_=UU[:, 0:64], pattern=[[1, 64]],
                                                compare_op=ALU.is_ge, fill=0.0, base=0,
                                                channel_multiplier=-1),
             reads=["H_UU"], writes=["H_UU"])
        S.op("pool", lambda e: e.memset(tmpm[:], 1.0), writes=["H_tmpm"])
        S.op("pool", lambda e: e.affine_select(out=tmpm[:], in_=tmpm[:], pattern=[[0, 64]],
                                                compare_op=ALU.is_ge, fill=0.0, base=31,
                                                channel_multiplier=-1),
             reads=["H_tmpm"], writes=["H_tmpm"])
        S.op("pool", lambda e: e.tensor_tensor(out=UU[:, 64:128], in0=UU[:, 0:64], in1=tmpm[:],
                                                op=ALU.subtract),
             reads=["H_UU", "H_tmpm"], writes=["H_UU"])
        S.op("pool", lambda e: e.memset(UL[:], 1.0), writes=["H_UL"])
        S.op("pool", lambda e: e.affine_select(out=UL[:], in_=UL[:], pattern=[[-1, 64]],
                                                compare_op=ALU.is_gt, fill=0.0, base=0,
                                                channel_multiplier=1),
             reads=["H_UL"], writes=["H_UL"])
        lb = sb("H_lb", [64, 256], F32)
        oml = sb("H_oml", [64, 256], F32)
        if l == 0:
            S.op("pool", lambda e: e.memset(lb[:], 0.0), writes=["H_lb"])
        else:
            r0 = sb("H_r0", [64, 256], F32)
            S.dma(out=r0[:], in_=bcast_rows(self.hg_lb[0:1, :], 64), sem="H_r0", writes=["H_r0"])
            S.dma(out=lb[:], in_=bcast_rows(self.hg_lb[1:2, :], 64), sem="H_lbl", writes=["H_lb"])
            S.op("dve", lambda e: e.tensor_tensor(out=lb[:], in0=lb[:], in1=r0[:], op=ALU.subtract),
                 reads=["H_lb", "H_r0"], writes=["H_lb"])
            S.op("act", lambda e: e.activation(out=lb[:], in_=lb[:], func=AF.Sigmoid),
                 reads=["H_lb"], writes=["H_lb"])
        S.op("dve", lambda e: e.tensor_scalar(out=oml[:], in0=lb[:], scalar1=-1.0, scalar2=1.0,
                                               op0=ALU.mult, op1=ALU.add),
             reads=["H_lb"], writes=["H_oml"])
        lbT = sb("H_lbT", [64, 4, 2], F32)
        for h in range(4):
            for which, src, ksrc in ((0, lb, "H_lb"), (1, oml, "H_oml")):
                ps = self.ps[(2 * h + which) % 4]
                kps = ("ps", (2 * h + which) % 4)
                S.op("pe", lambda e, ps=ps, src=src, h=h: e.transpose(
                    out=ps[:64, :64], in_=src[:, h * 64:(h + 1) * 64],
                    identity=self.ident_f[:64, :64]), reads=[ksrc, "ident_f"], writes=[kps])
                S.op("dve", lambda e, ps=ps, h=h, which=which: e.tensor_copy(
                    out=lbT[:, h, which:which + 1], in_=ps[:64, 0:1]), reads=[kps],
                    writes=["H_lbT"])
        ng = sb("H_ng", [64, 64], F32)
        S.dma(out=ng[:], in_=bcast_rows(self.hg_norm[l:l + 1, :], 64), sem="H_ng", writes=["H_ng"])

        qf = [sb("H_qf%d" % i, [64, 2, 4, 512], F32) for i in range(2)]
        sq = [sb("H_sq%d" % i, [64, 4, 512], F32) for i in range(2)]
        kc = [sb("H_kc%d" % i, [64, 4, 512], F32) for i in range(2)]
        tk = [sb("H_tk%d" % i, [64, 768], F32) for i in range(3)]
        St = [sb("H_S%d" % i, [64, 4, 64], F32) for i in range(2)]
        Sb = [sb("H_Sb%d" % i, [64, 4, 64], BF16) for i in range(2)]
        pf_q = self.pfeat[FM_ROW[2764]:FM_ROW[2764] + 256, :].rearrange("(h k) t -> k h t", k=64)
        pf_f = self.pfeat[FM_ROW[3020]:FM_ROW[3020] + 256, :].rearrange("(h k) t -> k h t", k=64)
        NB = 3

        def t3(name, shape, dtype):
            return [sb("%s%d" % (name, i), shape, dtype) for i in range(NB)]
        sig, fg, logf, kct, kd = (t3("H_sig", [64, 256], F32), t3("H_fg", [64, 256], F32),
                                  t3("H_logf", [64, 256], F32), t3("H_kct", [64, 256], F32),
                                  t3("H_kd", [64, 256], BF16))
        vb = t3("H_vb", [64, 256], BF16)
        gs = t3("H_gs", [64, 4, 64], F32)
        EB, EN = t3("H_EB", [64, 4, 128], F32), t3("H_EN", [64, 4, 64], F32)
        qd, qp, kp = (t3("H_qd", [64, 4, 64], BF16), t3("H_qp", [64, 4, 64], BF16),
                      t3("H_kp", [64, 4, 64], BF16))
        Am = t3("H_Am", [64, 4, 64], BF16)
        osb, osq = t3("H_osb", [64, 4, 64], F32), t3("H_osq", [64, 4, 64], F32)
        stt = t3("H_stt", [64, 8], F32)
        stmp = t3("H_stmp", [64, 4, 64], F32)
        def seq_thread(s):
            pb = lambda i: 4 * s + i % 4
            for blk8 in range(SEQ // 512):
                slot = (blk8 * NSEQ + s) % 2
                tb = s * SEQ + blk8 * 512
                kqf = ("H_qf", slot)
                S.dma(out=qf[slot][:, 0, :, :], in_=pf_q[:, :, tb:tb + 512], sem="H_qfq%d" % slot,
                      writes=[(kqf, 0)])
                S.dma(out=qf[slot][:, 1, :, :], in_=pf_f[:, :, tb:tb + 512], sem="H_qff%d" % slot,
                      writes=[(kqf, 1)])
                S.op("act", lambda e, slot=slot: e.activation(out=sq[slot][:], in_=qf[slot][:, 0],
                                                               func=AF.Silu),
                     reads=[(kqf, 0)], writes=[("H_sq", slot)])
                S.op("act", lambda e, slot=slot: e.activation(out=kc[slot][:], in_=qf[slot][:, 1],
                                                               func=AF.Sigmoid),
                     reads=[(kqf, 1)], writes=[("H_kc", slot)])
                for h in range(4):
                    S.op("dve", lambda e, slot=slot, h=h: e.tensor_scalar(
                        out=kc[slot][:, h, :], in0=kc[slot][:, h, :], scalar1=lbT[:, h, 1:2],
                        scalar2=-1.0, op0=ALU.mult, op1=ALU.mult),
                        reads=[("H_kc", slot), "H_lbT"], writes=[("H_kc", slot)])
                    S.op("dve", lambda e, slot=slot, h=h: e.tensor_scalar(
                        out=kc[slot][:, h, :], in0=kc[slot][:, h, :], scalar1=lbT[:, h, 1:2],
                        scalar2=None, op0=ALU.add),
                        reads=[("H_kc", slot), "H_lbT"], writes=[("H_kc", slot)])
                for cn in range(8):
                    b = s
                    c0 = cn * 64
                    tok = tb + c0
                    ktk = ("H_tk", b)
                    S.dma(out=tk[b][:], in_=self.ptok[tok:tok + 64, TM_CF:TM_CF + 768],
                          sem="H_tk%d" % b, writes=[ktk])
                    first = (blk8 == 0 and cn == 0)
                    S.op("act", lambda e, b=b: e.activation(out=sig[b][:], in_=tk[b][:, 0:256],
                                                             func=AF.Sigmoid),
                         reads=[ktk], writes=[("H_sig", b)])
                    S.op("dve", lambda e, b=b: e.tensor_tensor(out=fg[b][:], in0=sig[b][:],
                                                                in1=oml[:], op=ALU.mult),
                         reads=[("H_sig", b), "H_oml"], writes=[("H_fg", b)])
                    S.op("dve", lambda e, b=b: e.tensor_tensor(out=fg[b][:], in0=fg[b][:],
                                                                in1=lb[:], op=ALU.add),
                         reads=[("H_fg", b), "H_lb"], writes=[("H_fg", b)])
                    S.op("act", lambda e, b=b: e.activation(out=logf[b][:], in_=fg[b][:],
                                                             func=AF.Ln),
                         reads=[("H_fg", b)], writes=[("H_logf", b)])
                    S.op("pool", lambda e, b=b: e.tensor_scalar(out=kct[b][:], in0=fg[b][:],
                                                                 scalar1=-1.0, scalar2=1.0,
                                                                 op0=ALU.mult, op1=ALU.add),
                         reads=[("H_fg", b)], writes=[("H_kct", b)])
                    S.op("pool", lambda e, b=b: e.tensor_copy(out=vb[b][:], in_=tk[b][:, 256:512]),
                         reads=[ktk], writes=[("H_vb", b)])
                    S.op("act", lambda e, b=b: e.activation(
                        out=gs[b][:], in_=tk[b][:, 512:768].rearrange("p (h v) -> p h v", h=4),
                        func=AF.Silu), reads=[ktk], writes=[("H_gs", b)])
                    S.op("pool", lambda e, b=b: e.tensor_tensor(
                        out=gs[b][:], in0=gs[b][:],
                        in1=ng[:].unsqueeze(1).to_broadcast([64, 4, 64]), op=ALU.mult),
                        reads=[("H_gs", b), "H_ng"], writes=[("H_gs", b)])
                    p0, kp0 = self.ps8[pb(0)], ("ps", pb(0))

                    def mmb(e, b=b, p0=p0):
                        ins = None
                        for h in range(4):
                            ins = e.matmul(p0[:64, h * 128:(h + 1) * 128],
                                           lhsT=logf[b][:, h * 64:(h + 1) * 64], rhs=UU[:, :],
                                           start=True, stop=True)
                        return ins
                    S.op("pe", mmb, reads=[("H_logf", b), "H_UU"], writes=[kp0])
                    p0v = p0[:64, :].rearrange("p (h t) -> p h t", h=4)
                    S.op("act", lambda e, b=b, p0v=p0v: e.activation(out=EB[b][:], in_=p0v,
                                                                      func=AF.Exp),
                         reads=[kp0], writes=[("H_EB", b)])
                    S.op("act", lambda e, b=b, p0v=p0v: e.activation(out=EN[b][:],
                                                                      in_=p0v[:, :, 64:128],
                                                                      func=AF.Exp, scale=-1.0),
                         reads=[kp0], writes=[("H_EN", b)])
                    p1, kp1 = self.ps8[pb(1)], ("ps", pb(1))
                    S.op("pe", lambda e, b=b, p1=p1: e.matmul(p1[:64, :256], lhsT=UL[:, :],
                                                              rhs=logf[b][:, :], start=True,
                                                              stop=True),
                         reads=[("H_logf", b), "H_UL"], writes=[kp1])
                    S.op("act", lambda e, b=b, p1=p1: e.activation(out=sig[b][:], in_=p1[:64, :256],
                                                                    func=AF.Exp),
                         reads=[kp1], writes=[("H_sig", b)])
                    S.op("dve", lambda e, b=b: e.tensor_tensor(out=kd[b][:], in0=sig[b][:],
                                                                in1=kct[b][:], op=ALU.mult),
                         reads=[("H_sig", b), ("H_kct", b)], writes=[("H_kd", b)])
                    sqv = sq[slot][:, :, c0:c0 + 64]
                    kcv = kc[slot][:, :, c0:c0 + 64]
                    S.op("dve", lambda e, b=b, sqv=sqv: e.tensor_tensor(
                        out=qd[b][:], in0=sqv, in1=EB[b][:, :, 0:64], op=ALU.mult),
                        reads=[("H_sq", slot), ("H_EB", b)], writes=[("H_qd", b)])
                    S.op("pool", lambda e, b=b, sqv=sqv: e.tensor_tensor(
                        out=qp[b][:], in0=sqv, in1=EB[b][:, :, 64:128], op=ALU.mult),
                        reads=[("H_sq", slot), ("H_EB", b)], writes=[("H_qp", b)])
                    S.op("dve", lambda e, b=b, kcv=kcv: e.tensor_tensor(
                        out=kp[b][:], in0=kcv, in1=EN[b][:], op=ALU.mult),
                        reads=[("H_kc", slot), ("H_EN", b)], writes=[("H_kp", b)])
                    p2, kp2 = self.ps8[pb(2)], ("ps", pb(2))

                    def mma(e, b=b, p2=p2):
                        ins = None
                        for h in range(4):
                            ins = e.matmul(p2[:64, h * 64:(h + 1) * 64], lhsT=kp[b][:, h, :],
                                           rhs=qp[b][:, h, :], start=True, stop=True)
                        return ins
                    S.op("pe", mma, reads=[("H_kp", b), ("H_qp", b)], writes=[kp2])
                    S.op("dve", lambda e, b=b, p2=p2: e.tensor_tensor(
                        out=Am[b][:], in0=p2[:64, :256].rearrange("p (h c) -> p h c", h=4),
                        in1=UU[:, 0:64].unsqueeze(1).to_broadcast([64, 4, 64]), op=ALU.mult),
                        reads=[kp2, "H_UU"], writes=[("H_Am", b)])
                    if first:
                        S.op("pool", lambda e, s=s: e.memset(St[s][:], 0.0), writes=[("H_S", s)])
                        S.op("pool", lambda e, s=s: e.memset(Sb[s][:], 0.0), writes=[("H_Sb", s)])
                    p3, kp3 = self.ps8[pb(3)], ("ps", pb(3))

                    def mmo(e, b=b, p3=p3, s=s):
                        ins = None
                        for h in range(4):
                            e.matmul(p3[:64, h * 64:(h + 1) * 64], lhsT=qd[b][:, h, :],
                                     rhs=Sb[s][:, h, :], start=True, stop=False)
                            ins = e.matmul(p3[:64, h * 64:(h + 1) * 64], lhsT=Am[b][:, h, :],
                                           rhs=vb[b][:, h * 64:(h + 1) * 64], start=False, stop=True)
                        return ins
                    S.op("pe", mmo, reads=[("H_qd", b), ("H_Sb", s), ("H_Am", b), ("H_vb", b)],
                         writes=[kp3])
                    p4, kp4 = self.ps8[pb(4)], ("ps", pb(4))

                    def mms(e, b=b, p4=p4):
                        ins = None
                        for h in range(4):
                            ins = e.matmul(p4[:64, h * 64:(h + 1) * 64],
                                           lhsT=kd[b][:, h * 64:(h + 1) * 64],
                                           rhs=vb[b][:, h * 64:(h + 1) * 64], start=True, stop=True)
                        return ins
                    S.op("pe", mms, reads=[("H_kd", b), ("H_vb", b)], writes=[kp4])
                    S.op("dve", lambda e, b=b, s=s: e.tensor_tensor(
                        out=stmp[b][:], in0=St[s][:],
                        in1=EB[b][:, :, 63:64].to_broadcast([64, 4, 64]), op=ALU.mult),
                        reads=[("H_S", s), ("H_EB", b)], writes=[("H_stmp", b)])
                    S.op("dve", lambda e, b=b, s=s, p4=p4: e.tensor_tensor(
                        out=St[s][:], in0=stmp[b][:],
                        in1=p4[:64, :256].rearrange("p (h v) -> p h v", h=4), op=ALU.add),
                        reads=[("H_stmp", b), kp4], writes=[("H_S", s)])
                    S.op("act", lambda e, s=s: e.copy(out=Sb[s][:], in_=St[s][:]),
                         reads=[("H_S", s)], writes=[("H_Sb", s)])
                    S.op("act", lambda e, b=b, p3=p3: e.copy(
                        out=osb[b][:], in_=p3[:64, :256].rearrange("p (h v) -> p h v", h=4)),
                        reads=[kp3], writes=[("H_osb", b)])
                    S.op("pool", lambda e, b=b: e.tensor_tensor(out=osq[b][:], in0=osb[b][:],
                                                                 in1=osb[b][:], op=ALU.mult),
                         reads=[("H_osb", b)], writes=[("H_osq", b)])
                    S.op("dve", lambda e, b=b: e.tensor_reduce(out=stt[b][:, 0:4], in_=osq[b][:],
                                                                axis=AX.X, op=ALU.add),
                         reads=[("H_osq", b)], writes=[("H_stt", b)])
                    self.rstd_from_ss(stt[b][:, 0:4], ("H_stt", b), stt[b][:, 4:8], ("H_rs", b), 64)
                    S.op("dve", lambda e, b=b: e.tensor_tensor(
                        out=osb[b][:], in0=osb[b][:],
                        in1=stt[b][:, 4:8].unsqueeze(2).to_broadcast([64, 4, 64]), op=ALU.mult),
                        reads=[("H_osb", b), ("H_rs", b)], writes=[("H_osb", b)])
                    S.op("pool", lambda e, b=b: e.tensor_tensor(out=osq[b][:], in0=osb[b][:],
                                                                 in1=gs[b][:], op=ALU.mult),
                         reads=[("H_osb", b), ("H_gs", b)], writes=[("H_osq", b)])
                    S.dma(out=self.mixin[tok:tok + 64, 768:1024],
                          in_=osq[b][:].rearrange("p h v -> p (h v)"), sem="H_out%d" % b,
                          reads=[("H_osq", b)])
        S.interleave([lambda s=s: seq_thread(s) for s in range(NSEQ)])
        S.barrier()

    def phase_S(self, l):
        S = self.S
        self.sbuf_reset()
        sb = self.sb
        BIG = -1.0e30
        KIT = 32
        kiT = sb("S_kiT", [64, SEQ], F32)
        kT = sb("S_kT", [64, SEQ], BF16)
        kst = sb("S_kst", [64, 1024], F32)
        vst = sb("S_vst", [128, 8, 64], F32)
        v1 = sb("S_v1", [128, 32, 65], BF16)
        junk = sb("S_junk", [128, SEQ], BF16)
        ckn = sb("S_ckn", [128, KIT + 1], F32)
        for k in range(KIT + 1):
            S.op("pool", lambda e, k=k: e.memset(ckn[:, k:k + 1], -2.1 / 2.0 ** (k + 1)),
                 writes=[("S_ckn", k)])
        kck = [("S_ckn", k) for k in range(KIT + 1)]

        NT = 2

        def t2(name, shape, dtype):
            return [sb("%s%d" % (name, i), shape, dtype) for i in range(NT)]
        qq = t2("S_qq", [64, 2, 4, 128], F32)
        qqb = t2("S_qqb", [64, 4, 128], BF16)
        acc = t2("S_acc", [128, SEQ], F32)
        wkA = t2("S_wkA", [128, SEQ], F32)
        gtt = t2("S_gt", [128, SEQ], BF16)
        eqt = t2("S_eq", [128, SEQ], BF16)
        selb = t2("S_selb", [128, SEQ], BF16)
        rr = [sb("S_rr%d" % i, [128, 512], F32) for i in range(2 * NT)]
        pT = [sb("S_pT%d" % i, [128, 512], BF16) for i in range(2 * NT)]
        wi = t2("S_wi", [128, 12], F32)
        sc = t2("S_sc", [128, 16], F32)
        nst = t2("S_nst", [128, KIT + 1], F32)
        oT = t2("S_oT", [65, 512], F32)
        osb = t2("S_osb", [128, 4, 64], F32)
        rc = t2("S_rc", [128, 4, 1], F32)
        pf_q = self.pfeat[FM_ROW[2056]:FM_ROW[2056] + 256, :].rearrange("(h k) t -> k h t", k=64)
        pf_qi = self.pfeat[FM_ROW[2440]:FM_ROW[2440] + 256, :].rearrange("(h k) t -> k h t", k=64)
        pf_k = self.pfeat[FM_ROW[2312]:FM_ROW[2312] + 64, :]
        pf_ki = self.pfeat[FM_ROW[2696]:FM_ROW[2696] + 64, :]

        def tile_thread(s, a2):
            t0 = s * SEQ
            nr = 0
            for j in range(a2, SEQ // 128, NT):
                tq = t0 + j * 128
                NK = (j + 1) * 128
                kqq = ("S_qq", a2)
                S.dma(out=qq[a2][:, 0], in_=pf_q[:, :, tq:tq + 128], sem="S_qq%da" % a2,
                      writes=[(kqq, 0)])
                S.dma(out=qq[a2][:, 1], in_=pf_qi[:, :, tq:tq + 128], sem="S_qq%db" % a2,
                      writes=[(kqq, 1)])
                S.op("pool", lambda e: e.tensor_copy(out=qqb[a2][:], in_=qq[a2][:, 0]),
                     reads=[(kqq, 0)], writes=[("S_qqb", a2)])
                kwi = ("S_wi", a2)
                S.dma(out=wi[a2][:, 0:4], in_=self.ptok[tq:tq + 128, TM_WI:TM_WI + 4],
                      sem="S_wi%d" % a2, writes=[(kwi, 0)])
                S.op("act", lambda e: e.activation(out=wi[a2][:, 4:8], in_=wi[a2][:, 0:4],
                                                    func=AF.Abs),
                     reads=[(kwi, 0)], writes=[(kwi, 1)])
                S.op("act", lambda e: e.activation(out=wi[a2][:, 8:12], in_=wi[a2][:, 0:4],
                                                    func=AF.Sign),
                     reads=[(kwi, 0)], writes=[(kwi, 2)])
                kacc = ("S_acc", a2)
                nkb = (NK + 511) // 512
                for kb in range(nkb):
                    w = min(512, NK - kb * 512)
                    for h in range(4):
                        pi = 2 * a2 + h % 2
                        ps, kps = self.ps8[pi], ("ps", pi)
                        S.op("pe", lambda e, ps=ps, h=h, kb=kb, w=w: e.matmul(
                            ps[:, :w], lhsT=qq[a2][:, 1, h, :],
                            rhs=kiT[:, kb * 512: kb * 512 + w], start=True, stop=True),
                            reads=[(kqq, 1), "S_kiT"], writes=[kps])
                        ri = 2 * a2 + nr % 2
                        r, kr = rr[ri], ("S_rr", ri)
                        nr += 1
                        S.op("act", lambda e, ps=ps, r=r, w=w, h=h: e.activation(
                            out=r[:, :w], in_=ps[:, :w], func=AF.Relu, scale=wi[a2][:, 4 + h:5 + h]),
                            reads=[kps, (kwi, 1)], writes=[kr])
                        av = acc[a2][:, kb * 512: kb * 512 + w]
                        if h == 0:
                            S.op("dve", lambda e, av=av, r=r, w=w, h=h: e.tensor_scalar(
                                out=av, in0=r[:, :w], scalar1=wi[a2][:, 8 + h:9 + h], scalar2=None,
                                op0=ALU.mult), reads=[kr, (kwi, 2)], writes=[(kacc, kb)])
                        else:
                            S.op("dve", lambda e, av=av, r=r, w=w, h=h: e.scalar_tensor_tensor(
                                out=av, in0=r[:, :w], scalar=wi[a2][:, 8 + h:9 + h], in1=av,
                                op0=ALU.mult, op1=ALU.add),
                                reads=[kr, (kwi, 2), (kacc, kb)], writes=[(kacc, kb)])
                kall = [(kacc, kb) for kb in range(nkb)]
                X = sc[a2]
                ksc = lambda nm: ("S_sc", a2, nm)
                accv = acc[a2][:, :NK]
                if j >= 2:
                    S.op("dve", lambda e: e.tensor_reduce(out=X[:, 0:1], in_=accv, axis=AX.X,
                                                           op=ALU.max, apply_absolute_value=True),
                         reads=kall, writes=[ksc("rm")])
                    S.op("dve", lambda e: e.tensor_scalar(out=X[:, 0:1], in0=X[:, 0:1],
                                                           scalar1=1.0e-20, scalar2=None,
                                                           op0=ALU.max),
                         reads=[ksc("rm")], writes=[ksc("rm")])
                    S.op("dve", lambda e: e.tensor_scalar(out=nst[a2][:], in0=ckn[:],
                                                           scalar1=X[:, 0:1], scalar2=None,
                                                           op0=ALU.mult),
                         reads=[ksc("rm")] + kck, writes=[("S_nst", a2)])
                    S.op("dve", lambda e: e.tensor_scalar(out=X[:, 1:2], in0=X[:, 0:1],
                                                           scalar1=-0.02, scalar2=None,
                                                           op0=ALU.mult),
                         reads=[ksc("rm")], writes=[ksc("nc")])
                    S.op("pool", lambda e: e.memset(X[:, 4:5], float(NK) - 511.5),
                         writes=[ksc("cb")])
                S.op("pool", lambda e: e.memset(acc[a2][0:64, NK - 64:NK], BIG),
                     reads=kall + [ksc("rm")], writes=[kacc])
                if j >= 2:
                    for k in range(KIT):
                        S.op("act", lambda e: e.activation(out=junk[:, :NK], in_=accv, func=AF.Sign,
                                                            bias=X[:, 1:2], scale=1.0,
                                                            accum_out=X[:, 2:3]),
                             reads=[kacc, ksc("nc")], writes=[ksc("sg")])
                        S.op("act", lambda e: e.activation(out=X[:, 3:4], in_=X[:, 2:3],
                                                            func=AF.Sign, bias=X[:, 4:5], scale=1.0),
                             reads=[ksc("sg"), ksc("cb")], writes=[ksc("dd")])
                        S.op("act", lambda e, k=k: e.activation(out=X[:, 1:2], in_=X[:, 3:4],
                                                                 func=AF.Identity,
                                                                 scale=nst[a2][:, k + 1:k + 2],
                                                                 bias=X[:, 1:2]),
                             reads=[ksc("dd"), ("S_nst", a2), ksc("nc")], writes=[ksc("nc")])
                    S.op("dve", lambda e: e.scalar_tensor_tensor(
                        out=X[:, 5:6], in0=X[:, 1:2], scalar=-1.0, in1=nst[a2][:, KIT:KIT + 1],
                        op0=ALU.mult, op1=ALU.add), reads=[ksc("nc"), ("S_nst", a2)],
                        writes=[ksc("thr")])
                else:
                    S.op("pool", lambda e: e.memset(X[:, 5:6], BIG / 2), writes=[ksc("thr")])
                kwk = ("S_wkA", a2)
                wv = wkA[a2][:, :NK]
                S.op("dve", lambda e: e.tensor_scalar(out=wv, in0=accv, scalar1=X[:, 5:6],
                                                       scalar2=3.0e30, op0=ALU.is_lt, op1=ALU.mult),
                     reads=[kacc, ksc("thr")], writes=[kwk])
                S.op("dve", lambda e: e.tensor_tensor(out=wv, in0=wv, in1=accv, op=ALU.add),
                     reads=[kacc, kwk], writes=[kwk])
                S.op("dve", lambda e: e.tensor_reduce(out=X[:, 6:7], in_=wv, axis=AX.X, op=ALU.min),
                     reads=[kwk], writes=[ksc("v")])
                gv, ev = gtt[a2][:, :NK], eqt[a2][:, :NK]
                S.op("dve", lambda e: e.tensor_scalar(out=gv, in0=accv, scalar1=X[:, 6:7],
                                                       scalar2=None, op0=ALU.is_gt, op1=ALU.add,
                                                       accum_out=X[:, 7:8]),
                     reads=[kacc, ksc("v")], writes=[("S_gt", a2), ksc("cg")])
                S.op("dve", lambda e: e.tensor_scalar(out=ev, in0=accv, scalar1=X[:, 6:7],
                                                       scalar2=None, op0=ALU.is_equal),
                     reads=[kacc, ksc("v")], writes=[("S_eq", a2)])
                S.op("dve", lambda e: e.tensor_tensor_scan(out=wv, data0=ev, data1=ev, initial=0.0,
                                                            op0=ALU.add, op1=ALU.max),
                     reads=[("S_eq", a2), kwk], writes=[kwk])
                S.op("dve", lambda e: e.tensor_scalar(out=X[:, 8:9], in0=X[:, 7:8], scalar1=-1.0,
                                                       scalar2=256.0, op0=ALU.mult, op1=ALU.add),
                     reads=[ksc("cg")], writes=[ksc("need")])
                S.op("dve", lambda e: e.scalar_tensor_tensor(out=ev, in0=wv, scalar=X[:, 8:9],
                                                              in1=ev, op0=ALU.is_le, op1=ALU.mult),
                     reads=[kwk, ksc("need"), ("S_eq", a2)], writes=[("S_eq", a2)])
                S.op("pool", lambda e: e.tensor_tensor(out=gv, in0=gv, in1=ev, op=ALU.add),
                     reads=[("S_gt", a2), ("S_eq", a2)], writes=[("S_gt", a2)])
                ksel = ("S_selb", a2)
                S.op("pool", lambda e: e.tensor_scalar(out=selb[a2][:, :NK], in0=gv, scalar1=-1.0,
                                                        scalar2=-NEG, op0=ALU.add, op1=ALU.mult),
                     reads=[("S_gt", a2)], writes=[ksel])
                po, kpo = self.ps8[4 + a2], ("ps", 4 + a2)
                for kt in range(j + 1):
                    li = (0, 1, 6, 7)[2 * a2 + kt % 2]
                    pl, kpl = self.ps8[li], ("ps", li)

                    def mml(e, pl=pl, kt=kt):
                        e.matmul(pl[:, :].rearrange("p (h t) -> p h t", h=4),
                                 lhsT=kT[:, kt * 128:(kt + 1) * 128], rhs=qqb[a2][:, :, :],
                                 start=True, stop=False)
                        ins = None
                        for h in range(4):
                            ins = e.matmul(pl[:, h * 128:(h + 1) * 128],
                                           lhsT=selb[a2][:, kt * 128:(kt + 1) * 128],
                                           rhs=self.ident_b[:, :], start=False, stop=(h == 3))
                        return ins
                    S.op("pe", mml, reads=["S_kT", ("S_qqb", a2), ksel, "ident_b"], writes=[kpl])
                    pi_ = 2 * a2 + kt % 2
                    p, kp = pT[pi_], ("S_pT", pi_)
                    S.op("act", lambda e, p=p, pl=pl: e.activation(out=p[:], in_=pl[:, :],
                                                                    func=AF.Exp, scale=0.125),
                         reads=[kpl], writes=[kp])
                    S.op("pe", lambda e, p=p, kt=kt: e.matmul(
                        po[:65, :], lhsT=v1[:, kt, :], rhs=p[:], start=(kt == 0), stop=(kt == j)),
                        reads=["S_v1", kp], writes=[kpo])
                koT = ("S_oT", a2)
                S.op("act", lambda e: e.copy(out=oT[a2][:], in_=po[:65, :]),
                     reads=[kpo], writes=[koT])
                pt, kpt = self.ps8[2 * a2], ("ps", 2 * a2)

                def trs(e):
                    ins = None
                    for h in range(4):
                        ins = e.transpose(out=pt[:, h * 65:(h + 1) * 65],
                                          in_=oT[a2][:, h * 128:(h + 1) * 128],
                                          identity=self.ident_f[:65, :65])
                    return ins
                S.op("pe", trs, reads=[koT, "ident_f"], writes=[kpt])
                ptv = pt[:, :260].rearrange("p (h d) -> p h d", h=4)
                S.op("dve", lambda e: e.reciprocal(out=rc[a2][:], in_=ptv[:, :, 64:65]),
                     reads=[kpt], writes=[("S_rc", a2)])
                S.op("dve", lambda e: e.tensor_tensor(
                    out=osb[a2][:], in0=ptv[:, :, 0:64], in1=rc[a2][:].to_broadcast([128, 4, 64]),
                    op=ALU.mult), reads=[kpt, ("S_rc", a2)], writes=[("S_osb", a2)])
                S.dma(out=self.mixin[tq:tq + 128, 512:768],
                      in_=osb[a2][:].rearrange("p h d -> p (h d)"), sem="S_out%d" % a2,
                      reads=[("S_osb", a2)])

        for s in range(NSEQ):
            t0 = s * SEQ
            S.dma(out=kiT[:], in_=pf_ki[:, t0:t0 + SEQ], sem="S_kiT", writes=["S_kiT"])
            for q4 in range(4):
                S.dma(out=kst[:], in_=pf_k[:, t0 + q4 * 1024:t0 + (q4 + 1) * 1024], sem="S_kst",
                      writes=["S_kst"])
                S.op("pool", lambda e, q4=q4: e.tensor_copy(out=kT[:, q4 * 1024:(q4 + 1) * 1024],
                                                            in_=kst[:]),
                     reads=["S_kst"], writes=["S_kT"])
            S.op("pool", lambda e: e.memset(v1[:], 1.0), writes=["S_v1"])
            for q4 in range(4):
                S.dma(out=vst[:],
                      in_=self.ptok[t0 + q4 * 1024: t0 + (q4 + 1) * 1024,
                                    TM_BV:TM_BV + 64].rearrange("(kt p) d -> p kt d", p=128),
                      sem="S_vst", writes=["S_vst"])
                S.op("pool", lambda e, q4=q4: e.tensor_copy(out=v1[:, q4 * 8:(q4 + 1) * 8, 0:64],
                                                            in_=vst[:]),
                     reads=["S_vst"], writes=["S_v1"])
            S.interleave([lambda s=s, a=a: tile_thread(s, a) for a in range(NT)])
        S.barrier()

    def phase_D(self, l):
        S = self.S
        self.sbuf_reset()
        sb = self.sb
        NB = 2
        U64 = sb("D_U", [64, 64], F32)
        UL = sb("D_UL", [64, 64], F32)
        Lst = sb("D_Lst", [64, 64], F32)
        mb = sb("D_mb", [64, 64], F32)
        S.op("pool", lambda e: e.memset(U64[:], 1.0), writes=["D_U"])
        S.op("pool", lambda e: e.affine_select(out=U64[:], in_=U64[:], pattern=[[1, 64]],
                                                compare_op=ALU.is_ge, fill=0.0, base=0,
                                                channel_multiplier=-1), reads=["D_U"],
             writes=["D_U"])
        for t_, kk_ in ((UL, "D_UL"), (Lst, "D_Lst")):
            S.op("pool", lambda e, t_=t_: e.memset(t_[:], 1.0), writes=[kk_])
            S.op("pool", lambda e, t_=t_: e.affine_select(out=t_[:], in_=t_[:], pattern=[[-1, 64]],
                                                          compare_op=ALU.is_gt, fill=0.0, base=0,
                                                          channel_multiplier=1), reads=[kk_],
                 writes=[kk_])
        S.op("pool", lambda e: e.memset(mb[:], 0.0), writes=["D_mb"])
        S.op("pool", lambda e: e.affine_select(out=mb[:], in_=mb[:], pattern=[[-1, 64]],
                                                compare_op=ALU.is_ge, fill=-1.0e4, base=0,
                                                channel_multiplier=1), reads=["D_mb"],
             writes=["D_mb"])
        cw = self.load_convw("D_cw", self.dn_conv[l], 4, 12)
        alog = sb("D_alog", [64, 4], F32)
        dtb = sb("D_dtb", [64, 4], F32)
        S.dma(out=alog[:], in_=bcast_rows(self.dn_a_log[l:l + 1, :], 64), sem="D_alog",
              writes=["D_alog"])
        S.dma(out=dtb[:], in_=bcast_rows(self.dn_dt_bias[l:l + 1, :], 64), sem="D_dtb",
              writes=["D_dtb"])
        S.op("act", lambda e: e.activation(out=alog[:], in_=alog[:], func=AF.Exp), reads=["D_alog"],
             writes=["D_alog"])
        S.op("dve", lambda e: e.tensor_scalar(out=alog[:], in0=alog[:], scalar1=-1.0, scalar2=None,
                                               op0=ALU.mult), reads=["D_alog"], writes=["D_alog"])
        ng = sb("D_ng", [64, 128], F32)
        S.dma(out=ng[:], in_=bcast_rows(self.dn_norm[l:l + 1, :], 64), sem="D_ng", writes=["D_ng"])

        X = [sb("D_X%d" % i, [128, 515], F32) for i in range(4)]
        Cc = [sb("D_C%d" % i, [128, 512], F32) for i in range(4)]
        Y = [sb("D_Y%d" % i, [128, 12, 512], F32) for i in range(2)]
        St = [sb("D_S%d" % i, [128, 4, 128], F32) for i in range(NSEQ)]
        Sb = [sb("D_Sb%d" % i, [128, 4, 128], BF16) for i in range(NSEQ)]

        def t2(name, shape, dtype):
            return [sb("%s%d" % (name, i), shape, dtype) for i in range(NB)]
        tg = t2("D_tg", [64, 8], F32)
        gt = t2("D_gt", [64, 24], F32)
        zt = t2("D_zt", [64, 4, 128], F32)
        gs = t2("D_gs", [64, 4, 128], F32)
        QK = t2("D_QK", [64, 8, 128], F32)
        Vt = t2("D_Vt", [64, 4, 128], F32)
        sqq = t2("D_sqq", [64, 8, 128], F32)
        nrm = t2("D_nrm", [64, 16], F32)
        qdk = t2("D_qdk", [64, 4, 128], F32)
        TT = t2("D_TT", [128, 12, 64], BF16)
        kd = t2("D_kd", [64, 4, 128], BF16)
        bk = t2("D_bk", [64, 4, 128], BF16)
        bv = t2("D_bv", [64, 4, 128], BF16)
        gU = t2("D_gU", [64, 8, 64], F32)
        dec = t2("D_dec", [64, 4, 64], F32)
        Mm = t2("D_M", [64, 4, 64], F32)
        QKm = t2("D_QKm", [64, 4, 64], F32)
        QKT = t2("D_QKT", [64, 4, 64], BF16)
        Qa = t2("D_Qa", [64, 4, 64], F32)
        Qta = t2("D_Qta", [64, 4, 64], F32)
        Qb = t2("D_Qb", [64, 4, 64], F32)
        Qtb = t2("D_Qtb", [64, 4, 64], F32)
        Bt = t2("D_Bt", [64, 4, 64], F32)
        Tb = t2("D_Tb", [64, 4, 64], BF16)
        u0 = t2("D_u0", [64, 4, 128], F32)
        ub = t2("D_ub", [64, 4, 128], BF16)
        wkT = t2("D_wkT", [128, 4, 64], BF16)
        glb = t2("D_glb", [128, 4], F32)
        osb = t2("D_osb", [64, 4, 128], F32)
        osq = t2("D_osq", [64, 4, 128], F32)
        identI = sb("D_I", [64, 4, 64], F32)
        for h in range(4):
            S.op("pool", lambda e, h=h: e.tensor_copy(out=identI[:, h, :], in_=self.ident_f[:64, :64]),
                 reads=["ident_f"], writes=["D_I"])

        def v4(ps, w):
            return ps[:64, :4 * w].rearrange("p (h x) -> p h x", h=4)

        def bc(ap_h1, w):
            a = ap_h1 if len(ap_h1.shape) == 3 else ap_h1.unsqueeze(2)
            return a.to_broadcast([64, 4, w])

        def seq_thread(s):
            pb = lambda i: 4 * s + i % 4
            for blk8 in range(SEQ // 512):
                ys = (blk8 * NSEQ + s) % 2
                tb = s * SEQ + blk8 * 512
                for ct in range(12):
                    xi = 2 * s + ct % 2
                    x = X[xi]
                    kx = ("D_X", xi)
                    if blk8 == 0:
                        S.op("pool", lambda e, x=x: e.memset(x[:, 0:3], 0.0), writes=[(kx, "h")])
                        S.dma(out=x[:, 3:515], in_=self.pfeat[ct * 128:(ct + 1) * 128, tb:tb + 512],
                              sem="D_X%d" % xi, writes=[(kx, "b")])
                    else:
                        S.dma(out=x[:, :], in_=self.pfeat[ct * 128:(ct + 1) * 128, tb - 3:tb + 512],
                              sem="D_X%d" % xi, writes=[(kx, "h"), (kx, "b")])
                    c, kc = Cc[xi], ("D_C", xi)
                    S.op("act", lambda e, x=x, c=c, ct=ct: e.activation(
                        out=c[:], in_=x[:, 0:512], func=AF.Copy, scale=cw[:, 0, ct:ct + 1]),
                        reads=[(kx, "h"), (kx, "b"), "D_cw"], writes=[kc])
                    for tap in (1, 2, 3):
                        S.op("dve", lambda e, x=x, c=c, ct=ct, tap=tap: e.scalar_tensor_tensor(
                            out=c[:], in0=x[:, tap:tap + 512], scalar=cw[:, tap, ct:ct + 1],
                            in1=c[:], op0=ALU.mult, op1=ALU.add),
                            reads=[(kx, "h"), (kx, "b"), "D_cw", kc], writes=[kc])
                    S.op("act", lambda e, c=c, ct=ct, ys=ys: e.activation(out=Y[ys][:, ct, :],
                                                                           in_=c[:], func=AF.Silu),
                         reads=[kc], writes=[("D_Y", ys, ct)])
                ykeys = [("D_Y", ys, ct) for ct in range(12)]
                for cn in range(8):
                    b = s
                    c0 = cn * 64
                    tok = tb + c0
                    K = lambda nm: (nm, b)
                    first = (blk8 == 0 and cn == 0)
                    if first:
                        S.op("pool", lambda e, s=s: e.memset(St[s][:], 0.0), writes=[("D_S", s)])
                        S.op("pool", lambda e, s=s: e.memset(Sb[s][:], 0.0), writes=[("D_Sb", s)])
                    S.dma(out=tg[b][:], in_=self.ptok[tok:tok + 64, TM_AB:TM_AB + 8],
                          sem="D_tg%d" % b, writes=[K("tg")])
                    S.dma(out=zt[b][:].rearrange("p h v -> p (h v)"),
                          in_=self.ptok[tok:tok + 64, TM_AZ:TM_AZ + 512], sem="D_zt%d" % b,
                          writes=[K("zt")])
                    G = gt[b]
                    S.op("act", lambda e, b=b, G=G: e.activation(out=G[:, 0:4], in_=tg[b][:, 0:4],
                                                                  func=AF.Sigmoid),
                         reads=[K("tg")], writes=[K("beta")])
                    S.op("dve", lambda e, b=b, G=G: e.tensor_tensor(out=G[:, 20:24], in0=tg[b][:, 4:8],
                                                                     in1=dtb[:], op=ALU.add),
                         reads=[K("tg"), "D_dtb"], writes=[K("gtmp")])
                    S.op("act", lambda e, G=G: e.activation(out=G[:, 20:24], in_=G[:, 20:24],
                                                             func=AF.Exp),
                         reads=[K("gtmp")], writes=[K("gtmp")])
                    S.op("act", lambda e, G=G: e.activation(out=G[:, 20:24], in_=G[:, 20:24],
                                                             func=AF.Ln, bias=1.0),
                         reads=[K("gtmp")], writes=[K("gtmp")])
                    S.op("dve", lambda e, G=G: e.tensor_tensor(out=G[:, 4:8], in0=G[:, 20:24],
                                                                in1=alog[:], op=ALU.mult),
                         reads=[K("gtmp"), "D_alog"], writes=[K("g")])
                    pg, kpg = self.ps8[pb(5)], ("ps", pb(5))

                    def mmg(e, G=G, pg=pg):
                        e.matmul(pg[:64, 0:4], lhsT=U64[:, :], rhs=G[:, 4:8], start=True, stop=True)
                        e.matmul(pg[:64, 4:8], lhsT=UL[:, :], rhs=G[:, 4:8], start=True, stop=True)
                        return e.matmul(pg[:, 8:12], lhsT=self.ones_f[:64, :], rhs=G[:, 4:8],
                                        start=True, stop=True)
                    S.op("pe", mmg, reads=[K("g"), "D_U", "D_UL", "ones_f"], writes=[kpg])
                    S.op("act", lambda e, G=G, pg=pg: e.activation(out=G[:, 8:16], in_=pg[:64, 0:8],
                                                                    func=AF.Exp),
                         reads=[kpg], writes=[K("eG")])
                    S.op("act", lambda e, b=b, pg=pg: e.activation(out=glb[b][:], in_=pg[:, 8:12],
                                                                    func=AF.Exp),
                         reads=[kpg], writes=[K("glb")])
                    S.op("dve", lambda e, G=G: e.tensor_tensor(out=G[:, 16:20], in0=G[:, 0:4],
                                                                in1=G[:, 8:12], op=ALU.mult),
                         reads=[K("beta"), K("eG")], writes=[K("beG")])
                    S.op("dve", lambda e, b=b, G=G: e.tensor_tensor(
                        out=gU[b][:, 0:4, :], in0=U64[:].unsqueeze(1).to_broadcast([64, 4, 64]),
                        in1=bc(G[:, 4:8], 64), op=ALU.mult), reads=["D_U", K("g")],
                        writes=[K("gU")])
                    S.op("pool", lambda e, b=b: e.tensor_scalar(out=gU[b][:, 4:8, :],
                                                                 in0=gU[b][:, 0:4, :], scalar1=-1.0,
                                                                 scalar2=None, op0=ALU.mult),
                         reads=[K("gU")], writes=[K("ngU")])
                    pd, kpd = self.ps8[pb(4)], ("ps", pb(4))

                    def mmd(e, b=b, pd=pd):
                        ins = None
                        for h in range(4):
                            e.matmul(pd[:64, h * 64:(h + 1) * 64], lhsT=gU[b][:, h, :],
                                     rhs=self.ones_f[:64, :64], start=True, stop=False)
                            ins = e.matmul(pd[:64, h * 64:(h + 1) * 64], lhsT=self.ones_f[:64, :64],
                                           rhs=gU[b][:, 4 + h, :], start=False, stop=True)
                        return ins
                    S.op("pe", mmd, reads=[K("gU"), K("ngU"), "ones_f"], writes=[kpd])
                    S.op("dve", lambda e, b=b, pd=pd: e.tensor_tensor(
                        out=dec[b][:], in0=v4(pd, 64),
                        in1=mb[:].unsqueeze(1).to_broadcast([64, 4, 64]), op=ALU.add),
                        reads=[kpd, "D_mb"], writes=[K("dec")])
                    S.op("act", lambda e, b=b: e.activation(out=dec[b][:], in_=dec[b][:],
                                                             func=AF.Exp),
                         reads=[K("dec")], writes=[K("dec")])
                    for grp in range(3):
                        pt, kpt = self.ps8[pb(grp)], ("ps", pb(grp))

                        def trq(e, grp=grp, pt=pt):
                            ins = None
                            for h in range(4):
                                ins = e.transpose(out=pt[:64, h * 128:(h + 1) * 128],
                                                  in_=Y[ys][:, grp * 4 + h, c0:c0 + 64],
                                                  identity=self.ident_f[:, :])
                            return ins
                        S.op("pe", trq, reads=ykeys + ["ident_f"], writes=[kpt])
                        dstv = Vt[b][:] if grp == 2 else QK[b][:, grp * 4:(grp + 1) * 4, :]
                        S.op("act", lambda e, dstv=dstv, pt=pt: e.copy(out=dstv, in_=v4(pt, 128)),
                             reads=[kpt], writes=[K("QK%d" % grp)])
                    S.op("pool", lambda e, b=b: e.tensor_tensor(out=sqq[b][:], in0=QK[b][:],
                                                                 in1=QK[b][:], op=ALU.mult),
                         reads=[K("QK0"), K("QK1")], writes=[K("sqq")])
                    S.op("dve", lambda e, b=b: e.tensor_reduce(out=nrm[b][:, 0:8], in_=sqq[b][:],
                                                                axis=AX.X, op=ALU.add),
                         reads=[K("sqq")], writes=[K("nrm")])
                    S.op("dve", lambda e, b=b: e.tensor_scalar(out=nrm[b][:, 0:8], in0=nrm[b][:, 0:8],
                                                                scalar1=1.0e-6, scalar2=None,
                                                                op0=ALU.add),
                         reads=[K("nrm")], writes=[K("nrm")])
                    S.op("act", lambda e, b=b: e.sqrt(out=nrm[b][:, 0:8], in_=nrm[b][:, 0:8]),
                         reads=[K("nrm")], writes=[K("nrm")])
                    S.op("dve", lambda e, b=b: e.reciprocal(out=nrm[b][:, 8:16], in_=nrm[b][:, 0:8]),
                         reads=[K("nrm")], writes=[K("rn")])
                    S.op("dve", lambda e, b=b: e.tensor_scalar(out=nrm[b][:, 8:12],
                                                                in0=nrm[b][:, 8:12],
                                                                scalar1=128.0 ** -0.5, scalar2=None,
                                                                op0=ALU.mult),
                         reads=[K("rn")], writes=[K("rn")])
                    S.op("dve", lambda e, b=b: e.tensor_tensor(
                        out=QK[b][:], in0=QK[b][:],
                        in1=nrm[b][:, 8:16].unsqueeze(2).to_broadcast([64, 8, 128]), op=ALU.mult),
                        reads=[K("QK0"), K("QK1"), K("rn")], writes=[K("QKn")])
                    for h in range(4):
                        for dst_, src_, col, kd_, kr_ in ((qdk[b], QK[b][:, h, :], 8, "qdk", "eG"),
                                                       (kd[b], QK[b][:, 4 + h, :], 12, "kd", "eG"),
                                                       (bk[b], QK[b][:, 4 + h, :], 16, "bk", "beG"),
                                                       (bv[b], Vt[b][:, h, :], 0, "bv", "beta")):
                            S.op("dve", lambda e, dst_=dst_, src_=src_, col=col, h=h, G=G:
                                 e.tensor_scalar(out=dst_[:, h, :], in0=src_,
                                                 scalar1=G[:, col + h:col + h + 1], scalar2=None,
                                                 op0=ALU.mult),
                                 reads=[K("QKn"), K("QK2"), K(kr_)], writes=[(K(kd_), h)])
                    for grp, (src, ksrc) in enumerate(((qdk[b][:], [*[(K("qdk"), h_) for h_ in range(4)]]),
                                                       (QK[b][:, 4:8, :], [K("QKn")]),
                                                       (QK[b][:, 0:4, :], [K("QKn")]))):
                        pt, kpt = self.ps8[pb(grp)], ("ps", pb(grp))

                        def trt(e, src=src, pt=pt):
                            ins = None
                            for h in range(4):
                                ins = e.transpose(out=pt[:, h * 64:(h + 1) * 64], in_=src[:, h, :],
                                                  identity=self.ident_f[:64, :64])
                            return ins
                        S.op("pe", trt, reads=ksrc + ["ident_f"], writes=[kpt])
                        self.evac(grp, TT[b][:, grp * 4:(grp + 1) * 4, :],
                                  pt[:, :256].rearrange("p (h t) -> p h t", h=4), [kpt],
                                  [K("TT%d" % grp)])
                    pk, kpk = self.ps8[pb(3)], ("ps", pb(3))

                    def mmk(e, b=b, pk=pk):
                        ins = None
                        for h in range(4):
                            e.matmul(pk[:64, h * 64:(h + 1) * 64], lhsT=TT[b][:, 4 + h, :],
                                     rhs=TT[b][:, 4 + h, :], start=True, stop=True)
                            ins = e.matmul(pk[:64, 256 + h * 64:256 + (h + 1) * 64],
                                           lhsT=TT[b][:, 8 + h, :], rhs=TT[b][:, 4 + h, :],
                                           start=True, stop=True)
                        return ins
                    S.op("pe", mmk, reads=[K("TT1"), K("TT2")], writes=[kpk])
                    S.op("dve", lambda e, b=b, pk=pk: e.tensor_tensor(
                        out=Mm[b][:], in0=v4(pk, 64), in1=dec[b][:], op=ALU.mult),
                        reads=[kpk, K("dec")], writes=[K("M")])
                    S.op("dve", lambda e, b=b, pk=pk: e.tensor_tensor(
                        out=QKm[b][:], in0=pk[:64, 256:512].rearrange("p (h x) -> p h x", h=4),
                        in1=dec[b][:], op=ALU.mult), reads=[kpk, K("dec")], writes=[K("QKm")])
                    S.op("pool", lambda e, b=b: e.tensor_tensor(
                        out=Mm[b][:], in0=Mm[b][:],
                        in1=Lst[:].unsqueeze(1).to_broadcast([64, 4, 64]), op=ALU.mult),
                        reads=[K("M"), "D_Lst"], writes=[K("M")])
                    S.op("dve", lambda e, b=b, G=G: e.tensor_tensor(
                        out=Mm[b][:], in0=Mm[b][:], in1=bc(G[:, 0:4], 64), op=ALU.mult),
                        reads=[K("M"), K("beta")], writes=[K("M")])
                    pn, kpn = self.ps8[pb(0)], ("ps", pb(0))

                    def trn(e, b=b, pn=pn):
                        ins = None
                        for h in range(4):
                            e.transpose(out=pn[:64, h * 64:(h + 1) * 64], in_=Mm[b][:, h, :],
                                        identity=self.ident_f[:64, :64])
                            ins = e.transpose(out=pn[:64, 256 + h * 64:256 + (h + 1) * 64],
                                              in_=QKm[b][:, h, :], identity=self.ident_f[:64, :64])
                        return ins
                    S.op("pe", trn, reads=[K("M"), K("QKm"), "ident_f"], writes=[kpn])
                    S.op("act", lambda e, b=b, pn=pn: e.copy(out=Qa[b][:], in_=v4(pn, 64)),
                         reads=[kpn], writes=[K("Qa")])
                    S.op("act", lambda e, b=b, pn=pn: e.copy(
                        out=QKT[b][:], in_=pn[:64, 256:512].rearrange("p (h x) -> p h x", h=4)),
                        reads=[kpn], writes=[K("QKT")])
                    S.op("dve", lambda e, b=b: e.tensor_tensor(out=Bt[b][:], in0=identI[:],
                                                                in1=Qa[b][:], op=ALU.subtract),
                         reads=[K("Qa"), "D_I"], writes=[K("Bt")])
                    Q, Qt, kQ, kQt = Qa[b], Mm[b], K("Qa"), K("M")
                    alt = [(Qb[b], Qtb[b], K("Qb"), K("Qtb")), (Qa[b], Qta[b], K("Qa"), K("Qta"))]
                    for step in range(5):
                        Q2, Qt2, kQ2, kQt2 = alt[step % 2]
                        p1, kp1 = self.ps8[pb(1)], ("ps", pb(1))

                        def mq(e, Q=Q, Qt=Qt, p1=p1, step=step):
                            ins = None
                            for h in range(4):
                                ins = e.matmul(p1[:64, h * 64:(h + 1) * 64], lhsT=Q[:, h, :],
                                               rhs=Qt[:, h, :], start=True, stop=True)
                                if step < 4:
                                    ins = e.matmul(p1[:64, 256 + h * 64:256 + (h + 1) * 64],
                                                   lhsT=Qt[:, h, :], rhs=Q[:, h, :], start=True,
                                                   stop=True)
                            return ins
                        S.op("pe", mq, reads=[kQ, kQt], writes=[kp1])
                        S.op("act", lambda e, Qt2=Qt2, p1=p1: e.copy(out=Qt2[:], in_=v4(p1, 64)),
                             reads=[kp1], writes=[kQt2])
                        if step < 4:
                            S.op("act", lambda e, Q2=Q2, p1=p1: e.copy(
                                out=Q2[:], in_=p1[:64, 256:512].rearrange("p (h x) -> p h x", h=4)),
                                reads=[kp1], writes=[kQ2])
                        p2, kp2 = self.ps8[pb(2)], ("ps", pb(2))

                        def mbm(e, Qt2=Qt2, b=b, p2=p2):
                            ins = None
                            for h in range(4):
                                ins = e.matmul(p2[:64, h * 64:(h + 1) * 64], lhsT=Qt2[:, h, :],
                                               rhs=Bt[b][:, h, :], start=True, stop=True)
                            return ins
                        S.op("pe", mbm, reads=[kQt2, K("Bt")], writes=[kp2])
                        S.op("dve", lambda e, b=b, p2=p2: e.tensor_tensor(out=Bt[b][:], in0=Bt[b][:],
                                                                           in1=v4(p2, 64),
                                                                           op=ALU.add),
                             reads=[kp2, K("Bt")], writes=[K("Bt")])
                        Q, Qt, kQ, kQt = Q2, Qt2, kQ2, kQt2
                    S.op("act", lambda e, b=b: e.copy(out=Tb[b][:], in_=Bt[b][:]), reads=[K("Bt")],
                         writes=[K("Tb")])
                    pu0, kpu0 = self.ps8[pb(3)], ("ps", pb(3))

                    def mu0(e, b=b, pu0=pu0):
                        ins = None
                        for h in range(4):
                            ins = e.matmul(pu0[:64, h * 128:(h + 1) * 128], lhsT=Tb[b][:, h, :],
                                           rhs=bv[b][:, h, :], start=True, stop=True)
                        return ins
                    S.op("pe", mu0, reads=[K("Tb"), *[(K("bv"), h_) for h_ in range(4)]], writes=[kpu0])
                    S.op("act", lambda e, b=b, pu0=pu0: e.copy(out=u0[b][:], in_=v4(pu0, 128)),
                         reads=[kpu0], writes=[K("u0")])
                    pw, kpw = self.ps8[pb(4)], ("ps", pb(4))

                    def mwk(e, b=b, pw=pw):
                        ins = None
                        for h in range(4):
                            ins = e.matmul(pw[:, h * 64:(h + 1) * 64], lhsT=bk[b][:, h, :],
                                           rhs=Tb[b][:, h, :], start=True, stop=True)
                        return ins
                    S.op("pe", mwk, reads=[K("Tb"), *[(K("bk"), h_) for h_ in range(4)]], writes=[kpw])
                    S.op("act", lambda e, b=b, pw=pw: e.copy(
                        out=wkT[b][:], in_=pw[:, :256].rearrange("p (h t) -> p h t", h=4)),
                        reads=[kpw], writes=[K("wkT")])
                    pu, kpu = self.ps8[pb(5)], ("ps", pb(5))

                    def mpu(e, b=b, pu=pu, s=s):
                        ins = None
                        for h in range(4):
                            ins = e.matmul(pu[:64, h * 128:(h + 1) * 128], lhsT=wkT[b][:, h, :],
                                           rhs=Sb[s][:, h, :], start=True, stop=True)
                        return ins
                    S.op("pe", mpu, reads=[K("wkT"), ("D_Sb", s)], writes=[kpu])
                    S.op("dve", lambda e, b=b, pu=pu: e.tensor_tensor(out=ub[b][:], in0=u0[b][:],
                                                                       in1=v4(pu, 128),
                                                                       op=ALU.subtract),
                         reads=[K("u0"), kpu], writes=[K("ub")])
                    po, kpo = self.ps8[pb(0)], ("ps", pb(0))

                    def mpo(e, b=b, po=po, s=s):
                        ins = None
                        for h in range(4):
                            e.matmul(po[:64, h * 128:(h + 1) * 128], lhsT=TT[b][:, h, :],
                                     rhs=Sb[s][:, h, :], start=True, stop=False)
                            ins = e.matmul(po[:64, h * 128:(h + 1) * 128], lhsT=QKT[b][:, h, :],
                                           rhs=ub[b][:, h, :], start=False, stop=True)
                        return ins
                    S.op("pe", mpo, reads=[K("TT0"), ("D_Sb", s), K("QKT"), K("ub")], writes=[kpo])
                    psn, kpsn = self.ps8[pb(1)], ("ps", pb(1))

                    def mps(e, b=b, psn=psn):
                        ins = None
                        for h in range(4):
                            ins = e.matmul(psn[:, h * 128:(h + 1) * 128], lhsT=kd[b][:, h, :],
                                           rhs=ub[b][:, h, :], start=True, stop=True)
                        return ins
                    S.op("pe", mps, reads=[*[(K("kd"), h_) for h_ in range(4)], K("ub")], writes=[kpsn])
                    for h in range(4):
                        S.op("dve", lambda e, b=b, s=s, h=h, psn=psn: e.scalar_tensor_tensor(
                            out=St[s][:, h, :], in0=St[s][:, h, :], scalar=glb[b][:, h:h + 1],
                            in1=psn[:, h * 128:(h + 1) * 128], op0=ALU.mult, op1=ALU.add),
                            reads=[("D_S", s), K("glb"), kpsn], writes=[("D_S", s)])
                    S.op("act", lambda e, s=s: e.copy(out=Sb[s][:], in_=St[s][:]),
                         reads=[("D_S", s)], writes=[("D_Sb", s)])
                    S.op("act", lambda e, b=b: e.activation(out=gs[b][:], in_=zt[b][:], func=AF.Silu),
                         reads=[K("zt")], writes=[K("gs")])
                    S.op("pool", lambda e, b=b: e.tensor_tensor(
                        out=gs[b][:], in0=gs[b][:],
                        in1=ng[:].unsqueeze(1).to_broadcast([64, 4, 128]), op=ALU.mult),
                        reads=[K("gs"), "D_ng"], writes=[K("gs")])
                    S.op("act", lambda e, b=b, po=po: e.copy(out=osb[b][:], in_=v4(po, 128)),
                         reads=[kpo], writes=[K("osb")])
                    S.op("pool", lambda e, b=b: e.tensor_tensor(out=osq[b][:], in0=osb[b][:],
                                                                 in1=osb[b][:], op=ALU.mult),
                         reads=[K("osb")], writes=[K("osq")])
                    S.op("dve", lambda e, b=b: e.tensor_reduce(out=nrm[b][:, 0:4], in_=osq[b][:],
                                                                axis=AX.X, op=ALU.add),
                         reads=[K("osq")], writes=[K("oss")])
                    self.rstd_from_ss(nrm[b][:, 0:4], K("oss"), nrm[b][:, 4:8], K("ors"), 128)
                    S.op("dve", lambda e, b=b: e.tensor_tensor(out=osb[b][:], in0=osb[b][:],
                                                                in1=bc(nrm[b][:, 4:8], 128),
                                                                op=ALU.mult),
                         reads=[K("osb"), K("ors")], writes=[K("osb")])
                    S.op("pool", lambda e, b=b: e.tensor_tensor(out=osq[b][:], in0=osb[b][:],
                                                                 in1=gs[b][:], op=ALU.mult),
                         reads=[K("osb"), K("gs")], writes=[K("osq")])
                    S.dma(out=self.mixin[tok:tok + 64, 0:512],
                          in_=osq[b][:].rearrange("p h v -> p (h v)"), sem="D_out%d" % b,
                          reads=[K("osq")])
        S.interleave([lambda s=s: seq_thread(s) for s in range(NSEQ)])
        S.barrier()


def build_program():
    P = Prog()
    src = P.x_in
    for l in range(DEPTH):
        dst = P.xmid if l < DEPTH - 1 else P.y
        P.phase_A(l, src)
        P.phase_D(l)
        P.phase_S(l)
        P.phase_H(l)
        P.phase_E(l, src)
        P.phase_F(l, dst)
        src = dst
    return P


def kernel(**inputs):
    n = 8
    P = build_program()
    x = np.ascontiguousarray(inputs["x"], dtype=np.float32)
    shared = {k: np.ascontiguousarray(v, dtype=np.float32) for k, v in inputs.items() if k != "x"}
    in_maps = []
    for c in range(n):
        m = dict(shared)
        m["x"] = np.ascontiguousarray(x[c * NSEQ:(c + 1) * NSEQ].reshape(NTOK, D))
        in_maps.append(m)
    res = run_bass_kernel_spmd(P.nc, in_maps, core_ids=list(range(n)))
    out = np.stack([np.asarray(r["y"]).reshape(NSEQ, SEQ, D) for r in res.results], axis=0)
    return out.reshape(n * NSEQ, SEQ, D).astype(np.float32)
```

```python
import numpy as np
import concourse.bass as bass
import concourse.mybir as mybir
from concourse.bass_utils import run_bass_kernel_spmd

F32 = mybir.dt.float32
BF16 = mybir.dt.bfloat16
AF = mybir.ActivationFunctionType
ALU = mybir.AluOpType
AX = mybir.AxisListType

D = 1024
SEQ = 4096
NSEQ = 2
NTOK = NSEQ * SEQ
DEPTH = 2
D_IN = 3788
D_FF = 2816
EPS = 1e-6
NEG = -30000.0

TM_GROUPS = [(1536, 2056), (2376, 2440), (2760, 2764), (3020, 3788)]
TM_W = sum(b - a for a, b in TM_GROUPS)
TM_AZ, TM_AB, TM_AA = 0, 512, 516
TM_BV = 520
TM_WI = 584
TM_CF, TM_CI, TM_CG = 588, 844, 1100
FM_GROUPS = [(i * 128, 128) for i in range(12)] + [(2056, 128), (2184, 128), (2312, 64),
             (2440, 128), (2568, 128), (2696, 64), (2764, 128), (2892, 128), (3020, 128), (3148, 128)]
FM_ROW = {}
_r = 0
for _c, _n in FM_GROUPS:
    FM_ROW[_c] = _r
    _r += _n
FM_H = _r


class Sched:
    def __init__(self, nc):
        self.nc = nc
        self.eng = {"pe": nc.tensor, "act": nc.scalar, "dve": nc.vector, "pool": nc.gpsimd,
                    "sp": nc.sync}
        self.sems = {}
        self.cnt = {}
        for k in ("pe", "act", "dve", "pool"):
            self.sems[k] = nc.alloc_semaphore("s_" + k)
            self.cnt[k] = 0
        self.seen = {k: {} for k in self.eng}
        self.bufs = {}
        self.ninstr = 0

    def _buf(self, key):
        b = self.bufs.get(key)
        if b is None:
            b = {"w": None, "r": {}}
            self.bufs[key] = b
        return b

    def _deps(self, engine, reads, writes):
        deps = {}

        def add(ev, same_ok):
            if ev is None:
                return
            sk, val = ev
            if sk == engine and not same_ok:
                return
            if deps.get(sk, 0) < val:
                deps[sk] = val

        for k in reads:
            b = self._buf(k)
            add(b["w"], engine != "pe")
            if isinstance(k, tuple) and k[0] in ("ps", "psb"):
                for sk, val in b["r"].items():
                    add((sk, val), False)
        for k in writes:
            b = self._buf(k)
            add(b["w"], engine != "pe")
            for sk, val in b["r"].items():
                add((sk, val), False)
        return deps

    def _emit_waits(self, engine, deps):
        e = self.eng[engine]
        seen = self.seen[engine]
        for sk, val in deps.items():
            if seen.get(sk, 0) >= val:
                continue
            e.wait_ge(self.sems[sk], val)
            self.ninstr += 1
            seen[sk] = val

    def _record(self, ev, reads, writes):
        for k in writes:
            b = self._buf(k)
            b["w"] = ev
            b["r"] = {}
        for k in reads:
            b = self._buf(k)
            if b["r"].get(ev[0], 0) < ev[1]:
                b["r"][ev[0]] = ev[1]

    def op(self, engine, fn, reads=(), writes=()):
        deps = self._deps(engine, reads, writes)
        self._emit_waits(engine, deps)
        ins = fn(self.eng[engine])
        self.cnt[engine] += 1
        ins.then_inc(self.sems[engine], 1)
        self.ninstr += 1
        self._record((engine, self.cnt[engine]), reads, writes)
        self._yield()

    def dma(self, out, in_, sem, reads=(), writes=(), q="sp"):
        if sem not in self.sems:
            self.sems[sem] = self.nc.alloc_semaphore("d_" + sem)
            self.cnt[sem] = 0
        deps = self._deps(q, reads, writes)
        self._emit_waits(q, deps)
        ins = self.eng[q].dma_start(out=out, in_=in_)
        self.cnt[sem] += 16
        ins.then_inc(self.sems[sem], 16)
        self.ninstr += 1
        self._record((sem, self.cnt[sem]), reads, writes)
        self._yield()

    def interleave(self, fns):
        import threading
        n = len(fns)
        st = {"cur": 0, "alive": [True] * n, "err": None}
        cond = threading.Condition()

        def nxt(i):
            for d in range(1, n + 1):
                k = (i + d) % n
                if st["alive"][k]:
                    return k
            return -1

        def pass_turn(me):
            with cond:
                st["cur"] = nxt(me)
                cond.notify_all()
                while st["alive"][me] and st["cur"] != me and st["err"] is None:
                    cond.wait()
                if st["err"] is not None and st["alive"][me]:
                    raise RuntimeError("interleave aborted")

        def worker(i):
            with cond:
                while st["cur"] != i and st["err"] is None:
                    cond.wait()
            try:
                if st["err"] is None:
                    self._tl.me = i
                    fns[i]()
            except BaseException as e:
                if st["err"] is None:
                    st["err"] = e
            finally:
                with cond:
                    st["alive"][i] = False
                    if st["cur"] == i:
                        st["cur"] = nxt(i)
                    cond.notify_all()

        self._tl = threading.local()
        self._pass = pass_turn
        ths = [threading.Thread(target=worker, args=(i,)) for i in range(n)]
        for t in ths:
            t.start()
        for t in ths:
            t.join()
        self._pass = None
        if st["err"] is not None:
            raise st["err"]

    def _yield(self):
        p = getattr(self, "_pass", None)
        if p is not None:
            p(self._tl.me)

    def barrier(self):
        allv = {k: v for k, v in self.cnt.items() if v > 0}
        for engine in self.eng:
            self._emit_waits(engine, dict(allv))
        self.bufs = {}


def bcast_rows(ap2d_row, nparts):
    return ap2d_row.partition_broadcast(nparts)


class Prog:
    def __init__(self, layers=(0, 1), phases="AHDSEF", dbg=()):
        self.nc = nc = bass.Bass("TRN2", target_bir_lowering=False)
        self.S = Sched(nc)
        self.dbg = dbg
        dt = nc.dram_tensor
        self.x_in = dt("x", [NTOK, D], F32, kind="ExternalInput").ap()
        self.w_in = dt("w_in", [DEPTH, D, D_IN], F32, kind="ExternalInput").ap()
        self.dn_conv = dt("dn_conv", [DEPTH, 4, 1536], F32, kind="ExternalInput").ap()
        self.dn_a_log = dt("dn_a_log", [DEPTH, 4], F32, kind="ExternalInput").ap()
        self.dn_dt_bias = dt("dn_dt_bias", [DEPTH, 4], F32, kind="ExternalInput").ap()
        self.dn_norm = dt("dn_norm", [DEPTH, 128], F32, kind="ExternalInput").ap()
        self.hg_lb = dt("hg_lb", [DEPTH, 256], F32, kind="ExternalInput").ap()
        self.hg_norm = dt("hg_norm", [DEPTH, 64], F32, kind="ExternalInput").ap()
        self.w_out = dt("w_out", [DEPTH, D, D], F32, kind="ExternalInput").ap()
        self.g_mix_pre = dt("g_mix_pre", [DEPTH, D], F32, kind="ExternalInput").ap()
        self.g_mix_post = dt("g_mix_post", [DEPTH, D], F32, kind="ExternalInput").ap()
        self.g_ffn_pre = dt("g_ffn_pre", [DEPTH, D], F32, kind="ExternalInput").ap()
        self.g_ffn_post = dt("g_ffn_post", [DEPTH, D], F32, kind="ExternalInput").ap()
        self.w_up = dt("ffn_w_up", [DEPTH, D, 2 * D_FF], F32, kind="ExternalInput").ap()
        self.ffn_conv = dt("ffn_conv", [DEPTH, 3, 2 * D_FF], F32, kind="ExternalInput").ap()
        self.w_down = dt("ffn_w_down", [DEPTH, D_FF, D], F32, kind="ExternalInput").ap()
        self.y = dt("y", [NTOK, D], F32, kind="ExternalOutput").ap()

        def scratch(name, shape, dtype):
            kind = "ExternalOutput" if name in dbg else "Internal"
            return dt(name, shape, dtype, kind=kind).ap()

        self.ptok = scratch("ptok", [NTOK, TM_W], F32)
        self.pfeat = scratch("pfeat", [FM_H, NTOK], F32)
        self.mixin = scratch("mixin", [NTOK, D], F32)
        self.x1 = scratch("x1", [NTOK, D], F32)
        self.h2T = scratch("h2T", [D, NTOK], BF16)
        self.xmid = scratch("xmid", [NTOK, D], F32)

        self.ps = [nc.alloc_psum_tensor("ps%d" % i, [128, 512], F32) for i in range(6)]
        self.psb = [nc.alloc_psum_tensor("psb%d" % i, [128, 1024], BF16) for i in range(2)]
        self.ps8 = [p[:, :] for p in self.ps] + [p[:, :].bitcast(F32) for p in self.psb]
        self.ident_b = nc.alloc_sbuf_tensor("ident_b", [128, 128], BF16)
        self.ident_f = nc.alloc_sbuf_tensor("ident_f", [128, 128], F32)
        self.ones_f = nc.alloc_sbuf_tensor("ones_f", [128, 128], F32)
        self._consts()
        self.sb_base = nc.sbuf_base
        self.layers = layers
        self.phases = phases

    def _consts(self):
        nc, S = self.nc, self.S
        S.op("pool", lambda e: e.memset(self.ones_f[:], 1.0), writes=["ones_f"])
        S.op("pool", lambda e: e.memset(self.ident_f[:], 0.0), writes=["ident_f"])
        S.op("pool", lambda e: e.affine_select(out=self.ident_f[:], in_=self.ident_f[:],
                                                pattern=[[-1, 128]], compare_op=ALU.not_equal,
                                                fill=1.0, base=0, channel_multiplier=1),
             reads=["ident_f"], writes=["ident_f"])
        S.op("dve", lambda e: e.tensor_copy(out=self.ident_b[:], in_=self.ident_f[:]),
             reads=["ident_f"], writes=["ident_b"])

    def sbuf_reset(self):
        self.nc.sbuf_base = self.sb_base

    def sb(self, name, shape, dtype):
        self._uid = getattr(self, "_uid", 0) + 1
        return self.nc.alloc_sbuf_tensor("%s_u%d" % (name, self._uid), shape, dtype)

    def load_bcast(self, name, row_ap, n):
        t = self.sb(name, [128, n], F32)
        self.S.dma(out=t[:], in_=bcast_rows(row_ap, 128), sem="ld_" + name, writes=[name])
        return t

    def load_weight_bf16(self, name, w_ap, K, N, stage, stage_keys):
        S = self.S
        wt = self.sb(name, [128, K, N], BF16)
        CH = stage[0].shape[1]
        i = 0
        for k in range(K):
            for c0 in range(0, N, CH):
                cw = min(CH, N - c0)
                st, sk = stage[i % 2], stage_keys[i % 2]
                S.dma(out=st[:, :cw], in_=w_ap[k * 128:(k + 1) * 128, c0:c0 + cw], sem=sk,
                      writes=[sk])
                eng = ("dve", "pool", "act")[i % 3]
                if eng == "act":
                    S.op("act", lambda e, st=st, k=k, c0=c0, cw=cw: e.copy(
                        out=wt[:, k, c0:c0 + cw], in_=st[:, :cw]), reads=[sk], writes=[name])
                else:
                    S.op(eng, lambda e, st=st, k=k, c0=c0, cw=cw: e.tensor_copy(
                        out=wt[:, k, c0:c0 + cw], in_=st[:, :cw]), reads=[sk], writes=[name])
                i += 1
        return wt

    def evac(self, i, out, in_, reads, writes):
        if i % 2 == 0:
            self.S.op("act", lambda e: e.copy(out=out, in_=in_), reads=reads, writes=writes)
        else:
            self.S.op("dve", lambda e: e.tensor_copy(out=out, in_=in_), reads=reads, writes=writes)

    def norm_transpose(self, src_dram, tok0, gbc, gkey, T, tag, xt, hb, hT, ss, rs, slot,
                       do_norm=True):
        S = self.S
        kx = lambda j: (tag + "xt", slot, j)
        for j in range(4):
            S.dma(out=xt[slot][:, j, :], in_=src_dram[tok0 + j * 128: tok0 + (j + 1) * 128, :],
                  sem="%sxt%d_%d" % (tag, slot, j), writes=[kx(j)])
        kss, krs = (tag + "ss", slot), (tag + "rs", slot)
        if do_norm:
            for j in range(4):
                S.op("act", lambda e, j=j: e.activation(out=T["junk"][:], in_=xt[slot][:, j, :],
                                                         func=AF.Square,
                                                         accum_out=ss[slot][:, j:j + 1]),
                     reads=[kx(j)], writes=[kss])
            S.op("dve", lambda e: e.tensor_scalar(out=rs[slot][:], in0=ss[slot][:],
                                                   scalar1=1.0 / D, scalar2=EPS, op0=ALU.mult,
                                                   op1=ALU.add), reads=[kss], writes=[krs])
            S.op("act", lambda e: e.sqrt(out=rs[slot][:], in_=rs[slot][:]), reads=[krs],
                 writes=[krs])
            S.op("dve", lambda e: e.reciprocal(out=rs[slot][:], in_=rs[slot][:]), reads=[krs],
                 writes=[krs])
        for j in range(4):
            khb = (tag + "hb", j % 2)
            hbj = hb[j % 2]
            if do_norm:
                S.op("dve", lambda e, j=j, hbj=hbj: e.scalar_tensor_tensor(
                    out=hbj[:], in0=xt[slot][:, j, :], scalar=rs[slot][:, j:j + 1], in1=gbc[:],
                    op0=ALU.mult, op1=ALU.mult), reads=[kx(j), krs, gkey], writes=[khb])
            else:
                S.op("pool", lambda e, j=j, hbj=hbj: e.tensor_copy(out=hbj[:],
                                                                  in_=xt[slot][:, j, :]),
                     reads=[kx(j)], writes=[khb])
            pb = self.psb[j % 2]
            kpb = ("psb", j % 2)

            def tr(e, hbj=hbj, pb=pb):
                ins = None
                for k in range(8):
                    ins = e.transpose(out=pb[:, k * 128:(k + 1) * 128],
                                      in_=hbj[:, k * 128:(k + 1) * 128], identity=self.ident_b[:])
                return ins
            S.op("pe", tr, reads=[khb, "ident_b"], writes=[kpb])
            self.evac(j, hT[slot][:, :, j * 128:(j + 1) * 128],
                      pb[:].rearrange("p (k t) -> p k t", k=8), [kpb], [(tag + "hT", slot, j)])

    def phase_A(self, l, xsrc):
        nc, S = self.nc, self.S
        self.sbuf_reset()
        stage = [self.sb("A_stage%d" % i, [128, 3788], F32) for i in range(2)]
        Wi = self.load_weight_bf16("A_Wi", self.w_in[l], 8, D_IN, stage, ["A_stg0", "A_stg1"])
        gbc = self.load_bcast("A_gbc", self.g_mix_pre[l:l + 1, :], D)
        S.barrier()
        xt = [self.sb("A_xt%d" % i, [128, 4, D], F32) for i in range(2)]
        hb = [self.sb("A_hb%d" % i, [128, D], BF16) for i in range(2)]
        hT = [self.sb("A_hT%d" % i, [128, 8, 512], BF16) for i in range(2)]
        ss = [self.sb("A_ss%d" % i, [128, 4], F32) for i in range(2)]
        rs = [self.sb("A_rs%d" % i, [128, 4], F32) for i in range(2)]
        T = {"junk": self.sb("A_junk", [128, D], F32)}
        ofm = [self.sb("A_ofm%d" % i, [128, 512], F32) for i in range(4)]
        otm = [self.sb("A_otm%d" % i, [128, TM_W], F32) for i in range(2)]
        tmch = []
        off = 0
        for a, b in TM_GROUPS:
            c = a
            while c < b:
                w = min(512, b - c)
                tmch.append((c, w, off))
                off += w
                c += w
        nev = 0
        for blk in range(NTOK // 512):
            slot = blk % 2
            tok0 = blk * 512
            self.norm_transpose(xsrc, tok0, gbc, "A_gbc", T, "A_", xt, hb, hT, ss, rs, slot)
            hkeys = [("A_hT", slot, j) for j in range(4)]
            for gi, (c0, n) in enumerate(FM_GROUPS):
                ps = self.ps[gi % 4]
                kps = ("ps", gi % 4)

                def mm(e, ps=ps, c0=c0, n=n):
                    ins = None
                    for k in range(8):
                        ins = e.matmul(ps[:n, :], lhsT=Wi[:, k, c0:c0 + n], rhs=hT[slot][:, k, :],
                                       start=(k == 0), stop=(k == 7))
                    return ins
                S.op("pe", mm, reads=["A_Wi"] + hkeys, writes=[kps])
                o = ofm[gi % 4]
                ko = ("A_ofm", gi % 4)
                self.evac(nev, o[:n, :], ps[:n, :], [kps], [ko])
                nev += 1
                r0 = FM_ROW[c0]
                S.dma(out=self.pfeat[r0:r0 + n, tok0:tok0 + 512], in_=o[:n, :],
                      sem="A_ofm%d" % (gi % 4), reads=[ko], q="pool")
            for j in range(4):
                o = otm[j % 2]
                ko = ("A_otm", j % 2)
                for ci, (c, w, dst) in enumerate(tmch):
                    ps = self.ps[4 + ci % 2]
                    kps = ("ps", 4 + ci % 2)

                    def mm(e, ps=ps, c=c, w=w, j=j):
                        ins = None
                        for k in range(8):
                            ins = e.matmul(ps[:, :w], lhsT=hT[slot][:, k, j * 128:(j + 1) * 128],
                                           rhs=Wi[:, k, c:c + w], start=(k == 0), stop=(k == 7))
                        return ins
                    S.op("pe", mm, reads=["A_Wi", hkeys[j]], writes=[kps])
                    self.evac(nev, o[:, dst:dst + w], ps[:, :w], [kps], [(ko, ci)])
                    nev += 1
                S.dma(out=self.ptok[tok0 + j * 128: tok0 + (j + 1) * 128, :], in_=o[:, :],
                      sem="A_otm%d" % (j % 2), reads=[(ko, ci) for ci in range(len(tmch))],
                      q="pool")
        S.barrier()

    def transp8(self, hbj, khb, dst, kdst, j):
        pb = self.psb[j % 2]
        kpb = ("psb", j % 2)

        def tr(e):
            ins = None
            for k in range(8):
                ins = e.transpose(out=pb[:, k * 128:(k + 1) * 128],
                                  in_=hbj[:, k * 128:(k + 1) * 128], identity=self.ident_b[:])
            return ins
        self.S.op("pe", tr, reads=[khb, "ident_b"], writes=[kpb])
        self.evac(j, dst, pb[:].rearrange("p (k t) -> p k t", k=8), [kpb], [kdst])

    def rstd_from_ss(self, ss, kss, rs, krs, n):
        S = self.S
        S.op("dve", lambda e: e.tensor_scalar(out=rs, in0=ss, scalar1=1.0 / n, scalar2=EPS,
                                               op0=ALU.mult, op1=ALU.add), reads=[kss],
             writes=[krs])
        S.op("act", lambda e: e.sqrt(out=rs, in_=rs), reads=[krs], writes=[krs])
        S.op("dve", lambda e: e.reciprocal(out=rs, in_=rs), reads=[krs], writes=[krs])

    def phase_E(self, l, xsrc):
        S = self.S
        self.sbuf_reset()
        stage = [self.sb("E_stage%d" % i, [128, 1024], F32) for i in range(2)]
        Wo = self.load_weight_bf16("E_Wo", self.w_out[l], 8, D, stage, ["E_stg0", "E_stg1"])
        gpost = self.load_bcast("E_gpost", self.g_mix_post[l:l + 1, :], D)
        gpre = self.load_bcast("E_gpre", self.g_ffn_pre[l:l + 1, :], D)
        S.barrier()
        xt = [self.sb("E_xt%d" % i, [128, 4, D], F32) for i in range(2)]
        xr = [self.sb("E_xr%d" % i, [128, 4, D], F32) for i in range(2)]
        hb = [self.sb("E_hb%d" % i, [128, D], BF16) for i in range(2)]
        mT = [self.sb("E_mT%d" % i, [128, 8, 512], BF16) for i in range(2)]
        h2s = [self.sb("E_h2s%d" % i, [128, 8, 512], BF16) for i in range(2)]
        yt = [self.sb("E_yt%d" % i, [128, D], F32) for i in range(2)]
        x1t = [self.sb("E_x1t%d" % i, [128, D], F32) for i in range(2)]
        h2b = [self.sb("E_h2b%d" % i, [128, D], BF16) for i in range(2)]
        junk = self.sb("E_junk", [128, D], F32)
        st = [self.sb("E_st%d" % i, [128, 8], F32) for i in range(2)]
        h2T_v = self.h2T.rearrange("(k p) t -> p k t", p=128)
        for blk in range(NTOK // 512):
            slot = blk % 2
            tok0 = blk * 512
            self.norm_transpose(self.mixin, tok0, None, None, None, "E_", xt, hb, mT, None, None,
                                slot, do_norm=False)
            for j in range(4):
                S.dma(out=xr[slot][:, j, :], in_=xsrc[tok0 + j * 128: tok0 + (j + 1) * 128, :],
                      sem="E_xr%d_%d" % (slot, j), writes=[("E_xr", slot, j)])
            for j in range(4):
                p2 = j % 2
                kss, krs = ("E_ss", p2), ("E_rs", p2)
                for hf in range(2):
                    ps = self.ps[2 * p2 + hf]
                    kps = ("ps", 2 * p2 + hf)

                    def mm(e, ps=ps, hf=hf, j=j):
                        ins = None
                        for k in range(8):
                            ins = e.matmul(ps[:, :], lhsT=mT[slot][:, k, j * 128:(j + 1) * 128],
                                           rhs=Wo[:, k, hf * 512:(hf + 1) * 512], start=(k == 0),
                                           stop=(k == 7))
                        return ins
                    S.op("pe", mm, reads=["E_Wo", ("E_hT", slot, j)], writes=[kps])
                    S.op("act", lambda e, ps=ps, hf=hf, p2=p2: e.activation(
                        out=junk[:, :512], in_=ps[:, :], func=AF.Square,
                        accum_out=st[p2][:, hf:hf + 1]), reads=[kps], writes=[(kss, hf)])
                S.op("dve", lambda e, p2=p2: e.tensor_tensor(out=st[p2][:, 2:3], in0=st[p2][:, 0:1],
                                                              in1=st[p2][:, 1:2], op=ALU.add),
                     reads=[(kss, 0), (kss, 1)], writes=[kss])
                self.rstd_from_ss(st[p2][:, 2:3], kss, st[p2][:, 3:4], krs, D)
                kyt = ("E_yt", p2)
                for hf in range(2):
                    ps = self.ps[2 * p2 + hf]
                    kps = ("ps", 2 * p2 + hf)
                    S.op("act", lambda e, ps=ps, hf=hf, p2=p2: e.activation(
                        out=yt[p2][:, hf * 512:(hf + 1) * 512], in_=ps[:, :], func=AF.Copy,
                        scale=st[p2][:, 3:4]), reads=[kps, krs], writes=[(kyt, hf)])
                S.op("dve", lambda e, p2=p2: e.tensor_tensor(out=yt[p2][:], in0=yt[p2][:],
                                                              in1=gpost[:], op=ALU.mult),
                     reads=[(kyt, 0), (kyt, 1), "E_gpost"], writes=[kyt])
                kx1 = ("E_x1t", p2)
                S.op("pool", lambda e, p2=p2, j=j: e.tensor_tensor(out=x1t[p2][:], in0=yt[p2][:],
                                                                    in1=xr[slot][:, j, :],
                                                                    op=ALU.add),
                     reads=[kyt, ("E_xr", slot, j)], writes=[kx1])
                S.dma(out=self.x1[tok0 + j * 128: tok0 + (j + 1) * 128, :], in_=x1t[p2][:],
                      sem="E_x1t%d" % p2, reads=[kx1], q="pool")
                kss2, krs2 = ("E_ss2", p2), ("E_rs2", p2)
                S.op("act", lambda e, p2=p2: e.activation(out=junk[:], in_=x1t[p2][:],
                                                           func=AF.Square,
                                                           accum_out=st[p2][:, 4:5]),
                     reads=[kx1], writes=[kss2])
                self.rstd_from_ss(st[p2][:, 4:5], kss2, st[p2][:, 5:6], krs2, D)
                kh2b = ("E_h2b", p2)
                S.op("dve", lambda e, p2=p2: e.scalar_tensor_tensor(
                    out=h2b[p2][:], in0=x1t[p2][:], scalar=st[p2][:, 5:6], in1=gpre[:],
                    op0=ALU.mult, op1=ALU.mult), reads=[kx1, krs2, "E_gpre"], writes=[kh2b])
                self.transp8(h2b[p2], kh2b, h2s[slot][:, :, j * 128:(j + 1) * 128],
                             ("E_h2s", slot, j), j)
            S.dma(out=h2T_v[:, :, tok0:tok0 + 512], in_=h2s[slot][:],
                  sem="E_h2s%d" % slot, reads=[("E_h2s", slot, j) for j in range(4)], q="pool")
        S.barrier()

    def load_convw(self, name, conv_ap, ntaps, ntiles):
        S = self.S
        raw = self.sb(name + "_raw", [ntiles, ntaps, 128], F32)
        cw = self.sb(name, [128, ntaps, ntiles], F32)
        S.dma(out=raw[:], in_=conv_ap.rearrange("j (t p) -> t j p", p=128), sem="ld_" + name,
              writes=[name + "_raw"])
        for j in range(ntaps):
            ps = self.ps[j % 2]
            kps = ("ps", j % 2)
            S.op("pe", lambda e, j=j, ps=ps: e.transpose(out=ps[:, :ntiles], in_=raw[:, j, :],
                                                         identity=self.ident_f[:ntiles, :ntiles]),
                 reads=[name + "_raw", "ident_f"], writes=[kps])
            S.op("dve", lambda e, j=j, ps=ps: e.tensor_copy(out=cw[:, j, :], in_=ps[:, :ntiles]),
                 reads=[kps], writes=[name])
        return cw

    def phase_F(self, l, dst):
        S = self.S
        self.sbuf_reset()
        NT = 22
        stage = [self.sb("F_stage%d" % i, [128, 512], F32) for i in range(2)]
        Wu = self.load_weight_bf16("F_Wu", self.w_up[l], 8, 2 * D_FF, stage, ["F_stg0", "F_stg1"])
        Wd = self.load_weight_bf16("F_Wd", self.w_down[l], NT, D, stage, ["F_stg0", "F_stg1"])
        gpost = self.load_bcast("F_gpost", self.g_ffn_post[l:l + 1, :], D)
        cw = self.load_convw("F_cw", self.ffn_conv[l], 3, 2 * NT)
        S.barrier()
        hT = self.sb("F_hT", [128, 8, 512], BF16)
        gT = self.sb("F_gT", [128, NT, 512], BF16)
        U = [self.sb("F_U%d" % i, [128, 514], F32) for i in range(2)]
        C = [self.sb("F_C%d" % i, [128, 512], F32) for i in range(2)]
        GL = self.sb("F_GL", [128, 512], F32)
        halo = self.sb("F_halo", [128, 2 * NT, 2], F32)
        x1t = [self.sb("F_x1t%d" % i, [128, D], F32) for i in range(2)]
        yt = [self.sb("F_yt%d" % i, [128, D], F32) for i in range(2)]
        junk = self.sb("F_junk", [128, 512], F32)
        st = [self.sb("F_st%d" % i, [128, 8], F32) for i in range(2)]
        h2T_v = self.h2T.rearrange("(k p) t -> p k t", p=128)
        for blk in range(NTOK // 512):
            tok0 = blk * 512
            if blk % (SEQ // 512) == 0:
                S.op("pool", lambda e: e.memset(halo[:], 0.0), writes=["F_halo"])
            S.dma(out=hT[:], in_=h2T_v[:, :, tok0:tok0 + 512], sem="F_hT", writes=["F_hT"])
            for i in range(NT):
                for gv in range(2):
                    ti = gv * NT + i
                    c0 = ti * 128
                    ps = self.ps[gv * 2 + i % 2]
                    kps = ("ps", gv * 2 + i % 2)

                    def mm(e, ps=ps, c0=c0):
                        ins = None
                        for k in range(8):
                            ins = e.matmul(ps[:, :], lhsT=Wu[:, k, c0:c0 + 128], rhs=hT[:, k, :],
                                           start=(k == 0), stop=(k == 7))
                        return ins
                    S.op("pe", mm, reads=["F_Wu", "F_hT"], writes=[kps])
                    u, ku = U[gv], ("F_U", gv)
                    c, kc = C[gv], ("F_C", gv)
                    S.op("act", lambda e, u=u, ps=ps: e.copy(out=u[:, 2:514], in_=ps[:, :]),
                         reads=[kps], writes=[(ku, "b")])
                    S.op("pool", lambda e, u=u, ti=ti: e.tensor_copy(out=u[:, 0:2],
                                                                     in_=halo[:, ti, :]),
                         reads=["F_halo"], writes=[(ku, "h")])
                    S.op("act", lambda e, u=u, c=c, ti=ti: e.activation(
                        out=c[:], in_=u[:, 0:512], func=AF.Copy, scale=cw[:, 0, ti:ti + 1]),
                        reads=[(ku, "b"), (ku, "h"), "F_cw"], writes=[kc])
                    for tap in (1, 2):
                        S.op("dve", lambda e, u=u, c=c, ti=ti, tap=tap: e.scalar_tensor_tensor(
                            out=c[:], in0=u[:, tap:tap + 512], scalar=cw[:, tap, ti:ti + 1],
                            in1=c[:], op0=ALU.mult, op1=ALU.add),
                            reads=[(ku, "b"), (ku, "h"), "F_cw", kc], writes=[kc])
                    S.op("pool", lambda e, u=u, ti=ti: e.tensor_copy(out=halo[:, ti, :],
                                                                     in_=u[:, 512:514]),
                         reads=[(ku, "b")], writes=["F_halo"])
                S.op("act", lambda e: e.activation(out=GL[:], in_=C[0][:],
                                                    func=AF.Gelu_apprx_tanh),
                     reads=[("F_C", 0)], writes=["F_GL"])
                S.op("pool", lambda e, i=i: e.tensor_tensor(out=gT[:, i, :], in0=GL[:],
                                                             in1=C[1][:], op=ALU.mult),
                     reads=["F_GL", ("F_C", 1)], writes=[("F_gT", i)])
            gkeys = [("F_gT", i) for i in range(NT)]
            for j in range(4):
                p2 = j % 2
                S.dma(out=x1t[p2][:], in_=self.x1[tok0 + j * 128: tok0 + (j + 1) * 128, :],
                      sem="F_x1t%d" % p2, writes=[("F_x1t", p2)])
                kss, krs = ("F_ss", p2), ("F_rs", p2)
                for hf in range(2):
                    ps = self.ps[4 + hf]
                    kps = ("ps", 4 + hf)

                    def mm(e, ps=ps, hf=hf, j=j):
                        ins = None
                        for k in range(NT):
                            ins = e.matmul(ps[:, :], lhsT=gT[:, k, j * 128:(j + 1) * 128],
                                           rhs=Wd[:, k, hf * 512:(hf + 1) * 512], start=(k == 0),
                                           stop=(k == NT - 1))
                        return ins
                    S.op("pe", mm, reads=["F_Wd"] + gkeys, writes=[kps])
                    S.op("act", lambda e, ps=ps, hf=hf, p2=p2: e.activation(
                        out=junk[:, :], in_=ps[:, :], func=AF.Square,
                        accum_out=st[p2][:, hf:hf + 1]), reads=[kps], writes=[(kss, hf)])
                S.op("dve", lambda e, p2=p2: e.tensor_tensor(out=st[p2][:, 2:3], in0=st[p2][:, 0:1],
                                                              in1=st[p2][:, 1:2], op=ALU.add),
                     reads=[(kss, 0), (kss, 1)], writes=[kss])
                self.rstd_from_ss(st[p2][:, 2:3], kss, st[p2][:, 3:4], krs, D)
                kyt = ("F_yt", p2)
                for hf in range(2):
                    ps = self.ps[4 + hf]
                    kps = ("ps", 4 + hf)
                    S.op("act", lambda e, ps=ps, hf=hf, p2=p2: e.activation(
                        out=yt[p2][:, hf * 512:(hf + 1) * 512], in_=ps[:, :], func=AF.Copy,
                        scale=st[p2][:, 3:4]), reads=[kps, krs], writes=[(kyt, hf)])
                S.op("dve", lambda e, p2=p2: e.tensor_tensor(out=yt[p2][:], in0=yt[p2][:],
                                                              in1=gpost[:], op=ALU.mult),
                     reads=[(kyt, 0), (kyt, 1), "F_gpost"], writes=[kyt])
                S.op("pool", lambda e, p2=p2: e.tensor_tensor(out=yt[p2][:], in0=yt[p2][:],
                                                               in1=x1t[p2][:], op=ALU.add),
                     reads=[kyt, ("F_x1t", p2)], writes=[kyt])
                S.dma(out=dst[tok0 + j * 128: tok0 + (j + 1) * 128, :], in_=yt[p2][:],
                      sem="F_yt%d" % p2, reads=[kyt])
        S.barrier()

    def phase_H(self, l):
        S = self.S
        self.sbuf_reset()
        sb = self.sb
        UU = sb("H_UU", [64, 128], F32)
        UL = sb("H_UL", [64, 64], F32)
        tmpm = sb("H_tmpm", [64, 64], F32)
        S.op("pool", lambda e: e.memset(UU[:], 1.0), writes=["H_UU"])
        S.op("pool", lambda e: e.affine_select(out=UU[:, 0:64], in_=UU[:, 0:64], pattern=[[1, 64]],
                                                compare_op=ALU.is_ge, fill=0.0, base=0,
                                                channel_multiplier=-1),
             reads=["H_UU"], writes=["H_UU"])
        S.op("pool", lambda e: e.memset(tmpm[:], 1.0), writes=["H_tmpm"])
        S.op("pool", lambda e: e.affine_select(out=tmpm[:], in_=tmpm[:], pattern=[[0, 64]],
                                                compare_op=ALU.is_ge, fill=0.0, base=31,
                                                channel_multiplier=-1),
             reads=["H_tmpm"], writes=["H_tmpm"])
        S.op("pool", lambda e: e.tensor_tensor(out=UU[:, 64:128], in0=UU[:, 0:64], in1=tmpm[:],
                                                op=ALU.subtract),
             reads=["H_UU", "H_tmpm"], writes=["H_UU"])
        S.op("pool", lambda e: e.memset(UL[:], 1.0), writes=["H_UL"])
        S.op("pool", lambda e: e.affine_select(out=UL[:], in_=UL[:], pattern=[[-1, 64]],
                                                compare_op=ALU.is_gt, fill=0.0, base=0,
                                                channel_multiplier=1),
             reads=["H_UL"], writes=["H_UL"])
        lb = sb("H_lb", [64, 256], F32)
        oml = sb("H_oml", [64, 256], F32)
        if l == 0:
            S.op("pool", lambda e: e.memset(lb[:], 0.0), writes=["H_lb"])
        else:
            r0 = sb("H_r0", [64, 256], F32)
            S.dma(out=r0[:], in_=bcast_rows(self.hg_lb[0:1, :], 64), sem="H_r0", writes=["H_r0"])
            S.dma(out=lb[:], in_=bcast_rows(self.hg_lb[1:2, :], 64), sem="H_lbl", writes=["H_lb"])
            S.op("dve", lambda e: e.tensor_tensor(out=lb[:], in0=lb[:], in1=r0[:], op=ALU.subtract),
                 reads=["H_lb", "H_r0"], writes=["H_lb"])
            S.op("act", lambda e: e.activation(out=lb[:], in_=lb[:], func=AF.Sigmoid),
                 reads=["H_lb"], writes=["H_lb"])
        S.op("dve", lambda e: e.tensor_scalar(out=oml[:], in0=lb[:], scalar1=-1.0, scalar2=1.0,
                                               op0=ALU.mult, op1=ALU.add),
             reads=["H_lb"], writes=["H_oml"])
        lbT = sb("H_lbT", [64, 4, 2], F32)
        for h in range(4):
            for which, src, ksrc in ((0, lb, "H_lb"), (1, oml, "H_oml")):
                ps = self.ps[(2 * h + which) % 4]
                kps = ("ps", (2 * h + which) % 4)
                S.op("pe", lambda e, ps=ps, src=src, h=h: e.transpose(
                    out=ps[:64, :64], in_=src[:, h * 64:(h + 1) * 64],
                    identity=self.ident_f[:64, :64]), reads=[ksrc, "ident_f"], writes=[kps])
                S.op("dve", lambda e, ps=ps, h=h, which=which: e.tensor_copy(
                    out=lbT[:, h, which:which + 1], in_=ps[:64, 0:1]), reads=[kps],
                    writes=["H_lbT"])
        ng = sb("H_ng", [64, 64], F32)
        S.dma(out=ng[:], in_=bcast_rows(self.hg_norm[l:l + 1, :], 64), sem="H_ng", writes=["H_ng"])

        qf = [sb("H_qf%d" % i, [64, 2, 4, 512], F32) for i in range(2)]
        sq = [sb("H_sq%d" % i, [64, 4, 512], F32) for i in range(2)]
        kc = [sb("H_kc%d" % i, [64, 4, 512], F32) for i in range(2)]
        tk = [sb("H_tk%d" % i, [64, 768], F32) for i in range(3)]
        St = [sb("H_S%d" % i, [64, 4, 64], F32) for i in range(2)]
        Sb = [sb("H_Sb%d" % i, [64, 4, 64], BF16) for i in range(2)]
        pf_q = self.pfeat[FM_ROW[2764]:FM_ROW[2764] + 256, :].rearrange("(h k) t -> k h t", k=64)
        pf_f = self.pfeat[FM_ROW[3020]:FM_ROW[3020] + 256, :].rearrange("(h k) t -> k h t", k=64)
        NB = 3

        def t3(name, shape, dtype):
            return [sb("%s%d" % (name, i), shape, dtype) for i in range(NB)]
        sig, fg, logf, kct, kd = (t3("H_sig", [64, 256], F32), t3("H_fg", [64, 256], F32),
                                  t3("H_logf", [64, 256], F32), t3("H_kct", [64, 256], F32),
                                  t3("H_kd", [64, 256], BF16))
        vb = t3("H_vb", [64, 256], BF16)
        gs = t3("H_gs", [64, 4, 64], F32)
        EB, EN = t3("H_EB", [64, 4, 128], F32), t3("H_EN", [64, 4, 64], F32)
        qd, qp, kp = (t3("H_qd", [64, 4, 64], BF16), t3("H_qp", [64, 4, 64], BF16),
                      t3("H_kp", [64, 4, 64], BF16))
        Am = t3("H_Am", [64, 4, 64], BF16)
        osb, osq = t3("H_osb", [64, 4, 64], F32), t3("H_osq", [64, 4, 64], F32)
        stt = t3("H_stt", [64, 8], F32)
        stmp = t3("H_stmp", [64, 4, 64], F32)
        def seq_thread(s):
            pb = lambda i: 4 * s + i % 4
            for blk8 in range(SEQ // 512):
                slot = (blk8 * NSEQ + s) % 2
                tb = s * SEQ + blk8 * 512
                kqf = ("H_qf", slot)
                S.dma(out=qf[slot][:, 0, :, :], in_=pf_q[:, :, tb:tb + 512], sem="H_qfq%d" % slot,
                      writes=[(kqf, 0)])
                S.dma(out=qf[slot][:, 1, :, :], in_=pf_f[:, :, tb:tb + 512], sem="H_qff%d" % slot,
                      writes=[(kqf, 1)])
                S.op("act", lambda e, slot=slot: e.activation(out=sq[slot][:], in_=qf[slot][:, 0],
                                                               func=AF.Silu),
                     reads=[(kqf, 0)], writes=[("H_sq", slot)])
                S.op("act", lambda e, slot=slot: e.activation(out=kc[slot][:], in_=qf[slot][:, 1],
                                                               func=AF.Sigmoid),
                     reads=[(kqf, 1)], writes=[("H_kc", slot)])
                for h in range(4):
                    S.op("dve", lambda e, slot=slot, h=h: e.tensor_scalar(
                        out=kc[slot][:, h, :], in0=kc[slot][:, h, :], scalar1=lbT[:, h, 1:2],
                        scalar2=-1.0, op0=ALU.mult, op1=ALU.mult),
                        reads=[("H_kc", slot), "H_lbT"], writes=[("H_kc", slot)])
                    S.op("dve", lambda e, slot=slot, h=h: e.tensor_scalar(
                        out=kc[slot][:, h, :], in0=kc[slot][:, h, :], scalar1=lbT[:, h, 1:2],
                        scalar2=None, op0=ALU.add),
                        reads=[("H_kc", slot), "H_lbT"], writes=[("H_kc", slot)])
                for cn in range(8):
                    b = s
                    c0 = cn * 64
                    tok = tb + c0
                    ktk = ("H_tk", b)
                    S.dma(out=tk[b][:], in_=self.ptok[tok:tok + 64, TM_CF:TM_CF + 768],
                          sem="H_tk%d" % b, writes=[ktk])
                    first = (blk8 == 0 and cn == 0)
                    S.op("act", lambda e, b=b: e.activation(out=sig[b][:], in_=tk[b][:, 0:256],
                                                             func=AF.Sigmoid),
                         reads=[ktk], writes=[("H_sig", b)])
                    S.op("dve", lambda e, b=b: e.tensor_tensor(out=fg[b][:], in0=sig[b][:],
                                                                in1=oml[:], op=ALU.mult),
                         reads=[("H_sig", b), "H_oml"], writes=[("H_fg", b)])
                    S.op("dve", lambda e, b=b: e.tensor_tensor(out=fg[b][:], in0=fg[b][:],
                                                                in1=lb[:], op=ALU.add),
                         reads=[("H_fg", b), "H_lb"], writes=[("H_fg", b)])
                    S.op("act", lambda e, b=b: e.activation(out=logf[b][:], in_=fg[b][:],
                                                             func=AF.Ln),
                         reads=[("H_fg", b)], writes=[("H_logf", b)])
                    S.op("pool", lambda e, b=b: e.tensor_scalar(out=kct[b][:], in0=fg[b][:],
                                                                 scalar1=-1.0, scalar2=1.0,
                                                                 op0=ALU.mult, op1=ALU.add),
                         reads=[("H_fg", b)], writes=[("H_kct", b)])
                    S.op("pool", lambda e, b=b: e.tensor_copy(out=vb[b][:], in_=tk[b][:, 256:512]),
                         reads=[ktk], writes=[("H_vb", b)])
                    S.op("act", lambda e, b=b: e.activation(
                        out=gs[b][:], in_=tk[b][:, 512:768].rearrange("p (h v) -> p h v", h=4),
                        func=AF.Silu), reads=[ktk], writes=[("H_gs", b)])
                    S.op("pool", lambda e, b=b: e.tensor_tensor(
                        out=gs[b][:], in0=gs[b][:],
                        in1=ng[:].unsqueeze(1).to_broadcast([64, 4, 64]), op=ALU.mult),
                        reads=[("H_gs", b), "H_ng"], writes=[("H_gs", b)])
                    p0, kp0 = self.ps8[pb(0)], ("ps", pb(0))

                    def mmb(e, b=b, p0=p0):
                        ins = None
                        for h in range(4):
                            ins = e.matmul(p0[:64, h * 128:(h + 1) * 128],
                                           lhsT=logf[b][:, h * 64:(h + 1) * 64], rhs=UU[:, :],
                                           start=True, stop=True)
                        return ins
                    S.op("pe", mmb, reads=[("H_logf", b), "H_UU"], writes=[kp0])
                    p0v = p0[:64, :].rearrange("p (h t) -> p h t", h=4)
                    S.op("act", lambda e, b=b, p0v=p0v: e.activation(out=EB[b][:], in_=p0v,
                                                                      func=AF.Exp),
                         reads=[kp0], writes=[("H_EB", b)])
                    S.op("act", lambda e, b=b, p0v=p0v: e.activation(out=EN[b][:],
                                                                      in_=p0v[:, :, 64:128],
                                                                      func=AF.Exp, scale=-1.0),
                         reads=[kp0], writes=[("H_EN", b)])
                    p1, kp1 = self.ps8[pb(1)], ("ps", pb(1))
                    S.op("pe", lambda e, b=b, p1=p1: e.matmul(p1[:64, :256], lhsT=UL[:, :],
                                                              rhs=logf[b][:, :], start=True,
                                                              stop=True),
                         reads=[("H_logf", b), "H_UL"], writes=[kp1])
                    S.op("act", lambda e, b=b, p1=p1: e.activation(out=sig[b][:], in_=p1[:64, :256],
                                                                    func=AF.Exp),
                         reads=[kp1], writes=[("H_sig", b)])
                    S.op("dve", lambda e, b=b: e.tensor_tensor(out=kd[b][:], in0=sig[b][:],
                                                                in1=kct[b][:], op=ALU.mult),
                         reads=[("H_sig", b), ("H_kct", b)], writes=[("H_kd", b)])
                    sqv = sq[slot][:, :, c0:c0 + 64]
                    kcv = kc[slot][:, :, c0:c0 + 64]
                    S.op("dve", lambda e, b=b, sqv=sqv: e.tensor_tensor(
                        out=qd[b][:], in0=sqv, in1=EB[b][:, :, 0:64], op=ALU.mult),
                        reads=[("H_sq", slot), ("H_EB", b)], writes=[("H_qd", b)])
                    S.op("pool", lambda e, b=b, sqv=sqv: e.tensor_tensor(
                        out=qp[b][:], in0=sqv, in1=EB[b][:, :, 64:128], op=ALU.mult),
                        reads=[("H_sq", slot), ("H_EB", b)], writes=[("H_qp", b)])
                    S.op("dve", lambda e, b=b, kcv=kcv: e.tensor_tensor(
                        out=kp[b][:], in0=kcv, in1=EN[b][:], op=ALU.mult),
                        reads=[("H_kc", slot), ("H_EN", b)], writes=[("H_kp", b)])
                    p2, kp2 = self.ps8[pb(2)], ("ps", pb(2))

                    def mma(e, b=b, p2=p2):
                        ins = None
                        for h in range(4):
                            ins = e.matmul(p2[:64, h * 64:(h + 1) * 64], lhsT=kp[b][:, h, :],
                                           rhs=qp[b][:, h, :], start=True, stop=True)
                        return ins
                    S.op("pe", mma, reads=[("H_kp", b), ("H_qp", b)], writes=[kp2])
                    S.op("dve", lambda e, b=b, p2=p2: e.tensor_tensor(
                        out=Am[b][:], in0=p2[:64, :256].rearrange("p (h c) -> p h c", h=4),
                        in1=UU[:, 0:64].unsqueeze(1).to_broadcast([64, 4, 64]), op=ALU.mult),
                        reads=[kp2, "H_UU"], writes=[("H_Am", b)])
                    if first:
                        S.op("pool", lambda e, s=s: e.memset(St[s][:], 0.0), writes=[("H_S", s)])
                        S.op("pool", lambda e, s=s: e.memset(Sb[s][:], 0.0), writes=[("H_Sb", s)])
                    p3, kp3 = self.ps8[pb(3)], ("ps", pb(3))

                    def mmo(e, b=b, p3=p3, s=s):
                        ins = None
                        for h in range(4):
                            e.matmul(p3[:64, h * 64:(h + 1) * 64], lhsT=qd[b][:, h, :],
                                     rhs=Sb[s][:, h, :], start=True, stop=False)
                            ins = e.matmul(p3[:64, h * 64:(h + 1) * 64], lhsT=Am[b][:, h, :],
                                           rhs=vb[b][:, h * 64:(h + 1) * 64], start=False, stop=True)
                        return ins
                    S.op("pe", mmo, reads=[("H_qd", b), ("H_Sb", s), ("H_Am", b), ("H_vb", b)],
                         writes=[kp3])
                    p4, kp4 = self.ps8[pb(4)], ("ps", pb(4))

                    def mms(e, b=b, p4=p4):
                        ins = None
                        for h in range(4):
                            ins = e.matmul(p4[:64, h * 64:(h + 1) * 64],
                                           lhsT=kd[b][:, h * 64:(h + 1) * 64],
                                           rhs=vb[b][:, h * 64:(h + 1) * 64], start=True, stop=True)
                        return ins
                    S.op("pe", mms, reads=[("H_kd", b), ("H_vb", b)], writes=[kp4])
                    S.op("dve", lambda e, b=b, s=s: e.tensor_tensor(
                        out=stmp[b][:], in0=St[s][:],
                        in1=EB[b][:, :, 63:64].to_broadcast([64, 4, 64]), op=ALU.mult),
                        reads=[("H_S", s), ("H_EB", b)], writes=[("H_stmp", b)])
                    S.op("dve", lambda e, b=b, s=s, p4=p4: e.tensor_tensor(
                        out=St[s][:], in0=stmp[b][:],
                        in1=p4[:64, :256].rearrange("p (h v) -> p h v", h=4), op=ALU.add),
                        reads=[("H_stmp", b), kp4], writes=[("H_S", s)])
                    S.op("act", lambda e, s=s: e.copy(out=Sb[s][:], in_=St[s][:]),
                         reads=[("H_S", s)], writes=[("H_Sb", s)])
                    S.op("act", lambda e, b=b, p3=p3: e.copy(
                        out=osb[b][:], in_=p3[:64, :256].rearrange("p (h v) -> p h v", h=4)),
                        reads=[kp3], writes=[("H_osb", b)])
                    S.op("pool", lambda e, b=b: e.tensor_tensor(out=osq[b][:], in0=osb[b][:],
                                                                 in1=osb[b][:], op=ALU.mult),
                         reads=[("H_osb", b)], writes=[("H_osq", b)])
                    S.op("dve", lambda e, b=b: e.tensor_reduce(out=stt[b][:, 0:4], in_=osq[b][:],
                                                                axis=AX.X, op=ALU.add),
                         reads=[("H_osq", b)], writes=[("H_stt", b)])
                    self.rstd_from_ss(stt[b][:, 0:4], ("H_stt", b), stt[b][:, 4:8], ("H_rs", b), 64)
                    S.op("dve", lambda e, b=b: e.tensor_tensor(
                        out=osb[b][:], in0=osb[b][:],
                        in1=stt[b][:, 4:8].unsqueeze(2).to_broadcast([64, 4, 64]), op=ALU.mult),
                        reads=[("H_osb", b), ("H_rs", b)], writes=[("H_osb", b)])
                    S.op("pool", lambda e, b=b: e.tensor_tensor(out=osq[b][:], in0=osb[b][:],
                                                                 in1=gs[b][:], op=ALU.mult),
                         reads=[("H_osb", b), ("H_gs", b)], writes=[("H_osq", b)])
                    S.dma(out=self.mixin[tok:tok + 64, 768:1024],
                          in_=osq[b][:].rearrange("p h v -> p (h v)"), sem="H_out%d" % b,
                          reads=[("H_osq", b)])
        S.interleave([lambda s=s: seq_thread(s) for s in range(NSEQ)])
        S.barrier()

    def phase_S(self, l):
        S = self.S
        self.sbuf_reset()
        sb = self.sb
        BIG = -1.0e30
        KIT = 32
        kiT = sb("S_kiT", [64, SEQ], F32)
        kT = sb("S_kT", [64, SEQ], BF16)
        kst = sb("S_kst", [64, 1024], F32)
        vst = sb("S_vst", [128, 8, 64], F32)
        v1 = sb("S_v1", [128, 32, 65], BF16)
        junk = sb("S_junk", [128, SEQ], BF16)
        ckn = sb("S_ckn", [128, KIT + 1], F32)
        for k in range(KIT + 1):
            S.op("pool", lambda e, k=k: e.memset(ckn[:, k:k + 1], -2.1 / 2.0 ** (k + 1)),
                 writes=[("S_ckn", k)])
        kck = [("S_ckn", k) for k in range(KIT + 1)]

        NT = 2

        def t2(name, shape, dtype):
            return [sb("%s%d" % (name, i), shape, dtype) for i in range(NT)]
        qq = t2("S_qq", [64, 2, 4, 128], F32)
        qqb = t2("S_qqb", [64, 4, 128], BF16)
        acc = t2("S_acc", [128, SEQ], F32)
        wkA = t2("S_wkA", [128, SEQ], F32)
        gtt = t2("S_gt", [128, SEQ], BF16)
        eqt = t2("S_eq", [128, SEQ], BF16)
        selb = t2("S_selb", [128, SEQ], BF16)
        rr = [sb("S_rr%d" % i, [128, 512], F32) for i in range(2 * NT)]
        pT = [sb("S_pT%d" % i, [128, 512], BF16) for i in range(2 * NT)]
        wi = t2("S_wi", [128, 12], F32)
        sc = t2("S_sc", [128, 16], F32)
        nst = t2("S_nst", [128, KIT + 1], F32)
        oT = t2("S_oT", [65, 512], F32)
        osb = t2("S_osb", [128, 4, 64], F32)
        rc = t2("S_rc", [128, 4, 1], F32)
        pf_q = self.pfeat[FM_ROW[2056]:FM_ROW[2056] + 256, :].rearrange("(h k) t -> k h t", k=64)
        pf_qi = self.pfeat[FM_ROW[2440]:FM_ROW[2440] + 256, :].rearrange("(h k) t -> k h t", k=64)
        pf_k = self.pfeat[FM_ROW[2312]:FM_ROW[2312] + 64, :]
        pf_ki = self.pfeat[FM_ROW[2696]:FM_ROW[2696] + 64, :]

        def tile_thread(s, a2):
            t0 = s * SEQ
            nr = 0
            for j in range(a2, SEQ // 128, NT):
                tq = t0 + j * 128
                NK = (j + 1) * 128
                kqq = ("S_qq", a2)
                S.dma(out=qq[a2][:, 0], in_=pf_q[:, :, tq:tq + 128], sem="S_qq%da" % a2,
                      writes=[(kqq, 0)])
                S.dma(out=qq[a2][:, 1], in_=pf_qi[:, :, tq:tq + 128], sem="S_qq%db" % a2,
                      writes=[(kqq, 1)])
                S.op("pool", lambda e: e.tensor_copy(out=qqb[a2][:], in_=qq[a2][:, 0]),
                     reads=[(kqq, 0)], writes=[("S_qqb", a2)])
                kwi = ("S_wi", a2)
                S.dma(out=wi[a2][:, 0:4], in_=self.ptok[tq:tq + 128, TM_WI:TM_WI + 4],
                      sem="S_wi%d" % a2, writes=[(kwi, 0)])
                S.op("act", lambda e: e.activation(out=wi[a2][:, 4:8], in_=wi[a2][:, 0:4],
                                                    func=AF.Abs),
                     reads=[(kwi, 0)], writes=[(kwi, 1)])
                S.op("act", lambda e: e.activation(out=wi[a2][:, 8:12], in_=wi[a2][:, 0:4],
                                                    func=AF.Sign),
                     reads=[(kwi, 0)], writes=[(kwi, 2)])
                kacc = ("S_acc", a2)
                nkb = (NK + 511) // 512
                for kb in range(nkb):
                    w = min(512, NK - kb * 512)
                    for h in range(4):
                        pi = 2 * a2 + h % 2
                        ps, kps = self.ps8[pi], ("ps", pi)
                        S.op("pe", lambda e, ps=ps, h=h, kb=kb, w=w: e.matmul(
                            ps[:, :w], lhsT=qq[a2][:, 1, h, :],
                            rhs=kiT[:, kb * 512: kb * 512 + w], start=True, stop=True),
                            reads=[(kqq, 1), "S_kiT"], writes=[kps])
                        ri = 2 * a2 + nr % 2
                        r, kr = rr[ri], ("S_rr", ri)
                        nr += 1
                        S.op("act", lambda e, ps=ps, r=r, w=w, h=h: e.activation(
                            out=r[:, :w], in_=ps[:, :w], func=AF.Relu, scale=wi[a2][:, 4 + h:5 + h]),
                            reads=[kps, (kwi, 1)], writes=[kr])
                        av = acc[a2][:, kb * 512: kb * 512 + w]
                        if h == 0:
                            S.op("dve", lambda e, av=av, r=r, w=w, h=h: e.tensor_scalar(
                                out=av, in0=r[:, :w], scalar1=wi[a2][:, 8 + h:9 + h], scalar2=None,
                                op0=ALU.mult), reads=[kr, (kwi, 2)], writes=[(kacc, kb), kacc])
                        else:
                            S.op("dve", lambda e, av=av, r=r, w=w, h=h: e.scalar_tensor_tensor(
                                out=av, in0=r[:, :w], scalar=wi[a2][:, 8 + h:9 + h], in1=av,
                                op0=ALU.mult, op1=ALU.add),
                                reads=[kr, (kwi, 2), (kacc, kb)], writes=[(kacc, kb)])
                kall = [(kacc, kb) for kb in range(nkb)]
                X = sc[a2]
                ksc = lambda nm: ("S_sc", a2, nm)
                accv = acc[a2][:, :NK]
                if j >= 2:
                    S.op("dve", lambda e: e.tensor_reduce(out=X[:, 0:1], in_=accv, axis=AX.X,
                                                           op=ALU.max, apply_absolute_value=True),
                         reads=kall, writes=[ksc("rm")])
                    S.op("dve", lambda e: e.tensor_scalar(out=X[:, 0:1], in0=X[:, 0:1],
                                                           scalar1=1.0e-20, scalar2=None,
                                                           op0=ALU.max),
                         reads=[ksc("rm")], writes=[ksc("rm")])
                    S.op("dve", lambda e: e.tensor_scalar(out=nst[a2][:], in0=ckn[:],
                                                           scalar1=X[:, 0:1], scalar2=None,
                                                           op0=ALU.mult),
                         reads=[ksc("rm")] + kck, writes=[("S_nst", a2)])
                    S.op("dve", lambda e: e.tensor_scalar(out=X[:, 1:2], in0=X[:, 0:1],
                                                           scalar1=-0.02, scalar2=None,
                                                           op0=ALU.mult),
                         reads=[ksc("rm")], writes=[ksc("nc")])
                    S.op("pool", lambda e: e.memset(X[:, 4:5], float(NK) - 511.5),
                         writes=[ksc("cb")])
                S.op("pool", lambda e: e.memset(acc[a2][0:64, NK - 64:NK], BIG),
                     reads=kall + [ksc("rm")], writes=[kacc])
                if j >= 2:
                    for k in range(KIT):
                        S.op("act", lambda e: e.activation(out=junk[:, :NK], in_=accv, func=AF.Sign,
                                                            bias=X[:, 1:2], scale=1.0,
                                                            accum_out=X[:, 2:3]),
                             reads=[kacc, ksc("nc")], writes=[ksc("sg")])
                        S.op("act", lambda e: e.activation(out=X[:, 3:4], in_=X[:, 2:3],
                                                            func=AF.Sign, bias=X[:, 4:5], scale=1.0),
                             reads=[ksc("sg"), ksc("cb")], writes=[ksc("dd")])
                        S.op("act", lambda e, k=k: e.activation(out=X[:, 1:2], in_=X[:, 3:4],
                                                                 func=AF.Identity,
                                                                 scale=nst[a2][:, k + 1:k + 2],
                                                                 bias=X[:, 1:2]),
                             reads=[ksc("dd"), ("S_nst", a2), ksc("nc")], writes=[ksc("nc")])
                    S.op("dve", lambda e: e.scalar_tensor_tensor(
                        out=X[:, 5:6], in0=X[:, 1:2], scalar=-1.0, in1=nst[a2][:, KIT:KIT + 1],
                        op0=ALU.mult, op1=ALU.add), reads=[ksc("nc"), ("S_nst", a2)],
                        writes=[ksc("thr")])
                else:
                    S.op("pool", lambda e: e.memset(X[:, 5:6], BIG / 2), writes=[ksc("thr")])
                kwk = ("S_wkA", a2)
                wv = wkA[a2][:, :NK]
                S.op("dve", lambda e: e.tensor_scalar(out=wv, in0=accv, scalar1=X[:, 5:6],
                                                       scalar2=3.0e30, op0=ALU.is_lt, op1=ALU.mult),
                     reads=[kacc, ksc("thr")], writes=[kwk])
                S.op("dve", lambda e: e.tensor_tensor(out=wv, in0=wv, in1=accv, op=ALU.add),
                     reads=[kacc, kwk], writes=[kwk])
                S.op("dve", lambda e: e.tensor_reduce(out=X[:, 6:7], in_=wv, axis=AX.X, op=ALU.min),
                     reads=[kwk], writes=[ksc("v")])
                gv, ev = gtt[a2][:, :NK], eqt[a2][:, :NK]
                S.op("dve", lambda e: e.tensor_scalar(out=gv, in0=accv, scalar1=X[:, 6:7],
                                                       scalar2=None, op0=ALU.is_gt, op1=ALU.add,
                                                       accum_out=X[:, 7:8]),
                     reads=[kacc, ksc("v")], writes=[("S_gt", a2), ksc("cg")])
                S.op("dve", lambda e: e.tensor_scalar(out=ev, in0=accv, scalar1=X[:, 6:7],
                                                       scalar2=None, op0=ALU.is_equal),
                     reads=[kacc, ksc("v")], writes=[("S_eq", a2)])
                S.op("dve", lambda e: e.tensor_tensor_scan(out=wv, data0=ev, data1=ev, initial=0.0,
                                                            op0=ALU.add, op1=ALU.max),
                     reads=[("S_eq", a2), kwk], writes=[kwk])
                S.op("dve", lambda e: e.tensor_scalar(out=X[:, 8:9], in0=X[:, 7:8], scalar1=-1.0,
                                                       scalar2=256.0, op0=ALU.mult, op1=ALU.add),
                     reads=[ksc("cg")], writes=[ksc("need")])
                S.op("dve", lambda e: e.scalar_tensor_tensor(out=ev, in0=wv, scalar=X[:, 8:9],
                                                              in1=ev, op0=ALU.is_le, op1=ALU.mult),
                     reads=[kwk, ksc("need"), ("S_eq", a2)], writes=[("S_eq", a2)])
                S.op("pool", lambda e: e.tensor_tensor(out=gv, in0=gv, in1=ev, op=ALU.add),
                     reads=[("S_gt", a2), ("S_eq", a2)], writes=[("S_gt", a2)])
                ksel = ("S_selb", a2)
                S.op("pool", lambda e: e.tensor_scalar(out=selb[a2][:, :NK], in0=gv, scalar1=-1.0,
                                                        scalar2=-NEG, op0=ALU.add, op1=ALU.mult),
                     reads=[("S_gt", a2)], writes=[ksel])
                po, kpo = self.ps8[4 + a2], ("ps", 4 + a2)
                for kt in range(j + 1):
                    li = (0, 1, 6, 7)[2 * a2 + kt % 2]
                    pl, kpl = self.ps8[li], ("ps", li)

                    def mml(e, pl=pl, kt=kt):
                        e.matmul(pl[:, :].rearrange("p (h t) -> p h t", h=4),
                                 lhsT=kT[:, kt * 128:(kt + 1) * 128], rhs=qqb[a2][:, :, :],
                                 start=True, stop=False)
                        ins = None
                        for h in range(4):
                            ins = e.matmul(pl[:, h * 128:(h + 1) * 128],
                                           lhsT=selb[a2][:, kt * 128:(kt + 1) * 128],
                                           rhs=self.ident_b[:, :], start=False, stop=(h == 3))
                        return ins
                    S.op("pe", mml, reads=["S_kT", ("S_qqb", a2), ksel, "ident_b"], writes=[kpl])
                    pi_ = 2 * a2 + kt % 2
                    p, kp = pT[pi_], ("S_pT", pi_)
                    S.op("act", lambda e, p=p, pl=pl: e.activation(out=p[:], in_=pl[:, :],
                                                                    func=AF.Exp, scale=0.125),
                         reads=[kpl], writes=[kp])
                    S.op("pe", lambda e, p=p, kt=kt: e.matmul(
                        po[:65, :], lhsT=v1[:, kt, :], rhs=p[:], start=(kt == 0), stop=(kt == j)),
                        reads=["S_v1", kp], writes=[kpo])
                koT = ("S_oT", a2)
                S.op("act", lambda e: e.copy(out=oT[a2][:], in_=po[:65, :]),
                     reads=[kpo], writes=[koT])
                pt, kpt = self.ps8[2 * a2], ("ps", 2 * a2)

                def trs(e):
                    ins = None
                    for h in range(4):
                        ins = e.transpose(out=pt[:, h * 65:(h + 1) * 65],
                                          in_=oT[a2][:, h * 128:(h + 1) * 128],
                                          identity=self.ident_f[:65, :65])
                    return ins
                S.op("pe", trs, reads=[koT, "ident_f"], writes=[kpt])
                ptv = pt[:, :260].rearrange("p (h d) -> p h d", h=4)
                S.op("dve", lambda e: e.reciprocal(out=rc[a2][:], in_=ptv[:, :, 64:65]),
                     reads=[kpt], writes=[("S_rc", a2)])
                S.op("dve", lambda e: e.tensor_tensor(
                    out=osb[a2][:], in0=ptv[:, :, 0:64], in1=rc[a2][:].to_broadcast([128, 4, 64]),
                    op=ALU.mult), reads=[kpt, ("S_rc", a2)], writes=[("S_osb", a2)])
                S.dma(out=self.mixin[tq:tq + 128, 512:768],
                      in_=osb[a2][:].rearrange("p h d -> p (h d)"), sem="S_out%d" % a2,
                      reads=[("S_osb", a2)])

        for s in range(NSEQ):
            t0 = s * SEQ
            S.dma(out=kiT[:], in_=pf_ki[:, t0:t0 + SEQ], sem="S_kiT", writes=["S_kiT"])
            for q4 in range(4):
                S.dma(out=kst[:], in_=pf_k[:, t0 + q4 * 1024:t0 + (q4 + 1) * 1024], sem="S_kst",
                      writes=["S_kst"])
                S.op("pool", lambda e, q4=q4: e.tensor_copy(out=kT[:, q4 * 1024:(q4 + 1) * 1024],
                                                            in_=kst[:]),
                     reads=["S_kst"], writes=["S_kT"])
            S.op("pool", lambda e: e.memset(v1[:], 1.0), writes=["S_v1"])
            for q4 in range(4):
                S.dma(out=vst[:],
                      in_=self.ptok[t0 + q4 * 1024: t0 + (q4 + 1) * 1024,
                                    TM_BV:TM_BV + 64].rearrange("(kt p) d -> p kt d", p=128),
                      sem="S_vst", writes=["S_vst"])
                S.op("pool", lambda e, q4=q4: e.tensor_copy(out=v1[:, q4 * 8:(q4 + 1) * 8, 0:64],
                                                            in_=vst[:]),
                     reads=["S_vst"], writes=["S_v1"])
            S.interleave([lambda s=s, a=a: tile_thread(s, a) for a in range(NT)])
        S.barrier()

    def phase_D(self, l):
        S = self.S
        self.sbuf_reset()
        sb = self.sb
        NB = 2
        U64 = sb("D_U", [64, 64], F32)
        UL = sb("D_UL", [64, 64], F32)
        Lst = sb("D_Lst", [64, 64], F32)
        mb = sb("D_mb", [64, 64], F32)
        S.op("pool", lambda e: e.memset(U64[:], 1.0), writes=["D_U"])
        S.op("pool", lambda e: e.affine_select(out=U64[:], in_=U64[:], pattern=[[1, 64]],
                                                compare_op=ALU.is_ge, fill=0.0, base=0,
                                                channel_multiplier=-1), reads=["D_U"],
             writes=["D_U"])
        for t_, kk_ in ((UL, "D_UL"), (Lst, "D_Lst")):
            S.op("pool", lambda e, t_=t_: e.memset(t_[:], 1.0), writes=[kk_])
            S.op("pool", lambda e, t_=t_: e.affine_select(out=t_[:], in_=t_[:], pattern=[[-1, 64]],
                                                          compare_op=ALU.is_gt, fill=0.0, base=0,
                                                          channel_multiplier=1), reads=[kk_],
                 writes=[kk_])
        S.op("pool", lambda e: e.memset(mb[:], 0.0), writes=["D_mb"])
        S.op("pool", lambda e: e.affine_select(out=mb[:], in_=mb[:], pattern=[[-1, 64]],
                                                compare_op=ALU.is_ge, fill=-1.0e4, base=0,
                                                channel_multiplier=1), reads=["D_mb"],
             writes=["D_mb"])
        cw = self.load_convw("D_cw", self.dn_conv[l], 4, 12)
        alog = sb("D_alog", [64, 4], F32)
        dtb = sb("D_dtb", [64, 4], F32)
        S.dma(out=alog[:], in_=bcast_rows(self.dn_a_log[l:l + 1, :], 64), sem="D_alog",
              writes=["D_alog"])
        S.dma(out=dtb[:], in_=bcast_rows(self.dn_dt_bias[l:l + 1, :], 64), sem="D_dtb",
              writes=["D_dtb"])
        S.op("act", lambda e: e.activation(out=alog[:], in_=alog[:], func=AF.Exp), reads=["D_alog"],
             writes=["D_alog"])
        S.op("dve", lambda e: e.tensor_scalar(out=alog[:], in0=alog[:], scalar1=-1.0, scalar2=None,
                                               op0=ALU.mult), reads=["D_alog"], writes=["D_alog"])
        ng = sb("D_ng", [64, 128], F32)
        S.dma(out=ng[:], in_=bcast_rows(self.dn_norm[l:l + 1, :], 64), sem="D_ng", writes=["D_ng"])

        X = [sb("D_X%d" % i, [128, 515], F32) for i in range(4)]
        Cc = [sb("D_C%d" % i, [128, 512], F32) for i in range(4)]
        Y = [sb("D_Y%d" % i, [128, 12, 512], F32) for i in range(2)]
        St = [sb("D_S%d" % i, [128, 4, 128], F32) for i in range(NSEQ)]
        Sb = [sb("D_Sb%d" % i, [128, 4, 128], BF16) for i in range(NSEQ)]

        def t2(name, shape, dtype):
            return [sb("%s%d" % (name, i), shape, dtype) for i in range(NB)]
        tg = t2("D_tg", [64, 8], F32)
        gt = t2("D_gt", [64, 24], F32)
        zt = t2("D_zt", [64, 4, 128], F32)
        gs = t2("D_gs", [64, 4, 128], F32)
        QK = t2("D_QK", [64, 8, 128], F32)
        Vt = t2("D_Vt", [64, 4, 128], F32)
        sqq = t2("D_sqq", [64, 8, 128], F32)
        nrm = t2("D_nrm", [64, 16], F32)
        qdk = t2("D_qdk", [64, 4, 128], F32)
        TT = t2("D_TT", [128, 12, 64], BF16)
        kd = t2("D_kd", [64, 4, 128], BF16)
        bk = t2("D_bk", [64, 4, 128], BF16)
        bv = t2("D_bv", [64, 4, 128], BF16)
        gU = t2("D_gU", [64, 8, 64], F32)
        dec = t2("D_dec", [64, 4, 64], F32)
        Mm = t2("D_M", [64, 4, 64], F32)
        QKm = t2("D_QKm", [64, 4, 64], F32)
        QKT = t2("D_QKT", [64, 4, 64], BF16)
        Qa = t2("D_Qa", [64, 4, 64], F32)
        Qta = t2("D_Qta", [64, 4, 64], F32)
        Qb = t2("D_Qb", [64, 4, 64], F32)
        Qtb = t2("D_Qtb", [64, 4, 64], F32)
        Bt = t2("D_Bt", [64, 4, 64], F32)
        Tb = t2("D_Tb", [64, 4, 64], BF16)
        u0 = t2("D_u0", [64, 4, 128], F32)
        ub = t2("D_ub", [64, 4, 128], BF16)
        wkT = t2("D_wkT", [128, 4, 64], BF16)
        glb = t2("D_glb", [128, 4], F32)
        osb = t2("D_osb", [64, 4, 128], F32)
        osq = t2("D_osq", [64, 4, 128], F32)
        identI = sb("D_I", [64, 4, 64], F32)
        for h in range(4):
            S.op("pool", lambda e, h=h: e.tensor_copy(out=identI[:, h, :], in_=self.ident_f[:64, :64]),
                 reads=["ident_f"], writes=["D_I"])

        def v4(ps, w):
            return ps[:64, :4 * w].rearrange("p (h x) -> p h x", h=4)

        def bc(ap_h1, w):
            a = ap_h1 if len(ap_h1.shape) == 3 else ap_h1.unsqueeze(2)
            return a.to_broadcast([64, 4, w])

        def seq_thread(s):
            pb = lambda i: 4 * s + i % 4
            for blk8 in range(SEQ // 512):
                ys = (blk8 * NSEQ + s) % 2
                tb = s * SEQ + blk8 * 512
                for ct in range(12):
                    xi = 2 * s + ct % 2
                    x = X[xi]
                    kx = ("D_X", xi)
                    if blk8 == 0:
                        S.op("pool", lambda e, x=x: e.memset(x[:, 0:3], 0.0), writes=[(kx, "h")])
                        S.dma(out=x[:, 3:515], in_=self.pfeat[ct * 128:(ct + 1) * 128, tb:tb + 512],
                              sem="D_X%d" % xi, writes=[(kx, "b")])
                    else:
                        S.dma(out=x[:, :], in_=self.pfeat[ct * 128:(ct + 1) * 128, tb - 3:tb + 512],
                              sem="D_X%d" % xi, writes=[(kx, "h"), (kx, "b")])
                    c, kc = Cc[xi], ("D_C", xi)
                    S.op("act", lambda e, x=x, c=c, ct=ct: e.activation(
                        out=c[:], in_=x[:, 0:512], func=AF.Copy, scale=cw[:, 0, ct:ct + 1]),
                        reads=[(kx, "h"), (kx, "b"), "D_cw"], writes=[kc])
                    for tap in (1, 2, 3):
                        S.op("dve", lambda e, x=x, c=c, ct=ct, tap=tap: e.scalar_tensor_tensor(
                            out=c[:], in0=x[:, tap:tap + 512], scalar=cw[:, tap, ct:ct + 1],
                            in1=c[:], op0=ALU.mult, op1=ALU.add),
                            reads=[(kx, "h"), (kx, "b"), "D_cw", kc], writes=[kc])
                    S.op("act", lambda e, c=c, ct=ct, ys=ys: e.activation(out=Y[ys][:, ct, :],
                                                                           in_=c[:], func=AF.Silu),
                         reads=[kc], writes=[("D_Y", ys, ct)])
                ykeys = [("D_Y", ys, ct) for ct in range(12)]
                for cn in range(8):
                    b = s
                    c0 = cn * 64
                    tok = tb + c0
                    K = lambda nm: (nm, b)
                    first = (blk8 == 0 and cn == 0)
                    if first:
                        S.op("pool", lambda e, s=s: e.memset(St[s][:], 0.0), writes=[("D_S", s)])
                        S.op("pool", lambda e, s=s: e.memset(Sb[s][:], 0.0), writes=[("D_Sb", s)])
                    S.dma(out=tg[b][:], in_=self.ptok[tok:tok + 64, TM_AB:TM_AB + 8],
                          sem="D_tg%d" % b, writes=[K("tg")])
                    S.dma(out=zt[b][:].rearrange("p h v -> p (h v)"),
                          in_=self.ptok[tok:tok + 64, TM_AZ:TM_AZ + 512], sem="D_zt%d" % b,
                          writes=[K("zt")])
                    G = gt[b]
                    S.op("act", lambda e, b=b, G=G: e.activation(out=G[:, 0:4], in_=tg[b][:, 0:4],
                                                                  func=AF.Sigmoid),
                         reads=[K("tg")], writes=[K("beta")])
                    S.op("dve", lambda e, b=b, G=G: e.tensor_tensor(out=G[:, 20:24], in0=tg[b][:, 4:8],
                                                                     in1=dtb[:], op=ALU.add),
                         reads=[K("tg"), "D_dtb"], writes=[K("gtmp")])
                    S.op("act", lambda e, G=G: e.activation(out=G[:, 20:24], in_=G[:, 20:24],
                                                             func=AF.Exp),
                         reads=[K("gtmp")], writes=[K("gtmp")])
                    S.op("act", lambda e, G=G: e.activation(out=G[:, 20:24], in_=G[:, 20:24],
                                                             func=AF.Ln, bias=1.0),
                         reads=[K("gtmp")], writes=[K("gtmp")])
                    S.op("dve", lambda e, G=G: e.tensor_tensor(out=G[:, 4:8], in0=G[:, 20:24],
                                                                in1=alog[:], op=ALU.mult),
                         reads=[K("gtmp"), "D_alog"], writes=[K("g")])
                    pg, kpg = self.ps8[pb(5)], ("ps", pb(5))

                    def mmg(e, G=G, pg=pg):
                        e.matmul(pg[:64, 0:4], lhsT=U64[:, :], rhs=G[:, 4:8], start=True, stop=True)
                        e.matmul(pg[:64, 4:8], lhsT=UL[:, :], rhs=G[:, 4:8], start=True, stop=True)
                        return e.matmul(pg[:, 8:12], lhsT=self.ones_f[:64, :], rhs=G[:, 4:8],
                                        start=True, stop=True)
                    S.op("pe", mmg, reads=[K("g"), "D_U", "D_UL", "ones_f"], writes=[kpg])
                    S.op("act", lambda e, G=G, pg=pg: e.activation(out=G[:, 8:16], in_=pg[:64, 0:8],
                                                                    func=AF.Exp),
                         reads=[kpg], writes=[K("eG")])
                    S.op("act", lambda e, b=b, pg=pg: e.activation(out=glb[b][:], in_=pg[:, 8:12],
                                                                    func=AF.Exp),
                         reads=[kpg], writes=[K("glb")])
                    S.op("dve", lambda e, G=G: e.tensor_tensor(out=G[:, 16:20], in0=G[:, 0:4],
                                                                in1=G[:, 8:12], op=ALU.mult),
                         reads=[K("beta"), K("eG")], writes=[K("beG")])
                    S.op("dve", lambda e, b=b, G=G: e.tensor_tensor(
                        out=gU[b][:, 0:4, :], in0=U64[:].unsqueeze(1).to_broadcast([64, 4, 64]),
                        in1=bc(G[:, 4:8], 64), op=ALU.mult), reads=["D_U", K("g")],
                        writes=[K("gU")])
                    S.op("pool", lambda e, b=b: e.tensor_scalar(out=gU[b][:, 4:8, :],
                                                                 in0=gU[b][:, 0:4, :], scalar1=-1.0,
                                                                 scalar2=None, op0=ALU.mult),
                         reads=[K("gU")], writes=[K("ngU")])
                    pd, kpd = self.ps8[pb(4)], ("ps", pb(4))

                    def mmd(e, b=b, pd=pd):
                        ins = None
                        for h in range(4):
                            e.matmul(pd[:64, h * 64:(h + 1) * 64], lhsT=gU[b][:, h, :],
                                     rhs=self.ones_f[:64, :64], start=True, stop=False)
                            ins = e.matmul(pd[:64, h * 64:(h + 1) * 64], lhsT=self.ones_f[:64, :64],
                                           rhs=gU[b][:, 4 + h, :], start=False, stop=True)
                        return ins
                    S.op("pe", mmd, reads=[K("gU"), K("ngU"), "ones_f"], writes=[kpd])
                    S.op("dve", lambda e, b=b, pd=pd: e.tensor_tensor(
                        out=dec[b][:], in0=v4(pd, 64),
                        in1=mb[:].unsqueeze(1).to_broadcast([64, 4, 64]), op=ALU.add),
                        reads=[kpd, "D_mb"], writes=[K("dec")])
                    S.op("act", lambda e, b=b: e.activation(out=dec[b][:], in_=dec[b][:],
                                                             func=AF.Exp),
                         reads=[K("dec")], writes=[K("dec")])
                    for grp in range(3):
                        pt, kpt = self.ps8[pb(grp)], ("ps", pb(grp))

                        def trq(e, grp=grp, pt=pt):
                            ins = None
                            for h in range(4):
                                ins = e.transpose(out=pt[:64, h * 128:(h + 1) * 128],
                                                  in_=Y[ys][:, grp * 4 + h, c0:c0 + 64],
                                                  identity=self.ident_f[:, :])
                            return ins
                        S.op("pe", trq, reads=ykeys + ["ident_f"], writes=[kpt])
                        dstv = Vt[b][:] if grp == 2 else QK[b][:, grp * 4:(grp + 1) * 4, :]
                        S.op("act", lambda e, dstv=dstv, pt=pt: e.copy(out=dstv, in_=v4(pt, 128)),
                             reads=[kpt], writes=[K("QK%d" % grp)])
                    S.op("pool", lambda e, b=b: e.tensor_tensor(out=sqq[b][:], in0=QK[b][:],
                                                                 in1=QK[b][:], op=ALU.mult),
                         reads=[K("QK0"), K("QK1")], writes=[K("sqq")])
                    S.op("dve", lambda e, b=b: e.tensor_reduce(out=nrm[b][:, 0:8], in_=sqq[b][:],
                                                                axis=AX.X, op=ALU.add),
                         reads=[K("sqq")], writes=[K("nrm")])
                    S.op("dve", lambda e, b=b: e.tensor_scalar(out=nrm[b][:, 0:8], in0=nrm[b][:, 0:8],
                                                                scalar1=1.0e-6, scalar2=None,
                                                                op0=ALU.add),
                         reads=[K("nrm")], writes=[K("nrm")])
                    S.op("act", lambda e, b=b: e.sqrt(out=nrm[b][:, 0:8], in_=nrm[b][:, 0:8]),
                         reads=[K("nrm")], writes=[K("nrm")])
                    S.op("dve", lambda e, b=b: e.reciprocal(out=nrm[b][:, 8:16], in_=nrm[b][:, 0:8]),
                         reads=[K("nrm")], writes=[K("rn")])
                    S.op("dve", lambda e, b=b: e.tensor_scalar(out=nrm[b][:, 8:12],
                                                                in0=nrm[b][:, 8:12],
                                                                scalar1=128.0 ** -0.5, scalar2=None,
                                                                op0=ALU.mult),
                         reads=[K("rn")], writes=[K("rn")])
                    S.op("dve", lambda e, b=b: e.tensor_tensor(
                        out=QK[b][:], in0=QK[b][:],
                        in1=nrm[b][:, 8:16].unsqueeze(2).to_broadcast([64, 8, 128]), op=ALU.mult),
                        reads=[K("QK0"), K("QK1"), K("rn")], writes=[K("QKn")])
                    for h in range(4):
                        for dst_, src_, col, kd_, kr_ in ((qdk[b], QK[b][:, h, :], 8, "qdk", "eG"),
                                                       (kd[b], QK[b][:, 4 + h, :], 12, "kd", "eG"),
                                                       (bk[b], QK[b][:, 4 + h, :], 16, "bk", "beG"),
                                                       (bv[b], Vt[b][:, h, :], 0, "bv", "beta")):
                            S.op("dve", lambda e, dst_=dst_, src_=src_, col=col, h=h, G=G:
                                 e.tensor_scalar(out=dst_[:, h, :], in0=src_,
                                                 scalar1=G[:, col + h:col + h + 1], scalar2=None,
                                                 op0=ALU.mult),
                                 reads=[K("QKn"), K("QK2"), K(kr_)], writes=[(K(kd_), h)])
                    for grp, (src, ksrc) in enumerate(((qdk[b][:], [*[(K("qdk"), h_) for h_ in range(4)]]),
                                                       (QK[b][:, 4:8, :], [K("QKn")]),
                                                       (QK[b][:, 0:4, :], [K("QKn")]))):
                        pt, kpt = self.ps8[pb(grp)], ("ps", pb(grp))

                        def trt(e, src=src, pt=pt):
                            ins = None
                            for h in range(4):
                                ins = e.transpose(out=pt[:, h * 64:(h + 1) * 64], in_=src[:, h, :],
                                                  identity=self.ident_f[:64, :64])
                            return ins
                        S.op("pe", trt, reads=ksrc + ["ident_f"], writes=[kpt])
                        self.evac(grp, TT[b][:, grp * 4:(grp + 1) * 4, :],
                                  pt[:, :256].rearrange("p (h t) -> p h t", h=4), [kpt],
                                  [K("TT%d" % grp)])
                    pk, kpk = self.ps8[pb(3)], ("ps", pb(3))

                    def mmk(e, b=b, pk=pk):
                        ins = None
                        for h in range(4):
                            e.matmul(pk[:64, h * 64:(h + 1) * 64], lhsT=TT[b][:, 4 + h, :],
                                     rhs=TT[b][:, 4 + h, :], start=True, stop=True)
                            ins = e.matmul(pk[:64, 256 + h * 64:256 + (h + 1) * 64],
                                           lhsT=TT[b][:, 8 + h, :], rhs=TT[b][:, 4 + h, :],
                                           start=True, stop=True)
                        return ins
                    S.op("pe", mmk, reads=[K("TT1"), K("TT2")], writes=[kpk])
                    S.op("dve", lambda e, b=b, pk=pk: e.tensor_tensor(
                        out=Mm[b][:], in0=v4(pk, 64), in1=dec[b][:], op=ALU.mult),
                        reads=[kpk, K("dec")], writes=[K("M")])
                    S.op("dve", lambda e, b=b, pk=pk: e.tensor_tensor(
                        out=QKm[b][:], in0=pk[:64, 256:512].rearrange("p (h x) -> p h x", h=4),
                        in1=dec[b][:], op=ALU.mult), reads=[kpk, K("dec")], writes=[K("QKm")])
                    S.op("pool", lambda e, b=b: e.tensor_tensor(
                        out=Mm[b][:], in0=Mm[b][:],
                        in1=Lst[:].unsqueeze(1).to_broadcast([64, 4, 64]), op=ALU.mult),
                        reads=[K("M"), "D_Lst"], writes=[K("M")])
                    S.op("dve", lambda e, b=b, G=G: e.tensor_tensor(
                        out=Mm[b][:], in0=Mm[b][:], in1=bc(G[:, 0:4], 64), op=ALU.mult),
                        reads=[K("M"), K("beta")], writes=[K("M")])
                    pn, kpn = self.ps8[pb(0)], ("ps", pb(0))

                    def trn(e, b=b, pn=pn):
                        ins = None
                        for h in range(4):
                            e.transpose(out=pn[:64, h * 64:(h + 1) * 64], in_=Mm[b][:, h, :],
                                        identity=self.ident_f[:64, :64])
                            ins = e.transpose(out=pn[:64, 256 + h * 64:256 + (h + 1) * 64],
                                              in_=QKm[b][:, h, :], identity=self.ident_f[:64, :64])
                        return ins
                    S.op("pe", trn, reads=[K("M"), K("QKm"), "ident_f"], writes=[kpn])
                    S.op("act", lambda e, b=b, pn=pn: e.copy(out=Qa[b][:], in_=v4(pn, 64)),
                         reads=[kpn], writes=[K("Qa")])
                    S.op("act", lambda e, b=b, pn=pn: e.copy(
                        out=QKT[b][:], in_=pn[:64, 256:512].rearrange("p (h x) -> p h x", h=4)),
                        reads=[kpn], writes=[K("QKT")])
                    S.op("dve", lambda e, b=b: e.tensor_tensor(out=Bt[b][:], in0=identI[:],
                                                                in1=Qa[b][:], op=ALU.subtract),
                         reads=[K("Qa"), "D_I"], writes=[K("Bt")])
                    Q, Qt, kQ, kQt = Qa[b], Mm[b], K("Qa"), K("M")
                    alt = [(Qb[b], Qtb[b], K("Qb"), K("Qtb")), (Qa[b], Qta[b], K("Qa"), K("Qta"))]
                    for step in range(5):
                        Q2, Qt2, kQ2, kQt2 = alt[step % 2]
                        p1, kp1 = self.ps8[pb(1)], ("ps", pb(1))

                        def mq(e, Q=Q, Qt=Qt, p1=p1, step=step):
                            ins = None
                            for h in range(4):
                                ins = e.matmul(p1[:64, h * 64:(h + 1) * 64], lhsT=Q[:, h, :],
                                               rhs=Qt[:, h, :], start=True, stop=True)
                                if step < 4:
                                    ins = e.matmul(p1[:64, 256 + h * 64:256 + (h + 1) * 64],
                                                   lhsT=Qt[:, h, :], rhs=Q[:, h, :], start=True,
                                                   stop=True)
                            return ins
                        S.op("pe", mq, reads=[kQ, kQt], writes=[kp1])
                        S.op("act", lambda e, Qt2=Qt2, p1=p1: e.copy(out=Qt2[:], in_=v4(p1, 64)),
                             reads=[kp1], writes=[kQt2])
                        if step < 4:
                            S.op("act", lambda e, Q2=Q2, p1=p1: e.copy(
                                out=Q2[:], in_=p1[:64, 256:512].rearrange("p (h x) -> p h x", h=4)),
                                reads=[kp1], writes=[kQ2])
                        p2, kp2 = self.ps8[pb(2)], ("ps", pb(2))

                        def mbm(e, Qt2=Qt2, b=b, p2=p2):
                            ins = None
                            for h in range(4):
                                ins = e.matmul(p2[:64, h * 64:(h + 1) * 64], lhsT=Qt2[:, h, :],
                                               rhs=Bt[b][:, h, :], start=True, stop=True)
                            return ins
                        S.op("pe", mbm, reads=[kQt2, K("Bt")], writes=[kp2])
                        S.op("dve", lambda e, b=b, p2=p2: e.tensor_tensor(out=Bt[b][:], in0=Bt[b][:],
                                                                           in1=v4(p2, 64),
                                                                           op=ALU.add),
                             reads=[kp2, K("Bt")], writes=[K("Bt")])
                        Q, Qt, kQ, kQt = Q2, Qt2, kQ2, kQt2
                    S.op("act", lambda e, b=b: e.copy(out=Tb[b][:], in_=Bt[b][:]), reads=[K("Bt")],
                         writes=[K("Tb")])
                    pu0, kpu0 = self.ps8[pb(3)], ("ps", pb(3))

                    def mu0(e, b=b, pu0=pu0):
                        ins = None
                        for h in range(4):
                            ins = e.matmul(pu0[:64, h * 128:(h + 1) * 128], lhsT=Tb[b][:, h, :],
                                           rhs=bv[b][:, h, :], start=True, stop=True)
                        return ins
                    S.op("pe", mu0, reads=[K("Tb"), *[(K("bv"), h_) for h_ in range(4)]], writes=[kpu0])
                    S.op("act", lambda e, b=b, pu0=pu0: e.copy(out=u0[b][:], in_=v4(pu0, 128)),
                         reads=[kpu0], writes=[K("u0")])
                    pw, kpw = self.ps8[pb(4)], ("ps", pb(4))

                    def mwk(e, b=b, pw=pw):
                        ins = None
                        for h in range(4):
                            ins = e.matmul(pw[:, h * 64:(h + 1) * 64], lhsT=bk[b][:, h, :],
                                           rhs=Tb[b][:, h, :], start=True, stop=True)
                        return ins
                    S.op("pe", mwk, reads=[K("Tb"), *[(K("bk"), h_) for h_ in range(4)]], writes=[kpw])
                    S.op("act", lambda e, b=b, pw=pw: e.copy(
                        out=wkT[b][:], in_=pw[:, :256].rearrange("p (h t) -> p h t", h=4)),
                        reads=[kpw], writes=[K("wkT")])
                    pu, kpu = self.ps8[pb(5)], ("ps", pb(5))

                    def mpu(e, b=b, pu=pu, s=s):
                        ins = None
                        for h in range(4):
                            ins = e.matmul(pu[:64, h * 128:(h + 1) * 128], lhsT=wkT[b][:, h, :],
                                           rhs=Sb[s][:, h, :], start=True, stop=True)
                        return ins
                    S.op("pe", mpu, reads=[K("wkT"), ("D_Sb", s)], writes=[kpu])
                    S.op("dve", lambda e, b=b, pu=pu: e.tensor_tensor(out=ub[b][:], in0=u0[b][:],
                                                                       in1=v4(pu, 128),
                                                                       op=ALU.subtract),
                         reads=[K("u0"), kpu], writes=[K("ub")])
                    po, kpo = self.ps8[pb(0)], ("ps", pb(0))

                    def mpo(e, b=b, po=po, s=s):
                        ins = None
                        for h in range(4):
                            e.matmul(po[:64, h * 128:(h + 1) * 128], lhsT=TT[b][:, h, :],
                                     rhs=Sb[s][:, h, :], start=True, stop=False)
                            ins = e.matmul(po[:64, h * 128:(h + 1) * 128], lhsT=QKT[b][:, h, :],
                                           rhs=ub[b][:, h, :], start=False, stop=True)
                        return ins
                    S.op("pe", mpo, reads=[K("TT0"), ("D_Sb", s), K("QKT"), K("ub")], writes=[kpo])
                    psn, kpsn = self.ps8[pb(1)], ("ps", pb(1))

                    def mps(e, b=b, psn=psn):
                        ins = None
                        for h in range(4):
                            ins = e.matmul(psn[:, h * 128:(h + 1) * 128], lhsT=kd[b][:, h, :],
                                           rhs=ub[b][:, h, :], start=True, stop=True)
                        return ins
                    S.op("pe", mps, reads=[*[(K("kd"), h_) for h_ in range(4)], K("ub")], writes=[kpsn])
                    for h in range(4):
                        S.op("dve", lambda e, b=b, s=s, h=h, psn=psn: e.scalar_tensor_tensor(
                            out=St[s][:, h, :], in0=St[s][:, h, :], scalar=glb[b][:, h:h + 1],
                            in1=psn[:, h * 128:(h + 1) * 128], op0=ALU.mult, op1=ALU.add),
                            reads=[("D_S", s), K("glb"), kpsn], writes=[("D_S", s)])
                    S.op("act", lambda e, s=s: e.copy(out=Sb[s][:], in_=St[s][:]),
                         reads=[("D_S", s)], writes=[("D_Sb", s)])
                    S.op("act", lambda e, b=b: e.activation(out=gs[b][:], in_=zt[b][:], func=AF.Silu),
                         reads=[K("zt")], writes=[K("gs")])
                    S.op("pool", lambda e, b=b: e.tensor_tensor(
                        out=gs[b][:], in0=gs[b][:],
                        in1=ng[:].unsqueeze(1).to_broadcast([64, 4, 128]), op=ALU.mult),
                        reads=[K("gs"), "D_ng"], writes=[K("gs")])
                    S.op("act", lambda e, b=b, po=po: e.copy(out=osb[b][:], in_=v4(po, 128)),
                         reads=[kpo], writes=[K("osb")])
                    S.op("pool", lambda e, b=b: e.tensor_tensor(out=osq[b][:], in0=osb[b][:],
                                                                 in1=osb[b][:], op=ALU.mult),
                         reads=[K("osb")], writes=[K("osq")])
                    S.op("dve", lambda e, b=b: e.tensor_reduce(out=nrm[b][:, 0:4], in_=osq[b][:],
                                                                axis=AX.X, op=ALU.add),
                         reads=[K("osq")], writes=[K("oss")])
                    self.rstd_from_ss(nrm[b][:, 0:4], K("oss"), nrm[b][:, 4:8], K("ors"), 128)
                    S.op("dve", lambda e, b=b: e.tensor_tensor(out=osb[b][:], in0=osb[b][:],
                                                                in1=bc(nrm[b][:, 4:8], 128),
                                                                op=ALU.mult),
                         reads=[K("osb"), K("ors")], writes=[K("osb")])
                    S.op("pool", lambda e, b=b: e.tensor_tensor(out=osq[b][:], in0=osb[b][:],
                                                                 in1=gs[b][:], op=ALU.mult),
                         reads=[K("osb"), K("gs")], writes=[K("osq")])
                    S.dma(out=self.mixin[tok:tok + 64, 0:512],
                          in_=osq[b][:].rearrange("p h v -> p (h v)"), sem="D_out%d" % b,
                          reads=[K("osq")])
        S.interleave([lambda s=s: seq_thread(s) for s in range(NSEQ)])
        S.barrier()


def build_program():
    P = Prog()
    src = P.x_in
    for l in range(DEPTH):
        dst = P.xmid if l < DEPTH - 1 else P.y
        P.phase_A(l, src)
        P.phase_D(l)
        P.phase_S(l)
        P.phase_H(l)
        P.phase_E(l, src)
        P.phase_F(l, dst)
        src = dst
    return P


def kernel(**inputs):
    n = 8
    P = build_program()
    x = np.ascontiguousarray(inputs["x"], dtype=np.float32)
    shared = {k: np.ascontiguousarray(v, dtype=np.float32) for k, v in inputs.items() if k != "x"}
    in_maps = []
    for c in range(n):
        m = dict(shared)
        m["x"] = np.ascontiguousarray(x[c * NSEQ:(c + 1) * NSEQ].reshape(NTOK, D))
        in_maps.append(m)
    res = run_bass_kernel_spmd(P.nc, in_maps, core_ids=list(range(n)))
    out = np.stack([np.asarray(r["y"]).reshape(NSEQ, SEQ, D) for r in res.results], axis=0)
    return out.reshape(n * NSEQ, SEQ, D).astype(np.float32)
```

```python
import numpy as np
import concourse.bass as bass
import concourse.mybir as mybir
from concourse.bass_utils import run_bass_kernel_spmd

F32 = mybir.dt.float32
BF16 = mybir.dt.bfloat16
AF = mybir.ActivationFunctionType
ALU = mybir.AluOpType
AX = mybir.AxisListType

D = 1024
SEQ = 4096
NSEQ = 2
NTOK = NSEQ * SEQ
DEPTH = 2
D_IN = 3788
D_FF = 2816
EPS = 1e-6
NEG = -30000.0

TM_GROUPS = [(1536, 2056), (2376, 2440), (2760, 2764), (3020, 3788)]
TM_W = sum(b - a for a, b in TM_GROUPS)
TM_AZ, TM_AB, TM_AA = 0, 512, 516
TM_BV = 520
TM_WI = 584
TM_CF, TM_CI, TM_CG = 588, 844, 1100
FM_GROUPS = [(i * 128, 128) for i in range(12)] + [(2056, 128), (2184, 128), (2312, 64),
             (2440, 128), (2568, 128), (2696, 64), (2764, 128), (2892, 128), (3020, 128), (3148, 128)]
FM_ROW = {}
_r = 0
for _c, _n in FM_GROUPS:
    FM_ROW[_c] = _r
    _r += _n
FM_H = _r


class Sched:
    def __init__(self, nc):
        self.nc = nc
        self.eng = {"pe": nc.tensor, "act": nc.scalar, "dve": nc.vector, "pool": nc.gpsimd,
                    "sp": nc.sync}
        self.sems = {}
        self.cnt = {}
        for k in ("pe", "act", "dve", "pool"):
            self.sems[k] = nc.alloc_semaphore("s_" + k)
            self.cnt[k] = 0
        self.seen = {k: {} for k in self.eng}
        self.bufs = {}
        self.ninstr = 0

    def _buf(self, key):
        b = self.bufs.get(key)
        if b is None:
            b = {"w": None, "r": {}}
            self.bufs[key] = b
        return b

    def _deps(self, engine, reads, writes):
        deps = {}

        def add(ev, same_ok):
            if ev is None:
                return
            sk, val = ev
            if sk == engine and not same_ok:
                return
            if deps.get(sk, 0) < val:
                deps[sk] = val

        for k in reads:
            b = self._buf(k)
            add(b["w"], engine != "pe")
            if isinstance(k, tuple) and k[0] in ("ps", "psb"):
                for sk, val in b["r"].items():
                    add((sk, val), False)
        for k in writes:
            b = self._buf(k)
            add(b["w"], engine != "pe")
            for sk, val in b["r"].items():
                add((sk, val), False)
        return deps

    def _emit_waits(self, engine, deps):
        e = self.eng[engine]
        seen = self.seen[engine]
        for sk, val in deps.items():
            if seen.get(sk, 0) >= val:
                continue
            e.wait_ge(self.sems[sk], val)
            self.ninstr += 1
            seen[sk] = val

    def _record(self, ev, reads, writes):
        for k in writes:
            b = self._buf(k)
            b["w"] = ev
            b["r"] = {}
        for k in reads:
            b = self._buf(k)
            if b["r"].get(ev[0], 0) < ev[1]:
                b["r"][ev[0]] = ev[1]

    def op(self, engine, fn, reads=(), writes=()):
        deps = self._deps(engine, reads, writes)
        self._emit_waits(engine, deps)
        ins = fn(self.eng[engine])
        self.cnt[engine] += 1
        ins.then_inc(self.sems[engine], 1)
        self.ninstr += 1
        self._record((engine, self.cnt[engine]), reads, writes)
        self._yield()

    def dma(self, out, in_, sem, reads=(), writes=(), q="sp"):
        if sem not in self.sems:
            self.sems[sem] = self.nc.alloc_semaphore("d_" + sem)
            self.cnt[sem] = 0
        deps = self._deps(q, reads, writes)
        self._emit_waits(q, deps)
        ins = self.eng[q].dma_start(out=out, in_=in_)
        self.cnt[sem] += 16
        ins.then_inc(self.sems[sem], 16)
        self.ninstr += 1
        self._record((sem, self.cnt[sem]), reads, writes)
        self._yield()

    def interleave(self, fns):
        import threading
        n = len(fns)
        st = {"cur": 0, "alive": [True] * n, "err": None}
        cond = threading.Condition()

        def nxt(i):
            for d in range(1, n + 1):
                k = (i + d) % n
                if st["alive"][k]:
                    return k
            return -1

        def pass_turn(me):
            with cond:
                st["cur"] = nxt(me)
                cond.notify_all()
                while st["alive"][me] and st["cur"] != me and st["err"] is None:
                    cond.wait()
                if st["err"] is not None and st["alive"][me]:
                    raise RuntimeError("interleave aborted")

        def worker(i):
            with cond:
                while st["cur"] != i and st["err"] is None:
                    cond.wait()
            try:
                if st["err"] is None:
                    self._tl.me = i
                    fns[i]()
            except BaseException as e:
                if st["err"] is None:
                    st["err"] = e
            finally:
                with cond:
                    st["alive"][i] = False
                    if st["cur"] == i:
                        st["cur"] = nxt(i)
                    cond.notify_all()

        self._tl = threading.local()
        self._pass = pass_turn
        ths = [threading.Thread(target=worker, args=(i,)) for i in range(n)]
        for t in ths:
            t.start()
        for t in ths:
            t.join()
        self._pass = None
        if st["err"] is not None:
            raise st["err"]

    def _yield(self):
        p = getattr(self, "_pass", None)
        if p is not None:
            p(self._tl.me)

    def barrier(self):
        allv = {k: v for k, v in self.cnt.items() if v > 0}
        for engine in self.eng:
            self._emit_waits(engine, dict(allv))
        self.bufs = {}


def bcast_rows(ap2d_row, nparts):
    return ap2d_row.partition_broadcast(nparts)


class Prog:
    def __init__(self, layers=(0, 1), phases="AHDSEF", dbg=()):
        self.nc = nc = bass.Bass("TRN2", target_bir_lowering=False)
        self.S = Sched(nc)
        self.dbg = dbg
        dt = nc.dram_tensor
        self.x_in = dt("x", [NTOK, D], F32, kind="ExternalInput").ap()
        self.w_in = dt("w_in", [DEPTH, D, D_IN], F32, kind="ExternalInput").ap()
        self.dn_conv = dt("dn_conv", [DEPTH, 4, 1536], F32, kind="ExternalInput").ap()
        self.dn_a_log = dt("dn_a_log", [DEPTH, 4], F32, kind="ExternalInput").ap()
        self.dn_dt_bias = dt("dn_dt_bias", [DEPTH, 4], F32, kind="ExternalInput").ap()
        self.dn_norm = dt("dn_norm", [DEPTH, 128], F32, kind="ExternalInput").ap()
        self.hg_lb = dt("hg_lb", [DEPTH, 256], F32, kind="ExternalInput").ap()
        self.hg_norm = dt("hg_norm", [DEPTH, 64], F32, kind="ExternalInput").ap()
        self.w_out = dt("w_out", [DEPTH, D, D], F32, kind="ExternalInput").ap()
        self.g_mix_pre = dt("g_mix_pre", [DEPTH, D], F32, kind="ExternalInput").ap()
        self.g_mix_post = dt("g_mix_post", [DEPTH, D], F32, kind="ExternalInput").ap()
        self.g_ffn_pre = dt("g_ffn_pre", [DEPTH, D], F32, kind="ExternalInput").ap()
        self.g_ffn_post = dt("g_ffn_post", [DEPTH, D], F32, kind="ExternalInput").ap()
        self.w_up = dt("ffn_w_up", [DEPTH, D, 2 * D_FF], F32, kind="ExternalInput").ap()
        self.ffn_conv = dt("ffn_conv", [DEPTH, 3, 2 * D_FF], F32, kind="ExternalInput").ap()
        self.w_down = dt("ffn_w_down", [DEPTH, D_FF, D], F32, kind="ExternalInput").ap()
        self.y = dt("y", [NTOK, D], F32, kind="ExternalOutput").ap()

        def scratch(name, shape, dtype):
            kind = "ExternalOutput" if name in dbg else "Internal"
            return dt(name, shape, dtype, kind=kind).ap()

        self.ptok = scratch("ptok", [NTOK, TM_W], F32)
        self.pfeat = scratch("pfeat", [FM_H, NTOK], F32)
        self.mixin = scratch("mixin", [NTOK, D], F32)
        self.x1 = scratch("x1", [NTOK, D], F32)
        self.h2T = scratch("h2T", [D, NTOK], BF16)
        self.xmid = scratch("xmid", [NTOK, D], F32)

        self.ps = [nc.alloc_psum_tensor("ps%d" % i, [128, 512], F32) for i in range(6)]
        self.psb = [nc.alloc_psum_tensor("psb%d" % i, [128, 1024], BF16) for i in range(2)]
        self.ps8 = [p[:, :] for p in self.ps] + [p[:, :].bitcast(F32) for p in self.psb]
        self.ident_b = nc.alloc_sbuf_tensor("ident_b", [128, 128], BF16)
        self.ident_f = nc.alloc_sbuf_tensor("ident_f", [128, 128], F32)
        self.ones_f = nc.alloc_sbuf_tensor("ones_f", [128, 128], F32)
        self._consts()
        self.sb_base = nc.sbuf_base
        self.layers = layers
        self.phases = phases

    def _consts(self):
        nc, S = self.nc, self.S
        S.op("pool", lambda e: e.memset(self.ones_f[:], 1.0), writes=["ones_f"])
        S.op("pool", lambda e: e.memset(self.ident_f[:], 0.0), writes=["ident_f"])
        S.op("pool", lambda e: e.affine_select(out=self.ident_f[:], in_=self.ident_f[:],
                                                pattern=[[-1, 128]], compare_op=ALU.not_equal,
                                                fill=1.0, base=0, channel_multiplier=1),
             reads=["ident_f"], writes=["ident_f"])
        S.op("dve", lambda e: e.tensor_copy(out=self.ident_b[:], in_=self.ident_f[:]),
             reads=["ident_f"], writes=["ident_b"])

    def sbuf_reset(self):
        self.nc.sbuf_base = self.sb_base

    def sb(self, name, shape, dtype):
        self._uid = getattr(self, "_uid", 0) + 1
        return self.nc.alloc_sbuf_tensor("%s_u%d" % (name, self._uid), shape, dtype)

    def load_bcast(self, name, row_ap, n):
        t = self.sb(name, [128, n], F32)
        self.S.dma(out=t[:], in_=bcast_rows(row_ap, 128), sem="ld_" + name, writes=[name])
        return t

    def load_weight_bf16(self, name, w_ap, K, N, stage, stage_keys):
        S = self.S
        wt = self.sb(name, [128, K, N], BF16)
        CH = stage[0].shape[1]
        i = 0
        for k in range(K):
            for c0 in range(0, N, CH):
                cw = min(CH, N - c0)
                st, sk = stage[i % 2], stage_keys[i % 2]
                S.dma(out=st[:, :cw], in_=w_ap[k * 128:(k + 1) * 128, c0:c0 + cw], sem=sk,
                      writes=[sk])
                eng = ("dve", "pool", "act")[i % 3]
                if eng == "act":
                    S.op("act", lambda e, st=st, k=k, c0=c0, cw=cw: e.copy(
                        out=wt[:, k, c0:c0 + cw], in_=st[:, :cw]), reads=[sk], writes=[name])
                else:
                    S.op(eng, lambda e, st=st, k=k, c0=c0, cw=cw: e.tensor_copy(
                        out=wt[:, k, c0:c0 + cw], in_=st[:, :cw]), reads=[sk], writes=[name])
                i += 1
        return wt

    def evac(self, i, out, in_, reads, writes):
        if i % 2 == 0:
            self.S.op("act", lambda e: e.copy(out=out, in_=in_), reads=reads, writes=writes)
        else:
            self.S.op("dve", lambda e: e.tensor_copy(out=out, in_=in_), reads=reads, writes=writes)

    def norm_transpose(self, src_dram, tok0, gbc, gkey, T, tag, xt, hb, hT, ss, rs, slot,
                       do_norm=True):
        S = self.S
        kx = lambda j: (tag + "xt", slot, j)
        for j in range(4):
            S.dma(out=xt[slot][:, j, :], in_=src_dram[tok0 + j * 128: tok0 + (j + 1) * 128, :],
                  sem="%sxt%d_%d" % (tag, slot, j), writes=[kx(j)])
        kss, krs = (tag + "ss", slot), (tag + "rs", slot)
        if do_norm:
            for j in range(4):
                S.op("act", lambda e, j=j: e.activation(out=T["junk"][:], in_=xt[slot][:, j, :],
                                                         func=AF.Square,
                                                         accum_out=ss[slot][:, j:j + 1]),
                     reads=[kx(j)], writes=[kss])
            S.op("dve", lambda e: e.tensor_scalar(out=rs[slot][:], in0=ss[slot][:],
                                                   scalar1=1.0 / D, scalar2=EPS, op0=ALU.mult,
                                                   op1=ALU.add), reads=[kss], writes=[krs])
            S.op("act", lambda e: e.sqrt(out=rs[slot][:], in_=rs[slot][:]), reads=[krs],
                 writes=[krs])
            S.op("dve", lambda e: e.reciprocal(out=rs[slot][:], in_=rs[slot][:]), reads=[krs],
                 writes=[krs])
        for j in range(4):
            khb = (tag + "hb", j % 2)
            hbj = hb[j % 2]
            if do_norm:
                S.op("dve", lambda e, j=j, hbj=hbj: e.scalar_tensor_tensor(
                    out=hbj[:], in0=xt[slot][:, j, :], scalar=rs[slot][:, j:j + 1], in1=gbc[:],
                    op0=ALU.mult, op1=ALU.mult), reads=[kx(j), krs, gkey], writes=[khb])
            else:
                S.op("pool", lambda e, j=j, hbj=hbj: e.tensor_copy(out=hbj[:],
                                                                  in_=xt[slot][:, j, :]),
                     reads=[kx(j)], writes=[khb])
            pb = self.psb[j % 2]
            kpb = ("psb", j % 2)

            def tr(e, hbj=hbj, pb=pb):
                ins = None
                for k in range(8):
                    ins = e.transpose(out=pb[:, k * 128:(k + 1) * 128],
                                      in_=hbj[:, k * 128:(k + 1) * 128], identity=self.ident_b[:])
                return ins
            S.op("pe", tr, reads=[khb, "ident_b"], writes=[kpb])
            self.evac(j, hT[slot][:, :, j * 128:(j + 1) * 128],
                      pb[:].rearrange("p (k t) -> p k t", k=8), [kpb], [(tag + "hT", slot, j)])

    def phase_A(self, l, xsrc):
        nc, S = self.nc, self.S
        self.sbuf_reset()
        stage = [self.sb("A_stage%d" % i, [128, 3788], F32) for i in range(2)]
        Wi = self.load_weight_bf16("A_Wi", self.w_in[l], 8, D_IN, stage, ["A_stg0", "A_stg1"])
        gbc = self.load_bcast("A_gbc", self.g_mix_pre[l:l + 1, :], D)
        S.barrier()
        xt = [self.sb("A_xt%d" % i, [128, 4, D], F32) for i in range(2)]
        hb = [self.sb("A_hb%d" % i, [128, D], BF16) for i in range(2)]
        hT = [self.sb("A_hT%d" % i, [128, 8, 512], BF16) for i in range(2)]
        ss = [self.sb("A_ss%d" % i, [128, 4], F32) for i in range(2)]
        rs = [self.sb("A_rs%d" % i, [128, 4], F32) for i in range(2)]
        T = {"junk": self.sb("A_junk", [128, D], F32)}
        ofm = [self.sb("A_ofm%d" % i, [128, 512], F32) for i in range(4)]
        otm = [self.sb("A_otm%d" % i, [128, TM_W], F32) for i in range(2)]
        tmch = []
        off = 0
        for a, b in TM_GROUPS:
            c = a
            while c < b:
                w = min(512, b - c)
                tmch.append((c, w, off))
                off += w
                c += w
        nev = 0
        for blk in range(NTOK // 512):
            slot = blk % 2
            tok0 = blk * 512
            self.norm_transpose(xsrc, tok0, gbc, "A_gbc", T, "A_", xt, hb, hT, ss, rs, slot)
            hkeys = [("A_hT", slot, j) for j in range(4)]
            for gi, (c0, n) in enumerate(FM_GROUPS):
                ps = self.ps[gi % 4]
                kps = ("ps", gi % 4)

                def mm(e, ps=ps, c0=c0, n=n):
                    ins = None
                    for k in range(8):
                        ins = e.matmul(ps[:n, :], lhsT=Wi[:, k, c0:c0 + n], rhs=hT[slot][:, k, :],
                                       start=(k == 0), stop=(k == 7))
                    return ins
                S.op("pe", mm, reads=["A_Wi"] + hkeys, writes=[kps])
                o = ofm[gi % 4]
                ko = ("A_ofm", gi % 4)
                self.evac(nev, o[:n, :], ps[:n, :], [kps], [ko])
                nev += 1
                r0 = FM_ROW[c0]
                S.dma(out=self.pfeat[r0:r0 + n, tok0:tok0 + 512], in_=o[:n, :],
                      sem="A_ofm%d" % (gi % 4), reads=[ko], q="pool")
            for j in range(4):
                o = otm[j % 2]
                ko = ("A_otm", j % 2)
                for ci, (c, w, dst) in enumerate(tmch):
                    ps = self.ps[4 + ci % 2]
                    kps = ("ps", 4 + ci % 2)

                    def mm(e, ps=ps, c=c, w=w, j=j):
                        ins = None
                        for k in range(8):
                            ins = e.matmul(ps[:, :w], lhsT=hT[slot][:, k, j * 128:(j + 1) * 128],
                                           rhs=Wi[:, k, c:c + w], start=(k == 0), stop=(k == 7))
                        return ins
                    S.op("pe", mm, reads=["A_Wi", hkeys[j]], writes=[kps])
                    self.evac(nev, o[:, dst:dst + w], ps[:, :w], [kps], [(ko, ci)])
                    nev += 1
                S.dma(out=self.ptok[tok0 + j * 128: tok0 + (j + 1) * 128, :], in_=o[:, :],
                      sem="A_otm%d" % (j % 2), reads=[(ko, ci) for ci in range(len(tmch))],
                      q="pool")
        S.barrier()

    def transp8(self, hbj, khb, dst, kdst, j):
        pb = self.psb[j % 2]
        kpb = ("psb", j % 2)

        def tr(e):
            ins = None
            for k in range(8):
                ins = e.transpose(out=pb[:, k * 128:(k + 1) * 128],
                                  in_=hbj[:, k * 128:(k + 1) * 128], identity=self.ident_b[:])
            return ins
        self.S.op("pe", tr, reads=[khb, "ident_b"], writes=[kpb])
        self.evac(j, dst, pb[:].rearrange("p (k t) -> p k t", k=8), [kpb], [kdst])

    def rstd_from_ss(self, ss, kss, rs, krs, n):
        S = self.S
        S.op("dve", lambda e: e.tensor_scalar(out=rs, in0=ss, scalar1=1.0 / n, scalar2=EPS,
                                               op0=ALU.mult, op1=ALU.add), reads=[kss],
             writes=[krs])
        S.op("act", lambda e: e.sqrt(out=rs, in_=rs), reads=[krs], writes=[krs])
        S.op("dve", lambda e: e.reciprocal(out=rs, in_=rs), reads=[krs], writes=[krs])

    def phase_E(self, l, xsrc):
        S = self.S
        self.sbuf_reset()
        stage = [self.sb("E_stage%d" % i, [128, 1024], F32) for i in range(2)]
        Wo = self.load_weight_bf16("E_Wo", self.w_out[l], 8, D, stage, ["E_stg0", "E_stg1"])
        gpost = self.load_bcast("E_gpost", self.g_mix_post[l:l + 1, :], D)
        gpre = self.load_bcast("E_gpre", self.g_ffn_pre[l:l + 1, :], D)
        S.barrier()
        xt = [self.sb("E_xt%d" % i, [128, 4, D], F32) for i in range(2)]
        xr = [self.sb("E_xr%d" % i, [128, 4, D], F32) for i in range(2)]
        hb = [self.sb("E_hb%d" % i, [128, D], BF16) for i in range(2)]
        mT = [self.sb("E_mT%d" % i, [128, 8, 512], BF16) for i in range(2)]
        h2s = [self.sb("E_h2s%d" % i, [128, 8, 512], BF16) for i in range(2)]
        yt = [self.sb("E_yt%d" % i, [128, D], F32) for i in range(2)]
        x1t = [self.sb("E_x1t%d" % i, [128, D], F32) for i in range(2)]
        h2b = [self.sb("E_h2b%d" % i, [128, D], BF16) for i in range(2)]
        junk = self.sb("E_junk", [128, D], F32)
        st = [self.sb("E_st%d" % i, [128, 8], F32) for i in range(2)]
        h2T_v = self.h2T.rearrange("(k p) t -> p k t", p=128)
        for blk in range(NTOK // 512):
            slot = blk % 2
            tok0 = blk * 512
            self.norm_transpose(self.mixin, tok0, None, None, None, "E_", xt, hb, mT, None, None,
                                slot, do_norm=False)
            for j in range(4):
                S.dma(out=xr[slot][:, j, :], in_=xsrc[tok0 + j * 128: tok0 + (j + 1) * 128, :],
                      sem="E_xr%d_%d" % (slot, j), writes=[("E_xr", slot, j)])
            for j in range(4):
                p2 = j % 2
                kss, krs = ("E_ss", p2), ("E_rs", p2)
                for hf in range(2):
                    ps = self.ps[2 * p2 + hf]
                    kps = ("ps", 2 * p2 + hf)

                    def mm(e, ps=ps, hf=hf, j=j):
                        ins = None
                        for k in range(8):
                            ins = e.matmul(ps[:, :], lhsT=mT[slot][:, k, j * 128:(j + 1) * 128],
                                           rhs=Wo[:, k, hf * 512:(hf + 1) * 512], start=(k == 0),
                                           stop=(k == 7))
                        return ins
                    S.op("pe", mm, reads=["E_Wo", ("E_hT", slot, j)], writes=[kps])
                    S.op("act", lambda e, ps=ps, hf=hf, p2=p2: e.activation(
                        out=junk[:, :512], in_=ps[:, :], func=AF.Square,
                        accum_out=st[p2][:, hf:hf + 1]), reads=[kps], writes=[(kss, hf)])
                S.op("dve", lambda e, p2=p2: e.tensor_tensor(out=st[p2][:, 2:3], in0=st[p2][:, 0:1],
                                                              in1=st[p2][:, 1:2], op=ALU.add),
                     reads=[(kss, 0), (kss, 1)], writes=[kss])
                self.rstd_from_ss(st[p2][:, 2:3], kss, st[p2][:, 3:4], krs, D)
                kyt = ("E_yt", p2)
                for hf in range(2):
                    ps = self.ps[2 * p2 + hf]
                    kps = ("ps", 2 * p2 + hf)
                    S.op("act", lambda e, ps=ps, hf=hf, p2=p2: e.activation(
                        out=yt[p2][:, hf * 512:(hf + 1) * 512], in_=ps[:, :], func=AF.Copy,
                        scale=st[p2][:, 3:4]), reads=[kps, krs], writes=[(kyt, hf)])
                S.op("dve", lambda e, p2=p2: e.tensor_tensor(out=yt[p2][:], in0=yt[p2][:],
                                                              in1=gpost[:], op=ALU.mult),
                     reads=[(kyt, 0), (kyt, 1), "E_gpost"], writes=[kyt])
                kx1 = ("E_x1t", p2)
                S.op("pool", lambda e, p2=p2, j=j: e.tensor_tensor(out=x1t[p2][:], in0=yt[p2][:],
                                                                    in1=xr[slot][:, j, :],
                                                                    op=ALU.add),
                     reads=[kyt, ("E_xr", slot, j)], writes=[kx1])
                S.dma(out=self.x1[tok0 + j * 128: tok0 + (j + 1) * 128, :], in_=x1t[p2][:],
                      sem="E_x1t%d" % p2, reads=[kx1], q="pool")
                kss2, krs2 = ("E_ss2", p2), ("E_rs2", p2)
                S.op("act", lambda e, p2=p2: e.activation(out=junk[:], in_=x1t[p2][:],
                                                           func=AF.Square,
                                                           accum_out=st[p2][:, 4:5]),
                     reads=[kx1], writes=[kss2])
                self.rstd_from_ss(st[p2][:, 4:5], kss2, st[p2][:, 5:6], krs2, D)
                kh2b = ("E_h2b", p2)
                S.op("dve", lambda e, p2=p2: e.scalar_tensor_tensor(
                    out=h2b[p2][:], in0=x1t[p2][:], scalar=st[p2][:, 5:6], in1=gpre[:],
                    op0=ALU.mult, op1=ALU.mult), reads=[kx1, krs2, "E_gpre"], writes=[kh2b])
                self.transp8(h2b[p2], kh2b, h2s[slot][:, :, j * 128:(j + 1) * 128],
                             ("E_h2s", slot, j), j)
            S.dma(out=h2T_v[:, :, tok0:tok0 + 512], in_=h2s[slot][:],
                  sem="E_h2s%d" % slot, reads=[("E_h2s", slot, j) for j in range(4)], q="pool")
        S.barrier()

    def load_convw(self, name, conv_ap, ntaps, ntiles):
        S = self.S
        raw = self.sb(name + "_raw", [ntiles, ntaps, 128], F32)
        cw = self.sb(name, [128, ntaps, ntiles], F32)
        S.dma(out=raw[:], in_=conv_ap.rearrange("j (t p) -> t j p", p=128), sem="ld_" + name,
              writes=[name + "_raw"])
        for j in range(ntaps):
            ps = self.ps[j % 2]
            kps = ("ps", j % 2)
            S.op("pe", lambda e, j=j, ps=ps: e.transpose(out=ps[:, :ntiles], in_=raw[:, j, :],
                                                         identity=self.ident_f[:ntiles, :ntiles]),
                 reads=[name + "_raw", "ident_f"], writes=[kps])
            S.op("dve", lambda e, j=j, ps=ps: e.tensor_copy(out=cw[:, j, :], in_=ps[:, :ntiles]),
                 reads=[kps], writes=[name])
        return cw

    def phase_F(self, l, dst):
        S = self.S
        self.sbuf_reset()
        NT = 22
        stage = [self.sb("F_stage%d" % i, [128, 512], F32) for i in range(2)]
        Wu = self.load_weight_bf16("F_Wu", self.w_up[l], 8, 2 * D_FF, stage, ["F_stg0", "F_stg1"])
        Wd = self.load_weight_bf16("F_Wd", self.w_down[l], NT, D, stage, ["F_stg0", "F_stg1"])
        gpost = self.load_bcast("F_gpost", self.g_ffn_post[l:l + 1, :], D)
        cw = self.load_convw("F_cw", self.ffn_conv[l], 3, 2 * NT)
        S.barrier()
        hT = self.sb("F_hT", [128, 8, 512], BF16)
        gT = self.sb("F_gT", [128, NT, 512], BF16)
        U = [self.sb("F_U%d" % i, [128, 514], F32) for i in range(2)]
        C = [self.sb("F_C%d" % i, [128, 512], F32) for i in range(2)]
        GL = self.sb("F_GL", [128, 512], F32)
        halo = self.sb("F_halo", [128, 2 * NT, 2], F32)
        x1t = [self.sb("F_x1t%d" % i, [128, D], F32) for i in range(2)]
        yt = [self.sb("F_yt%d" % i, [128, D], F32) for i in range(2)]
        junk = self.sb("F_junk", [128, 512], F32)
        st = [self.sb("F_st%d" % i, [128, 8], F32) for i in range(2)]
        h2T_v = self.h2T.rearrange("(k p) t -> p k t", p=128)
        for blk in range(NTOK // 512):
            tok0 = blk * 512
            if blk % (SEQ // 512) == 0:
                S.op("pool", lambda e: e.memset(halo[:], 0.0), writes=["F_halo"])
            S.dma(out=hT[:], in_=h2T_v[:, :, tok0:tok0 + 512], sem="F_hT", writes=["F_hT"])
            for i in range(NT):
                for gv in range(2):
                    ti = gv * NT + i
                    c0 = ti * 128
                    ps = self.ps[gv * 2 + i % 2]
                    kps = ("ps", gv * 2 + i % 2)

                    def mm(e, ps=ps, c0=c0):
                        ins = None
                        for k in range(8):
                            ins = e.matmul(ps[:, :], lhsT=Wu[:, k, c0:c0 + 128], rhs=hT[:, k, :],
                                           start=(k == 0), stop=(k == 7))
                        return ins
                    S.op("pe", mm, reads=["F_Wu", "F_hT"], writes=[kps])
                    u, ku = U[gv], ("F_U", gv)
                    c, kc = C[gv], ("F_C", gv)
                    S.op("act", lambda e, u=u, ps=ps: e.copy(out=u[:, 2:514], in_=ps[:, :]),
                         reads=[kps], writes=[(ku, "b")])
                    S.op("pool", lambda e, u=u, ti=ti: e.tensor_copy(out=u[:, 0:2],
                                                                     in_=halo[:, ti, :]),
                         reads=["F_halo"], writes=[(ku, "h")])
                    S.op("act", lambda e, u=u, c=c, ti=ti: e.activation(
                        out=c[:], in_=u[:, 0:512], func=AF.Copy, scale=cw[:, 0, ti:ti + 1]),
                        reads=[(ku, "b"), (ku, "h"), "F_cw"], writes=[kc])
                    for tap in (1, 2):
                        S.op("dve", lambda e, u=u, c=c, ti=ti, tap=tap: e.scalar_tensor_tensor(
                            out=c[:], in0=u[:, tap:tap + 512], scalar=cw[:, tap, ti:ti + 1],
                            in1=c[:], op0=ALU.mult, op1=ALU.add),
                            reads=[(ku, "b"), (ku, "h"), "F_cw", kc], writes=[kc])
                    S.op("pool", lambda e, u=u, ti=ti: e.tensor_copy(out=halo[:, ti, :],
                                                                     in_=u[:, 512:514]),
                         reads=[(ku, "b")], writes=["F_halo"])
                S.op("act", lambda e: e.activation(out=GL[:], in_=C[0][:],
                                                    func=AF.Gelu_apprx_tanh),
                     reads=[("F_C", 0)], writes=["F_GL"])
                S.op("pool", lambda e, i=i: e.tensor_tensor(out=gT[:, i, :], in0=GL[:],
                                                             in1=C[1][:], op=ALU.mult),
                     reads=["F_GL", ("F_C", 1)], writes=[("F_gT", i)])
            gkeys = [("F_gT", i) for i in range(NT)]
            for j in range(4):
                p2 = j % 2
                S.dma(out=x1t[p2][:], in_=self.x1[tok0 + j * 128: tok0 + (j + 1) * 128, :],
                      sem="F_x1t%d" % p2, writes=[("F_x1t", p2)])
                kss, krs = ("F_ss", p2), ("F_rs", p2)
                for hf in range(2):
                    ps = self.ps[4 + hf]
                    kps = ("ps", 4 + hf)

                    def mm(e, ps=ps, hf=hf, j=j):
                        ins = None
                        for k in range(NT):
                            ins = e.matmul(ps[:, :], lhsT=gT[:, k, j * 128:(j + 1) * 128],
                                           rhs=Wd[:, k, hf * 512:(hf + 1) * 512], start=(k == 0),
                                           stop=(k == NT - 1))
                        return ins
                    S.op("pe", mm, reads=["F_Wd"] + gkeys, writes=[kps])
                    S.op("act", lambda e, ps=ps, hf=hf, p2=p2: e.activation(
                        out=junk[:, :], in_=ps[:, :], func=AF.Square,
                        accum_out=st[p2][:, hf:hf + 1]), reads=[kps], writes=[(kss, hf)])
                S.op("dve", lambda e, p2=p2: e.tensor_tensor(out=st[p2][:, 2:3], in0=st[p2][:, 0:1],
                                                              in1=st[p2][:, 1:2], op=ALU.add),
                     reads=[(kss, 0), (kss, 1)], writes=[kss])
                self.rstd_from_ss(st[p2][:, 2:3], kss, st[p2][:, 3:4], krs, D)
                kyt = ("F_yt", p2)
                for hf in range(2):
                    ps = self.ps[4 + hf]
                    kps = ("ps", 4 + hf)
                    S.op("act", lambda e, ps=ps, hf=hf, p2=p2: e.activation(
                        out=yt[p2][:, hf * 512:(hf + 1) * 512], in_=ps[:, :], func=AF.Copy,
                        scale=st[p2][:, 3:4]), reads=[kps, krs], writes=[(kyt, hf)])
                S.op("dve", lambda e, p2=p2: e.tensor_tensor(out=yt[p2][:], in0=yt[p2][:],
                                                              in1=gpost[:], op=ALU.mult),
                     reads=[(kyt, 0), (kyt, 1), "F_gpost"], writes=[kyt])
                S.op("pool", lambda e, p2=p2: e.tensor_tensor(out=yt[p2][:], in0=yt[p2][:],
                                                               in1=x1t[p2][:], op=ALU.add),
                     reads=[kyt, ("F_x1t", p2)], writes=[kyt])
                S.dma(out=dst[tok0 + j * 128: tok0 + (j + 1) * 128, :], in_=yt[p2][:],
                      sem="F_yt%d" % p2, reads=[kyt], q="pool")
        S.barrier()

    def phase_H(self, l):
        S = self.S
        self.sbuf_reset()
        sb = self.sb
        UU = sb("H_UU", [64, 128], F32)
        UL = sb("H_UL", [64, 64], F32)
        tmpm = sb("H_tmpm", [64, 64], F32)
        S.op("pool", lambda e: e.memset(UU[:], 1.0), writes=["H_UU"])
        S.op("pool", lambda e: e.affine_select(out=UU[:, 0:64], in_=UU[:, 0:64], pattern=[[1, 64]],
                                                compare_op=ALU.is_ge, fill=0.0, base=0,
                                                channel_multiplier=-1),
             reads=["H_UU"], writes=["H_UU"])
        S.op("pool", lambda e: e.memset(tmpm[:], 1.0), writes=["H_tmpm"])
        S.op("pool", lambda e: e.affine_select(out=tmpm[:], in_=tmpm[:], pattern=[[0, 64]],
                                                compare_op=ALU.is_ge, fill=0.0, base=31,
                                                channel_multiplier=-1),
             reads=["H_tmpm"], writes=["H_tmpm"])
        S.op("pool", lambda e: e.tensor_tensor(out=UU[:, 64:128], in0=UU[:, 0:64], in1=tmpm[:],
                                                op=ALU.subtract),
             reads=["H_UU", "H_tmpm"], writes=["H_UU"])
        S.op("pool", lambda e: e.memset(UL[:], 1.0), writes=["H_UL"])
        S.op("pool", lambda e: e.affine_select(out=UL[:], in_=UL[:], pattern=[[-1, 64]],
                                                compare_op=ALU.is_gt, fill=0.0, base=0,
                                                channel_multiplier=1),
             reads=["H_UL"], writes=["H_UL"])
        lb = sb("H_lb", [64, 256], F32)
        oml = sb("H_oml", [64, 256], F32)
        if l == 0:
            S.op("pool", lambda e: e.memset(lb[:], 0.0), writes=["H_lb"])
        else:
            r0 = sb("H_r0", [64, 256], F32)
            S.dma(out=r0[:], in_=bcast_rows(self.hg_lb[0:1, :], 64), sem="H_r0", writes=["H_r0"])
            S.dma(out=lb[:], in_=bcast_rows(self.hg_lb[1:2, :], 64), sem="H_lbl", writes=["H_lb"])
            S.op("dve", lambda e: e.tensor_tensor(out=lb[:], in0=lb[:], in1=r0[:], op=ALU.subtract),
                 reads=["H_lb", "H_r0"], writes=["H_lb"])
            S.op("act", lambda e: e.activation(out=lb[:], in_=lb[:], func=AF.Sigmoid),
                 reads=["H_lb"], writes=["H_lb"])
        S.op("dve", lambda e: e.tensor_scalar(out=oml[:], in0=lb[:], scalar1=-1.0, scalar2=1.0,
                                               op0=ALU.mult, op1=ALU.add),
             reads=["H_lb"], writes=["H_oml"])
        lbT = sb("H_lbT", [64, 4, 2], F32)
        for h in range(4):
            for which, src, ksrc in ((0, lb, "H_lb"), (1, oml, "H_oml")):
                ps = self.ps[(2 * h + which) % 4]
                kps = ("ps", (2 * h + which) % 4)
                S.op("pe", lambda e, ps=ps, src=src, h=h: e.transpose(
                    out=ps[:64, :64], in_=src[:, h * 64:(h + 1) * 64],
                    identity=self.ident_f[:64, :64]), reads=[ksrc, "ident_f"], writes=[kps])
                S.op("dve", lambda e, ps=ps, h=h, which=which: e.tensor_copy(
                    out=lbT[:, h, which:which + 1], in_=ps[:64, 0:1]), reads=[kps],
                    writes=["H_lbT"])
        ng = sb("H_ng", [64, 64], F32)
        S.dma(out=ng[:], in_=bcast_rows(self.hg_norm[l:l + 1, :], 64), sem="H_ng", writes=["H_ng"])

        qf = [sb("H_qf%d" % i, [64, 2, 4, 512], F32) for i in range(2)]
        sq = [sb("H_sq%d" % i, [64, 4, 512], F32) for i in range(2)]
        kc = [sb("H_kc%d" % i, [64, 4, 512], F32) for i in range(2)]
        tk = [sb("H_tk%d" % i, [64, 768], F32) for i in range(3)]
        St = [sb("H_S%d" % i, [64, 4, 64], F32) for i in range(2)]
        Sb = [sb("H_Sb%d" % i, [64, 4, 64], BF16) for i in range(2)]
        pf_q = self.pfeat[FM_ROW[2764]:FM_ROW[2764] + 256, :].rearrange("(h k) t -> k h t", k=64)
        pf_f = self.pfeat[FM_ROW[3020]:FM_ROW[3020] + 256, :].rearrange("(h k) t -> k h t", k=64)
        NB = 3

        def t3(name, shape, dtype):
            return [sb("%s%d" % (name, i), shape, dtype) for i in range(NB)]
        sig, fg, logf, kct, kd = (t3("H_sig", [64, 256], F32), t3("H_fg", [64, 256], F32),
                                  t3("H_logf", [64, 256], F32), t3("H_kct", [64, 256], F32),
                                  t3("H_kd", [64, 256], BF16))
        vb = t3("H_vb", [64, 256], BF16)
        gs = t3("H_gs", [64, 4, 64], F32)
        EB, EN = t3("H_EB", [64, 4, 128], F32), t3("H_EN", [64, 4, 64], F32)
        qd, qp, kp = (t3("H_qd", [64, 4, 64], BF16), t3("H_qp", [64, 4, 64], BF16),
                      t3("H_kp", [64, 4, 64], BF16))
        Am = t3("H_Am", [64, 4, 64], BF16)
        osb, osq = t3("H_osb", [64, 4, 64], F32), t3("H_osq", [64, 4, 64], F32)
        stt = t3("H_stt", [64, 8], F32)
        stmp = t3("H_stmp", [64, 4, 64], F32)
        def seq_thread(s):
            pb = lambda i: 4 * s + i % 4
            for blk8 in range(SEQ // 512):
                slot = (blk8 * NSEQ + s) % 2
                tb = s * SEQ + blk8 * 512
                kqf = ("H_qf", slot)
                S.dma(out=qf[slot][:, 0, :, :], in_=pf_q[:, :, tb:tb + 512], sem="H_qfq%d" % slot,
                      writes=[(kqf, 0)])
                S.dma(out=qf[slot][:, 1, :, :], in_=pf_f[:, :, tb:tb + 512], sem="H_qff%d" % slot,
                      writes=[(kqf, 1)])
                S.op("act", lambda e, slot=slot: e.activation(out=sq[slot][:], in_=qf[slot][:, 0],
                                                               func=AF.Silu),
                     reads=[(kqf, 0)], writes=[("H_sq", slot)])
                S.op("act", lambda e, slot=slot: e.activation(out=kc[slot][:], in_=qf[slot][:, 1],
                                                               func=AF.Sigmoid),
                     reads=[(kqf, 1)], writes=[("H_kc", slot)])
                for h in range(4):
                    S.op("dve", lambda e, slot=slot, h=h: e.tensor_scalar(
                        out=kc[slot][:, h, :], in0=kc[slot][:, h, :], scalar1=lbT[:, h, 1:2],
                        scalar2=-1.0, op0=ALU.mult, op1=ALU.mult),
                        reads=[("H_kc", slot), "H_lbT"], writes=[("H_kc", slot)])
                    S.op("dve", lambda e, slot=slot, h=h: e.tensor_scalar(
                        out=kc[slot][:, h, :], in0=kc[slot][:, h, :], scalar1=lbT[:, h, 1:2],
                        scalar2=None, op0=ALU.add),
                        reads=[("H_kc", slot), "H_lbT"], writes=[("H_kc", slot)])
                for cn in range(8):
                    b = s
                    c0 = cn * 64
                    tok = tb + c0
                    ktk = ("H_tk", b)
                    S.dma(out=tk[b][:], in_=self.ptok[tok:tok + 64, TM_CF:TM_CF + 768],
                          sem="H_tk%d" % b, writes=[ktk])
                    first = (blk8 == 0 and cn == 0)
                    S.op("act", lambda e, b=b: e.activation(out=sig[b][:], in_=tk[b][:, 0:256],
                                                             func=AF.Sigmoid),
                         reads=[ktk], writes=[("H_sig", b)])
                    S.op("dve", lambda e, b=b: e.tensor_tensor(out=fg[b][:], in0=sig[b][:],
                                                                in1=oml[:], op=ALU.mult),
                         reads=[("H_sig", b), "H_oml"], writes=[("H_fg", b)])
                    S.op("dve", lambda e, b=b: e.tensor_tensor(out=fg[b][:], in0=fg[b][:],
                                                                in1=lb[:], op=ALU.add),
                         reads=[("H_fg", b), "H_lb"], writes=[("H_fg", b)])
                    S.op("act", lambda e, b=b: e.activation(out=logf[b][:], in_=fg[b][:],
                                                             func=AF.Ln),
                         reads=[("H_fg", b)], writes=[("H_logf", b)])
                    S.op("pool", lambda e, b=b: e.tensor_scalar(out=kct[b][:], in0=fg[b][:],
                                                                 scalar1=-1.0, scalar2=1.0,
                                                                 op0=ALU.mult, op1=ALU.add),
                         reads=[("H_fg", b)], writes=[("H_kct", b)])
                    S.op("pool", lambda e, b=b: e.tensor_copy(out=vb[b][:], in_=tk[b][:, 256:512]),
                         reads=[ktk], writes=[("H_vb", b)])
                    S.op("act", lambda e, b=b: e.activation(
                        out=gs[b][:], in_=tk[b][:, 512:768].rearrange("p (h v) -> p h v", h=4),
                        func=AF.Silu), reads=[ktk], writes=[("H_gs", b)])
                    S.op("pool", lambda e, b=b: e.tensor_tensor(
                        out=gs[b][:], in0=gs[b][:],
                        in1=ng[:].unsqueeze(1).to_broadcast([64, 4, 64]), op=ALU.mult),
                        reads=[("H_gs", b), "H_ng"], writes=[("H_gs", b)])
                    p0, kp0 = self.ps8[pb(0)], ("ps", pb(0))

                    def mmb(e, b=b, p0=p0):
                        ins = None
                        for h in range(4):
                            ins = e.matmul(p0[:64, h * 128:(h + 1) * 128],
                                           lhsT=logf[b][:, h * 64:(h + 1) * 64], rhs=UU[:, :],
                                           start=True, stop=True)
                        return ins
                    S.op("pe", mmb, reads=[("H_logf", b), "H_UU"], writes=[kp0])
                    p0v = p0[:64, :].rearrange("p (h t) -> p h t", h=4)
                    S.op("act", lambda e, b=b, p0v=p0v: e.activation(out=EB[b][:], in_=p0v,
                                                                      func=AF.Exp),
                         reads=[kp0], writes=[("H_EB", b)])
                    S.op("act", lambda e, b=b, p0v=p0v: e.activation(out=EN[b][:],
                                                                      in_=p0v[:, :, 64:128],
                                                                      func=AF.Exp, scale=-1.0),
                         reads=[kp0], writes=[("H_EN", b)])
                    p1, kp1 = self.ps8[pb(1)], ("ps", pb(1))
                    S.op("pe", lambda e, b=b, p1=p1: e.matmul(p1[:64, :256], lhsT=UL[:, :],
                                                              rhs=logf[b][:, :], start=True,
                                                              stop=True),
                         reads=[("H_logf", b), "H_UL"], writes=[kp1])
                    S.op("act", lambda e, b=b, p1=p1: e.activation(out=sig[b][:], in_=p1[:64, :256],
                                                                    func=AF.Exp),
                         reads=[kp1], writes=[("H_sig", b)])
                    S.op("dve", lambda e, b=b: e.tensor_tensor(out=kd[b][:], in0=sig[b][:],
                                                                in1=kct[b][:], op=ALU.mult),
                         reads=[("H_sig", b), ("H_kct", b)], writes=[("H_kd", b)])
                    sqv = sq[slot][:, :, c0:c0 + 64]
                    kcv = kc[slot][:, :, c0:c0 + 64]
                    S.op("dve", lambda e, b=b, sqv=sqv: e.tensor_tensor(
                        out=qd[b][:], in0=sqv, in1=EB[b][:, :, 0:64], op=ALU.mult),
                        reads=[("H_sq", slot), ("H_EB", b)], writes=[("H_qd", b)])
                    S.op("pool", lambda e, b=b, sqv=sqv: e.tensor_tensor(
                        out=qp[b][:], in0=sqv, in1=EB[b][:, :, 64:128], op=ALU.mult),
                        reads=[("H_sq", slot), ("H_EB", b)], writes=[("H_qp", b)])
                    S.op("dve", lambda e, b=b, kcv=kcv: e.tensor_tensor(
                        out=kp[b][:], in0=kcv, in1=EN[b][:], op=ALU.mult),
                        reads=[("H_kc", slot), ("H_EN", b)], writes=[("H_kp", b)])
                    p2, kp2 = self.ps8[pb(2)], ("ps", pb(2))

                    def mma(e, b=b, p2=p2):
                        ins = None
                        for h in range(4):
                            ins = e.matmul(p2[:64, h * 64:(h + 1) * 64], lhsT=kp[b][:, h, :],
                                           rhs=qp[b][:, h, :], start=True, stop=True)
                        return ins
                    S.op("pe", mma, reads=[("H_kp", b), ("H_qp", b)], writes=[kp2])
                    S.op("dve", lambda e, b=b, p2=p2: e.tensor_tensor(
                        out=Am[b][:], in0=p2[:64, :256].rearrange("p (h c) -> p h c", h=4),
                        in1=UU[:, 0:64].unsqueeze(1).to_broadcast([64, 4, 64]), op=ALU.mult),
                        reads=[kp2, "H_UU"], writes=[("H_Am", b)])
                    if first:
                        S.op("pool", lambda e, s=s: e.memset(St[s][:], 0.0), writes=[("H_S", s)])
                        S.op("pool", lambda e, s=s: e.memset(Sb[s][:], 0.0), writes=[("H_Sb", s)])
                    p3, kp3 = self.ps8[pb(3)], ("ps", pb(3))

                    def mmo(e, b=b, p3=p3, s=s):
                        ins = None
                        for h in range(4):
                            e.matmul(p3[:64, h * 64:(h + 1) * 64], lhsT=qd[b][:, h, :],
                                     rhs=Sb[s][:, h, :], start=True, stop=False)
                            ins = e.matmul(p3[:64, h * 64:(h + 1) * 64], lhsT=Am[b][:, h, :],
                                           rhs=vb[b][:, h * 64:(h + 1) * 64], start=False, stop=True)
                        return ins
                    S.op("pe", mmo, reads=[("H_qd", b), ("H_Sb", s), ("H_Am", b), ("H_vb", b)],
                         writes=[kp3])
                    p4, kp4 = self.ps8[pb(4)], ("ps", pb(4))

                    def mms(e, b=b, p4=p4):
                        ins = None
                        for h in range(4):
                            ins = e.matmul(p4[:64, h * 64:(h + 1) * 64],
                                           lhsT=kd[b][:, h * 64:(h + 1) * 64],
                                           rhs=vb[b][:, h * 64:(h + 1) * 64], start=True, stop=True)
                        return ins
                    S.op("pe", mms, reads=[("H_kd", b), ("H_vb", b)], writes=[kp4])
                    S.op("dve", lambda e, b=b, s=s: e.tensor_tensor(
                        out=stmp[b][:], in0=St[s][:],
                        in1=EB[b][:, :, 63:64].to_broadcast([64, 4, 64]), op=ALU.mult),
                        reads=[("H_S", s), ("H_EB", b)], writes=[("H_stmp", b)])
                    S.op("dve", lambda e, b=b, s=s, p4=p4: e.tensor_tensor(
                        out=St[s][:], in0=stmp[b][:],
                        in1=p4[:64, :256].rearrange("p (h v) -> p h v", h=4), op=ALU.add),
                        reads=[("H_stmp", b), kp4], writes=[("H_S", s)])
                    S.op("act", lambda e, s=s: e.copy(out=Sb[s][:], in_=St[s][:]),
                         reads=[("H_S", s)], writes=[("H_Sb", s)])
                    S.op("act", lambda e, b=b, p3=p3: e.copy(
                        out=osb[b][:], in_=p3[:64, :256].rearrange("p (h v) -> p h v", h=4)),
                        reads=[kp3], writes=[("H_osb", b)])
                    S.op("pool", lambda e, b=b: e.tensor_tensor(out=osq[b][:], in0=osb[b][:],
                                                                 in1=osb[b][:], op=ALU.mult),
                         reads=[("H_osb", b)], writes=[("H_osq", b)])
                    S.op("dve", lambda e, b=b: e.tensor_reduce(out=stt[b][:, 0:4], in_=osq[b][:],
                                                                axis=AX.X, op=ALU.add),
                         reads=[("H_osq", b)], writes=[("H_stt", b)])
                    self.rstd_from_ss(stt[b][:, 0:4], ("H_stt", b), stt[b][:, 4:8], ("H_rs", b), 64)
                    S.op("dve", lambda e, b=b: e.tensor_tensor(
                        out=osb[b][:], in0=osb[b][:],
                        in1=stt[b][:, 4:8].unsqueeze(2).to_broadcast([64, 4, 64]), op=ALU.mult),
                        reads=[("H_osb", b), ("H_rs", b)], writes=[("H_osb", b)])
                    S.op("pool", lambda e, b=b: e.tensor_tensor(out=osq[b][:], in0=osb[b][:],
                                                                 in1=gs[b][:], op=ALU.mult),
                         reads=[("H_osb", b), ("H_gs", b)], writes=[("H_osq", b)])
                    S.dma(out=self.mixin[tok:tok + 64, 768:1024],
                          in_=osq[b][:].rearrange("p h v -> p (h v)"), sem="H_out%d" % b,
                          reads=[("H_osq", b)])
        S.interleave([lambda s=s: seq_thread(s) for s in range(NSEQ)])
        S.barrier()

    def phase_S(self, l):
        S = self.S
        self.sbuf_reset()
        sb = self.sb
        BIG = -1.0e30
        KIT = 32
        kiT = sb("S_kiT", [64, SEQ], F32)
        kT = sb("S_kT", [64, SEQ], BF16)
        kst = sb("S_kst", [64, 1024], F32)
        vst = sb("S_vst", [128, 8, 64], F32)
        v1 = sb("S_v1", [128, 32, 65], BF16)
        junk = sb("S_junk", [128, SEQ], BF16)
        ckn = sb("S_ckn", [128, KIT + 1], F32)
        for k in range(KIT + 1):
            S.op("pool", lambda e, k=k: e.memset(ckn[:, k:k + 1], -2.1 / 2.0 ** (k + 1)),
                 writes=[("S_ckn", k)])
        kck = [("S_ckn", k) for k in range(KIT + 1)]

        NT = 2

        def t2(name, shape, dtype):
            return [sb("%s%d" % (name, i), shape, dtype) for i in range(NT)]
        qq = t2("S_qq", [64, 2, 4, 128], F32)
        qqb = t2("S_qqb", [64, 4, 128], BF16)
        acc = t2("S_acc", [128, SEQ], F32)
        wkA = t2("S_wkA", [128, SEQ], F32)
        gtt = t2("S_gt", [128, SEQ], BF16)
        eqt = t2("S_eq", [128, SEQ], BF16)
        selb = t2("S_selb", [128, SEQ], BF16)
        rr = [sb("S_rr%d" % i, [128, 512], F32) for i in range(2 * NT)]
        pT = [sb("S_pT%d" % i, [128, 512], BF16) for i in range(2 * NT)]
        wi = t2("S_wi", [128, 12], F32)
        sc = t2("S_sc", [128, 16], F32)
        nst = t2("S_nst", [128, KIT + 1], F32)
        oT = t2("S_oT", [65, 512], F32)
        osb = t2("S_osb", [128, 4, 64], F32)
        rc = t2("S_rc", [128, 4, 1], F32)
        pf_q = self.pfeat[FM_ROW[2056]:FM_ROW[2056] + 256, :].rearrange("(h k) t -> k h t", k=64)
        pf_qi = self.pfeat[FM_ROW[2440]:FM_ROW[2440] + 256, :].rearrange("(h k) t -> k h t", k=64)
        pf_k = self.pfeat[FM_ROW[2312]:FM_ROW[2312] + 64, :]
        pf_ki = self.pfeat[FM_ROW[2696]:FM_ROW[2696] + 64, :]

        def tile_thread(s, a2):
            t0 = s * SEQ
            nr = 0
            for j in range(a2, SEQ // 128, NT):
                tq = t0 + j * 128
                NK = (j + 1) * 128
                kqq = ("S_qq", a2)
                S.dma(out=qq[a2][:, 0], in_=pf_q[:, :, tq:tq + 128], sem="S_qq%da" % a2,
                      writes=[(kqq, 0)])
                S.dma(out=qq[a2][:, 1], in_=pf_qi[:, :, tq:tq + 128], sem="S_qq%db" % a2,
                      writes=[(kqq, 1)])
                S.op("pool", lambda e: e.tensor_copy(out=qqb[a2][:], in_=qq[a2][:, 0]),
                     reads=[(kqq, 0)], writes=[("S_qqb", a2)])
                kwi = ("S_wi", a2)
                S.dma(out=wi[a2][:, 0:4], in_=self.ptok[tq:tq + 128, TM_WI:TM_WI + 4],
                      sem="S_wi%d" % a2, writes=[(kwi, 0)])
                S.op("act", lambda e: e.activation(out=wi[a2][:, 4:8], in_=wi[a2][:, 0:4],
                                                    func=AF.Abs),
                     reads=[(kwi, 0)], writes=[(kwi, 1)])
                S.op("act", lambda e: e.activation(out=wi[a2][:, 8:12], in_=wi[a2][:, 0:4],
                                                    func=AF.Sign),
                     reads=[(kwi, 0)], writes=[(kwi, 2)])
                kacc = ("S_acc", a2)
                nkb = (NK + 511) // 512
                for kb in range(nkb):
                    w = min(512, NK - kb * 512)
                    for h in range(4):
                        pi = 2 * a2 + h % 2
                        ps, kps = self.ps8[pi], ("ps", pi)
                        S.op("pe", lambda e, ps=ps, h=h, kb=kb, w=w: e.matmul(
                            ps[:, :w], lhsT=qq[a2][:, 1, h, :],
                            rhs=kiT[:, kb * 512: kb * 512 + w], start=True, stop=True),
                            reads=[(kqq, 1), "S_kiT"], writes=[kps])
                        ri = 2 * a2 + nr % 2
                        r, kr = rr[ri], ("S_rr", ri)
                        nr += 1
                        S.op("act", lambda e, ps=ps, r=r, w=w, h=h: e.activation(
                            out=r[:, :w], in_=ps[:, :w], func=AF.Relu, scale=wi[a2][:, 4 + h:5 + h]),
                            reads=[kps, (kwi, 1)], writes=[kr])
                        av = acc[a2][:, kb * 512: kb * 512 + w]
                        if h == 0:
                            S.op("dve", lambda e, av=av, r=r, w=w, h=h: e.tensor_scalar(
                                out=av, in0=r[:, :w], scalar1=wi[a2][:, 8 + h:9 + h], scalar2=None,
                                op0=ALU.mult), reads=[kr, (kwi, 2)], writes=[(kacc, kb), kacc])
                        else:
                            S.op("dve", lambda e, av=av, r=r, w=w, h=h: e.scalar_tensor_tensor(
                                out=av, in0=r[:, :w], scalar=wi[a2][:, 8 + h:9 + h], in1=av,
                                op0=ALU.mult, op1=ALU.add),
                                reads=[kr, (kwi, 2), (kacc, kb)], writes=[(kacc, kb)])
                kall = [(kacc, kb) for kb in range(nkb)]
                X = sc[a2]
                ksc = lambda nm: ("S_sc", a2, nm)
                accv = acc[a2][:, :NK]
                if j >= 2:
                    S.op("dve", lambda e: e.tensor_reduce(out=X[:, 0:1], in_=accv, axis=AX.X,
                                                           op=ALU.max, apply_absolute_value=True),
                         reads=kall, writes=[ksc("rm")])
                    S.op("dve", lambda e: e.tensor_scalar(out=X[:, 0:1], in0=X[:, 0:1],
                                                           scalar1=1.0e-20, scalar2=None,
                                                           op0=ALU.max),
                         reads=[ksc("rm")], writes=[ksc("rm")])
                    S.op("dve", lambda e: e.tensor_scalar(out=nst[a2][:], in0=ckn[:],
                                                           scalar1=X[:, 0:1], scalar2=None,
                                                           op0=ALU.mult),
                         reads=[ksc("rm")] + kck, writes=[("S_nst", a2)])
                    S.op("dve", lambda e: e.tensor_scalar(out=X[:, 1:2], in0=X[:, 0:1],
                                                           scalar1=-0.02, scalar2=None,
                                                           op0=ALU.mult),
                         reads=[ksc("rm")], writes=[ksc("nc")])
                    S.op("pool", lambda e: e.memset(X[:, 4:5], float(NK) - 511.5),
                         writes=[ksc("cb")])
                S.op("pool", lambda e: e.memset(acc[a2][0:64, NK - 64:NK], BIG),
                     reads=kall + [ksc("rm")], writes=[kacc])
                if j >= 2:
                    for k in range(KIT):
                        S.op("act", lambda e: e.activation(out=junk[:, :NK], in_=accv, func=AF.Sign,
                                                            bias=X[:, 1:2], scale=1.0,
                                                            accum_out=X[:, 2:3]),
                             reads=[kacc, ksc("nc")], writes=[ksc("sg")])
                        S.op("act", lambda e: e.activation(out=X[:, 3:4], in_=X[:, 2:3],
                                                            func=AF.Sign, bias=X[:, 4:5], scale=1.0),
                             reads=[ksc("sg"), ksc("cb")], writes=[ksc("dd")])
                        S.op("act", lambda e, k=k: e.activation(out=X[:, 1:2], in_=X[:, 3:4],
                                                                 func=AF.Identity,
                                                                 scale=nst[a2][:, k + 1:k + 2],
                                                                 bias=X[:, 1:2]),
                             reads=[ksc("dd"), ("S_nst", a2), ksc("nc")], writes=[ksc("nc")])
                    S.op("dve", lambda e: e.scalar_tensor_tensor(
                        out=X[:, 5:6], in0=X[:, 1:2], scalar=-1.0, in1=nst[a2][:, KIT:KIT + 1],
                        op0=ALU.mult, op1=ALU.add), reads=[ksc("nc"), ("S_nst", a2)],
                        writes=[ksc("thr")])
                else:
                    S.op("pool", lambda e: e.memset(X[:, 5:6], BIG / 2), writes=[ksc("thr")])
                kwk = ("S_wkA", a2)
                wv = wkA[a2][:, :NK]
                S.op("dve", lambda e: e.tensor_scalar(out=wv, in0=accv, scalar1=X[:, 5:6],
                                                       scalar2=3.0e30, op0=ALU.is_lt, op1=ALU.mult),
                     reads=[kacc, ksc("thr")], writes=[kwk])
                S.op("dve", lambda e: e.tensor_tensor(out=wv, in0=wv, in1=accv, op=ALU.add),
                     reads=[kacc, kwk], writes=[kwk])
                S.op("dve", lambda e: e.tensor_reduce(out=X[:, 6:7], in_=wv, axis=AX.X, op=ALU.min),
                     reads=[kwk], writes=[ksc("v")])
                gv, ev = gtt[a2][:, :NK], eqt[a2][:, :NK]
                S.op("dve", lambda e: e.tensor_scalar(out=gv, in0=accv, scalar1=X[:, 6:7],
                                                       scalar2=None, op0=ALU.is_gt, op1=ALU.add,
                                                       accum_out=X[:, 7:8]),
                     reads=[kacc, ksc("v")], writes=[("S_gt", a2), ksc("cg")])
                S.op("dve", lambda e: e.tensor_scalar(out=ev, in0=accv, scalar1=X[:, 6:7],
                                                       scalar2=None, op0=ALU.is_equal),
                     reads=[kacc, ksc("v")], writes=[("S_eq", a2)])
                S.op("dve", lambda e: e.tensor_tensor_scan(out=wv, data0=ev, data1=ev, initial=0.0,
                                                            op0=ALU.add, op1=ALU.max),
                     reads=[("S_eq", a2), kwk], writes=[kwk])
                S.op("dve", lambda e: e.tensor_scalar(out=X[:, 8:9], in0=X[:, 7:8], scalar1=-1.0,
                                                       scalar2=256.0, op0=ALU.mult, op1=ALU.add),
                     reads=[ksc("cg")], writes=[ksc("need")])
                S.op("dve", lambda e: e.scalar_tensor_tensor(out=ev, in0=wv, scalar=X[:, 8:9],
                                                              in1=ev, op0=ALU.is_le, op1=ALU.mult),
                     reads=[kwk, ksc("need"), ("S_eq", a2)], writes=[("S_eq", a2)])
                S.op("pool", lambda e: e.tensor_tensor(out=gv, in0=gv, in1=ev, op=ALU.add),
                     reads=[("S_gt", a2), ("S_eq", a2)], writes=[("S_gt", a2)])
                ksel = ("S_selb", a2)
                S.op("pool", lambda e: e.tensor_scalar(out=selb[a2][:, :NK], in0=gv, scalar1=-1.0,
                                                        scalar2=-NEG, op0=ALU.add, op1=ALU.mult),
                     reads=[("S_gt", a2)], writes=[ksel])
                po, kpo = self.ps8[4 + a2], ("ps", 4 + a2)
                for kt in range(j + 1):
                    li = (0, 1, 6, 7)[2 * a2 + kt % 2]
                    pl, kpl = self.ps8[li], ("ps", li)

                    def mml(e, pl=pl, kt=kt):
                        e.matmul(pl[:, :].rearrange("p (h t) -> p h t", h=4),
                                 lhsT=kT[:, kt * 128:(kt + 1) * 128], rhs=qqb[a2][:, :, :],
                                 start=True, stop=False)
                        ins = None
                        for h in range(4):
                            ins = e.matmul(pl[:, h * 128:(h + 1) * 128],
                                           lhsT=selb[a2][:, kt * 128:(kt + 1) * 128],
                                           rhs=self.ident_b[:, :], start=False, stop=(h == 3))
                        return ins
                    S.op("pe", mml, reads=["S_kT", ("S_qqb", a2), ksel, "ident_b"], writes=[kpl])
                    pi_ = 2 * a2 + kt % 2
                    p, kp = pT[pi_], ("S_pT", pi_)
                    S.op("act", lambda e, p=p, pl=pl: e.activation(out=p[:], in_=pl[:, :],
                                                                    func=AF.Exp, scale=0.125),
                         reads=[kpl], writes=[kp])
                    S.op("pe", lambda e, p=p, kt=kt: e.matmul(
                        po[:65, :], lhsT=v1[:, kt, :], rhs=p[:], start=(kt == 0), stop=(kt == j)),
                        reads=["S_v1", kp], writes=[kpo])
                koT = ("S_oT", a2)
                S.op("act", lambda e: e.copy(out=oT[a2][:], in_=po[:65, :]),
                     reads=[kpo], writes=[koT])
                pt, kpt = self.ps8[2 * a2], ("ps", 2 * a2)

                def trs(e):
                    ins = None
                    for h in range(4):
                        ins = e.transpose(out=pt[:, h * 65:(h + 1) * 65],
                                          in_=oT[a2][:, h * 128:(h + 1) * 128],
                                          identity=self.ident_f[:65, :65])
                    return ins
                S.op("pe", trs, reads=[koT, "ident_f"], writes=[kpt])
                ptv = pt[:, :260].rearrange("p (h d) -> p h d", h=4)
                S.op("dve", lambda e: e.reciprocal(out=rc[a2][:], in_=ptv[:, :, 64:65]),
                     reads=[kpt], writes=[("S_rc", a2)])
                S.op("dve", lambda e: e.tensor_tensor(
                    out=osb[a2][:], in0=ptv[:, :, 0:64], in1=rc[a2][:].to_broadcast([128, 4, 64]),
                    op=ALU.mult), reads=[kpt, ("S_rc", a2)], writes=[("S_osb", a2)])
                S.dma(out=self.mixin[tq:tq + 128, 512:768],
                      in_=osb[a2][:].rearrange("p h d -> p (h d)"), sem="S_out%d" % a2,
                      reads=[("S_osb", a2)])

        for s in range(NSEQ):
            t0 = s * SEQ
            S.dma(out=kiT[:], in_=pf_ki[:, t0:t0 + SEQ], sem="S_kiT", writes=["S_kiT"])
            for q4 in range(4):
                S.dma(out=kst[:], in_=pf_k[:, t0 + q4 * 1024:t0 + (q4 + 1) * 1024], sem="S_kst",
                      writes=["S_kst"])
                S.op("pool", lambda e, q4=q4: e.tensor_copy(out=kT[:, q4 * 1024:(q4 + 1) * 1024],
                                                            in_=kst[:]),
                     reads=["S_kst"], writes=["S_kT"])
            S.op("pool", lambda e: e.memset(v1[:], 1.0), writes=["S_v1"])
            for q4 in range(4):
                S.dma(out=vst[:],
                      in_=self.ptok[t0 + q4 * 1024: t0 + (q4 + 1) * 1024,
                                    TM_BV:TM_BV + 64].rearrange("(kt p) d -> p kt d", p=128),
                      sem="S_vst", writes=["S_vst"])
                S.op("pool", lambda e, q4=q4: e.tensor_copy(out=v1[:, q4 * 8:(q4 + 1) * 8, 0:64],
                                                            in_=vst[:]),
                     reads=["S_vst"], writes=["S_v1"])
            S.interleave([lambda s=s, a=a: tile_thread(s, a) for a in range(NT)])
        S.barrier()

    def phase_D(self, l):
        S = self.S
        self.sbuf_reset()
        sb = self.sb
        NB = 2
        U64 = sb("D_U", [64, 64], F32)
        UL = sb("D_UL", [64, 64], F32)
        Lst = sb("D_Lst", [64, 64], F32)
        mb = sb("D_mb", [64, 64], F32)
        S.op("pool", lambda e: e.memset(U64[:], 1.0), writes=["D_U"])
        S.op("pool", lambda e: e.affine_select(out=U64[:], in_=U64[:], pattern=[[1, 64]],
                                                compare_op=ALU.is_ge, fill=0.0, base=0,
                                                channel_multiplier=-1), reads=["D_U"],
             writes=["D_U"])
        for t_, kk_ in ((UL, "D_UL"), (Lst, "D_Lst")):
            S.op("pool", lambda e, t_=t_: e.memset(t_[:], 1.0), writes=[kk_])
            S.op("pool", lambda e, t_=t_: e.affine_select(out=t_[:], in_=t_[:], pattern=[[-1, 64]],
                                                          compare_op=ALU.is_gt, fill=0.0, base=0,
                                                          channel_multiplier=1), reads=[kk_],
                 writes=[kk_])
        S.op("pool", lambda e: e.memset(mb[:], 0.0), writes=["D_mb"])
        S.op("pool", lambda e: e.affine_select(out=mb[:], in_=mb[:], pattern=[[-1, 64]],
                                                compare_op=ALU.is_ge, fill=-1.0e4, base=0,
                                                channel_multiplier=1), reads=["D_mb"],
             writes=["D_mb"])
        cw = self.load_convw("D_cw", self.dn_conv[l], 4, 12)
        alog = sb("D_alog", [64, 4], F32)
        dtb = sb("D_dtb", [64, 4], F32)
        S.dma(out=alog[:], in_=bcast_rows(self.dn_a_log[l:l + 1, :], 64), sem="D_alog",
              writes=["D_alog"])
        S.dma(out=dtb[:], in_=bcast_rows(self.dn_dt_bias[l:l + 1, :], 64), sem="D_dtb",
              writes=["D_dtb"])
        S.op("act", lambda e: e.activation(out=alog[:], in_=alog[:], func=AF.Exp), reads=["D_alog"],
             writes=["D_alog"])
        S.op("dve", lambda e: e.tensor_scalar(out=alog[:], in0=alog[:], scalar1=-1.0, scalar2=None,
                                               op0=ALU.mult), reads=["D_alog"], writes=["D_alog"])
        ng = sb("D_ng", [64, 128], F32)
        S.dma(out=ng[:], in_=bcast_rows(self.dn_norm[l:l + 1, :], 64), sem="D_ng", writes=["D_ng"])

        X = [sb("D_X%d" % i, [128, 515], F32) for i in range(4)]
        Cc = [sb("D_C%d" % i, [128, 512], F32) for i in range(4)]
        Y = [sb("D_Y%d" % i, [128, 12, 512], F32) for i in range(2)]
        St = [sb("D_S%d" % i, [128, 4, 128], F32) for i in range(NSEQ)]
        Sb = [sb("D_Sb%d" % i, [128, 4, 128], BF16) for i in range(NSEQ)]

        def t2(name, shape, dtype):
            return [sb("%s%d" % (name, i), shape, dtype) for i in range(NB)]
        tg = t2("D_tg", [64, 8], F32)
        gt = t2("D_gt", [64, 24], F32)
        zt = t2("D_zt", [64, 4, 128], F32)
        gs = t2("D_gs", [64, 4, 128], F32)
        QK = t2("D_QK", [64, 8, 128], F32)
        Vt = t2("D_Vt", [64, 4, 128], F32)
        sqq = t2("D_sqq", [64, 8, 128], F32)
        nrm = t2("D_nrm", [64, 16], F32)
        qdk = t2("D_qdk", [64, 4, 128], F32)
        TT = t2("D_TT", [128, 12, 64], BF16)
        kd = t2("D_kd", [64, 4, 128], BF16)
        bk = t2("D_bk", [64, 4, 128], BF16)
        bv = t2("D_bv", [64, 4, 128], BF16)
        gU = t2("D_gU", [64, 8, 64], F32)
        dec = t2("D_dec", [64, 4, 64], F32)
        Mm = t2("D_M", [64, 4, 64], F32)
        QKm = t2("D_QKm", [64, 4, 64], F32)
        QKT = t2("D_QKT", [64, 4, 64], BF16)
        Qa = t2("D_Qa", [64, 4, 64], F32)
        Qta = t2("D_Qta", [64, 4, 64], F32)
        Qb = t2("D_Qb", [64, 4, 64], F32)
        Qtb = t2("D_Qtb", [64, 4, 64], F32)
        Bt = t2("D_Bt", [64, 4, 64], F32)
        Tb = t2("D_Tb", [64, 4, 64], BF16)
        u0 = t2("D_u0", [64, 4, 128], F32)
        ub = t2("D_ub", [64, 4, 128], BF16)
        wkT = t2("D_wkT", [128, 4, 64], BF16)
        glb = t2("D_glb", [128, 4], F32)
        osb = t2("D_osb", [64, 4, 128], F32)
        osq = t2("D_osq", [64, 4, 128], F32)
        identI = sb("D_I", [64, 4, 64], F32)
        for h in range(4):
            S.op("pool", lambda e, h=h: e.tensor_copy(out=identI[:, h, :], in_=self.ident_f[:64, :64]),
                 reads=["ident_f"], writes=["D_I"])

        def v4(ps, w):
            return ps[:64, :4 * w].rearrange("p (h x) -> p h x", h=4)

        def bc(ap_h1, w):
            a = ap_h1 if len(ap_h1.shape) == 3 else ap_h1.unsqueeze(2)
            return a.to_broadcast([64, 4, w])

        def seq_thread(s):
            pb = lambda i: 4 * s + i % 4
            for blk8 in range(SEQ // 512):
                ys = (blk8 * NSEQ + s) % 2
                tb = s * SEQ + blk8 * 512
                for ct in range(12):
                    xi = 2 * s + ct % 2
                    x = X[xi]
                    kx = ("D_X", xi)
                    if blk8 == 0:
                        S.op("pool", lambda e, x=x: e.memset(x[:, 0:3], 0.0), writes=[(kx, "h")])
                        S.dma(out=x[:, 3:515], in_=self.pfeat[ct * 128:(ct + 1) * 128, tb:tb + 512],
                              sem="D_X%d" % xi, writes=[(kx, "b")])
                    else:
                        S.dma(out=x[:, :], in_=self.pfeat[ct * 128:(ct + 1) * 128, tb - 3:tb + 512],
                              sem="D_X%d" % xi, writes=[(kx, "h"), (kx, "b")])
                    c, kc = Cc[xi], ("D_C", xi)
                    S.op("act", lambda e, x=x, c=c, ct=ct: e.activation(
                        out=c[:], in_=x[:, 0:512], func=AF.Copy, scale=cw[:, 0, ct:ct + 1]),
                        reads=[(kx, "h"), (kx, "b"), "D_cw"], writes=[kc])
                    for tap in (1, 2, 3):
                        S.op("dve", lambda e, x=x, c=c, ct=ct, tap=tap: e.scalar_tensor_tensor(
                            out=c[:], in0=x[:, tap:tap + 512], scalar=cw[:, tap, ct:ct + 1],
                            in1=c[:], op0=ALU.mult, op1=ALU.add),
                            reads=[(kx, "h"), (kx, "b"), "D_cw", kc], writes=[kc])
                    S.op("act", lambda e, c=c, ct=ct, ys=ys: e.activation(out=Y[ys][:, ct, :],
                                                                           in_=c[:], func=AF.Silu),
                         reads=[kc], writes=[("D_Y", ys, ct)])
                ykeys = [("D_Y", ys, ct) for ct in range(12)]
                for cn in range(8):
                    b = s
                    c0 = cn * 64
                    tok = tb + c0
                    K = lambda nm: (nm, b)
                    first = (blk8 == 0 and cn == 0)
                    if first:
                        S.op("pool", lambda e, s=s: e.memset(St[s][:], 0.0), writes=[("D_S", s)])
                        S.op("pool", lambda e, s=s: e.memset(Sb[s][:], 0.0), writes=[("D_Sb", s)])
                    S.dma(out=tg[b][:], in_=self.ptok[tok:tok + 64, TM_AB:TM_AB + 8],
                          sem="D_tg%d" % b, writes=[K("tg")])
                    S.dma(out=zt[b][:].rearrange("p h v -> p (h v)"),
                          in_=self.ptok[tok:tok + 64, TM_AZ:TM_AZ + 512], sem="D_zt%d" % b,
                          writes=[K("zt")])
                    G = gt[b]
                    S.op("act", lambda e, b=b, G=G: e.activation(out=G[:, 0:4], in_=tg[b][:, 0:4],
                                                                  func=AF.Sigmoid),
                         reads=[K("tg")], writes=[K("beta")])
                    S.op("dve", lambda e, b=b, G=G: e.tensor_tensor(out=G[:, 20:24], in0=tg[b][:, 4:8],
                                                                     in1=dtb[:], op=ALU.add),
                         reads=[K("tg"), "D_dtb"], writes=[K("gtmp")])
                    S.op("act", lambda e, G=G: e.activation(out=G[:, 20:24], in_=G[:, 20:24],
                                                             func=AF.Exp),
                         reads=[K("gtmp")], writes=[K("gtmp")])
                    S.op("act", lambda e, G=G: e.activation(out=G[:, 20:24], in_=G[:, 20:24],
                                                             func=AF.Ln, bias=1.0),
                         reads=[K("gtmp")], writes=[K("gtmp")])
                    S.op("dve", lambda e, G=G: e.tensor_tensor(out=G[:, 4:8], in0=G[:, 20:24],
                                                                in1=alog[:], op=ALU.mult),
                         reads=[K("gtmp"), "D_alog"], writes=[K("g")])
                    pg, kpg = self.ps8[pb(5)], ("ps", pb(5))

                    def mmg(e, G=G, pg=pg):
                        e.matmul(pg[:64, 0:4], lhsT=U64[:, :], rhs=G[:, 4:8], start=True, stop=True)
                        e.matmul(pg[:64, 4:8], lhsT=UL[:, :], rhs=G[:, 4:8], start=True, stop=True)
                        return e.matmul(pg[:, 8:12], lhsT=self.ones_f[:64, :], rhs=G[:, 4:8],
                                        start=True, stop=True)
                    S.op("pe", mmg, reads=[K("g"), "D_U", "D_UL", "ones_f"], writes=[kpg])
                    S.op("act", lambda e, G=G, pg=pg: e.activation(out=G[:, 8:16], in_=pg[:64, 0:8],
                                                                    func=AF.Exp),
                         reads=[kpg], writes=[K("eG")])
                    S.op("act", lambda e, b=b, pg=pg: e.activation(out=glb[b][:], in_=pg[:, 8:12],
                                                                    func=AF.Exp),
                         reads=[kpg], writes=[K("glb")])
                    S.op("dve", lambda e, G=G: e.tensor_tensor(out=G[:, 16:20], in0=G[:, 0:4],
                                                                in1=G[:, 8:12], op=ALU.mult),
                         reads=[K("beta"), K("eG")], writes=[K("beG")])
                    S.op("dve", lambda e, b=b, G=G: e.tensor_tensor(
                        out=gU[b][:, 0:4, :], in0=U64[:].unsqueeze(1).to_broadcast([64, 4, 64]),
                        in1=bc(G[:, 4:8], 64), op=ALU.mult), reads=["D_U", K("g")],
                        writes=[K("gU")])
                    S.op("pool", lambda e, b=b: e.tensor_scalar(out=gU[b][:, 4:8, :],
                                                                 in0=gU[b][:, 0:4, :], scalar1=-1.0,
                                                                 scalar2=None, op0=ALU.mult),
                         reads=[K("gU")], writes=[K("ngU")])
                    pd, kpd = self.ps8[pb(4)], ("ps", pb(4))

                    def mmd(e, b=b, pd=pd):
                        ins = None
                        for h in range(4):
                            e.matmul(pd[:64, h * 64:(h + 1) * 64], lhsT=gU[b][:, h, :],
                                     rhs=self.ones_f[:64, :64], start=True, stop=False)
                            ins = e.matmul(pd[:64, h * 64:(h + 1) * 64], lhsT=self.ones_f[:64, :64],
                                           rhs=gU[b][:, 4 + h, :], start=False, stop=True)
                        return ins
                    S.op("pe", mmd, reads=[K("gU"), K("ngU"), "ones_f"], writes=[kpd])
                    S.op("dve", lambda e, b=b, pd=pd: e.tensor_tensor(
                        out=dec[b][:], in0=v4(pd, 64),
                        in1=mb[:].unsqueeze(1).to_broadcast([64, 4, 64]), op=ALU.add),
                        reads=[kpd, "D_mb"], writes=[K("dec")])
                    S.op("act", lambda e, b=b: e.activation(out=dec[b][:], in_=dec[b][:],
                                                             func=AF.Exp),
                         reads=[K("dec")], writes=[K("dec")])
                    for grp in range(3):
                        pt, kpt = self.ps8[pb(grp)], ("ps", pb(grp))

                        def trq(e, grp=grp, pt=pt):
                            ins = None
                            for h in range(4):
                                ins = e.transpose(out=pt[:64, h * 128:(h + 1) * 128],
                                                  in_=Y[ys][:, grp * 4 + h, c0:c0 + 64],
                                                  identity=self.ident_f[:, :])
                            return ins
                        S.op("pe", trq, reads=ykeys + ["ident_f"], writes=[kpt])
                        dstv = Vt[b][:] if grp == 2 else QK[b][:, grp * 4:(grp + 1) * 4, :]
                        S.op("act", lambda e, dstv=dstv, pt=pt: e.copy(out=dstv, in_=v4(pt, 128)),
                             reads=[kpt], writes=[K("QK%d" % grp)])
                    S.op("pool", lambda e, b=b: e.tensor_tensor(out=sqq[b][:], in0=QK[b][:],
                                                                 in1=QK[b][:], op=ALU.mult),
                         reads=[K("QK0"), K("QK1")], writes=[K("sqq")])
                    S.op("dve", lambda e, b=b: e.tensor_reduce(out=nrm[b][:, 0:8], in_=sqq[b][:],
                                                                axis=AX.X, op=ALU.add),
                         reads=[K("sqq")], writes=[K("nrm")])
                    S.op("dve", lambda e, b=b: e.tensor_scalar(out=nrm[b][:, 0:8], in0=nrm[b][:, 0:8],
                                                                scalar1=1.0e-6, scalar2=None,
                                                                op0=ALU.add),
                         reads=[K("nrm")], writes=[K("nrm")])
                    S.op("act", lambda e, b=b: e.sqrt(out=nrm[b][:, 0:8], in_=nrm[b][:, 0:8]),
                         reads=[K("nrm")], writes=[K("nrm")])
                    S.op("dve", lambda e, b=b: e.reciprocal(out=nrm[b][:, 8:16], in_=nrm[b][:, 0:8]),
                         reads=[K("nrm")], writes=[K("rn")])
                    S.op("dve", lambda e, b=b: e.tensor_scalar(out=nrm[b][:, 8:12],
                                                                in0=nrm[b][:, 8:12],
                                                                scalar1=128.0 ** -0.5, scalar2=None,
                                                                op0=ALU.mult),
                         reads=[K("rn")], writes=[K("rn")])
                    S.op("dve", lambda e, b=b: e.tensor_tensor(
                        out=QK[b][:], in0=QK[b][:],
                        in1=nrm[b][:, 8:16].unsqueeze(2).to_broadcast([64, 8, 128]), op=ALU.mult),
                        reads=[K("QK0"), K("QK1"), K("rn")], writes=[K("QKn")])
                    for h in range(4):
                        for dst_, src_, col, kd_, kr_ in ((qdk[b], QK[b][:, h, :], 8, "qdk", "eG"),
                                                       (kd[b], QK[b][:, 4 + h, :], 12, "kd", "eG"),
                                                       (bk[b], QK[b][:, 4 + h, :], 16, "bk", "beG"),
                                                       (bv[b], Vt[b][:, h, :], 0, "bv", "beta")):
                            S.op("dve", lambda e, dst_=dst_, src_=src_, col=col, h=h, G=G:
                                 e.tensor_scalar(out=dst_[:, h, :], in0=src_,
                                                 scalar1=G[:, col + h:col + h + 1], scalar2=None,
                                                 op0=ALU.mult),
                                 reads=[K("QKn"), K("QK2"), K(kr_)], writes=[(K(kd_), h)])
                    for grp, (src, ksrc) in enumerate(((qdk[b][:], [*[(K("qdk"), h_) for h_ in range(4)]]),
                                                       (QK[b][:, 4:8, :], [K("QKn")]),
                                                       (QK[b][:, 0:4, :], [K("QKn")]))):
                        pt, kpt = self.ps8[pb(grp)], ("ps", pb(grp))

                        def trt(e, src=src, pt=pt):
                            ins = None
                            for h in range(4):
                                ins = e.transpose(out=pt[:, h * 64:(h + 1) * 64], in_=src[:, h, :],
                                                  identity=self.ident_f[:64, :64])
                            return ins
                        S.op("pe", trt, reads=ksrc + ["ident_f"], writes=[kpt])
                        self.evac(grp, TT[b][:, grp * 4:(grp + 1) * 4, :],
                                  pt[:, :256].rearrange("p (h t) -> p h t", h=4), [kpt],
                                  [K("TT%d" % grp)])
                    pk, kpk = self.ps8[pb(3)], ("ps", pb(3))

                    def mmk(e, b=b, pk=pk):
                        ins = None
                        for h in range(4):
                            e.matmul(pk[:64, h * 64:(h + 1) * 64], lhsT=TT[b][:, 4 + h, :],
                                     rhs=TT[b][:, 4 + h, :], start=True, stop=True)
                            ins = e.matmul(pk[:64, 256 + h * 64:256 + (h + 1) * 64],
                                           lhsT=TT[b][:, 8 + h, :], rhs=TT[b][:, 4 + h, :],
                                           start=True, stop=True)
                        return ins
                    S.op("pe", mmk, reads=[K("TT1"), K("TT2")], writes=[kpk])
                    S.op("dve", lambda e, b=b, pk=pk: e.tensor_tensor(
                        out=Mm[b][:], in0=v4(pk, 64), in1=dec[b][:], op=ALU.mult),
                        reads=[kpk, K("dec")], writes=[K("M")])
                    S.op("dve", lambda e, b=b, pk=pk: e.tensor_tensor(
                        out=QKm[b][:], in0=pk[:64, 256:512].rearrange("p (h x) -> p h x", h=4),
                        in1=dec[b][:], op=ALU.mult), reads=[kpk, K("dec")], writes=[K("QKm")])
                    S.op("pool", lambda e, b=b: e.tensor_tensor(
                        out=Mm[b][:], in0=Mm[b][:],
                        in1=Lst[:].unsqueeze(1).to_broadcast([64, 4, 64]), op=ALU.mult),
                        reads=[K("M"), "D_Lst"], writes=[K("M")])
                    S.op("dve", lambda e, b=b, G=G: e.tensor_tensor(
                        out=Mm[b][:], in0=Mm[b][:], in1=bc(G[:, 0:4], 64), op=ALU.mult),
                        reads=[K("M"), K("beta")], writes=[K("M")])
                    pn, kpn = self.ps8[pb(0)], ("ps", pb(0))

                    def trn(e, b=b, pn=pn):
                        ins = None
                        for h in range(4):
                            e.transpose(out=pn[:64, h * 64:(h + 1) * 64], in_=Mm[b][:, h, :],
                                        identity=self.ident_f[:64, :64])
                            ins = e.transpose(out=pn[:64, 256 + h * 64:256 + (h + 1) * 64],
                                              in_=QKm[b][:, h, :], identity=self.ident_f[:64, :64])
                        return ins
                    S.op("pe", trn, reads=[K("M"), K("QKm"), "ident_f"], writes=[kpn])
                    S.op("act", lambda e, b=b, pn=pn: e.copy(out=Qa[b][:], in_=v4(pn, 64)),
                         reads=[kpn], writes=[K("Qa")])
                    S.op("act", lambda e, b=b, pn=pn: e.copy(
                        out=QKT[b][:], in_=pn[:64, 256:512].rearrange("p (h x) -> p h x", h=4)),
                        reads=[kpn], writes=[K("QKT")])
                    S.op("dve", lambda e, b=b: e.tensor_tensor(out=Bt[b][:], in0=identI[:],
                                                                in1=Qa[b][:], op=ALU.subtract),
                         reads=[K("Qa"), "D_I"], writes=[K("Bt")])
                    Q, Qt, kQ, kQt = Qa[b], Mm[b], K("Qa"), K("M")
                    alt = [(Qb[b], Qtb[b], K("Qb"), K("Qtb")), (Qa[b], Qta[b], K("Qa"), K("Qta"))]
                    for step in range(5):
                        Q2, Qt2, kQ2, kQt2 = alt[step % 2]
                        p1, kp1 = self.ps8[pb(1)], ("ps", pb(1))

                        def mq(e, Q=Q, Qt=Qt, p1=p1, step=step):
                            ins = None
                            for h in range(4):
                                ins = e.matmul(p1[:64, h * 64:(h + 1) * 64], lhsT=Q[:, h, :],
                                               rhs=Qt[:, h, :], start=True, stop=True)
                                if step < 4:
                                    ins = e.matmul(p1[:64, 256 + h * 64:256 + (h + 1) * 64],
                                                   lhsT=Qt[:, h, :], rhs=Q[:, h, :], start=True,
                                                   stop=True)
                            return ins
                        S.op("pe", mq, reads=[kQ, kQt], writes=[kp1])
                        S.op("act", lambda e, Qt2=Qt2, p1=p1: e.copy(out=Qt2[:], in_=v4(p1, 64)),
                             reads=[kp1], writes=[kQt2])
                        if step < 4:
                            S.op("act", lambda e, Q2=Q2, p1=p1: e.copy(
                                out=Q2[:], in_=p1[:64, 256:512].rearrange("p (h x) -> p h x", h=4)),
                                reads=[kp1], writes=[kQ2])
                        p2, kp2 = self.ps8[pb(2)], ("ps", pb(2))

                        def mbm(e, Qt2=Qt2, b=b, p2=p2):
                            ins = None
                            for h in range(4):
                                ins = e.matmul(p2[:64, h * 64:(h + 1) * 64], lhsT=Qt2[:, h, :],
                                               rhs=Bt[b][:, h, :], start=True, stop=True)
                            return ins
                        S.op("pe", mbm, reads=[kQt2, K("Bt")], writes=[kp2])
                        S.op("dve", lambda e, b=b, p2=p2: e.tensor_tensor(out=Bt[b][:], in0=Bt[b][:],
                                                                           in1=v4(p2, 64),
                                                                           op=ALU.add),
                             reads=[kp2, K("Bt")], writes=[K("Bt")])
                        Q, Qt, kQ, kQt = Q2, Qt2, kQ2, kQt2
                    S.op("act", lambda e, b=b: e.copy(out=Tb[b][:], in_=Bt[b][:]), reads=[K("Bt")],
                         writes=[K("Tb")])
                    pu0, kpu0 = self.ps8[pb(3)], ("ps", pb(3))

                    def mu0(e, b=b, pu0=pu0):
                        ins = None
                        for h in range(4):
                            ins = e.matmul(pu0[:64, h * 128:(h + 1) * 128], lhsT=Tb[b][:, h, :],
                                           rhs=bv[b][:, h, :], start=True, stop=True)
                        return ins
                    S.op("pe", mu0, reads=[K("Tb"), *[(K("bv"), h_) for h_ in range(4)]], writes=[kpu0])
                    S.op("act", lambda e, b=b, pu0=pu0: e.copy(out=u0[b][:], in_=v4(pu0, 128)),
                         reads=[kpu0], writes=[K("u0")])
                    pw, kpw = self.ps8[pb(4)], ("ps", pb(4))

                    def mwk(e, b=b, pw=pw):
                        ins = None
                        for h in range(4):
                            ins = e.matmul(pw[:, h * 64:(h + 1) * 64], lhsT=bk[b][:, h, :],
                                           rhs=Tb[b][:, h, :], start=True, stop=True)
                        return ins
                    S.op("pe", mwk, reads=[K("Tb"), *[(K("bk"), h_) for h_ in range(4)]], writes=[kpw])
                    S.op("act", lambda e, b=b, pw=pw: e.copy(
                        out=wkT[b][:], in_=pw[:, :256].rearrange("p (h t) -> p h t", h=4)),
                        reads=[kpw], writes=[K("wkT")])
                    pu, kpu = self.ps8[pb(5)], ("ps", pb(5))

                    def mpu(e, b=b, pu=pu, s=s):
                        ins = None
                        for h in range(4):
                            ins = e.matmul(pu[:64, h * 128:(h + 1) * 128], lhsT=wkT[b][:, h, :],
                                           rhs=Sb[s][:, h, :], start=True, stop=True)
                        return ins
                    S.op("pe", mpu, reads=[K("wkT"), ("D_Sb", s)], writes=[kpu])
                    S.op("dve", lambda e, b=b, pu=pu: e.tensor_tensor(out=ub[b][:], in0=u0[b][:],
                                                                       in1=v4(pu, 128),
                                                                       op=ALU.subtract),
                         reads=[K("u0"), kpu], writes=[K("ub")])
                    po, kpo = self.ps8[pb(0)], ("ps", pb(0))

                    def mpo(e, b=b, po=po, s=s):
                        ins = None
                        for h in range(4):
                            e.matmul(po[:64, h * 128:(h + 1) * 128], lhsT=TT[b][:, h, :],
                                     rhs=Sb[s][:, h, :], start=True, stop=False)
                            ins = e.matmul(po[:64, h * 128:(h + 1) * 128], lhsT=QKT[b][:, h, :],
                                           rhs=ub[b][:, h, :], start=False, stop=True)
                        return ins
                    S.op("pe", mpo, reads=[K("TT0"), ("D_Sb", s), K("QKT"), K("ub")], writes=[kpo])
                    psn, kpsn = self.ps8[pb(1)], ("ps", pb(1))

                    def mps(e, b=b, psn=psn):
                        ins = None
                        for h in range(4):
                            ins = e.matmul(psn[:, h * 128:(h + 1) * 128], lhsT=kd[b][:, h, :],
                                           rhs=ub[b][:, h, :], start=True, stop=True)
                        return ins
                    S.op("pe", mps, reads=[*[(K("kd"), h_) for h_ in range(4)], K("ub")], writes=[kpsn])
                    for h in range(4):
                        S.op("dve", lambda e, b=b, s=s, h=h, psn=psn: e.scalar_tensor_tensor(
                            out=St[s][:, h, :], in0=St[s][:, h, :], scalar=glb[b][:, h:h + 1],
                            in1=psn[:, h * 128:(h + 1) * 128], op0=ALU.mult, op1=ALU.add),
                            reads=[("D_S", s), K("glb"), kpsn], writes=[("D_S", s)])
                    S.op("act", lambda e, s=s: e.copy(out=Sb[s][:], in_=St[s][:]),
                         reads=[("D_S", s)], writes=[("D_Sb", s)])
                    S.op("act", lambda e, b=b: e.activation(out=gs[b][:], in_=zt[b][:], func=AF.Silu),
                         reads=[K("zt")], writes=[K("gs")])
                    S.op("pool", lambda e, b=b: e.tensor_tensor(
                        out=gs[b][:], in0=gs[b][:],
                        in1=ng[:].unsqueeze(1).to_broadcast([64, 4, 128]), op=ALU.mult),
                        reads=[K("gs"), "D_ng"], writes=[K("gs")])
                    S.op("act", lambda e, b=b, po=po: e.copy(out=osb[b][:], in_=v4(po, 128)),
                         reads=[kpo], writes=[K("osb")])
                    S.op("pool", lambda e, b=b: e.tensor_tensor(out=osq[b][:], in0=osb[b][:],
                                                                 in1=osb[b][:], op=ALU.mult),
                         reads=[K("osb")], writes=[K("osq")])
                    S.op("dve", lambda e, b=b: e.tensor_reduce(out=nrm[b][:, 0:4], in_=osq[b][:],
                                                                axis=AX.X, op=ALU.add),
                         reads=[K("osq")], writes=[K("oss")])
                    self.rstd_from_ss(nrm[b][:, 0:4], K("oss"), nrm[b][:, 4:8], K("ors"), 128)
                    S.op("dve", lambda e, b=b: e.tensor_tensor(out=osb[b][:], in0=osb[b][:],
                                                                in1=bc(nrm[b][:, 4:8], 128),
                                                                op=ALU.mult),
                         reads=[K("osb"), K("ors")], writes=[K("osb")])
                    S.op("pool", lambda e, b=b: e.tensor_tensor(out=osq[b][:], in0=osb[b][:],
                                                                 in1=gs[b][:], op=ALU.mult),
                         reads=[K("osb"), K("gs")], writes=[K("osq")])
                    S.dma(out=self.mixin[tok:tok + 64, 0:512],
                          in_=osq[b][:].rearrange("p h v -> p (h v)"), sem="D_out%d" % b,
                          reads=[K("osq")])
        S.interleave([lambda s=s: seq_thread(s) for s in range(NSEQ)])
        S.barrier()


def build_program():
    P = Prog()
    src = P.x_in
    for l in range(DEPTH):
        dst = P.xmid if l < DEPTH - 1 else P.y
        P.phase_A(l, src)
        P.phase_D(l)
        P.phase_S(l)
        P.phase_H(l)
        P.phase_E(l, src)
        P.phase_F(l, dst)
        src = dst
    return P


def kernel(**inputs):
    n = 8
    P = build_program()
    x = np.ascontiguousarray(inputs["x"], dtype=np.float32)
    shared = {k: np.ascontiguousarray(v, dtype=np.float32) for k, v in inputs.items() if k != "x"}
    in_maps = []
    for c in range(n):
        m = dict(shared)
        m["x"] = np.ascontiguousarray(x[c * NSEQ:(c + 1) * NSEQ].reshape(NTOK, D))
        in_maps.append(m)
    res = run_bass_kernel_spmd(P.nc, in_maps, core_ids=list(range(n)))
    out = np.stack([np.asarray(r["y"]).reshape(NSEQ, SEQ, D) for r in res.results], axis=0)
    return out.reshape(n * NSEQ, SEQ, D).astype(np.float32)
```

```python
import numpy as np
import concourse.bass as bass
import concourse.mybir as mybir
from concourse.bass_utils import run_bass_kernel_spmd

F32 = mybir.dt.float32
BF16 = mybir.dt.bfloat16
AF = mybir.ActivationFunctionType
ALU = mybir.AluOpType
AX = mybir.AxisListType

D = 1024
SEQ = 4096
NSEQ = 2
NTOK = NSEQ * SEQ
DEPTH = 2
D_IN = 3788
D_FF = 2816
EPS = 1e-6
NEG = -30000.0

TM_GROUPS = [(1536, 2056), (2376, 2440), (2760, 2764), (3020, 3788)]
TM_W = sum(b - a for a, b in TM_GROUPS)
TM_AZ, TM_AB, TM_AA = 0, 512, 516
TM_BV = 520
TM_WI = 584
TM_CF, TM_CI, TM_CG = 588, 844, 1100
FM_GROUPS = [(i * 128, 128) for i in range(12)] + [(2056, 128), (2184, 128), (2312, 64),
             (2440, 128), (2568, 128), (2696, 64), (2764, 128), (2892, 128), (3020, 128), (3148, 128)]
FM_ROW = {}
_r = 0
for _c, _n in FM_GROUPS:
    FM_ROW[_c] = _r
    _r += _n
FM_H = _r


class Sched:
    def __init__(self, nc):
        self.nc = nc
        self.eng = {"pe": nc.tensor, "act": nc.scalar, "dve": nc.vector, "pool": nc.gpsimd,
                    "sp": nc.sync}
        self.sems = {}
        self.cnt = {}
        for k in ("pe", "act", "dve", "pool"):
            self.sems[k] = nc.alloc_semaphore("s_" + k)
            self.cnt[k] = 0
        self.seen = {k: {} for k in self.eng}
        self.bufs = {}
        self.ninstr = 0

    def _buf(self, key):
        b = self.bufs.get(key)
        if b is None:
            b = {"w": None, "r": {}}
            self.bufs[key] = b
        return b

    def _deps(self, engine, reads, writes):
        deps = {}

        def add(ev, same_ok):
            if ev is None:
                return
            sk, val = ev
            if sk == engine and not same_ok:
                return
            if deps.get(sk, 0) < val:
                deps[sk] = val

        for k in reads:
            b = self._buf(k)
            add(b["w"], engine != "pe")
            if isinstance(k, tuple) and k[0] in ("ps", "psb"):
                for sk, val in b["r"].items():
                    add((sk, val), False)
        for k in writes:
            b = self._buf(k)
            add(b["w"], engine != "pe")
            for sk, val in b["r"].items():
                add((sk, val), False)
        return deps

    def _emit_waits(self, engine, deps):
        e = self.eng[engine]
        seen = self.seen[engine]
        for sk, val in deps.items():
            if seen.get(sk, 0) >= val:
                continue
            e.wait_ge(self.sems[sk], val)
            self.ninstr += 1
            seen[sk] = val

    def _record(self, ev, reads, writes):
        for k in writes:
            b = self._buf(k)
            b["w"] = ev
            b["r"] = {}
        for k in reads:
            b = self._buf(k)
            if b["r"].get(ev[0], 0) < ev[1]:
                b["r"][ev[0]] = ev[1]

    def op(self, engine, fn, reads=(), writes=()):
        deps = self._deps(engine, reads, writes)
        self._emit_waits(engine, deps)
        ins = fn(self.eng[engine])
        self.cnt[engine] += 1
        ins.then_inc(self.sems[engine], 1)
        self.ninstr += 1
        self._record((engine, self.cnt[engine]), reads, writes)
        self._yield()

    def dma(self, out, in_, sem, reads=(), writes=(), q="sp"):
        if sem not in self.sems:
            self.sems[sem] = self.nc.alloc_semaphore("d_" + sem)
            self.cnt[sem] = 0
        deps = self._deps(q, reads, writes)
        self._emit_waits(q, deps)
        ins = self.eng[q].dma_start(out=out, in_=in_)
        self.cnt[sem] += 16
        ins.then_inc(self.sems[sem], 16)
        self.ninstr += 1
        self._record((sem, self.cnt[sem]), reads, writes)
        self._yield()

    def interleave(self, fns):
        import threading
        n = len(fns)
        st = {"cur": 0, "alive": [True] * n, "err": None}
        cond = threading.Condition()

        def nxt(i):
            for d in range(1, n + 1):
                k = (i + d) % n
                if st["alive"][k]:
                    return k
            return -1

        def pass_turn(me):
            with cond:
                st["cur"] = nxt(me)
                cond.notify_all()
                while st["alive"][me] and st["cur"] != me and st["err"] is None:
                    cond.wait()
                if st["err"] is not None and st["alive"][me]:
                    raise RuntimeError("interleave aborted")

        def worker(i):
            with cond:
                while st["cur"] != i and st["err"] is None:
                    cond.wait()
            try:
                if st["err"] is None:
                    self._tl.me = i
                    fns[i]()
            except BaseException as e:
                if st["err"] is None:
                    st["err"] = e
            finally:
                with cond:
                    st["alive"][i] = False
                    if st["cur"] == i:
                        st["cur"] = nxt(i)
                    cond.notify_all()

        self._tl = threading.local()
        self._pass = pass_turn
        ths = [threading.Thread(target=worker, args=(i,)) for i in range(n)]
        for t in ths:
            t.start()
        for t in ths:
            t.join()
        self._pass = None
        if st["err"] is not None:
            raise st["err"]

    def _yield(self):
        p = getattr(self, "_pass", None)
        if p is not None:
            p(self._tl.me)

    def barrier(self):
        allv = {k: v for k, v in self.cnt.items() if v > 0}
        for engine in self.eng:
            self._emit_waits(engine, dict(allv))
        self.bufs = {}


def bcast_rows(ap2d_row, nparts):
    return ap2d_row.partition_broadcast(nparts)


class Prog:
    def __init__(self, layers=(0, 1), phases="AHDSEF", dbg=()):
        self.nc = nc = bass.Bass("TRN2", target_bir_lowering=False)
        self.S = Sched(nc)
        self.dbg = dbg
        dt = nc.dram_tensor
        self.x_in = dt("x", [NTOK, D], F32, kind="ExternalInput").ap()
        self.w_in = dt("w_in", [DEPTH, D, D_IN], F32, kind="ExternalInput").ap()
        self.dn_conv = dt("dn_conv", [DEPTH, 4, 1536], F32, kind="ExternalInput").ap()
        self.dn_a_log = dt("dn_a_log", [DEPTH, 4], F32, kind="ExternalInput").ap()
        self.dn_dt_bias = dt("dn_dt_bias", [DEPTH, 4], F32, kind="ExternalInput").ap()
        self.dn_norm = dt("dn_norm", [DEPTH, 128], F32, kind="ExternalInput").ap()
        self.hg_lb = dt("hg_lb", [DEPTH, 256], F32, kind="ExternalInput").ap()
        self.hg_norm = dt("hg_norm", [DEPTH, 64], F32, kind="ExternalInput").ap()
        self.w_out = dt("w_out", [DEPTH, D, D], F32, kind="ExternalInput").ap()
        self.g_mix_pre = dt("g_mix_pre", [DEPTH, D], F32, kind="ExternalInput").ap()
        self.g_mix_post = dt("g_mix_post", [DEPTH, D], F32, kind="ExternalInput").ap()
        self.g_ffn_pre = dt("g_ffn_pre", [DEPTH, D], F32, kind="ExternalInput").ap()
        self.g_ffn_post = dt("g_ffn_post", [DEPTH, D], F32, kind="ExternalInput").ap()
        self.w_up = dt("ffn_w_up", [DEPTH, D, 2 * D_FF], F32, kind="ExternalInput").ap()
        self.ffn_conv = dt("ffn_conv", [DEPTH, 3, 2 * D_FF], F32, kind="ExternalInput").ap()
        self.w_down = dt("ffn_w_down", [DEPTH, D_FF, D], F32, kind="ExternalInput").ap()
        self.y = dt("y", [NTOK, D], F32, kind="ExternalOutput").ap()

        def scratch(name, shape, dtype):
            kind = "ExternalOutput" if name in dbg else "Internal"
            return dt(name, shape, dtype, kind=kind).ap()

        self.ptok = scratch("ptok", [NTOK, TM_W], F32)
        self.pfeat = scratch("pfeat", [FM_H, NTOK], F32)
        self.mixin = scratch("mixin", [NTOK, D], F32)
        self.x1 = scratch("x1", [NTOK, D], F32)
        self.h2T = scratch("h2T", [D, NTOK], BF16)
        self.xmid = scratch("xmid", [NTOK, D], F32)

        self.ps = [nc.alloc_psum_tensor("ps%d" % i, [128, 512], F32) for i in range(6)]
        self.psb = [nc.alloc_psum_tensor("psb%d" % i, [128, 1024], BF16) for i in range(2)]
        self.ps8 = [p[:, :] for p in self.ps] + [p[:, :].bitcast(F32) for p in self.psb]
        self.ident_b = nc.alloc_sbuf_tensor("ident_b", [128, 128], BF16)
        self.ident_f = nc.alloc_sbuf_tensor("ident_f", [128, 128], F32)
        self.ones_f = nc.alloc_sbuf_tensor("ones_f", [128, 128], F32)
        self._consts()
        self.sb_base = nc.sbuf_base
        self.layers = layers
        self.phases = phases

    def _consts(self):
        nc, S = self.nc, self.S
        S.op("pool", lambda e: e.memset(self.ones_f[:], 1.0), writes=["ones_f"])
        S.op("pool", lambda e: e.memset(self.ident_f[:], 0.0), writes=["ident_f"])
        S.op("pool", lambda e: e.affine_select(out=self.ident_f[:], in_=self.ident_f[:],
                                                pattern=[[-1, 128]], compare_op=ALU.not_equal,
                                                fill=1.0, base=0, channel_multiplier=1),
             reads=["ident_f"], writes=["ident_f"])
        S.op("dve", lambda e: e.tensor_copy(out=self.ident_b[:], in_=self.ident_f[:]),
             reads=["ident_f"], writes=["ident_b"])

    def sbuf_reset(self):
        self.nc.sbuf_base = self.sb_base

    def sb(self, name, shape, dtype):
        self._uid = getattr(self, "_uid", 0) + 1
        return self.nc.alloc_sbuf_tensor("%s_u%d" % (name, self._uid), shape, dtype)

    def load_bcast(self, name, row_ap, n):
        t = self.sb(name, [128, n], F32)
        self.S.dma(out=t[:], in_=bcast_rows(row_ap, 128), sem="ld_" + name, writes=[name])
        return t

    def load_weight_bf16(self, name, w_ap, K, N, stage, stage_keys):
        S = self.S
        wt = self.sb(name, [128, K, N], BF16)
        CH = stage[0].shape[1]
        i = 0
        for k in range(K):
            for c0 in range(0, N, CH):
                cw = min(CH, N - c0)
                st, sk = stage[i % 2], stage_keys[i % 2]
                S.dma(out=st[:, :cw], in_=w_ap[k * 128:(k + 1) * 128, c0:c0 + cw], sem=sk,
                      writes=[sk])
                eng = ("dve", "pool", "act")[i % 3]
                if eng == "act":
                    S.op("act", lambda e, st=st, k=k, c0=c0, cw=cw: e.copy(
                        out=wt[:, k, c0:c0 + cw], in_=st[:, :cw]), reads=[sk], writes=[name])
                else:
                    S.op(eng, lambda e, st=st, k=k, c0=c0, cw=cw: e.tensor_copy(
                        out=wt[:, k, c0:c0 + cw], in_=st[:, :cw]), reads=[sk], writes=[name])
                i += 1
        return wt

    def evac(self, i, out, in_, reads, writes):
        if i % 2 == 0:
            self.S.op("act", lambda e: e.copy(out=out, in_=in_), reads=reads, writes=writes)
        else:
            self.S.op("dve", lambda e: e.tensor_copy(out=out, in_=in_), reads=reads, writes=writes)

    def norm_transpose(self, src_dram, tok0, gbc, gkey, T, tag, xt, hb, hT, ss, rs, slot,
                       do_norm=True):
        S = self.S
        kx = lambda j: (tag + "xt", slot, j)
        for j in range(4):
            S.dma(out=xt[slot][:, j, :], in_=src_dram[tok0 + j * 128: tok0 + (j + 1) * 128, :],
                  sem="%sxt%d_%d" % (tag, slot, j), writes=[kx(j)])
        kss, krs = (tag + "ss", slot), (tag + "rs", slot)
        if do_norm:
            for j in range(4):
                S.op("act", lambda e, j=j: e.activation(out=T["junk"][:], in_=xt[slot][:, j, :],
                                                         func=AF.Square,
                                                         accum_out=ss[slot][:, j:j + 1]),
                     reads=[kx(j)], writes=[kss])
            S.op("dve", lambda e: e.tensor_scalar(out=rs[slot][:], in0=ss[slot][:],
                                                   scalar1=1.0 / D, scalar2=EPS, op0=ALU.mult,
                                                   op1=ALU.add), reads=[kss], writes=[krs])
            S.op("act", lambda e: e.sqrt(out=rs[slot][:], in_=rs[slot][:]), reads=[krs],
                 writes=[krs])
            S.op("dve", lambda e: e.reciprocal(out=rs[slot][:], in_=rs[slot][:]), reads=[krs],
                 writes=[krs])
        for j in range(4):
            khb = (tag + "hb", j % 2)
            hbj = hb[j % 2]
            if do_norm:
                S.op("dve", lambda e, j=j, hbj=hbj: e.scalar_tensor_tensor(
                    out=hbj[:], in0=xt[slot][:, j, :], scalar=rs[slot][:, j:j + 1], in1=gbc[:],
                    op0=ALU.mult, op1=ALU.mult), reads=[kx(j), krs, gkey], writes=[khb])
            else:
                S.op("pool", lambda e, j=j, hbj=hbj: e.tensor_copy(out=hbj[:],
                                                                  in_=xt[slot][:, j, :]),
                     reads=[kx(j)], writes=[khb])
            pb = self.psb[j % 2]
            kpb = ("psb", j % 2)

            def tr(e, hbj=hbj, pb=pb):
                ins = None
                for k in range(8):
                    ins = e.transpose(out=pb[:, k * 128:(k + 1) * 128],
                                      in_=hbj[:, k * 128:(k + 1) * 128], identity=self.ident_b[:])
                return ins
            S.op("pe", tr, reads=[khb, "ident_b"], writes=[kpb])
            self.evac(j, hT[slot][:, :, j * 128:(j + 1) * 128],
                      pb[:].rearrange("p (k t) -> p k t", k=8), [kpb], [(tag + "hT", slot, j)])

    def phase_A(self, l, xsrc):
        nc, S = self.nc, self.S
        self.sbuf_reset()
        stage = [self.sb("A_stage%d" % i, [128, 3788], F32) for i in range(2)]
        Wi = self.load_weight_bf16("A_Wi", self.w_in[l], 8, D_IN, stage, ["A_stg0", "A_stg1"])
        gbc = self.load_bcast("A_gbc", self.g_mix_pre[l:l + 1, :], D)
        S.barrier()
        xt = [self.sb("A_xt%d" % i, [128, 4, D], F32) for i in range(2)]
        hb = [self.sb("A_hb%d" % i, [128, D], BF16) for i in range(2)]
        hT = [self.sb("A_hT%d" % i, [128, 8, 512], BF16) for i in range(2)]
        ss = [self.sb("A_ss%d" % i, [128, 4], F32) for i in range(2)]
        rs = [self.sb("A_rs%d" % i, [128, 4], F32) for i in range(2)]
        T = {"junk": self.sb("A_junk", [128, D], F32)}
        ofm = [self.sb("A_ofm%d" % i, [128, 512], F32) for i in range(4)]
        otm = [self.sb("A_otm%d" % i, [128, TM_W], F32) for i in range(2)]
        tmch = []
        off = 0
        for a, b in TM_GROUPS:
            c = a
            while c < b:
                w = min(512, b - c)
                tmch.append((c, w, off))
                off += w
                c += w
        nev = 0
        for blk in range(NTOK // 512):
            slot = blk % 2
            tok0 = blk * 512
            self.norm_transpose(xsrc, tok0, gbc, "A_gbc", T, "A_", xt, hb, hT, ss, rs, slot)
            hkeys = [("A_hT", slot, j) for j in range(4)]
            for gi, (c0, n) in enumerate(FM_GROUPS):
                ps = self.ps[gi % 4]
                kps = ("ps", gi % 4)

                def mm(e, ps=ps, c0=c0, n=n):
                    ins = None
                    for k in range(8):
                        ins = e.matmul(ps[:n, :], lhsT=Wi[:, k, c0:c0 + n], rhs=hT[slot][:, k, :],
                                       start=(k == 0), stop=(k == 7))
                    return ins
                S.op("pe", mm, reads=["A_Wi"] + hkeys, writes=[kps])
                o = ofm[gi % 4]
                ko = ("A_ofm", gi % 4)
                self.evac(nev, o[:n, :], ps[:n, :], [kps], [ko])
                nev += 1
                r0 = FM_ROW[c0]
                S.dma(out=self.pfeat[r0:r0 + n, tok0:tok0 + 512], in_=o[:n, :],
                      sem="A_ofm%d" % (gi % 4), reads=[ko], q="pool")
            for j in range(4):
                o = otm[j % 2]
                ko = ("A_otm", j % 2)
                for ci, (c, w, dst) in enumerate(tmch):
                    ps = self.ps[4 + ci % 2]
                    kps = ("ps", 4 + ci % 2)

                    def mm(e, ps=ps, c=c, w=w, j=j):
                        ins = None
                        for k in range(8):
                            ins = e.matmul(ps[:, :w], lhsT=hT[slot][:, k, j * 128:(j + 1) * 128],
                                           rhs=Wi[:, k, c:c + w], start=(k == 0), stop=(k == 7))
                        return ins
                    S.op("pe", mm, reads=["A_Wi", hkeys[j]], writes=[kps])
                    self.evac(nev, o[:, dst:dst + w], ps[:, :w], [kps], [(ko, ci)])
                    nev += 1
                S.dma(out=self.ptok[tok0 + j * 128: tok0 + (j + 1) * 128, :], in_=o[:, :],
                      sem="A_otm%d" % (j % 2), reads=[(ko, ci) for ci in range(len(tmch))],
                      q="pool")
        S.barrier()

    def transp8(self, hbj, khb, dst, kdst, j):
        pb = self.psb[j % 2]
        kpb = ("psb", j % 2)

        def tr(e):
            ins = None
            for k in range(8):
                ins = e.transpose(out=pb[:, k * 128:(k + 1) * 128],
                                  in_=hbj[:, k * 128:(k + 1) * 128], identity=self.ident_b[:])
            return ins
        self.S.op("pe", tr, reads=[khb, "ident_b"], writes=[kpb])
        self.evac(j, dst, pb[:].rearrange("p (k t) -> p k t", k=8), [kpb], [kdst])

    def rstd_from_ss(self, ss, kss, rs, krs, n):
        S = self.S
        S.op("dve", lambda e: e.tensor_scalar(out=rs, in0=ss, scalar1=1.0 / n, scalar2=EPS,
                                               op0=ALU.mult, op1=ALU.add), reads=[kss],
             writes=[krs])
        S.op("act", lambda e: e.sqrt(out=rs, in_=rs), reads=[krs], writes=[krs])
        S.op("dve", lambda e: e.reciprocal(out=rs, in_=rs), reads=[krs], writes=[krs])

    def phase_E(self, l, xsrc):
        S = self.S
        self.sbuf_reset()
        stage = [self.sb("E_stage%d" % i, [128, 1024], F32) for i in range(2)]
        Wo = self.load_weight_bf16("E_Wo", self.w_out[l], 8, D, stage, ["E_stg0", "E_stg1"])
        gpost = self.load_bcast("E_gpost", self.g_mix_post[l:l + 1, :], D)
        gpre = self.load_bcast("E_gpre", self.g_ffn_pre[l:l + 1, :], D)
        S.barrier()
        xt = [self.sb("E_xt%d" % i, [128, 4, D], F32) for i in range(2)]
        xr = [self.sb("E_xr%d" % i, [128, 4, D], F32) for i in range(2)]
        hb = [self.sb("E_hb%d" % i, [128, D], BF16) for i in range(2)]
        mT = [self.sb("E_mT%d" % i, [128, 8, 512], BF16) for i in range(2)]
        h2s = [self.sb("E_h2s%d" % i, [128, 8, 512], BF16) for i in range(2)]
        yt = [self.sb("E_yt%d" % i, [128, D], F32) for i in range(2)]
        x1t = [self.sb("E_x1t%d" % i, [128, D], F32) for i in range(2)]
        h2b = [self.sb("E_h2b%d" % i, [128, D], BF16) for i in range(2)]
        junk = self.sb("E_junk", [128, D], F32)
        st = [self.sb("E_st%d" % i, [128, 8], F32) for i in range(2)]
        h2T_v = self.h2T.rearrange("(k p) t -> p k t", p=128)
        for blk in range(NTOK // 512):
            slot = blk % 2
            tok0 = blk * 512
            self.norm_transpose(self.mixin, tok0, None, None, None, "E_", xt, hb, mT, None, None,
                                slot, do_norm=False)
            for j in range(4):
                S.dma(out=xr[slot][:, j, :], in_=xsrc[tok0 + j * 128: tok0 + (j + 1) * 128, :],
                      sem="E_xr%d_%d" % (slot, j), writes=[("E_xr", slot, j)])
            for j in range(4):
                p2 = j % 2
                kss, krs = ("E_ss", p2), ("E_rs", p2)
                for hf in range(2):
                    ps = self.ps[2 * p2 + hf]
                    kps = ("ps", 2 * p2 + hf)

                    def mm(e, ps=ps, hf=hf, j=j):
                        ins = None
                        for k in range(8):
                            ins = e.matmul(ps[:, :], lhsT=mT[slot][:, k, j * 128:(j + 1) * 128],
                                           rhs=Wo[:, k, hf * 512:(hf + 1) * 512], start=(k == 0),
                                           stop=(k == 7))
                        return ins
                    S.op("pe", mm, reads=["E_Wo", ("E_hT", slot, j)], writes=[kps])
                    S.op("act", lambda e, ps=ps, hf=hf, p2=p2: e.activation(
                        out=junk[:, :512], in_=ps[:, :], func=AF.Square,
                        accum_out=st[p2][:, hf:hf + 1]), reads=[kps], writes=[(kss, hf)])
                S.op("dve", lambda e, p2=p2: e.tensor_tensor(out=st[p2][:, 2:3], in0=st[p2][:, 0:1],
                                                              in1=st[p2][:, 1:2], op=ALU.add),
                     reads=[(kss, 0), (kss, 1)], writes=[kss])
                self.rstd_from_ss(st[p2][:, 2:3], kss, st[p2][:, 3:4], krs, D)
                kyt = ("E_yt", p2)
                for hf in range(2):
                    ps = self.ps[2 * p2 + hf]
                    kps = ("ps", 2 * p2 + hf)
                    S.op("act", lambda e, ps=ps, hf=hf, p2=p2: e.activation(
                        out=yt[p2][:, hf * 512:(hf + 1) * 512], in_=ps[:, :], func=AF.Copy,
                        scale=st[p2][:, 3:4]), reads=[kps, krs], writes=[(kyt, hf)])
                S.op("dve", lambda e, p2=p2: e.tensor_tensor(out=yt[p2][:], in0=yt[p2][:],
                                                              in1=gpost[:], op=ALU.mult),
                     reads=[(kyt, 0), (kyt, 1), "E_gpost"], writes=[kyt])
                kx1 = ("E_x1t", p2)
                S.op("pool", lambda e, p2=p2, j=j: e.tensor_tensor(out=x1t[p2][:], in0=yt[p2][:],
                                                                    in1=xr[slot][:, j, :],
                                                                    op=ALU.add),
                     reads=[kyt, ("E_xr", slot, j)], writes=[kx1])
                S.dma(out=self.x1[tok0 + j * 128: tok0 + (j + 1) * 128, :], in_=x1t[p2][:],
                      sem="E_x1t%d" % p2, reads=[kx1], q="pool")
                kss2, krs2 = ("E_ss2", p2), ("E_rs2", p2)
                S.op("act", lambda e, p2=p2: e.activation(out=junk[:], in_=x1t[p2][:],
                                                           func=AF.Square,
                                                           accum_out=st[p2][:, 4:5]),
                     reads=[kx1], writes=[kss2])
                self.rstd_from_ss(st[p2][:, 4:5], kss2, st[p2][:, 5:6], krs2, D)
                kh2b = ("E_h2b", p2)
                S.op("dve", lambda e, p2=p2: e.scalar_tensor_tensor(
                    out=h2b[p2][:], in0=x1t[p2][:], scalar=st[p2][:, 5:6], in1=gpre[:],
                    op0=ALU.mult, op1=ALU.mult), reads=[kx1, krs2, "E_gpre"], writes=[kh2b])
                self.transp8(h2b[p2], kh2b, h2s[slot][:, :, j * 128:(j + 1) * 128],
                             ("E_h2s", slot, j), j)
            S.dma(out=h2T_v[:, :, tok0:tok0 + 512], in_=h2s[slot][:],
                  sem="E_h2s%d" % slot, reads=[("E_h2s", slot, j) for j in range(4)], q="pool")
        S.barrier()

    def load_convw(self, name, conv_ap, ntaps, ntiles):
        S = self.S
        raw = self.sb(name + "_raw", [ntiles, ntaps, 128], F32)
        cw = self.sb(name, [128, ntaps, ntiles], F32)
        S.dma(out=raw[:], in_=conv_ap.rearrange("j (t p) -> t j p", p=128), sem="ld_" + name,
              writes=[name + "_raw"])
        for j in range(ntaps):
            ps = self.ps[j % 2]
            kps = ("ps", j % 2)
            S.op("pe", lambda e, j=j, ps=ps: e.transpose(out=ps[:, :ntiles], in_=raw[:, j, :],
                                                         identity=self.ident_f[:ntiles, :ntiles]),
                 reads=[name + "_raw", "ident_f"], writes=[kps])
            S.op("dve", lambda e, j=j, ps=ps: e.tensor_copy(out=cw[:, j, :], in_=ps[:, :ntiles]),
                 reads=[kps], writes=[name])
        return cw

    def phase_F(self, l, dst):
        S = self.S
        self.sbuf_reset()
        NT = 22
        stage = [self.sb("F_stage%d" % i, [128, 512], F32) for i in range(2)]
        Wu = self.load_weight_bf16("F_Wu", self.w_up[l], 8, 2 * D_FF, stage, ["F_stg0", "F_stg1"])
        Wd = self.load_weight_bf16("F_Wd", self.w_down[l], NT, D, stage, ["F_stg0", "F_stg1"])
        gpost = self.load_bcast("F_gpost", self.g_ffn_post[l:l + 1, :], D)
        cw = self.load_convw("F_cw", self.ffn_conv[l], 3, 2 * NT)
        S.barrier()
        hT = self.sb("F_hT", [128, 8, 512], BF16)
        gT = self.sb("F_gT", [128, NT, 512], BF16)
        U = [self.sb("F_U%d" % i, [128, 514], F32) for i in range(2)]
        C = [self.sb("F_C%d" % i, [128, 512], F32) for i in range(2)]
        GL = self.sb("F_GL", [128, 512], F32)
        halo = self.sb("F_halo", [128, 2 * NT, 2], F32)
        x1t = [self.sb("F_x1t%d" % i, [128, D], F32) for i in range(2)]
        yt = [self.sb("F_yt%d" % i, [128, D], F32) for i in range(2)]
        junk = self.sb("F_junk", [128, 512], F32)
        st = [self.sb("F_st%d" % i, [128, 8], F32) for i in range(2)]
        h2T_v = self.h2T.rearrange("(k p) t -> p k t", p=128)
        for blk in range(NTOK // 512):
            tok0 = blk * 512
            if blk % (SEQ // 512) == 0:
                S.op("pool", lambda e: e.memset(halo[:], 0.0), writes=["F_halo"])
            S.dma(out=hT[:], in_=h2T_v[:, :, tok0:tok0 + 512], sem="F_hT", writes=["F_hT"])
            for i in range(NT):
                for gv in range(2):
                    ti = gv * NT + i
                    c0 = ti * 128
                    ps = self.ps[gv * 2 + i % 2]
                    kps = ("ps", gv * 2 + i % 2)

                    def mm(e, ps=ps, c0=c0):
                        ins = None
                        for k in range(8):
                            ins = e.matmul(ps[:, :], lhsT=Wu[:, k, c0:c0 + 128], rhs=hT[:, k, :],
                                           start=(k == 0), stop=(k == 7))
                        return ins
                    S.op("pe", mm, reads=["F_Wu", "F_hT"], writes=[kps])
                    u, ku = U[gv], ("F_U", gv)
                    c, kc = C[gv], ("F_C", gv)
                    S.op("act", lambda e, u=u, ps=ps: e.copy(out=u[:, 2:514], in_=ps[:, :]),
                         reads=[kps], writes=[(ku, "b")])
                    S.op("pool", lambda e, u=u, ti=ti: e.tensor_copy(out=u[:, 0:2],
                                                                     in_=halo[:, ti, :]),
                         reads=["F_halo"], writes=[(ku, "h")])
                    S.op("act", lambda e, u=u, c=c, ti=ti: e.activation(
                        out=c[:], in_=u[:, 0:512], func=AF.Copy, scale=cw[:, 0, ti:ti + 1]),
                        reads=[(ku, "b"), (ku, "h"), "F_cw"], writes=[kc])
                    for tap in (1, 2):
                        S.op("dve", lambda e, u=u, c=c, ti=ti, tap=tap: e.scalar_tensor_tensor(
                            out=c[:], in0=u[:, tap:tap + 512], scalar=cw[:, tap, ti:ti + 1],
                            in1=c[:], op0=ALU.mult, op1=ALU.add),
                            reads=[(ku, "b"), (ku, "h"), "F_cw", kc], writes=[kc])
                    S.op("pool", lambda e, u=u, ti=ti: e.tensor_copy(out=halo[:, ti, :],
                                                                     in_=u[:, 512:514]),
                         reads=[(ku, "b")], writes=["F_halo"])
                S.op("act", lambda e: e.activation(out=GL[:], in_=C[0][:],
                                                    func=AF.Gelu_apprx_tanh),
                     reads=[("F_C", 0)], writes=["F_GL"])
                S.op("pool", lambda e, i=i: e.tensor_tensor(out=gT[:, i, :], in0=GL[:],
                                                             in1=C[1][:], op=ALU.mult),
                     reads=["F_GL", ("F_C", 1)], writes=[("F_gT", i)])
            gkeys = [("F_gT", i) for i in range(NT)]
            for j in range(4):
                p2 = j % 2
                S.dma(out=x1t[p2][:], in_=self.x1[tok0 + j * 128: tok0 + (j + 1) * 128, :],
                      sem="F_x1t%d" % p2, writes=[("F_x1t", p2)])
                kss, krs = ("F_ss", p2), ("F_rs", p2)
                for hf in range(2):
                    ps = self.ps[4 + hf]
                    kps = ("ps", 4 + hf)

                    def mm(e, ps=ps, hf=hf, j=j):
                        ins = None
                        for k in range(NT):
                            ins = e.matmul(ps[:, :], lhsT=gT[:, k, j * 128:(j + 1) * 128],
                                           rhs=Wd[:, k, hf * 512:(hf + 1) * 512], start=(k == 0),
                                           stop=(k == NT - 1))
                        return ins
                    S.op("pe", mm, reads=["F_Wd"] + gkeys, writes=[kps])
                    S.op("act", lambda e, ps=ps, hf=hf, p2=p2: e.activation(
                        out=junk[:, :], in_=ps[:, :], func=AF.Square,
                        accum_out=st[p2][:, hf:hf + 1]), reads=[kps], writes=[(kss, hf)])
                S.op("dve", lambda e, p2=p2: e.tensor_tensor(out=st[p2][:, 2:3], in0=st[p2][:, 0:1],
                                                              in1=st[p2][:, 1:2], op=ALU.add),
                     reads=[(kss, 0), (kss, 1)], writes=[kss])
                self.rstd_from_ss(st[p2][:, 2:3], kss, st[p2][:, 3:4], krs, D)
                kyt = ("F_yt", p2)
                for hf in range(2):
                    ps = self.ps[4 + hf]
                    kps = ("ps", 4 + hf)
                    S.op("act", lambda e, ps=ps, hf=hf, p2=p2: e.activation(
                        out=yt[p2][:, hf * 512:(hf + 1) * 512], in_=ps[:, :], func=AF.Copy,
                        scale=st[p2][:, 3:4]), reads=[kps, krs], writes=[(kyt, hf)])
                S.op("dve", lambda e, p2=p2: e.tensor_tensor(out=yt[p2][:], in0=yt[p2][:],
                                                              in1=gpost[:], op=ALU.mult),
                     reads=[(kyt, 0), (kyt, 1), "F_gpost"], writes=[kyt])
                S.op("pool", lambda e, p2=p2: e.tensor_tensor(out=yt[p2][:], in0=yt[p2][:],
                                                               in1=x1t[p2][:], op=ALU.add),
                     reads=[kyt, ("F_x1t", p2)], writes=[kyt])
                S.dma(out=dst[tok0 + j * 128: tok0 + (j + 1) * 128, :], in_=yt[p2][:],
                      sem="F_yt%d" % p2, reads=[kyt], q="pool")
        S.barrier()

    def phase_H(self, l):
        S = self.S
        self.sbuf_reset()
        sb = self.sb
        UU = sb("H_UU", [64, 128], F32)
        UL = sb("H_UL", [64, 64], F32)
        tmpm = sb("H_tmpm", [64, 64], F32)
        S.op("pool", lambda e: e.memset(UU[:], 1.0), writes=["H_UU"])
        S.op("pool", lambda e: e.affine_select(out=UU[:, 0:64], in_=UU[:, 0:64], pattern=[[1, 64]],
                                                compare_op=ALU.is_ge, fill=0.0, base=0,
                                                channel_multiplier=-1),
             reads=["H_UU"], writes=["H_UU"])
        S.op("pool", lambda e: e.memset(tmpm[:], 1.0), writes=["H_tmpm"])
        S.op("pool", lambda e: e.affine_select(out=tmpm[:], in_=tmpm[:], pattern=[[0, 64]],
                                                compare_op=ALU.is_ge, fill=0.0, base=31,
                                                channel_multiplier=-1),
             reads=["H_tmpm"], writes=["H_tmpm"])
        S.op("pool", lambda e: e.tensor_tensor(out=UU[:, 64:128], in0=UU[:, 0:64], in1=tmpm[:],
                                                op=ALU.subtract),
             reads=["H_UU", "H_tmpm"], writes=["H_UU"])
        S.op("pool", lambda e: e.memset(UL[:], 1.0), writes=["H_UL"])
        S.op("pool", lambda e: e.affine_select(out=UL[:], in_=UL[:], pattern=[[-1, 64]],
                                                compare_op=ALU.is_gt, fill=0.0, base=0,
                                                channel_multiplier=1),
             reads=["H_UL"], writes=["H_UL"])
        lb = sb("H_lb", [64, 256], F32)
        oml = sb("H_oml", [64, 256], F32)
        if l == 0:
            S.op("pool", lambda e: e.memset(lb[:], 0.0), writes=["H_lb"])
        else:
            r0 = sb("H_r0", [64, 256], F32)
            S.dma(out=r0[:], in_=bcast_rows(self.hg_lb[0:1, :], 64), sem="H_r0", writes=["H_r0"])
            S.dma(out=lb[:], in_=bcast_rows(self.hg_lb[1:2, :], 64), sem="H_lbl", writes=["H_lb"])
            S.op("dve", lambda e: e.tensor_tensor(out=lb[:], in0=lb[:], in1=r0[:], op=ALU.subtract),
                 reads=["H_lb", "H_r0"], writes=["H_lb"])
            S.op("act", lambda e: e.activation(out=lb[:], in_=lb[:], func=AF.Sigmoid),
                 reads=["H_lb"], writes=["H_lb"])
        S.op("dve", lambda e: e.tensor_scalar(out=oml[:], in0=lb[:], scalar1=-1.0, scalar2=1.0,
                                               op0=ALU.mult, op1=ALU.add),
             reads=["H_lb"], writes=["H_oml"])
        lbT = sb("H_lbT", [64, 4, 2], F32)
        for h in range(4):
            for which, src, ksrc in ((0, lb, "H_lb"), (1, oml, "H_oml")):
                ps = self.ps[(2 * h + which) % 4]
                kps = ("ps", (2 * h + which) % 4)
                S.op("pe", lambda e, ps=ps, src=src, h=h: e.transpose(
                    out=ps[:64, :64], in_=src[:, h * 64:(h + 1) * 64],
                    identity=self.ident_f[:64, :64]), reads=[ksrc, "ident_f"], writes=[kps])
                S.op("dve", lambda e, ps=ps, h=h, which=which: e.tensor_copy(
                    out=lbT[:, h, which:which + 1], in_=ps[:64, 0:1]), reads=[kps],
                    writes=["H_lbT"])
        ng = sb("H_ng", [64, 64], F32)
        S.dma(out=ng[:], in_=bcast_rows(self.hg_norm[l:l + 1, :], 64), sem="H_ng", writes=["H_ng"])

        qf = [sb("H_qf%d" % i, [64, 2, 4, 512], F32) for i in range(2)]
        sq = [sb("H_sq%d" % i, [64, 4, 512], F32) for i in range(2)]
        kc = [sb("H_kc%d" % i, [64, 4, 512], F32) for i in range(2)]
        tk = [sb("H_tk%d" % i, [64, 768], F32) for i in range(3)]
        St = [sb("H_S%d" % i, [64, 4, 64], F32) for i in range(2)]
        Sb = [sb("H_Sb%d" % i, [64, 4, 64], BF16) for i in range(2)]
        pf_q = self.pfeat[FM_ROW[2764]:FM_ROW[2764] + 256, :].rearrange("(h k) t -> k h t", k=64)
        pf_f = self.pfeat[FM_ROW[3020]:FM_ROW[3020] + 256, :].rearrange("(h k) t -> k h t", k=64)
        NB = 3

        def t3(name, shape, dtype):
            return [sb("%s%d" % (name, i), shape, dtype) for i in range(NB)]
        sig, fg, logf, kct, kd = (t3("H_sig", [64, 256], F32), t3("H_fg", [64, 256], F32),
                                  t3("H_logf", [64, 256], F32), t3("H_kct", [64, 256], F32),
                                  t3("H_kd", [64, 256], BF16))
        vb = t3("H_vb", [64, 256], BF16)
        gs = t3("H_gs", [64, 4, 64], F32)
        EB, EN = t3("H_EB", [64, 4, 128], F32), t3("H_EN", [64, 4, 64], F32)
        qd, qp, kp = (t3("H_qd", [64, 4, 64], BF16), t3("H_qp", [64, 4, 64], BF16),
                      t3("H_kp", [64, 4, 64], BF16))
        Am = t3("H_Am", [64, 4, 64], BF16)
        osb, osq = t3("H_osb", [64, 4, 64], F32), t3("H_osq", [64, 4, 64], F32)
        stt = t3("H_stt", [64, 8], F32)
        stmp = t3("H_stmp", [64, 4, 64], F32)
        def seq_thread(s):
            pb = lambda i: 4 * s + i % 4
            for blk8 in range(SEQ // 512):
                slot = (blk8 * NSEQ + s) % 2
                tb = s * SEQ + blk8 * 512
                kqf = ("H_qf", slot)
                S.dma(out=qf[slot][:, 0, :, :], in_=pf_q[:, :, tb:tb + 512], sem="H_qfq%d" % slot,
                      writes=[(kqf, 0)])
                S.dma(out=qf[slot][:, 1, :, :], in_=pf_f[:, :, tb:tb + 512], sem="H_qff%d" % slot,
                      writes=[(kqf, 1)])
                S.op("act", lambda e, slot=slot: e.activation(out=sq[slot][:], in_=qf[slot][:, 0],
                                                               func=AF.Silu),
                     reads=[(kqf, 0)], writes=[("H_sq", slot)])
                S.op("act", lambda e, slot=slot: e.activation(out=kc[slot][:], in_=qf[slot][:, 1],
                                                               func=AF.Sigmoid),
                     reads=[(kqf, 1)], writes=[("H_kc", slot)])
                for h in range(4):
                    S.op("dve", lambda e, slot=slot, h=h: e.tensor_scalar(
                        out=kc[slot][:, h, :], in0=kc[slot][:, h, :], scalar1=lbT[:, h, 1:2],
                        scalar2=-1.0, op0=ALU.mult, op1=ALU.mult),
                        reads=[("H_kc", slot), "H_lbT"], writes=[("H_kc", slot)])
                    S.op("dve", lambda e, slot=slot, h=h: e.tensor_scalar(
                        out=kc[slot][:, h, :], in0=kc[slot][:, h, :], scalar1=lbT[:, h, 1:2],
                        scalar2=None, op0=ALU.add),
                        reads=[("H_kc", slot), "H_lbT"], writes=[("H_kc", slot)])
                for cn in range(8):
                    b = s
                    c0 = cn * 64
                    tok = tb + c0
                    ktk = ("H_tk", b)
                    S.dma(out=tk[b][:], in_=self.ptok[tok:tok + 64, TM_CF:TM_CF + 768],
                          sem="H_tk%d" % b, writes=[ktk])
                    first = (blk8 == 0 and cn == 0)
                    S.op("act", lambda e, b=b: e.activation(out=sig[b][:], in_=tk[b][:, 0:256],
                                                             func=AF.Sigmoid),
                         reads=[ktk], writes=[("H_sig", b)])
                    S.op("dve", lambda e, b=b: e.tensor_tensor(out=fg[b][:], in0=sig[b][:],
                                                                in1=oml[:], op=ALU.mult),
                         reads=[("H_sig", b), "H_oml"], writes=[("H_fg", b)])
                    S.op("dve", lambda e, b=b: e.tensor_tensor(out=fg[b][:], in0=fg[b][:],
                                                                in1=lb[:], op=ALU.add),
                         reads=[("H_fg", b), "H_lb"], writes=[("H_fg", b)])
                    S.op("act", lambda e, b=b: e.activation(out=logf[b][:], in_=fg[b][:],
                                                             func=AF.Ln),
                         reads=[("H_fg", b)], writes=[("H_logf", b)])
                    S.op("pool", lambda e, b=b: e.tensor_scalar(out=kct[b][:], in0=fg[b][:],
                                                                 scalar1=-1.0, scalar2=1.0,
                                                                 op0=ALU.mult, op1=ALU.add),
                         reads=[("H_fg", b)], writes=[("H_kct", b)])
                    S.op("pool", lambda e, b=b: e.tensor_copy(out=vb[b][:], in_=tk[b][:, 256:512]),
                         reads=[ktk], writes=[("H_vb", b)])
                    S.op("act", lambda e, b=b: e.activation(
                        out=gs[b][:], in_=tk[b][:, 512:768].rearrange("p (h v) -> p h v", h=4),
                        func=AF.Silu), reads=[ktk], writes=[("H_gs", b)])
                    S.op("pool", lambda e, b=b: e.tensor_tensor(
                        out=gs[b][:], in0=gs[b][:],
                        in1=ng[:].unsqueeze(1).to_broadcast([64, 4, 64]), op=ALU.mult),
                        reads=[("H_gs", b), "H_ng"], writes=[("H_gs", b)])
                    p0, kp0 = self.ps8[pb(0)], ("ps", pb(0))

                    def mmb(e, b=b, p0=p0):
                        ins = None
                        for h in range(4):
                            ins = e.matmul(p0[:64, h * 128:(h + 1) * 128],
                                           lhsT=logf[b][:, h * 64:(h + 1) * 64], rhs=UU[:, :],
                                           start=True, stop=True)
                        return ins
                    S.op("pe", mmb, reads=[("H_logf", b), "H_UU"], writes=[kp0])
                    p0v = p0[:64, :].rearrange("p (h t) -> p h t", h=4)
                    S.op("act", lambda e, b=b, p0v=p0v: e.activation(out=EB[b][:], in_=p0v,
                                                                      func=AF.Exp),
                         reads=[kp0], writes=[("H_EB", b)])
                    S.op("act", lambda e, b=b, p0v=p0v: e.activation(out=EN[b][:],
                                                                      in_=p0v[:, :, 64:128],
                                                                      func=AF.Exp, scale=-1.0),
                         reads=[kp0], writes=[("H_EN", b)])
                    p1, kp1 = self.ps8[pb(1)], ("ps", pb(1))
                    S.op("pe", lambda e, b=b, p1=p1: e.matmul(p1[:64, :256], lhsT=UL[:, :],
                                                              rhs=logf[b][:, :], start=True,
                                                              stop=True),
                         reads=[("H_logf", b), "H_UL"], writes=[kp1])
                    S.op("act", lambda e, b=b, p1=p1: e.activation(out=sig[b][:], in_=p1[:64, :256],
                                                                    func=AF.Exp),
                         reads=[kp1], writes=[("H_sig", b)])
                    S.op("dve", lambda e, b=b: e.tensor_tensor(out=kd[b][:], in0=sig[b][:],
                                                                in1=kct[b][:], op=ALU.mult),
                         reads=[("H_sig", b), ("H_kct", b)], writes=[("H_kd", b)])
                    sqv = sq[slot][:, :, c0:c0 + 64]
                    kcv = kc[slot][:, :, c0:c0 + 64]
                    S.op("dve", lambda e, b=b, sqv=sqv: e.tensor_tensor(
                        out=qd[b][:], in0=sqv, in1=EB[b][:, :, 0:64], op=ALU.mult),
                        reads=[("H_sq", slot), ("H_EB", b)], writes=[("H_qd", b)])
                    S.op("pool", lambda e, b=b, sqv=sqv: e.tensor_tensor(
                        out=qp[b][:], in0=sqv, in1=EB[b][:, :, 64:128], op=ALU.mult),
                        reads=[("H_sq", slot), ("H_EB", b)], writes=[("H_qp", b)])
                    S.op("dve", lambda e, b=b, kcv=kcv: e.tensor_tensor(
                        out=kp[b][:], in0=kcv, in1=EN[b][:], op=ALU.mult),
                        reads=[("H_kc", slot), ("H_EN", b)], writes=[("H_kp", b)])
                    p2, kp2 = self.ps8[pb(2)], ("ps", pb(2))

                    def mma(e, b=b, p2=p2):
                        ins = None
                        for h in range(4):
                            ins = e.matmul(p2[:64, h * 64:(h + 1) * 64], lhsT=kp[b][:, h, :],
                                           rhs=qp[b][:, h, :], start=True, stop=True)
                        return ins
                    S.op("pe", mma, reads=[("H_kp", b), ("H_qp", b)], writes=[kp2])
                    S.op("dve", lambda e, b=b, p2=p2: e.tensor_tensor(
                        out=Am[b][:], in0=p2[:64, :256].rearrange("p (h c) -> p h c", h=4),
                        in1=UU[:, 0:64].unsqueeze(1).to_broadcast([64, 4, 64]), op=ALU.mult),
                        reads=[kp2, "H_UU"], writes=[("H_Am", b)])
                    if first:
                        S.op("pool", lambda e, s=s: e.memset(St[s][:], 0.0), writes=[("H_S", s)])
                        S.op("pool", lambda e, s=s: e.memset(Sb[s][:], 0.0), writes=[("H_Sb", s)])
                    p3, kp3 = self.ps8[pb(3)], ("ps", pb(3))

                    def mmo(e, b=b, p3=p3, s=s):
                        ins = None
                        for h in range(4):
                            e.matmul(p3[:64, h * 64:(h + 1) * 64], lhsT=qd[b][:, h, :],
                                     rhs=Sb[s][:, h, :], start=True, stop=False)
                            ins = e.matmul(p3[:64, h * 64:(h + 1) * 64], lhsT=Am[b][:, h, :],
                                           rhs=vb[b][:, h * 64:(h + 1) * 64], start=False, stop=True)
                        return ins
                    S.op("pe", mmo, reads=[("H_qd", b), ("H_Sb", s), ("H_Am", b), ("H_vb", b)],
                         writes=[kp3])
                    p4, kp4 = self.ps8[pb(4)], ("ps", pb(4))

                    def mms(e, b=b, p4=p4):
                        ins = None
                        for h in range(4):
                            ins = e.matmul(p4[:64, h * 64:(h + 1) * 64],
                                           lhsT=kd[b][:, h * 64:(h + 1) * 64],
                                           rhs=vb[b][:, h * 64:(h + 1) * 64], start=True, stop=True)
                        return ins
                    S.op("pe", mms, reads=[("H_kd", b), ("H_vb", b)], writes=[kp4])
                    S.op("dve", lambda e, b=b, s=s: e.tensor_tensor(
                        out=stmp[b][:], in0=St[s][:],
                        in1=EB[b][:, :, 63:64].to_broadcast([64, 4, 64]), op=ALU.mult),
                        reads=[("H_S", s), ("H_EB", b)], writes=[("H_stmp", b)])
                    S.op("dve", lambda e, b=b, s=s, p4=p4: e.tensor_tensor(
                        out=St[s][:], in0=stmp[b][:],
                        in1=p4[:64, :256].rearrange("p (h v) -> p h v", h=4), op=ALU.add),
                        reads=[("H_stmp", b), kp4], writes=[("H_S", s)])
                    S.op("act", lambda e, s=s: e.copy(out=Sb[s][:], in_=St[s][:]),
                         reads=[("H_S", s)], writes=[("H_Sb", s)])
                    S.op("act", lambda e, b=b, p3=p3: e.copy(
                        out=osb[b][:], in_=p3[:64, :256].rearrange("p (h v) -> p h v", h=4)),
                        reads=[kp3], writes=[("H_osb", b)])
                    S.op("pool", lambda e, b=b: e.tensor_tensor(out=osq[b][:], in0=osb[b][:],
                                                                 in1=osb[b][:], op=ALU.mult),
                         reads=[("H_osb", b)], writes=[("H_osq", b)])
                    S.op("dve", lambda e, b=b: e.tensor_reduce(out=stt[b][:, 0:4], in_=osq[b][:],
                                                                axis=AX.X, op=ALU.add),
                         reads=[("H_osq", b)], writes=[("H_stt", b)])
                    self.rstd_from_ss(stt[b][:, 0:4], ("H_stt", b), stt[b][:, 4:8], ("H_rs", b), 64)
                    S.op("dve", lambda e, b=b: e.tensor_tensor(
                        out=osb[b][:], in0=osb[b][:],
                        in1=stt[b][:, 4:8].unsqueeze(2).to_broadcast([64, 4, 64]), op=ALU.mult),
                        reads=[("H_osb", b), ("H_rs", b)], writes=[("H_osb", b)])
                    S.op("pool", lambda e, b=b: e.tensor_tensor(out=osq[b][:], in0=osb[b][:],
                                                                 in1=gs[b][:], op=ALU.mult),
                         reads=[("H_osb", b), ("H_gs", b)], writes=[("H_osq", b)])
                    S.dma(out=self.mixin[tok:tok + 64, 768:1024],
                          in_=osq[b][:].rearrange("p h v -> p (h v)"), sem="H_out%d" % b,
                          reads=[("H_osq", b)], q="pool")
        S.interleave([lambda s=s: seq_thread(s) for s in range(NSEQ)])
        S.barrier()

    def phase_S(self, l):
        S = self.S
        self.sbuf_reset()
        sb = self.sb
        BIG = -1.0e30
        KIT = 32
        kiT = sb("S_kiT", [64, SEQ], F32)
        kT = sb("S_kT", [64, SEQ], BF16)
        kst = sb("S_kst", [64, 1024], F32)
        vst = sb("S_vst", [128, 8, 64], F32)
        v1 = sb("S_v1", [128, 32, 65], BF16)
        junk = sb("S_junk", [128, SEQ], BF16)
        ckn = sb("S_ckn", [128, KIT + 1], F32)
        for k in range(KIT + 1):
            S.op("pool", lambda e, k=k: e.memset(ckn[:, k:k + 1], -2.1 / 2.0 ** (k + 1)),
                 writes=[("S_ckn", k)])
        kck = [("S_ckn", k) for k in range(KIT + 1)]

        NT = 2

        def t2(name, shape, dtype):
            return [sb("%s%d" % (name, i), shape, dtype) for i in range(NT)]
        qq = t2("S_qq", [64, 2, 4, 128], F32)
        qqb = t2("S_qqb", [64, 4, 128], BF16)
        acc = t2("S_acc", [128, SEQ], F32)
        wkA = t2("S_wkA", [128, SEQ], F32)
        gtt = t2("S_gt", [128, SEQ], BF16)
        eqt = t2("S_eq", [128, SEQ], BF16)
        selb = t2("S_selb", [128, SEQ], BF16)
        rr = [sb("S_rr%d" % i, [128, 512], F32) for i in range(2 * NT)]
        pT = [sb("S_pT%d" % i, [128, 512], BF16) for i in range(2 * NT)]
        wi = t2("S_wi", [128, 12], F32)
        sc = t2("S_sc", [128, 16], F32)
        nst = t2("S_nst", [128, KIT + 1], F32)
        oT = t2("S_oT", [65, 512], F32)
        osb = t2("S_osb", [128, 4, 64], F32)
        rc = t2("S_rc", [128, 4, 1], F32)
        pf_q = self.pfeat[FM_ROW[2056]:FM_ROW[2056] + 256, :].rearrange("(h k) t -> k h t", k=64)
        pf_qi = self.pfeat[FM_ROW[2440]:FM_ROW[2440] + 256, :].rearrange("(h k) t -> k h t", k=64)
        pf_k = self.pfeat[FM_ROW[2312]:FM_ROW[2312] + 64, :]
        pf_ki = self.pfeat[FM_ROW[2696]:FM_ROW[2696] + 64, :]

        def tile_thread(s, a2):
            t0 = s * SEQ
            nr = 0
            for j in range(a2, SEQ // 128, NT):
                tq = t0 + j * 128
                NK = (j + 1) * 128
                kqq = ("S_qq", a2)
                S.dma(out=qq[a2][:, 0], in_=pf_q[:, :, tq:tq + 128], sem="S_qq%da" % a2,
                      writes=[(kqq, 0)])
                S.dma(out=qq[a2][:, 1], in_=pf_qi[:, :, tq:tq + 128], sem="S_qq%db" % a2,
                      writes=[(kqq, 1)])
                S.op("pool", lambda e: e.tensor_copy(out=qqb[a2][:], in_=qq[a2][:, 0]),
                     reads=[(kqq, 0)], writes=[("S_qqb", a2)])
                kwi = ("S_wi", a2)
                S.dma(out=wi[a2][:, 0:4], in_=self.ptok[tq:tq + 128, TM_WI:TM_WI + 4],
                      sem="S_wi%d" % a2, writes=[(kwi, 0)])
                S.op("act", lambda e: e.activation(out=wi[a2][:, 4:8], in_=wi[a2][:, 0:4],
                                                    func=AF.Abs),
                     reads=[(kwi, 0)], writes=[(kwi, 1)])
                S.op("act", lambda e: e.activation(out=wi[a2][:, 8:12], in_=wi[a2][:, 0:4],
                                                    func=AF.Sign),
                     reads=[(kwi, 0)], writes=[(kwi, 2)])
                kacc = ("S_acc", a2)
                nkb = (NK + 511) // 512
                for kb in range(nkb):
                    w = min(512, NK - kb * 512)
                    for h in range(4):
                        pi = 2 * a2 + h % 2
                        ps, kps = self.ps8[pi], ("ps", pi)
                        S.op("pe", lambda e, ps=ps, h=h, kb=kb, w=w: e.matmul(
                            ps[:, :w], lhsT=qq[a2][:, 1, h, :],
                            rhs=kiT[:, kb * 512: kb * 512 + w], start=True, stop=True),
                            reads=[(kqq, 1), "S_kiT"], writes=[kps])
                        ri = 2 * a2 + nr % 2
                        r, kr = rr[ri], ("S_rr", ri)
                        nr += 1
                        S.op("act", lambda e, ps=ps, r=r, w=w, h=h: e.activation(
                            out=r[:, :w], in_=ps[:, :w], func=AF.Relu, scale=wi[a2][:, 4 + h:5 + h]),
                            reads=[kps, (kwi, 1)], writes=[kr])
                        av = acc[a2][:, kb * 512: kb * 512 + w]
                        if h == 0:
                            S.op("dve", lambda e, av=av, r=r, w=w, h=h: e.tensor_scalar(
                                out=av, in0=r[:, :w], scalar1=wi[a2][:, 8 + h:9 + h], scalar2=None,
                                op0=ALU.mult), reads=[kr, (kwi, 2)], writes=[(kacc, kb), kacc])
                        else:
                            S.op("dve", lambda e, av=av, r=r, w=w, h=h: e.scalar_tensor_tensor(
                                out=av, in0=r[:, :w], scalar=wi[a2][:, 8 + h:9 + h], in1=av,
                                op0=ALU.mult, op1=ALU.add),
                                reads=[kr, (kwi, 2), (kacc, kb)], writes=[(kacc, kb)])
                kall = [(kacc, kb) for kb in range(nkb)]
                X = sc[a2]
                ksc = lambda nm: ("S_sc", a2, nm)
                accv = acc[a2][:, :NK]
                if j >= 2:
                    S.op("dve", lambda e: e.tensor_reduce(out=X[:, 0:1], in_=accv, axis=AX.X,
                                                           op=ALU.max, apply_absolute_value=True),
                         reads=kall, writes=[ksc("rm")])
                    S.op("dve", lambda e: e.tensor_scalar(out=X[:, 0:1], in0=X[:, 0:1],
                                                           scalar1=1.0e-20, scalar2=None,
                                                           op0=ALU.max),
                         reads=[ksc("rm")], writes=[ksc("rm")])
                    S.op("dve", lambda e: e.tensor_scalar(out=nst[a2][:], in0=ckn[:],
                                                           scalar1=X[:, 0:1], scalar2=None,
                                                           op0=ALU.mult),
                         reads=[ksc("rm")] + kck, writes=[("S_nst", a2)])
                    S.op("dve", lambda e: e.tensor_scalar(out=X[:, 1:2], in0=X[:, 0:1],
                                                           scalar1=-0.02, scalar2=None,
                                                           op0=ALU.mult),
                         reads=[ksc("rm")], writes=[ksc("nc")])
                    S.op("pool", lambda e: e.memset(X[:, 4:5], float(NK) - 511.5),
                         writes=[ksc("cb")])
                S.op("pool", lambda e: e.memset(acc[a2][0:64, NK - 64:NK], BIG),
                     reads=kall + [ksc("rm")], writes=[kacc])
                if j >= 2:
                    for k in range(KIT):
                        S.op("act", lambda e: e.activation(out=junk[:, :NK], in_=accv, func=AF.Sign,
                                                            bias=X[:, 1:2], scale=1.0,
                                                            accum_out=X[:, 2:3]),
                             reads=[kacc, ksc("nc")], writes=[ksc("sg")])
                        S.op("act", lambda e: e.activation(out=X[:, 3:4], in_=X[:, 2:3],
                                                            func=AF.Sign, bias=X[:, 4:5], scale=1.0),
                             reads=[ksc("sg"), ksc("cb")], writes=[ksc("dd")])
                        S.op("act", lambda e, k=k: e.activation(out=X[:, 1:2], in_=X[:, 3:4],
                                                                 func=AF.Identity,
                                                                 scale=nst[a2][:, k + 1:k + 2],
                                                                 bias=X[:, 1:2]),
                             reads=[ksc("dd"), ("S_nst", a2), ksc("nc")], writes=[ksc("nc")])
                    S.op("dve", lambda e: e.scalar_tensor_tensor(
                        out=X[:, 5:6], in0=X[:, 1:2], scalar=-1.0, in1=nst[a2][:, KIT:KIT + 1],
                        op0=ALU.mult, op1=ALU.add), reads=[ksc("nc"), ("S_nst", a2)],
                        writes=[ksc("thr")])
                else:
                    S.op("pool", lambda e: e.memset(X[:, 5:6], BIG / 2), writes=[ksc("thr")])
                kwk = ("S_wkA", a2)
                wv = wkA[a2][:, :NK]
                S.op("dve", lambda e: e.tensor_scalar(out=wv, in0=accv, scalar1=X[:, 5:6],
                                                       scalar2=3.0e30, op0=ALU.is_lt, op1=ALU.mult),
                     reads=[kacc, ksc("thr")], writes=[kwk])
                S.op("dve", lambda e: e.tensor_tensor(out=wv, in0=wv, in1=accv, op=ALU.add),
                     reads=[kacc, kwk], writes=[kwk])
                S.op("dve", lambda e: e.tensor_reduce(out=X[:, 6:7], in_=wv, axis=AX.X, op=ALU.min),
                     reads=[kwk], writes=[ksc("v")])
                gv, ev = gtt[a2][:, :NK], eqt[a2][:, :NK]
                S.op("dve", lambda e: e.tensor_scalar(out=gv, in0=accv, scalar1=X[:, 6:7],
                                                       scalar2=None, op0=ALU.is_gt, op1=ALU.add,
                                                       accum_out=X[:, 7:8]),
                     reads=[kacc, ksc("v")], writes=[("S_gt", a2), ksc("cg")])
                S.op("dve", lambda e: e.tensor_scalar(out=ev, in0=accv, scalar1=X[:, 6:7],
                                                       scalar2=None, op0=ALU.is_equal),
                     reads=[kacc, ksc("v")], writes=[("S_eq", a2)])
                S.op("dve", lambda e: e.tensor_tensor_scan(out=wv, data0=ev, data1=ev, initial=0.0,
                                                            op0=ALU.add, op1=ALU.max),
                     reads=[("S_eq", a2), kwk], writes=[kwk])
                S.op("dve", lambda e: e.tensor_scalar(out=X[:, 8:9], in0=X[:, 7:8], scalar1=-1.0,
                                                       scalar2=256.0, op0=ALU.mult, op1=ALU.add),
                     reads=[ksc("cg")], writes=[ksc("need")])
                S.op("dve", lambda e: e.scalar_tensor_tensor(out=ev, in0=wv, scalar=X[:, 8:9],
                                                              in1=ev, op0=ALU.is_le, op1=ALU.mult),
                     reads=[kwk, ksc("need"), ("S_eq", a2)], writes=[("S_eq", a2)])
                S.op("pool", lambda e: e.tensor_tensor(out=gv, in0=gv, in1=ev, op=ALU.add),
                     reads=[("S_gt", a2), ("S_eq", a2)], writes=[("S_gt", a2)])
                ksel = ("S_selb", a2)
                S.op("pool", lambda e: e.tensor_scalar(out=selb[a2][:, :NK], in0=gv, scalar1=-1.0,
                                                        scalar2=-NEG, op0=ALU.add, op1=ALU.mult),
                     reads=[("S_gt", a2)], writes=[ksel])
                po, kpo = self.ps8[4 + a2], ("ps", 4 + a2)
                for kt in range(j + 1):
                    li = (0, 1, 6, 7)[2 * a2 + kt % 2]
                    pl, kpl = self.ps8[li], ("ps", li)

                    def mml(e, pl=pl, kt=kt):
                        e.matmul(pl[:, :].rearrange("p (h t) -> p h t", h=4),
                                 lhsT=kT[:, kt * 128:(kt + 1) * 128], rhs=qqb[a2][:, :, :],
                                 start=True, stop=False)
                        ins = None
                        for h in range(4):
                            ins = e.matmul(pl[:, h * 128:(h + 1) * 128],
                                           lhsT=selb[a2][:, kt * 128:(kt + 1) * 128],
                                           rhs=self.ident_b[:, :], start=False, stop=(h == 3))
                        return ins
                    S.op("pe", mml, reads=["S_kT", ("S_qqb", a2), ksel, "ident_b"], writes=[kpl])
                    pi_ = 2 * a2 + kt % 2
                    p, kp = pT[pi_], ("S_pT", pi_)
                    S.op("act", lambda e, p=p, pl=pl: e.activation(out=p[:], in_=pl[:, :],
                                                                    func=AF.Exp, scale=0.125),
                         reads=[kpl], writes=[kp])
                    S.op("pe", lambda e, p=p, kt=kt: e.matmul(
                        po[:65, :], lhsT=v1[:, kt, :], rhs=p[:], start=(kt == 0), stop=(kt == j)),
                        reads=["S_v1", kp], writes=[kpo])
                koT = ("S_oT", a2)
                S.op("act", lambda e: e.copy(out=oT[a2][:], in_=po[:65, :]),
                     reads=[kpo], writes=[koT])
                pt, kpt = self.ps8[2 * a2], ("ps", 2 * a2)

                def trs(e):
                    ins = None
                    for h in range(4):
                        ins = e.transpose(out=pt[:, h * 65:(h + 1) * 65],
                                          in_=oT[a2][:, h * 128:(h + 1) * 128],
                                          identity=self.ident_f[:65, :65])
                    return ins
                S.op("pe", trs, reads=[koT, "ident_f"], writes=[kpt])
                ptv = pt[:, :260].rearrange("p (h d) -> p h d", h=4)
                S.op("dve", lambda e: e.reciprocal(out=rc[a2][:], in_=ptv[:, :, 64:65]),
                     reads=[kpt], writes=[("S_rc", a2)])
                S.op("dve", lambda e: e.tensor_tensor(
                    out=osb[a2][:], in0=ptv[:, :, 0:64], in1=rc[a2][:].to_broadcast([128, 4, 64]),
                    op=ALU.mult), reads=[kpt, ("S_rc", a2)], writes=[("S_osb", a2)])
                S.dma(out=self.mixin[tq:tq + 128, 512:768],
                      in_=osb[a2][:].rearrange("p h d -> p (h d)"), sem="S_out%d" % a2,
                      reads=[("S_osb", a2)])

        for s in range(NSEQ):
            t0 = s * SEQ
            S.dma(out=kiT[:], in_=pf_ki[:, t0:t0 + SEQ], sem="S_kiT", writes=["S_kiT"])
            for q4 in range(4):
                S.dma(out=kst[:], in_=pf_k[:, t0 + q4 * 1024:t0 + (q4 + 1) * 1024], sem="S_kst",
                      writes=["S_kst"])
                S.op("pool", lambda e, q4=q4: e.tensor_copy(out=kT[:, q4 * 1024:(q4 + 1) * 1024],
                                                            in_=kst[:]),
                     reads=["S_kst"], writes=["S_kT"])
            S.op("pool", lambda e: e.memset(v1[:], 1.0), writes=["S_v1"])
            for q4 in range(4):
                S.dma(out=vst[:],
                      in_=self.ptok[t0 + q4 * 1024: t0 + (q4 + 1) * 1024,
                                    TM_BV:TM_BV + 64].rearrange("(kt p) d -> p kt d", p=128),
                      sem="S_vst", writes=["S_vst"])
                S.op("pool", lambda e, q4=q4: e.tensor_copy(out=v1[:, q4 * 8:(q4 + 1) * 8, 0:64],
                                                            in_=vst[:]),
                     reads=["S_vst"], writes=["S_v1"])
            S.interleave([lambda s=s, a=a: tile_thread(s, a) for a in range(NT)])
        S.barrier()

    def phase_D(self, l):
        S = self.S
        self.sbuf_reset()
        sb = self.sb
        NB = 2
        U64 = sb("D_U", [64, 64], F32)
        UL = sb("D_UL", [64, 64], F32)
        Lst = sb("D_Lst", [64, 64], F32)
        mb = sb("D_mb", [64, 64], F32)
        S.op("pool", lambda e: e.memset(U64[:], 1.0), writes=["D_U"])
        S.op("pool", lambda e: e.affine_select(out=U64[:], in_=U64[:], pattern=[[1, 64]],
                                                compare_op=ALU.is_ge, fill=0.0, base=0,
                                                channel_multiplier=-1), reads=["D_U"],
             writes=["D_U"])
        for t_, kk_ in ((UL, "D_UL"), (Lst, "D_Lst")):
            S.op("pool", lambda e, t_=t_: e.memset(t_[:], 1.0), writes=[kk_])
            S.op("pool", lambda e, t_=t_: e.affine_select(out=t_[:], in_=t_[:], pattern=[[-1, 64]],
                                                          compare_op=ALU.is_gt, fill=0.0, base=0,
                                                          channel_multiplier=1), reads=[kk_],
                 writes=[kk_])
        S.op("pool", lambda e: e.memset(mb[:], 0.0), writes=["D_mb"])
        S.op("pool", lambda e: e.affine_select(out=mb[:], in_=mb[:], pattern=[[-1, 64]],
                                                compare_op=ALU.is_ge, fill=-1.0e4, base=0,
                                                channel_multiplier=1), reads=["D_mb"],
             writes=["D_mb"])
        cw = self.load_convw("D_cw", self.dn_conv[l], 4, 12)
        alog = sb("D_alog", [64, 4], F32)
        dtb = sb("D_dtb", [64, 4], F32)
        S.dma(out=alog[:], in_=bcast_rows(self.dn_a_log[l:l + 1, :], 64), sem="D_alog",
              writes=["D_alog"])
        S.dma(out=dtb[:], in_=bcast_rows(self.dn_dt_bias[l:l + 1, :], 64), sem="D_dtb",
              writes=["D_dtb"])
        S.op("act", lambda e: e.activation(out=alog[:], in_=alog[:], func=AF.Exp), reads=["D_alog"],
             writes=["D_alog"])
        S.op("dve", lambda e: e.tensor_scalar(out=alog[:], in0=alog[:], scalar1=-1.0, scalar2=None,
                                               op0=ALU.mult), reads=["D_alog"], writes=["D_alog"])
        ng = sb("D_ng", [64, 128], F32)
        S.dma(out=ng[:], in_=bcast_rows(self.dn_norm[l:l + 1, :], 64), sem="D_ng", writes=["D_ng"])

        X = [sb("D_X%d" % i, [128, 515], F32) for i in range(4)]
        Cc = [sb("D_C%d" % i, [128, 512], F32) for i in range(4)]
        Y = [sb("D_Y%d" % i, [128, 12, 512], F32) for i in range(2)]
        St = [sb("D_S%d" % i, [128, 4, 128], F32) for i in range(NSEQ)]
        Sb = [sb("D_Sb%d" % i, [128, 4, 128], BF16) for i in range(NSEQ)]

        def t2(name, shape, dtype):
            return [sb("%s%d" % (name, i), shape, dtype) for i in range(NB)]
        tg = t2("D_tg", [64, 8], F32)
        gt = t2("D_gt", [64, 24], F32)
        zt = t2("D_zt", [64, 4, 128], F32)
        gs = t2("D_gs", [64, 4, 128], F32)
        QK = t2("D_QK", [64, 8, 128], F32)
        Vt = t2("D_Vt", [64, 4, 128], F32)
        sqq = t2("D_sqq", [64, 8, 128], F32)
        nrm = t2("D_nrm", [64, 16], F32)
        qdk = t2("D_qdk", [64, 4, 128], F32)
        TT = t2("D_TT", [128, 12, 64], BF16)
        kd = t2("D_kd", [64, 4, 128], BF16)
        bk = t2("D_bk", [64, 4, 128], BF16)
        bv = t2("D_bv", [64, 4, 128], BF16)
        gU = t2("D_gU", [64, 8, 64], F32)
        dec = t2("D_dec", [64, 4, 64], F32)
        Mm = t2("D_M", [64, 4, 64], F32)
        QKm = t2("D_QKm", [64, 4, 64], F32)
        QKT = t2("D_QKT", [64, 4, 64], BF16)
        Qa = t2("D_Qa", [64, 4, 64], F32)
        Qta = t2("D_Qta", [64, 4, 64], F32)
        Qb = t2("D_Qb", [64, 4, 64], F32)
        Qtb = t2("D_Qtb", [64, 4, 64], F32)
        Bt = t2("D_Bt", [64, 4, 64], F32)
        Tb = t2("D_Tb", [64, 4, 64], BF16)
        u0 = t2("D_u0", [64, 4, 128], F32)
        ub = t2("D_ub", [64, 4, 128], BF16)
        wkT = t2("D_wkT", [128, 4, 64], BF16)
        glb = t2("D_glb", [128, 4], F32)
        osb = t2("D_osb", [64, 4, 128], F32)
        osq = t2("D_osq", [64, 4, 128], F32)
        identI = sb("D_I", [64, 4, 64], F32)
        for h in range(4):
            S.op("pool", lambda e, h=h: e.tensor_copy(out=identI[:, h, :], in_=self.ident_f[:64, :64]),
                 reads=["ident_f"], writes=["D_I"])

        def v4(ps, w):
            return ps[:64, :4 * w].rearrange("p (h x) -> p h x", h=4)

        def bc(ap_h1, w):
            a = ap_h1 if len(ap_h1.shape) == 3 else ap_h1.unsqueeze(2)
            return a.to_broadcast([64, 4, w])

        def seq_thread(s):
            pb = lambda i: 4 * s + i % 4
            for blk8 in range(SEQ // 512):
                ys = (blk8 * NSEQ + s) % 2
                tb = s * SEQ + blk8 * 512
                for ct in range(12):
                    xi = 2 * s + ct % 2
                    x = X[xi]
                    kx = ("D_X", xi)
                    if blk8 == 0:
                        S.op("pool", lambda e, x=x: e.memset(x[:, 0:3], 0.0), writes=[(kx, "h")])
                        S.dma(out=x[:, 3:515], in_=self.pfeat[ct * 128:(ct + 1) * 128, tb:tb + 512],
                              sem="D_X%d" % xi, writes=[(kx, "b")])
                    else:
                        S.dma(out=x[:, :], in_=self.pfeat[ct * 128:(ct + 1) * 128, tb - 3:tb + 512],
                              sem="D_X%d" % xi, writes=[(kx, "h"), (kx, "b")])
                    c, kc = Cc[xi], ("D_C", xi)
                    S.op("act", lambda e, x=x, c=c, ct=ct: e.activation(
                        out=c[:], in_=x[:, 0:512], func=AF.Copy, scale=cw[:, 0, ct:ct + 1]),
                        reads=[(kx, "h"), (kx, "b"), "D_cw"], writes=[kc])
                    for tap in (1, 2, 3):
                        S.op("dve", lambda e, x=x, c=c, ct=ct, tap=tap: e.scalar_tensor_tensor(
                            out=c[:], in0=x[:, tap:tap + 512], scalar=cw[:, tap, ct:ct + 1],
                            in1=c[:], op0=ALU.mult, op1=ALU.add),
                            reads=[(kx, "h"), (kx, "b"), "D_cw", kc], writes=[kc])
                    S.op("act", lambda e, c=c, ct=ct, ys=ys: e.activation(out=Y[ys][:, ct, :],
                                                                           in_=c[:], func=AF.Silu),
                         reads=[kc], writes=[("D_Y", ys, ct)])
                ykeys = [("D_Y", ys, ct) for ct in range(12)]
                for cn in range(8):
                    b = s
                    c0 = cn * 64
                    tok = tb + c0
                    K = lambda nm: (nm, b)
                    first = (blk8 == 0 and cn == 0)
                    if first:
                        S.op("pool", lambda e, s=s: e.memset(St[s][:], 0.0), writes=[("D_S", s)])
                        S.op("pool", lambda e, s=s: e.memset(Sb[s][:], 0.0), writes=[("D_Sb", s)])
                    S.dma(out=tg[b][:], in_=self.ptok[tok:tok + 64, TM_AB:TM_AB + 8],
                          sem="D_tg%d" % b, writes=[K("tg")])
                    S.dma(out=zt[b][:].rearrange("p h v -> p (h v)"),
                          in_=self.ptok[tok:tok + 64, TM_AZ:TM_AZ + 512], sem="D_zt%d" % b,
                          writes=[K("zt")])
                    G = gt[b]
                    S.op("act", lambda e, b=b, G=G: e.activation(out=G[:, 0:4], in_=tg[b][:, 0:4],
                                                                  func=AF.Sigmoid),
                         reads=[K("tg")], writes=[K("beta")])
                    S.op("dve", lambda e, b=b, G=G: e.tensor_tensor(out=G[:, 20:24], in0=tg[b][:, 4:8],
                                                                     in1=dtb[:], op=ALU.add),
                         reads=[K("tg"), "D_dtb"], writes=[K("gtmp")])
                    S.op("act", lambda e, G=G: e.activation(out=G[:, 20:24], in_=G[:, 20:24],
                                                             func=AF.Exp),
                         reads=[K("gtmp")], writes=[K("gtmp")])
                    S.op("act", lambda e, G=G: e.activation(out=G[:, 20:24], in_=G[:, 20:24],
                                                             func=AF.Ln, bias=1.0),
                         reads=[K("gtmp")], writes=[K("gtmp")])
                    S.op("dve", lambda e, G=G: e.tensor_tensor(out=G[:, 4:8], in0=G[:, 20:24],
                                                                in1=alog[:], op=ALU.mult),
                         reads=[K("gtmp"), "D_alog"], writes=[K("g")])
                    pg, kpg = self.ps8[pb(5)], ("ps", pb(5))

                    def mmg(e, G=G, pg=pg):
                        e.matmul(pg[:64, 0:4], lhsT=U64[:, :], rhs=G[:, 4:8], start=True, stop=True)
                        e.matmul(pg[:64, 4:8], lhsT=UL[:, :], rhs=G[:, 4:8], start=True, stop=True)
                        return e.matmul(pg[:, 8:12], lhsT=self.ones_f[:64, :], rhs=G[:, 4:8],
                                        start=True, stop=True)
                    S.op("pe", mmg, reads=[K("g"), "D_U", "D_UL", "ones_f"], writes=[kpg])
                    S.op("act", lambda e, G=G, pg=pg: e.activation(out=G[:, 8:16], in_=pg[:64, 0:8],
                                                                    func=AF.Exp),
                         reads=[kpg], writes=[K("eG")])
                    S.op("act", lambda e, b=b, pg=pg: e.activation(out=glb[b][:], in_=pg[:, 8:12],
                                                                    func=AF.Exp),
                         reads=[kpg], writes=[K("glb")])
                    S.op("dve", lambda e, G=G: e.tensor_tensor(out=G[:, 16:20], in0=G[:, 0:4],
                                                                in1=G[:, 8:12], op=ALU.mult),
                         reads=[K("beta"), K("eG")], writes=[K("beG")])
                    S.op("dve", lambda e, b=b, G=G: e.tensor_tensor(
                        out=gU[b][:, 0:4, :], in0=U64[:].unsqueeze(1).to_broadcast([64, 4, 64]),
                        in1=bc(G[:, 4:8], 64), op=ALU.mult), reads=["D_U", K("g")],
                        writes=[K("gU")])
                    S.op("pool", lambda e, b=b: e.tensor_scalar(out=gU[b][:, 4:8, :],
                                                                 in0=gU[b][:, 0:4, :], scalar1=-1.0,
                                                                 scalar2=None, op0=ALU.mult),
                         reads=[K("gU")], writes=[K("ngU")])
                    pd, kpd = self.ps8[pb(4)], ("ps", pb(4))

                    def mmd(e, b=b, pd=pd):
                        ins = None
                        for h in range(4):
                            e.matmul(pd[:64, h * 64:(h + 1) * 64], lhsT=gU[b][:, h, :],
                                     rhs=self.ones_f[:64, :64], start=True, stop=False)
                            ins = e.matmul(pd[:64, h * 64:(h + 1) * 64], lhsT=self.ones_f[:64, :64],
                                           rhs=gU[b][:, 4 + h, :], start=False, stop=True)
                        return ins
                    S.op("pe", mmd, reads=[K("gU"), K("ngU"), "ones_f"], writes=[kpd])
                    S.op("dve", lambda e, b=b, pd=pd: e.tensor_tensor(
                        out=dec[b][:], in0=v4(pd, 64),
                        in1=mb[:].unsqueeze(1).to_broadcast([64, 4, 64]), op=ALU.add),
                        reads=[kpd, "D_mb"], writes=[K("dec")])
                    S.op("act", lambda e, b=b: e.activation(out=dec[b][:], in_=dec[b][:],
                                                             func=AF.Exp),
                         reads=[K("dec")], writes=[K("dec")])
                    for grp in range(3):
                        pt, kpt = self.ps8[pb(grp)], ("ps", pb(grp))

                        def trq(e, grp=grp, pt=pt):
                            ins = None
                            for h in range(4):
                                ins = e.transpose(out=pt[:64, h * 128:(h + 1) * 128],
                                                  in_=Y[ys][:, grp * 4 + h, c0:c0 + 64],
                                                  identity=self.ident_f[:, :])
                            return ins
                        S.op("pe", trq, reads=ykeys + ["ident_f"], writes=[kpt])
                        dstv = Vt[b][:] if grp == 2 else QK[b][:, grp * 4:(grp + 1) * 4, :]
                        S.op("act", lambda e, dstv=dstv, pt=pt: e.copy(out=dstv, in_=v4(pt, 128)),
                             reads=[kpt], writes=[K("QK%d" % grp)])
                    S.op("pool", lambda e, b=b: e.tensor_tensor(out=sqq[b][:], in0=QK[b][:],
                                                                 in1=QK[b][:], op=ALU.mult),
                         reads=[K("QK0"), K("QK1")], writes=[K("sqq")])
                    S.op("dve", lambda e, b=b: e.tensor_reduce(out=nrm[b][:, 0:8], in_=sqq[b][:],
                                                                axis=AX.X, op=ALU.add),
                         reads=[K("sqq")], writes=[K("nrm")])
                    S.op("dve", lambda e, b=b: e.tensor_scalar(out=nrm[b][:, 0:8], in0=nrm[b][:, 0:8],
                                                                scalar1=1.0e-6, scalar2=None,
                                                                op0=ALU.add),
                         reads=[K("nrm")], writes=[K("nrm")])
                    S.op("act", lambda e, b=b: e.sqrt(out=nrm[b][:, 0:8], in_=nrm[b][:, 0:8]),
                         reads=[K("nrm")], writes=[K("nrm")])
                    S.op("dve", lambda e, b=b: e.reciprocal(out=nrm[b][:, 8:16], in_=nrm[b][:, 0:8]),
                         reads=[K("nrm")], writes=[K("rn")])
                    S.op("dve", lambda e, b=b: e.tensor_scalar(out=nrm[b][:, 8:12],
                                                                in0=nrm[b][:, 8:12],
                                                                scalar1=128.0 ** -0.5, scalar2=None,
                                                                op0=ALU.mult),
                         reads=[K("rn")], writes=[K("rn")])
                    S.op("dve", lambda e, b=b: e.tensor_tensor(
                        out=QK[b][:], in0=QK[b][:],
                        in1=nrm[b][:, 8:16].unsqueeze(2).to_broadcast([64, 8, 128]), op=ALU.mult),
                        reads=[K("QK0"), K("QK1"), K("rn")], writes=[K("QKn")])
                    for h in range(4):
                        for dst_, src_, col, kd_, kr_ in ((qdk[b], QK[b][:, h, :], 8, "qdk", "eG"),
                                                       (kd[b], QK[b][:, 4 + h, :], 12, "kd", "eG"),
                                                       (bk[b], QK[b][:, 4 + h, :], 16, "bk", "beG"),
                                                       (bv[b], Vt[b][:, h, :], 0, "bv", "beta")):
                            S.op("dve", lambda e, dst_=dst_, src_=src_, col=col, h=h, G=G:
                                 e.tensor_scalar(out=dst_[:, h, :], in0=src_,
                                                 scalar1=G[:, col + h:col + h + 1], scalar2=None,
                                                 op0=ALU.mult),
                                 reads=[K("QKn"), K("QK2"), K(kr_)], writes=[(K(kd_), h)])
                    for grp, (src, ksrc) in enumerate(((qdk[b][:], [*[(K("qdk"), h_) for h_ in range(4)]]),
                                                       (QK[b][:, 4:8, :], [K("QKn")]),
                                                       (QK[b][:, 0:4, :], [K("QKn")]))):
                        pt, kpt = self.ps8[pb(grp)], ("ps", pb(grp))

                        def trt(e, src=src, pt=pt):
                            ins = None
                            for h in range(4):
                                ins = e.transpose(out=pt[:, h * 64:(h + 1) * 64], in_=src[:, h, :],
                                                  identity=self.ident_f[:64, :64])
                            return ins
                        S.op("pe", trt, reads=ksrc + ["ident_f"], writes=[kpt])
                        self.evac(grp, TT[b][:, grp * 4:(grp + 1) * 4, :],
                                  pt[:, :256].rearrange("p (h t) -> p h t", h=4), [kpt],
                                  [K("TT%d" % grp)])
                    pk, kpk = self.ps8[pb(3)], ("ps", pb(3))

                    def mmk(e, b=b, pk=pk):
                        ins = None
                        for h in range(4):
                            e.matmul(pk[:64, h * 64:(h + 1) * 64], lhsT=TT[b][:, 4 + h, :],
                                     rhs=TT[b][:, 4 + h, :], start=True, stop=True)
                            ins = e.matmul(pk[:64, 256 + h * 64:256 + (h + 1) * 64],
                                           lhsT=TT[b][:, 8 + h, :], rhs=TT[b][:, 4 + h, :],
                                           start=True, stop=True)
                        return ins
                    S.op("pe", mmk, reads=[K("TT1"), K("TT2")], writes=[kpk])
                    S.op("dve", lambda e, b=b, pk=pk: e.tensor_tensor(
                        out=Mm[b][:], in0=v4(pk, 64), in1=dec[b][:], op=ALU.mult),
                        reads=[kpk, K("dec")], writes=[K("M")])
                    S.op("dve", lambda e, b=b, pk=pk: e.tensor_tensor(
                        out=QKm[b][:], in0=pk[:64, 256:512].rearrange("p (h x) -> p h x", h=4),
                        in1=dec[b][:], op=ALU.mult), reads=[kpk, K("dec")], writes=[K("QKm")])
                    S.op("pool", lambda e, b=b: e.tensor_tensor(
                        out=Mm[b][:], in0=Mm[b][:],
                        in1=Lst[:].unsqueeze(1).to_broadcast([64, 4, 64]), op=ALU.mult),
                        reads=[K("M"), "D_Lst"], writes=[K("M")])
                    S.op("dve", lambda e, b=b, G=G: e.tensor_tensor(
                        out=Mm[b][:], in0=Mm[b][:], in1=bc(G[:, 0:4], 64), op=ALU.mult),
                        reads=[K("M"), K("beta")], writes=[K("M")])
                    pn, kpn = self.ps8[pb(0)], ("ps", pb(0))

                    def trn(e, b=b, pn=pn):
                        ins = None
                        for h in range(4):
                            e.transpose(out=pn[:64, h * 64:(h + 1) * 64], in_=Mm[b][:, h, :],
                                        identity=self.ident_f[:64, :64])
                            ins = e.transpose(out=pn[:64, 256 + h * 64:256 + (h + 1) * 64],
                                              in_=QKm[b][:, h, :], identity=self.ident_f[:64, :64])
                        return ins
                    S.op("pe", trn, reads=[K("M"), K("QKm"), "ident_f"], writes=[kpn])
                    S.op("act", lambda e, b=b, pn=pn: e.copy(out=Qa[b][:], in_=v4(pn, 64)),
                         reads=[kpn], writes=[K("Qa")])
                    S.op("act", lambda e, b=b, pn=pn: e.copy(
                        out=QKT[b][:], in_=pn[:64, 256:512].rearrange("p (h x) -> p h x", h=4)),
                        reads=[kpn], writes=[K("QKT")])
                    S.op("dve", lambda e, b=b: e.tensor_tensor(out=Bt[b][:], in0=identI[:],
                                                                in1=Qa[b][:], op=ALU.subtract),
                         reads=[K("Qa"), "D_I"], writes=[K("Bt")])
                    Q, Qt, kQ, kQt = Qa[b], Mm[b], K("Qa"), K("M")
                    alt = [(Qb[b], Qtb[b], K("Qb"), K("Qtb")), (Qa[b], Qta[b], K("Qa"), K("Qta"))]
                    for step in range(5):
                        Q2, Qt2, kQ2, kQt2 = alt[step % 2]
                        p1, kp1 = self.ps8[pb(1)], ("ps", pb(1))

                        def mq(e, Q=Q, Qt=Qt, p1=p1, step=step):
                            ins = None
                            for h in range(4):
                                ins = e.matmul(p1[:64, h * 64:(h + 1) * 64], lhsT=Q[:, h, :],
                                               rhs=Qt[:, h, :], start=True, stop=True)
                                if step < 4:
                                    ins = e.matmul(p1[:64, 256 + h * 64:256 + (h + 1) * 64],
                                                   lhsT=Qt[:, h, :], rhs=Q[:, h, :], start=True,
                                                   stop=True)
                            return ins
                        S.op("pe", mq, reads=[kQ, kQt], writes=[kp1])
                        S.op("act", lambda e, Qt2=Qt2, p1=p1: e.copy(out=Qt2[:], in_=v4(p1, 64)),
                             reads=[kp1], writes=[kQt2])
                        if step < 4:
                            S.op("act", lambda e, Q2=Q2, p1=p1: e.copy(
                                out=Q2[:], in_=p1[:64, 256:512].rearrange("p (h x) -> p h x", h=4)),
                                reads=[kp1], writes=[kQ2])
                        p2, kp2 = self.ps8[pb(2)], ("ps", pb(2))

                        def mbm(e, Qt2=Qt2, b=b, p2=p2):
                            ins = None
                            for h in range(4):
                                ins = e.matmul(p2[:64, h * 64:(h + 1) * 64], lhsT=Qt2[:, h, :],
                                               rhs=Bt[b][:, h, :], start=True, stop=True)
                            return ins
                        S.op("pe", mbm, reads=[kQt2, K("Bt")], writes=[kp2])
                        S.op("dve", lambda e, b=b, p2=p2: e.tensor_tensor(out=Bt[b][:], in0=Bt[b][:],
                                                                           in1=v4(p2, 64),
                                                                           op=ALU.add),
                             reads=[kp2, K("Bt")], writes=[K("Bt")])
                        Q, Qt, kQ, kQt = Q2, Qt2, kQ2, kQt2
                    S.op("act", lambda e, b=b: e.copy(out=Tb[b][:], in_=Bt[b][:]), reads=[K("Bt")],
                         writes=[K("Tb")])
                    pu0, kpu0 = self.ps8[pb(3)], ("ps", pb(3))

                    def mu0(e, b=b, pu0=pu0):
                        ins = None
                        for h in range(4):
                            ins = e.matmul(pu0[:64, h * 128:(h + 1) * 128], lhsT=Tb[b][:, h, :],
                                           rhs=bv[b][:, h, :], start=True, stop=True)
                        return ins
                    S.op("pe", mu0, reads=[K("Tb"), *[(K("bv"), h_) for h_ in range(4)]], writes=[kpu0])
                    S.op("act", lambda e, b=b, pu0=pu0: e.copy(out=u0[b][:], in_=v4(pu0, 128)),
                         reads=[kpu0], writes=[K("u0")])
                    pw, kpw = self.ps8[pb(4)], ("ps", pb(4))

                    def mwk(e, b=b, pw=pw):
                        ins = None
                        for h in range(4):
                            ins = e.matmul(pw[:, h * 64:(h + 1) * 64], lhsT=bk[b][:, h, :],
                                           rhs=Tb[b][:, h, :], start=True, stop=True)
                        return ins
                    S.op("pe", mwk, reads=[K("Tb"), *[(K("bk"), h_) for h_ in range(4)]], writes=[kpw])
                    S.op("act", lambda e, b=b, pw=pw: e.copy(
                        out=wkT[b][:], in_=pw[:, :256].rearrange("p (h t) -> p h t", h=4)),
                        reads=[kpw], writes=[K("wkT")])
                    pu, kpu = self.ps8[pb(5)], ("ps", pb(5))

                    def mpu(e, b=b, pu=pu, s=s):
                        ins = None
                        for h in range(4):
                            ins = e.matmul(pu[:64, h * 128:(h + 1) * 128], lhsT=wkT[b][:, h, :],
                                           rhs=Sb[s][:, h, :], start=True, stop=True)
                        return ins
                    S.op("pe", mpu, reads=[K("wkT"), ("D_Sb", s)], writes=[kpu])
                    S.op("dve", lambda e, b=b, pu=pu: e.tensor_tensor(out=ub[b][:], in0=u0[b][:],
                                                                       in1=v4(pu, 128),
                                                                       op=ALU.subtract),
                         reads=[K("u0"), kpu], writes=[K("ub")])
                    po, kpo = self.ps8[pb(0)], ("ps", pb(0))

                    def mpo(e, b=b, po=po, s=s):
                        ins = None
                        for h in range(4):
                            e.matmul(po[:64, h * 128:(h + 1) * 128], lhsT=TT[b][:, h, :],
                                     rhs=Sb[s][:, h, :], start=True, stop=False)
                            ins = e.matmul(po[:64, h * 128:(h + 1) * 128], lhsT=QKT[b][:, h, :],
                                           rhs=ub[b][:, h, :], start=False, stop=True)
                        return ins
                    S.op("pe", mpo, reads=[K("TT0"), ("D_Sb", s), K("QKT"), K("ub")], writes=[kpo])
                    psn, kpsn = self.ps8[pb(1)], ("ps", pb(1))

                    def mps(e, b=b, psn=psn):
                        ins = None
                        for h in range(4):
                            ins = e.matmul(psn[:, h * 128:(h + 1) * 128], lhsT=kd[b][:, h, :],
                                           rhs=ub[b][:, h, :], start=True, stop=True)
                        return ins
                    S.op("pe", mps, reads=[*[(K("kd"), h_) for h_ in range(4)], K("ub")], writes=[kpsn])
                    for h in range(4):
                        S.op("dve", lambda e, b=b, s=s, h=h, psn=psn: e.scalar_tensor_tensor(
                            out=St[s][:, h, :], in0=St[s][:, h, :], scalar=glb[b][:, h:h + 1],
                            in1=psn[:, h * 128:(h + 1) * 128], op0=ALU.mult, op1=ALU.add),
                            reads=[("D_S", s), K("glb"), kpsn], writes=[("D_S", s)])
                    S.op("act", lambda e, s=s: e.copy(out=Sb[s][:], in_=St[s][:]),
                         reads=[("D_S", s)], writes=[("D_Sb", s)])
                    S.op("act", lambda e, b=b: e.activation(out=gs[b][:], in_=zt[b][:], func=AF.Silu),
                         reads=[K("zt")], writes=[K("gs")])
                    S.op("pool", lambda e, b=b: e.tensor_tensor(
                        out=gs[b][:], in0=gs[b][:],
                        in1=ng[:].unsqueeze(1).to_broadcast([64, 4, 128]), op=ALU.mult),
                        reads=[K("gs"), "D_ng"], writes=[K("gs")])
                    S.op("act", lambda e, b=b, po=po: e.copy(out=osb[b][:], in_=v4(po, 128)),
                         reads=[kpo], writes=[K("osb")])
                    S.op("pool", lambda e, b=b: e.tensor_tensor(out=osq[b][:], in0=osb[b][:],
                                                                 in1=osb[b][:], op=ALU.mult),
                         reads=[K("osb")], writes=[K("osq")])
                    S.op("dve", lambda e, b=b: e.tensor_reduce(out=nrm[b][:, 0:4], in_=osq[b][:],
                                                                axis=AX.X, op=ALU.add),
                         reads=[K("osq")], writes=[K("oss")])
                    self.rstd_from_ss(nrm[b][:, 0:4], K("oss"), nrm[b][:, 4:8], K("ors"), 128)
                    S.op("dve", lambda e, b=b: e.tensor_tensor(out=osb[b][:], in0=osb[b][:],
                                                                in1=bc(nrm[b][:, 4:8], 128),
                                                                op=ALU.mult),
                         reads=[K("osb"), K("ors")], writes=[K("osb")])
                    S.op("pool", lambda e, b=b: e.tensor_tensor(out=osq[b][:], in0=osb[b][:],
                                                                 in1=gs[b][:], op=ALU.mult),
                         reads=[K("osb"), K("gs")], writes=[K("osq")])
                    S.dma(out=self.mixin[tok:tok + 64, 0:512],
                          in_=osq[b][:].rearrange("p h v -> p (h v)"), sem="D_out%d" % b,
                          reads=[K("osq")], q="pool")
        S.interleave([lambda s=s: seq_thread(s) for s in range(NSEQ)])
        S.barrier()


def build_program():
    P = Prog()
    src = P.x_in
    for l in range(DEPTH):
        dst = P.xmid if l < DEPTH - 1 else P.y
        P.phase_A(l, src)
        P.phase_D(l)
        P.phase_S(l)
        P.phase_H(l)
        P.phase_E(l, src)
        P.phase_F(l, dst)
        src = dst
    return P


def kernel(**inputs):
    n = 8
    P = build_program()
    x = np.ascontiguousarray(inputs["x"], dtype=np.float32)
    shared = {k: np.ascontiguousarray(v, dtype=np.float32) for k, v in inputs.items() if k != "x"}
    in_maps = []
    for c in range(n):
        m = dict(shared)
        m["x"] = np.ascontiguousarray(x[c * NSEQ:(c + 1) * NSEQ].reshape(NTOK, D))
        in_maps.append(m)
    res = run_bass_kernel_spmd(P.nc, in_maps, core_ids=list(range(n)))
    out = np.stack([np.asarray(r["y"]).reshape(NSEQ, SEQ, D) for r in res.results], axis=0)
    return out.reshape(n * NSEQ, SEQ, D).astype(np.float32)
```
